# Optimizing a Trainium2 kernel written in Bass

```python
import math
import jax
import jax.numpy as jnp
from jax import lax
import numpy as np

D_MODEL = 1024
BATCH = 8
SEQ = 4096
DEPTH = 4

CTX_LEN = 256
GRID_W = 64
N_MIXERS = 2
N_SSD_LAYERS = (DEPTH + 1) // 2
N_RET_LAYERS = DEPTH // 2

DEEPNORM_ALPHA = (2.0 * DEPTH) ** 0.25
DEEPNORM_BETA = (8.0 * DEPTH) ** -0.25
LN_EPS = 1e-5

SSD_D_INNER = 2 * D_MODEL
SSD_HEADDIM = 64
SSD_HEADS = SSD_D_INNER // SSD_HEADDIM
SSD_GROUPS = 4
SSD_HPG = SSD_HEADS // SSD_GROUPS
SSD_STATE = 128
SSD_CONV_W = 5
SSD_CHUNK = 128
SSD_BC_DIM = SSD_GROUPS * SSD_STATE
SSD_CONV_DIM = SSD_D_INNER + 2 * SSD_BC_DIM
SSD_IN_DIM = SSD_D_INNER + SSD_CONV_DIM + 2 * SSD_HEADS

RET_HEADS = D_MODEL // 256
RET_QK_DIM = D_MODEL // RET_HEADS
RET_VALUE = 2 * D_MODEL
RET_V_DIM = RET_VALUE // RET_HEADS
RET_CHUNK = 128
RET_IN_DIM = 2 * D_MODEL + 2 * RET_VALUE
ROPE_BASE = 10000.0

MOE_GROUPS = 4
MOE_EXPERTS_PER_GROUP = 8
MOE_EXPERTS = MOE_GROUPS * MOE_EXPERTS_PER_GROUP
MOE_TOP_K = 2
MOE_HIDDEN = D_MODEL // 2
MOE_BLOCK = 128

kernel_name = 'hybrid_ssd_retention_hmoe_diffusion'


def swap01(t):
    return jnp.swapaxes(t, 0, 1)


def rev(t):
    return jnp.flip(t, axis=1)


def layer_norm(x, g, b):
    xf = x.astype(jnp.float32)
    mu = jnp.mean(xf, -1, keepdims=True)
    var = jnp.mean(jnp.square(xf - mu), -1, keepdims=True)
    return ((xf - mu) * lax.rsqrt(var + LN_EPS)).astype(x.dtype) * g + b


def modulate(x, shift, scale):
    return x * (1 + scale) + shift


def dwconv_centred(x, w, b):
    k = w.shape[0]
    y = lax.conv_general_dilated(x, w[:, None, :].astype(x.dtype), window_strides=(1,),
                                 padding=[(k // 2, k // 2)],
                                 dimension_numbers=('NWC', 'WIO', 'NWC'),
                                 feature_group_count=x.shape[-1])
    return y + b


def axial_rope_tables(n_rows, dtype):
    rows, cols = jnp.meshgrid(jnp.arange(n_rows), jnp.arange(GRID_W), indexing='ij')
    rows = rows.reshape(-1).astype(jnp.float32)
    cols = cols.reshape(-1).astype(jnp.float32)
    n_freq = RET_QK_DIM // 4
    inv_freq = ROPE_BASE ** (-jnp.arange(n_freq, dtype=jnp.float32) / n_freq)
    ang = jnp.concatenate([rows[:, None] * inv_freq, cols[:, None] * inv_freq], -1)
    return jnp.cos(ang).astype(dtype), jnp.sin(ang).astype(dtype)


def apply_rope(t, cos, sin):
    half = t.shape[-1] // 2
    t1, t2 = t[..., :half], t[..., half:]
    cs, sn = cos[None, :, None, :], sin[None, :, None, :]
    return jnp.concatenate([t1 * cs - t2 * sn, t1 * sn + t2 * cs], -1)


def ssd_chunk_scan(xdt, log_a, bm, cm, h0, want_y):
    bsz, seqlen = xdt.shape[:2]
    nc = seqlen // SSD_CHUNK
    dtype = xdt.dtype

    def chunk(t):
        return t.reshape((bsz, nc, SSD_CHUNK) + t.shape[2:])

    xc, bc, cc = chunk(xdt), chunk(bm), chunk(cm)
    acs = jnp.cumsum(chunk(log_a), axis=2)
    xe = xc * jnp.exp(acs[:, :, -1:] - acs).astype(dtype)[..., None]
    chunk_decay = jnp.exp(acs[:, :, -1]).astype(dtype)

    def state_step(h, b_c, xe_c, d_c):
        return h * d_c[..., None, None] + jnp.einsum('blgn,blghp->bghpn', b_c, xe_c)

    if not want_y:
        h_final, _ = lax.scan(lambda h, inp: (state_step(h, *inp), None), h0,
                              (swap01(bc), swap01(xe), swap01(chunk_decay)))
        return None, h_final

    def step(h, inp):
        b_c, xe_c, d_c, c_c, ea_c = inp
        y_off = jnp.einsum('blgn,bghpn->blghp', c_c, h) * ea_c[..., None]
        return state_step(h, b_c, xe_c, d_c), y_off

    ea = jnp.exp(acs).astype(dtype)
    h_final, y_off = lax.scan(step, h0, (swap01(bc), swap01(xe), swap01(chunk_decay),
                                         swap01(cc), swap01(ea)))
    pos = jnp.arange(SSD_CHUNK)
    lower = (pos[:, None] >= pos[None, :])[:, :, None, None]
    seg = acs[:, :, :, None] - acs[:, :, None, :]
    decay = jnp.exp(jnp.where(lower, seg, -jnp.inf)).astype(dtype)
    cb = jnp.einsum('bclgn,bcsgn->bclsg', cc, bc)
    y_diag = jnp.einsum('bclsgh,bcsghp->bclghp', cb[..., None] * decay, xc)
    return (y_diag + swap01(y_off)).reshape(xdt.shape), h_final


def ssd_branch(u, h0_f, h0_b, want_y, in_w, conv_w, conv_b, dt_bias, a_log, d_skip, norm_w):
    bsz, seqlen, _ = u.shape
    proj = u @ in_w
    xbc = jax.nn.silu(dwconv_centred(proj[..., SSD_D_INNER:SSD_D_INNER + SSD_CONV_DIM], conv_w, conv_b))
    dtype = xbc.dtype
    xs = xbc[..., :SSD_D_INNER].reshape(bsz, seqlen, SSD_GROUPS, SSD_HPG, SSD_HEADDIM)
    bm = xbc[..., SSD_D_INNER:SSD_D_INNER + SSD_BC_DIM].reshape(bsz, seqlen, SSD_GROUPS, SSD_STATE)
    cm = xbc[..., SSD_D_INNER + SSD_BC_DIM:].reshape(bsz, seqlen, SSD_GROUPS, SSD_STATE)
    dt_raw = proj[..., SSD_D_INNER + SSD_CONV_DIM:].astype(jnp.float32)
    dt_raw = dt_raw.reshape(bsz, seqlen, 2, SSD_GROUPS, SSD_HPG)
    dt = jax.nn.softplus(dt_raw + dt_bias.astype(jnp.float32).reshape(2, SSD_GROUPS, SSD_HPG))
    log_a = dt * -jnp.exp(a_log.astype(jnp.float32)).reshape(2, SSD_GROUPS, SSD_HPG)
    if h0_f is None:
        h0_f = h0_b = jnp.zeros((bsz, SSD_GROUPS, SSD_HPG, SSD_HEADDIM, SSD_STATE), dtype)
    y_f, h_f = ssd_chunk_scan(xs * dt[:, :, 0, ..., None].astype(dtype), log_a[:, :, 0],
                              bm, cm, h0_f.astype(dtype), want_y)
    y_b, h_b = ssd_chunk_scan(rev(xs * dt[:, :, 1, ..., None].astype(dtype)), rev(log_a[:, :, 1]),
                              rev(bm), rev(cm), h0_b.astype(dtype), want_y)
    if not want_y:
        return None, h_f, h_b
    y = y_f + rev(y_b) + xs * d_skip.reshape(SSD_GROUPS, SSD_HPG)[..., None]
    y = y.reshape(bsz, seqlen, SSD_D_INNER) * jax.nn.silu(proj[..., :SSD_D_INNER])
    yg = y.reshape(bsz, seqlen, SSD_GROUPS, SSD_D_INNER // SSD_GROUPS).astype(jnp.float32)
    yg = yg * lax.rsqrt(jnp.mean(jnp.square(yg), -1, keepdims=True) + LN_EPS)
    return yg.reshape(bsz, seqlen, SSD_D_INNER).astype(dtype) * norm_w, h_f, h_b


def retention_chunk_scan(q, k, v, log_gamma, s0, want_y):
    bsz, seqlen, nh, _ = q.shape
    nc = seqlen // RET_CHUNK
    dtype = q.dtype

    def chunk(t):
        return t.reshape((bsz, nc, RET_CHUNK) + t.shape[2:])

    qc, kc, vc = chunk(q), chunk(k), chunk(v)
    pos = jnp.arange(RET_CHUNK, dtype=jnp.float32)
    zeta = jnp.exp((RET_CHUNK - 1 - pos)[:, None] * log_gamma).astype(dtype)
    chunk_decay = jnp.exp(RET_CHUNK * log_gamma).astype(dtype)[:, None, None]
    kz = kc * zeta[:, :, None]

    def state_step(s, k_c, v_c):
        return s * chunk_decay + jnp.einsum('blhd,blhe->bhde', k_c, v_c)

    if not want_y:
        s_final, _ = lax.scan(lambda s, inp: (state_step(s, *inp), None), s0, (swap01(kz), swap01(vc)))
        return None, s_final

    xi = jnp.exp((pos + 1)[:, None] * log_gamma).astype(dtype)

    def step(s, inp):
        k_c, v_c, q_c = inp
        return state_step(s, k_c, v_c), jnp.einsum('blhd,bhde->blhe', q_c, s)

    s_final, cross = lax.scan(step, s0, (swap01(kz), swap01(vc), swap01(qc * xi[:, :, None])))
    rel = pos[:, None] - pos[None, :]
    dmat = jnp.exp(jnp.where((rel >= 0)[:, :, None], rel[:, :, None] * log_gamma, -jnp.inf)).astype(dtype)
    scores = jnp.einsum('bclhd,bcshd->bclsh', qc, kc) * dmat
    inner = jnp.einsum('bclsh,bcshe->bclhe', scores, vc)
    return (inner + swap01(cross)).reshape(bsz, seqlen, nh, v.shape[-1]), s_final


def retention_branch(u, rope, s0_f, s0_b, want_y, in_w, decay_logit, gn_w, gn_b):
    bsz, seqlen, _ = u.shape
    proj = u @ in_w
    dtype = proj.dtype
    qk_shape = (bsz, seqlen, RET_HEADS, RET_QK_DIM)
    q = proj[..., :D_MODEL].reshape(qk_shape)
    k = proj[..., D_MODEL:2 * D_MODEL].reshape(qk_shape) * (RET_QK_DIM ** -0.5)
    v = proj[..., 2 * D_MODEL:2 * D_MODEL + RET_VALUE].reshape(bsz, seqlen, RET_HEADS, RET_V_DIM)
    if rope is not None:
        q = apply_rope(q, *rope)
        k = apply_rope(k, *rope)
    log_gamma = jax.nn.log_sigmoid(decay_logit.astype(jnp.float32))
    if s0_f is None:
        s0_f = s0_b = jnp.zeros((bsz, RET_HEADS, RET_QK_DIM, RET_V_DIM), dtype)
    o_f, s_f = retention_chunk_scan(q, k, v, log_gamma[0], s0_f.astype(dtype), want_y)
    o_b, s_b = retention_chunk_scan(rev(q), rev(k), rev(v), log_gamma[1], s0_b.astype(dtype), want_y)
    if not want_y:
        return None, s_f, s_b
    o = (o_f + rev(o_b)).astype(jnp.float32)
    mu = jnp.mean(o, -1, keepdims=True)
    var = jnp.mean(jnp.square(o - mu), -1, keepdims=True)
    o = ((o - mu) * lax.rsqrt(var + LN_EPS)).reshape(bsz, seqlen, RET_VALUE).astype(dtype) * gn_w + gn_b
    return o * jax.nn.silu(proj[..., 2 * D_MODEL + RET_VALUE:]), s_f, s_b


def hier_moe(h, w_group, b_group, w_expert, b_expert, w_gate_up, w_down):
    n_tok, d = h.shape
    hf = h.astype(jnp.float32)
    g_logits = hf @ w_group.astype(jnp.float32) + b_group.astype(jnp.float32)
    g_sel = jnp.argmax(g_logits, -1)
    g_gate = jnp.take_along_axis(jax.nn.softmax(g_logits, -1), g_sel[:, None], 1)[:, 0]
    e_logits = (hf @ w_expert.astype(jnp.float32) + b_expert.astype(jnp.float32))
    e_logits = e_logits.reshape(n_tok, MOE_GROUPS, MOE_EXPERTS_PER_GROUP)
    e_logits = jnp.take_along_axis(e_logits, g_sel[:, None, None], 1)[:, 0]
    top_p, top_i = lax.top_k(jax.nn.softmax(e_logits, -1), MOE_TOP_K)
    top_w = top_p / jnp.sum(top_p, -1, keepdims=True) * g_gate[:, None]
    flat_e = (g_sel[:, None] * MOE_EXPERTS_PER_GROUP + top_i).reshape(-1)
    n_assign = n_tok * MOE_TOP_K
    order = jnp.argsort(flat_e)
    sorted_e = flat_e[order]
    counts = jnp.bincount(flat_e, length=MOE_EXPERTS)
    starts = jnp.cumsum(counts) - counts
    padded = (counts + MOE_BLOCK - 1) // MOE_BLOCK * MOE_BLOCK
    pends = jnp.cumsum(padded)
    dest_sorted = (pends - padded)[sorted_e] + jnp.arange(n_assign) - starts[sorted_e]
    dest = jnp.zeros((n_assign,), dest_sorted.dtype).at[order].set(dest_sorted)
    n_rows = -(-n_assign // MOE_BLOCK) * MOE_BLOCK + MOE_EXPERTS * MOE_BLOCK
    n_blocks = n_rows // MOE_BLOCK
    buf = jnp.zeros((n_rows, d), h.dtype).at[dest].set(jnp.repeat(h, MOE_TOP_K, axis=0))
    block_e = jnp.minimum(jnp.searchsorted(pends, jnp.arange(n_blocks) * MOE_BLOCK, side='right'),
                          MOE_EXPERTS - 1)

    def expert_block(args):
        xb, e = args
        gu = xb @ w_gate_up[e]
        return (jax.nn.silu(gu[:, :MOE_HIDDEN]) * gu[:, MOE_HIDDEN:]) @ w_down[e]

    out = lax.map(expert_block, (buf.reshape(n_blocks, MOE_BLOCK, d), block_e)).reshape(n_rows, -1)
    return jnp.sum(out[dest].reshape(n_tok, MOE_TOP_K, -1) * top_w[..., None].astype(out.dtype), axis=1)


def setup_inputs(seed: int = 0) -> dict:
    key = jax.random.key(seed)
    keys = iter(jax.random.split(key, 40))
    f32 = jnp.float32

    def normal(shape, scale):
        return jax.random.normal(next(keys), shape, f32) * scale

    def uniform(shape, lo, hi):
        return jax.random.uniform(next(keys), shape, f32, lo, hi)

    d = D_MODEL
    ns, nr = N_SSD_LAYERS, N_RET_LAYERS
    x = normal((BATCH, SEQ, d), 1.0)
    c = normal((BATCH, d), 1.0)
    ctx = normal((BATCH, CTX_LEN, d), 1.0)
    c_ctx = normal((d,), 1.0)
    mod_w = normal((DEPTH, d, 6 * d), 0.5 * d ** -0.5)
    mod_b = normal((DEPTH, 6 * d), 0.02)
    ssd_in_w = normal((ns, d, SSD_IN_DIM), d ** -0.5)
    ssd_conv_w = normal((ns, SSD_CONV_W, SSD_CONV_DIM), SSD_CONV_W ** -0.5)
    ssd_conv_b = normal((ns, SSD_CONV_DIM), 0.02)
    dt0 = jnp.exp(uniform((ns, 2, SSD_HEADS), math.log(1e-3), math.log(1e-1)))
    ssd_dt_bias = dt0 + jnp.log(-jnp.expm1(-dt0))
    ssd_a_log = jnp.log(uniform((ns, 2, SSD_HEADS), 1.0, 16.0))
    ssd_d_skip = 1.0 + normal((ns, SSD_HEADS), 0.02)
    ssd_norm_w = 1.0 + normal((ns, SSD_D_INNER), 0.02)
    ssd_out_w = normal((ns, SSD_D_INNER, d), SSD_D_INNER ** -0.5 * DEEPNORM_BETA)
    ret_in_w = normal((nr, d, RET_IN_DIM), d ** -0.5)
    m = 5.0 + jnp.arange(RET_HEADS, dtype=f32)
    ret_decay_logit = jnp.log(jnp.exp2(m) - 1.0) + normal((nr, 2, RET_HEADS), 0.1)
    ret_gn_w = 1.0 + normal((nr, RET_VALUE), 0.02)
    ret_gn_b = normal((nr, RET_VALUE), 0.02)
    ret_out_w = normal((nr, RET_VALUE, d), RET_VALUE ** -0.5 * DEEPNORM_BETA)
    ln_mix_g = 1.0 + normal((DEPTH, d), 0.02)
    ln_mix_b = normal((DEPTH, d), 0.02)
    ln_ffn_g = 1.0 + normal((DEPTH, d), 0.02)
    ln_ffn_b = normal((DEPTH, d), 0.02)
    moe_group_w = normal((DEPTH, d, MOE_GROUPS), d ** -0.5)
    moe_group_b = normal((DEPTH, MOE_GROUPS), 0.01)
    moe_expert_w = normal((DEPTH, d, MOE_EXPERTS), d ** -0.5)
    moe_expert_b = normal((DEPTH, MOE_EXPERTS), 0.01)
    moe_w_gate_up = normal((DEPTH, MOE_EXPERTS, d, 2 * MOE_HIDDEN), d ** -0.5)
    moe_w_down = normal((DEPTH, MOE_EXPERTS, MOE_HIDDEN, d), MOE_HIDDEN ** -0.5 * DEEPNORM_BETA)
    return {'x': x, 'c': c, 'ctx': ctx, 'c_ctx': c_ctx, 'mod_w': mod_w, 'mod_b': mod_b,
            'ssd_in_w': ssd_in_w, 'ssd_conv_w': ssd_conv_w, 'ssd_conv_b': ssd_conv_b,
            'ssd_dt_bias': ssd_dt_bias, 'ssd_a_log': ssd_a_log, 'ssd_d_skip': ssd_d_skip,
            'ssd_norm_w': ssd_norm_w, 'ssd_out_w': ssd_out_w,
            'ret_in_w': ret_in_w, 'ret_decay_logit': ret_decay_logit, 'ret_gn_w': ret_gn_w,
            'ret_gn_b': ret_gn_b, 'ret_out_w': ret_out_w,
            'ln_mix_g': ln_mix_g, 'ln_mix_b': ln_mix_b, 'ln_ffn_g': ln_ffn_g, 'ln_ffn_b': ln_ffn_b,
            'moe_group_w': moe_group_w, 'moe_group_b': moe_group_b, 'moe_expert_w': moe_expert_w,
            'moe_expert_b': moe_expert_b, 'moe_w_gate_up': moe_w_gate_up, 'moe_w_down': moe_w_down}


def reference(x, c, ctx, c_ctx, mod_w, mod_b, ssd_in_w, ssd_conv_w, ssd_conv_b, ssd_dt_bias,
              ssd_a_log, ssd_d_skip, ssd_norm_w, ssd_out_w, ret_in_w, ret_decay_logit, ret_gn_w,
              ret_gn_b, ret_out_w, ln_mix_g, ln_mix_b, ln_ffn_g, ln_ffn_b, moe_group_w, moe_group_b,
              moe_expert_w, moe_expert_b, moe_w_gate_up, moe_w_down):
    bsz, seqlen, d = x.shape
    n_lat = bsz * seqlen
    n_grid_rows = seqlen // GRID_W
    rope = axial_rope_tables(n_grid_rows, x.dtype)
    silu_c = jax.nn.silu(c)
    silu_cc = jax.nn.silu(c_ctx)
    for i in range(DEPTH):
        last = i == DEPTH - 1
        j = i // N_MIXERS
        sh1, sc1, g1, sh2, sc2, g2 = jnp.split((silu_c @ mod_w[i] + mod_b[i])[:, None, :], 6, axis=-1)
        csh1, csc1, cg1, csh2, csc2, cg2 = jnp.split(silu_cc @ mod_w[i] + mod_b[i], 6, axis=-1)
        u_ctx = modulate(ctx, csh1, csc1)
        u_lat = modulate(x, sh1, sc1)
        if i % N_MIXERS == 0:
            ssd_p = (ssd_in_w[j], ssd_conv_w[j], ssd_conv_b[j], ssd_dt_bias[j], ssd_a_log[j],
                     ssd_d_skip[j], ssd_norm_w[j])
            y_ctx, st_f, st_b = ssd_branch(u_ctx, None, None, not last, *ssd_p)
            y_lat, _, _ = ssd_branch(u_lat, st_f, st_b, True, *ssd_p)
            out_w = ssd_out_w[j]
        else:
            ret_p = (ret_in_w[j], ret_decay_logit[j], ret_gn_w[j], ret_gn_b[j])
            y_ctx, st_f, st_b = retention_branch(u_ctx, None, None, None, not last, *ret_p)
            y_lat, _, _ = retention_branch(u_lat, rope, st_f, st_b, True, *ret_p)
            out_w = ret_out_w[j]
        x = layer_norm(DEEPNORM_ALPHA * x + g1 * (y_lat @ out_w), ln_mix_g[i], ln_mix_b[i])
        tokens = modulate(x, sh2, sc2).reshape(n_lat, d)
        if not last:
            ctx = layer_norm(DEEPNORM_ALPHA * ctx + cg1 * (y_ctx @ out_w), ln_mix_g[i], ln_mix_b[i])
            tokens = jnp.concatenate([tokens, modulate(ctx, csh2, csc2).reshape(-1, d)], axis=0)
        ffn = hier_moe(tokens, moe_group_w[i], moe_group_b[i], moe_expert_w[i], moe_expert_b[i],
                       moe_w_gate_up[i], moe_w_down[i])
        x = layer_norm(DEEPNORM_ALPHA * x + g2 * ffn[:n_lat].reshape(x.shape), ln_ffn_g[i], ln_ffn_b[i])
        if not last:
            ctx = layer_norm(DEEPNORM_ALPHA * ctx + cg2 * ffn[n_lat:].reshape(ctx.shape),
                             ln_ffn_g[i], ln_ffn_b[i])
    return x
```

```python
import numpy as np
import ml_dtypes
from contextlib import ExitStack, contextmanager
import concourse.bass as bass
import concourse.mybir as mybir
from concourse.bass_utils import run_bass_kernel_spmd

F32 = mybir.dt.float32
BF16 = mybir.dt.bfloat16
I32 = mybir.dt.int32
ALU = mybir.AluOpType
AF = mybir.ActivationFunctionType
AX = mybir.AxisListType

ENGS = ["tensor", "vector", "scalar", "gpsimd", "sync"]
EPOCH = 20000
NDMA_SEM = 24

D = 1024
T = 4352
NCH = 34
NCTX = 2
NB = 100
DEPTH = 4
ALPHA = (2.0 * DEPTH) ** 0.25
EPS = 1e-5


class Res:
    __slots__ = ("w", "r")

    def __init__(self):
        self.w = None
        self.r = {}


class Tl:
    def __init__(self, t):
        self.t = t
        self.r = Res()

    def __getitem__(self, k):
        return self.t[k]


class FW:
    def __init__(self, nc):
        self.nc = nc
        self.root = ExitStack()
        self.es = self.root
        self.cnt = {e: 0 for e in ENGS}
        self.sems = {}
        self.waited = {e: {} for e in ENGS}
        self.dma_i = {e: 0 for e in ENGS}
        self.dma_last = {}
        self.latest = {}
        self.uid = 0

    def sem(self, key):
        if key not in self.sems:
            self.sems[key] = self.root.enter_context(self.nc.semaphore("s_%s_%s" % key))
        return self.sems[key]

    def sb(self, name, shape, dt):
        self.uid += 1
        return Tl(self.es.enter_context(self.nc.sbuf_tensor("%s_%d" % (name, self.uid), list(shape), dt)))

    def ps(self, name, shape, dt):
        self.uid += 1
        return Tl(self.es.enter_context(self.nc.psum_tensor("%s_%d" % (name, self.uid), list(shape), dt)))

    @contextmanager
    def scope(self):
        old = self.es
        self.es = ExitStack()
        try:
            yield
        finally:
            self.barrier()
            self.es.close()
            self.es = old

    def barrier(self):
        for eng in ENGS:
            for key, val in list(self.latest.items()):
                self._wait(eng, (key, val))

    def _wait(self, eng, ev):
        if ev is None:
            return
        key, val = ev
        if self.waited[eng].get(key, 0) >= val:
            return
        self.waited[eng][key] = val
        getattr(self.nc, eng).wait_ge(self.sem(key), val)

    def _deps(self, eng, reads, writes):
        evs = []
        for r in reads:
            if r.w is not None:
                evs.append(r.w)
        for w in writes:
            if w.w is not None:
                evs.append(w.w)
            evs.extend(w.r.items())
        for ev in evs:
            if ev[0][0] == "tensor" and eng == "tensor":
                continue
            self._wait(eng, ev)

    def _record(self, ev, reads, writes):
        self.latest[ev[0]] = ev[1]
        for r in reads:
            if r.r.get(ev[0], 0) < ev[1]:
                r.r[ev[0]] = ev[1]
        for w in writes:
            w.w = ev
            w.r = {}

    def op(self, eng, fn, reads=(), writes=()):
        reads = [t.r for t in reads]
        writes = [t.r for t in writes]
        self._deps(eng, reads, writes)
        c = self.cnt[eng]
        key = (eng, c // EPOCH)
        val = c % EPOCH + 1
        self.cnt[eng] = c + 1
        fn(getattr(self.nc, eng)).then_inc(self.sem(key), 1)
        self._record((key, val), reads, writes)

    def dma(self, eng, fn, reads=(), writes=()):
        reads = [t.r for t in reads]
        writes = [t.r for t in writes]
        self._deps(eng, reads, writes)
        i = self.dma_i[eng]
        self.dma_i[eng] = i + 1
        key = ("d" + eng, i % NDMA_SEM)
        prev = self.dma_last.get(key, 0)
        if prev:
            self._wait(eng, (key, prev))
        val = prev + 16
        self.dma_last[key] = val
        fn(getattr(self.nc, eng)).then_inc(self.sem(key), 16)
        self._record((key, val), reads, writes)


def bcast(ap, shape):
    return ap.to_broadcast(list(shape))


class Builder:
    def __init__(self, nc, nlayers=DEPTH, stop=None):
        self.nc = nc
        self.fw = FW(nc)
        self.nlayers = nlayers
        self.stop = stop
        self.dram = {}
        self.debug = set()
        self.only = None

    def din(self, name, shape, dt):
        if self.only is not None and name not in self.only:
            return None
        a = self.nc.dram_tensor(name, list(shape), dt, kind="ExternalInput").ap()
        self.dram[name] = a
        return a

    def dscr(self, name, shape, dt):
        kind = "ExternalOutput" if name in self.debug else "Internal"
        a = self.nc.dram_tensor(name, list(shape), dt, kind=kind).ap()
        self.dram[name] = a
        return a

    def ld(self, tile, dst, src, eng="sync"):
        self.fw.dma(eng, lambda e: e.dma_start(out=dst, in_=src), writes=[tile])

    def st(self, dst, tile, src, eng="sync"):
        self.fw.dma(eng, lambda e: e.dma_start(out=dst, in_=src), reads=[tile])

    def V(self, fn, rd, wr):
        self.fw.op("vector", fn, rd, wr)

    def A(self, fn, rd, wr):
        self.fw.op("scalar", fn, rd, wr)

    def G(self, fn, rd, wr):
        self.fw.op("gpsimd", fn, rd, wr)

    def P(self, fn, rd, wr):
        self.fw.op("tensor", fn, rd, wr)

    def ld_row(self, tile, dst, src_row, n=128):
        self.ld(tile, dst, src_row.partition_broadcast(n))

    def declare(self):
        d = self.din
        self.xin = d("xin", [T, D], F32)
        self.cvec = d("cvec", [128, 8, 2], F32)
        self.mod_w = d("mod_w", [DEPTH, D, 6 * D], F32)
        self.mod_b = d("mod_b", [DEPTH, 6 * D], F32)
        self.ssd_in_w = d("ssd_in_w", [2, D, 5184], F32)
        self.convw = d("convw", [2, 128, 24, 5], F32)
        self.convb = d("convb", [2, 128, 24], F32)
        self.ssd_dt_bias = d("ssd_dt_bias", [2, 64], F32)
        self.ssd_a_log = d("ssd_a_log", [2, 64], F32)
        self.ssd_d_skip = d("ssd_d_skip", [2, 32], F32)
        self.ssd_norm_w = d("ssd_norm_w", [2, 2048], F32)
        self.ssd_out_w = d("ssd_out_w", [2, 2048, D], F32)
        self.ret_in_w = d("ret_in_w", [2, D, 6144], F32)
        self.ret_decay = d("ret_decay_logit", [2, 2, 4], F32)
        self.ret_gn_w = d("ret_gn_w", [2, 2048], F32)
        self.ret_gn_b = d("ret_gn_b", [2, 2048], F32)
        self.ret_out_w = d("ret_out_w", [2, 2048, D], F32)
        self.ln_mix_g = d("ln_mix_g", [DEPTH, D], F32)
        self.ln_mix_b = d("ln_mix_b", [DEPTH, D], F32)
        self.ln_ffn_g = d("ln_ffn_g", [DEPTH, D], F32)
        self.ln_ffn_b = d("ln_ffn_b", [DEPTH, D], F32)
        self.moe_rw = d("moe_rw", [DEPTH, D, 36], F32)
        self.moe_rb = d("moe_rb", [DEPTH, 36], F32)
        self.moe_wgu = d("moe_w_gate_up", [DEPTH * 32 * 128, 8 * D], F32)
        self.moe_wd = d("moe_w_down", [DEPTH * 32 * 128, 4 * D], F32)
        self.c_identb = d("c_identb", [128, 128], BF16)
        self.c_identf = d("c_identf", [128, 128], F32)
        self.c_trif = d("c_trif", [2, 128, 128], F32)
        self.c_ones = d("c_ones", [128, 128], F32)
        self.c_slow = d("c_slow", [128, 128], BF16)
        self.c_rope = d("c_rope", [2, 128, T], F32)
        self.c_iota = d("c_iota", [128, 512], F32)
        self.c_pidx = d("c_pidx", [128, 12], F32)
        self.yout = self.nc.dram_tensor("yout", [4096, D], F32, kind="ExternalOutput").ap()
        s = self.dscr
        self.xA = s("xA", [T, D], F32)
        self.xB = s("xB", [T, D], F32)
        self.modrow = s("modrow", [2, 6 * D], F32)
        self.QT = s("QT", [4, 2, 128, T], BF16)
        self.KT = s("KT", [4, 2, 128, T], BF16)
        self.Ktm = s("Ktm", [T, 1024], BF16)
        self.Vtm = s("Vtm", [T, 2048], BF16)
        self.gate = s("gate", [T, 2048], F32)
        self.latm = s("latm", [T, 64], F32)
        self.dttm = s("dttm", [T, 64], F32)
        self.yf = s("yf", [T, 2048], F32)
        self.tok = s("tok", [T, D], F32)
        self.xbuf = s("xbuf", [NB * 128, D], BF16)
        self.ybuf = s("ybuf", [NB * 128, D], F32)
        self.dbg = {}

    def phase_mod(self, li):
        fw = self.fw
        with fw.scope():
            cv = fw.sb("cv", [128, 8, 2], F32)
            sv = fw.sb("sv", [128, 8, 2], F32)
            mb = fw.sb("mb", [2, 6 * D], F32)
            mr = fw.sb("mr", [2, 6 * D], F32)
            wst = fw.sb("wst", [128, 8, 512], F32)
            pm = fw.ps("pm", [128, 512], F32)
            self.ld(cv, cv[:], self.cvec)
            self.A(lambda e: e.activation(out=sv[:], in_=cv[:], func=AF.Silu), [cv], [sv])
            self.ld(mb, mb[0:1, :], self.mod_b[li:li + 1, :])
            self.ld(mb, mb[1:2, :], self.mod_b[li:li + 1, :])
            wv = self.mod_w[li].rearrange("(k p) n -> p k n", p=128)
            for n in range(12):
                self.ld(wst, wst[:], wv[:, :, n * 512:(n + 1) * 512])
                for k in range(8):
                    self.P(lambda e, k=k: e.matmul(pm[0:2, :], lhsT=sv[:, k, :], rhs=wst[:, k, :],
                                                   start=(k == 0), stop=(k == 7)), [sv, wst], [pm])
                self.V(lambda e, n=n: e.tensor_tensor(out=mr[0:2, n * 512:(n + 1) * 512], in0=pm[0:2, :],
                                                      in1=mb[0:2, n * 512:(n + 1) * 512], op=ALU.add),
                       [pm, mb], [mr])
            self.st(self.modrow, mr, mr[0:2, :])

    def make_uT(self, xcur, uT, identb, ptr, sc1, sh1, xt, u32, ub, c):
        v = 1 if c < NCTX else 0
        self.ld(xt, xt[:], xcur[c * 128:(c + 1) * 128, :])
        self.V(lambda e: e.tensor_tensor(out=u32[:], in0=xt[:], in1=sc1[v][:], op=ALU.mult), [xt, sc1[v]], [u32])
        self.G(lambda e: e.tensor_tensor(out=ub[:], in0=u32[:], in1=sh1[v][:], op=ALU.add), [u32, sh1[v]], [ub])
        for k in range(8):
            self.P(lambda e, k=k: e.transpose(ptr[:, k, :], ub[:, k * 128:(k + 1) * 128], identb[:]),
                   [ub, identb], [ptr])

    def load_mod_rows(self, lo, names):
        out = {}
        for nm, idx in names:
            tl = []
            for v in range(2):
                t = self.fw.sb("row_%s%d" % (nm, v), [128, D], F32)
                self.ld_row(t, t[:], self.modrow[v:v + 1, idx * D:(idx + 1) * D])
                tl.append(t)
            out[nm] = tl
        return out

    def phase_in_ssd(self, li, j, xcur):
        fw = self.fw
        with fw.scope():
            identb = fw.sb("identb", [128, 128], BF16)
            self.ld(identb, identb[:], self.c_identb)
            rows = self.load_mod_rows(0, [("sh1", 0), ("sc1", 1)])
            sh1, sc1 = rows["sh1"], rows["sc1"]
            for v in range(2):
                self.V(lambda e, v=v: e.tensor_scalar_add(out=sc1[v][:], in0=sc1[v][:], scalar1=1.0), [sc1[v]], [sc1[v]])
            uT = fw.sb("uT", [128, 8, T], BF16)
            xt = fw.sb("xt", [128, D], F32)
            u32 = fw.sb("u32", [128, D], F32)
            ub = fw.sb("ub", [128, D], BF16)
            ptr = fw.ps("ptr", [128, 8, 128], BF16)
            for c in range(NCH):
                self.make_uT(xcur, uT, identb, ptr, sc1, sh1, xt, u32, ub, c)
                self.A(lambda e, c=c: e.copy(out=uT[:, :, c * 128:(c + 1) * 128], in_=ptr[:]), [ptr], [uT])
            cw = fw.sb("cw", [128, 24, 5], F32)
            cb = fw.sb("cb", [128, 24], F32)
            self.ld(cw, cw[:], self.convw[j])
            self.ld(cb, cb[:], self.convb[j])
            wst = fw.sb("wstA", [128, 8, 128], F32)
            wb = fw.sb("wbA", [128, 8, 128], BF16)
            raw = fw.sb("raw", [128, T], F32)
            o = fw.sb("o", [128, T], F32)
            ob = fw.sb("ob", [128, T], BF16)
            pp = [fw.ps("pp%d" % i, [128, 512], F32) for i in range(2)]
            trs = fw.sb("trs", [128, 8, 128], BF16)
            wv = self.ssd_in_w[j].rearrange("(k p) n -> p k n", p=128)
            segs = [(0, 256)] + [(256 + i * 512, 256 + (i + 1) * 512) for i in range(8)]
            seqs = [(0, 256), (256, T)]
            for f in range(24):
                col0 = 2048 + f * 128
                self.ld(wst, wst[:], wv[:, :, col0:col0 + 128])
                self.G(lambda e: e.tensor_copy(out=wb[:], in_=wst[:]), [wst], [wb])
                for si, (a, b) in enumerate(segs):
                    p = pp[si % 2]
                    for k in range(8):
                        self.P(lambda e, k=k, a=a, b=b, p=p: e.matmul(p[:, 0:b - a], lhsT=wb[:, k, :], rhs=uT[:, k, a:b],
                                                                       start=(k == 0), stop=(k == 7)), [wb, uT], [p])
                    self.A(lambda e, a=a, b=b, p=p: e.copy(out=raw[:, a:b], in_=p[:, 0:b - a]), [p], [raw])
                self.A(lambda e, f=f: e.activation(out=o[:], in_=raw[:], func=AF.Identity,
                                                   bias=cb[:, f:f + 1], scale=cw[:, f, 2:3]), [raw, cw, cb], [o])
                for (a, b) in seqs:
                    for kk, off in ((0, -2), (1, -1), (3, 1), (4, 2)):
                        if off < 0:
                            osl = (a - off, b)
                            isl = (a, b + off)
                        else:
                            osl = (a, b - off)
                            isl = (a + off, b)
                        self.V(lambda e, f=f, kk=kk, osl=osl, isl=isl: e.scalar_tensor_tensor(
                            out=o[:, osl[0]:osl[1]], in0=raw[:, isl[0]:isl[1]], scalar=cw[:, f, kk:kk + 1],
                            in1=o[:, osl[0]:osl[1]], op0=ALU.mult, op1=ALU.add), [raw, cw, o], [o])
                self.A(lambda e: e.activation(out=ob[:], in_=o[:], func=AF.Silu), [o], [ob])
                if f >= 16:
                    g = (f - 16) % 4
                    dst = self.KT if f < 20 else self.QT
                    self.st(dst[g, 0], ob, ob[:])
                if f < 20:
                    dstm = self.Vtm if f < 16 else self.Ktm
                    fc = f if f < 16 else f - 16
                    dv = dstm.rearrange("(c p) f -> p c f", p=128)
                    for c0 in range(0, NCH, 8):
                        n = min(8, NCH - c0)
                        for cc in range(n):
                            self.P(lambda e, cc=cc, c0=c0: e.transpose(ptr[:, cc, :], ob[:, (c0 + cc) * 128:(c0 + cc + 1) * 128],
                                                                       identb[:]), [ob, identb], [ptr])
                        self.V(lambda e, n=n: e.tensor_copy(out=trs[:, 0:n, :], in_=ptr[:, 0:n, :]), [ptr], [trs])
                        self.st(dv[:, c0:c0 + n, fc * 128:(fc + 1) * 128], trs, trs[:, 0:n, :])
            wst2 = fw.sb("wst2", [128, 8, 512], F32)
            wb2 = fw.sb("wb2", [128, 8, 512], BF16)
            zt = fw.sb("zt", [128, 512], F32)
            for n in range(4):
                self.ld(wst2, wst2[:], wv[:, :, n * 512:(n + 1) * 512])
                self.G(lambda e: e.tensor_copy(out=wb2[:], in_=wst2[:]), [wst2], [wb2])
                for c in range(NCH):
                    p = pp[c % 2]
                    for k in range(8):
                        self.P(lambda e, k=k, c=c, p=p: e.matmul(p[:], lhsT=uT[:, k, c * 128:(c + 1) * 128], rhs=wb2[:, k, :],
                                                                 start=(k == 0), stop=(k == 7)), [uT, wb2], [p])
                    self.A(lambda e, p=p: e.copy(out=zt[:], in_=p[:]), [p], [zt])
                    self.st(self.gate[c * 128:(c + 1) * 128, n * 512:(n + 1) * 512], zt, zt[:])
            dtb = fw.sb("dtb", [128, 64], F32)
            nega = fw.sb("nega", [128, 64], F32)
            self.ld_row(dtb, dtb[:], self.ssd_dt_bias[j:j + 1, :])
            self.ld_row(nega, nega[:], self.ssd_a_log[j:j + 1, :])
            self.A(lambda e: e.activation(out=nega[:], in_=nega[:], func=AF.Exp), [nega], [nega])
            self.V(lambda e: e.tensor_scalar_mul(out=nega[:], in0=nega[:], scalar1=-1.0), [nega], [nega])
            self.ld(wst2, wst2[:, :, 0:64], wv[:, :, 5120:5184])
            self.G(lambda e: e.tensor_copy(out=wb2[:, :, 0:64], in_=wst2[:, :, 0:64]), [wst2], [wb2])
            d0 = fw.sb("d0", [128, 64], F32)
            d1 = fw.sb("d1", [128, 64], F32)
            d2 = fw.sb("d2", [128, 64], F32)
            for c in range(NCH):
                p = pp[c % 2]
                for k in range(8):
                    self.P(lambda e, k=k, c=c, p=p: e.matmul(p[:, 0:64], lhsT=uT[:, k, c * 128:(c + 1) * 128], rhs=wb2[:, k, 0:64],
                                                             start=(k == 0), stop=(k == 7)), [uT, wb2], [p])
                self.V(lambda e, p=p: e.tensor_tensor(out=d0[:], in0=p[:, 0:64], in1=dtb[:], op=ALU.add), [p, dtb], [d0])
                self.V(lambda e: e.tensor_scalar_mul(out=d1[:], in0=d0[:], scalar1=-1.0), [d0], [d1])
                self.V(lambda e: e.tensor_tensor(out=d1[:], in0=d1[:], in1=d0[:], op=ALU.max), [d0, d1], [d1])
                self.A(lambda e: e.activation(out=d1[:], in_=d1[:], func=AF.Exp, scale=-1.0), [d1], [d1])
                self.A(lambda e: e.activation(out=d1[:], in_=d1[:], func=AF.Ln, bias=1.0), [d1], [d1])
                self.V(lambda e: e.scalar_tensor_tensor(out=d2[:], in0=d0[:], scalar=0.0, in1=d1[:], op0=ALU.max, op1=ALU.add),
                       [d0, d1], [d2])
                self.st(self.dttm[c * 128:(c + 1) * 128, :], d2, d2[:])
                self.V(lambda e: e.tensor_tensor(out=d0[:], in0=d2[:], in1=nega[:], op=ALU.mult), [d2, nega], [d0])
                self.st(self.latm[c * 128:(c + 1) * 128, :], d0, d0[:])

    def phase_in_ret(self, li, j, xcur):
        fw = self.fw
        with fw.scope():
            identb = fw.sb("identb", [128, 128], BF16)
            self.ld(identb, identb[:], self.c_identb)
            rows = self.load_mod_rows(0, [("sh1", 0), ("sc1", 1)])
            sh1, sc1 = rows["sh1"], rows["sc1"]
            for v in range(2):
                self.V(lambda e, v=v: e.tensor_scalar_add(out=sc1[v][:], in0=sc1[v][:], scalar1=1.0), [sc1[v]], [sc1[v]])
            W = fw.sb("Wret", [128, 8, 6144], BF16)
            wst = fw.sb("wstR", [128, 8, 512], F32)
            wv = self.ret_in_w[j].rearrange("(k p) n -> p k n", p=128)
            for n in range(12):
                self.ld(wst, wst[:], wv[:, :, n * 512:(n + 1) * 512])
                eng = self.G if n % 2 == 0 else self.A
                if n % 2 == 0:
                    self.G(lambda e, n=n: e.tensor_copy(out=W[:, :, n * 512:(n + 1) * 512], in_=wst[:]), [wst], [W])
                else:
                    self.A(lambda e, n=n: e.copy(out=W[:, :, n * 512:(n + 1) * 512], in_=wst[:]), [wst], [W])
            uT = fw.sb("uTs", [128, 8, 512], BF16)
            xt = fw.sb("xt", [128, D], F32)
            u32 = fw.sb("u32", [128, D], F32)
            ub = fw.sb("ub", [128, D], BF16)
            ptr = fw.ps("ptr", [128, 8, 128], BF16)
            pp = [fw.ps("pp%d" % i, [128, 512], F32) for i in range(4)]
            cs = fw.sb("cs", [128, 512], F32)
            sn = fw.sb("sn", [128, 512], F32)
            r1 = fw.sb("r1", [128, 512], F32)
            r2 = fw.sb("r2", [128, 512], F32)
            ta = fw.sb("ta", [128, 512], F32)
            tb = fw.sb("tb", [128, 512], F32)
            o1 = fw.sb("o1", [128, 512], BF16)
            o2 = fw.sb("o2", [128, 512], BF16)
            trs = fw.sb("trs", [128, 8, 128], BF16)
            zt = fw.sb("zt", [128, 512], F32)
            vb = fw.sb("vb", [128, 512], BF16)
            segs = [(0, 256)] + [(256 + i * 512, 256 + (i + 1) * 512) for i in range(8)]
            ktv = self.Ktm.rearrange("(c p) f -> p c f", p=128)
            for (a, b) in segs:
                n = b - a
                nt = n // 128
                c0 = a // 128
                for ci in range(nt):
                    self.make_uT(xcur, uT, identb, ptr, sc1, sh1, xt, u32, ub, c0 + ci)
                    self.A(lambda e, ci=ci: e.copy(out=uT[:, :, ci * 128:(ci + 1) * 128], in_=ptr[:]), [ptr], [uT])
                self.ld(cs, cs[:, 0:n], self.c_rope[0, :, a:b])
                self.ld(sn, sn[:, 0:n], self.c_rope[1, :, a:b])
                for which in range(2):
                    for h in range(4):
                        base = which * 1024 + h * 256
                        for half, (pt, rr) in enumerate(((pp[0], r1), (pp[1], r2))):
                            cb0 = base + half * 128
                            for k in range(8):
                                self.P(lambda e, k=k, cb0=cb0, pt=pt: e.matmul(pt[:, 0:n], lhsT=W[:, k, cb0:cb0 + 128], rhs=uT[:, k, 0:n],
                                                                               start=(k == 0), stop=(k == 7)), [W, uT], [pt])
                            sc = 1.0 if which == 0 else 0.0625
                            self.A(lambda e, pt=pt, rr=rr, sc=sc: e.activation(out=rr[:, 0:n], in_=pt[:, 0:n], func=AF.Copy, scale=sc),
                                   [pt], [rr])
                        self.V(lambda e: e.tensor_tensor(out=ta[:, 0:n], in0=r1[:, 0:n], in1=cs[:, 0:n], op=ALU.mult), [r1, cs], [ta])
                        self.G(lambda e: e.tensor_tensor(out=tb[:, 0:n], in0=r2[:, 0:n], in1=sn[:, 0:n], op=ALU.mult), [r2, sn], [tb])
                        self.V(lambda e: e.tensor_tensor(out=o1[:, 0:n], in0=ta[:, 0:n], in1=tb[:, 0:n], op=ALU.subtract), [ta, tb], [o1])
                        self.V(lambda e: e.tensor_tensor(out=ta[:, 0:n], in0=r1[:, 0:n], in1=sn[:, 0:n], op=ALU.mult), [r1, sn], [ta])
                        self.G(lambda e: e.tensor_tensor(out=tb[:, 0:n], in0=r2[:, 0:n], in1=cs[:, 0:n], op=ALU.mult), [r2, cs], [tb])
                        self.V(lambda e: e.tensor_tensor(out=o2[:, 0:n], in0=ta[:, 0:n], in1=tb[:, 0:n], op=ALU.add), [ta, tb], [o2])
                        dst = self.QT if which == 0 else self.KT
                        self.st(dst[h, 0, :, a:b], o1, o1[:, 0:n])
                        self.st(dst[h, 1, :, a:b], o2, o2[:, 0:n])
                        if which == 1:
                            for half, oo in enumerate((o1, o2)):
                                for ci in range(nt):
                                    self.P(lambda e, ci=ci, oo=oo, half=half: e.transpose(ptr[:, half * 4 + ci, :], oo[:, ci * 128:(ci + 1) * 128],
                                                                                          identb[:]), [oo, identb], [ptr])
                            self.V(lambda e: e.tensor_copy(out=trs[:], in_=ptr[:]), [ptr], [trs])
                            for half in range(2):
                                col = h * 256 + half * 128
                                self.st(ktv[:, c0:c0 + nt, col:col + 128], trs, trs[:, half * 4:half * 4 + nt, :])
                for ci in range(nt):
                    c = c0 + ci
                    for nn in range(8):
                        p = pp[2 + nn % 2]
                        colw = 2048 + nn * 512
                        for k in range(8):
                            self.P(lambda e, k=k, ci=ci, p=p, colw=colw: e.matmul(p[:], lhsT=uT[:, k, ci * 128:(ci + 1) * 128],
                                                                                  rhs=W[:, k, colw:colw + 512], start=(k == 0), stop=(k == 7)),
                                   [uT, W], [p])
                        if nn < 4:
                            self.A(lambda e, p=p: e.copy(out=vb[:], in_=p[:]), [p], [vb])
                            self.st(self.Vtm[c * 128:(c + 1) * 128, nn * 512:(nn + 1) * 512], vb, vb[:])
                        else:
                            self.V(lambda e, p=p: e.tensor_copy(out=zt[:], in_=p[:]), [p], [zt])
                            self.st(self.gate[c * 128:(c + 1) * 128, (nn - 4) * 512:(nn - 3) * 512], zt, zt[:])

    def phase_scan(self, li, j, ssd, xcur, xnext):
        fw = self.fw
        G = 4
        Hg = 8 if ssd else 1
        KC = 1 if ssd else 2
        H = G * Hg
        dv = 2048 // H
        dk = KC * 128
        with fw.scope():
            identb = fw.sb("identb", [128, 128], BF16)
            self.ld(identb, identb[:], self.c_identb)
            ones = fw.sb("ones", [128, 128], F32)
            self.ld(ones, ones[:], self.c_ones)
            tri = fw.sb("tri", [128, 128], F32)
            Wo = fw.sb("Wo", [128, 16, D], BF16)
            wst = fw.sb("wstO", [128, 4, D], F32)
            owv = (self.ssd_out_w if ssd else self.ret_out_w)[j].rearrange("(k p) n -> p k n", p=128)
            for q in range(4):
                self.ld(wst, wst[:], owv[:, q * 4:(q + 1) * 4, :])
                self.G(lambda e, q=q: e.tensor_copy(out=Wo[:, q * 4:(q + 1) * 4, :], in_=wst[:]), [wst], [Wo])
            rows = self.load_mod_rows(0, [("g1", 2), ("sh2", 3), ("sc2", 4)])
            g1, sh2, sc2 = rows["g1"], rows["sh2"], rows["sc2"]
            for v in range(2):
                self.V(lambda e, v=v: e.tensor_scalar_add(out=sc2[v][:], in0=sc2[v][:], scalar1=1.0), [sc2[v]], [sc2[v]])
            lng = fw.sb("lng", [128, D], F32)
            lnb = fw.sb("lnb", [128, D], F32)
            self.ld_row(lng, lng[:], self.ln_mix_g[li:li + 1, :])
            self.ld_row(lnb, lnb[:], self.ln_mix_b[li:li + 1, :])
            nw = fw.sb("nw", [128, 2048], F32)
            self.ld_row(nw, nw[:], (self.ssd_norm_w if ssd else self.ret_gn_w)[j:j + 1, :])
            if ssd:
                dsk = fw.sb("dsk", [128, 32], F32)
                self.ld_row(dsk, dsk[:], self.ssd_d_skip[j:j + 1, :])
            else:
                nb_ = fw.sb("nb", [128, 2048], F32)
                self.ld_row(nb_, nb_[:], self.ret_gn_b[j:j + 1, :])
            S = fw.sb("S", [128, KC, 2048], F32)
            Sb = fw.sb("Sb", [128, KC, 2048], BF16)
            qt = fw.sb("qt", [128, G * KC, 128], BF16)
            kt = fw.sb("kt", [128, G * KC, 128], BF16)
            ktm = fw.sb("ktm", [128, G * dk], BF16)
            vt = fw.sb("vt", [128, 2048], BF16)
            la = fw.sb("la", [128, 64], F32)
            dt = fw.sb("dt", [128, 64], F32)
            acs = fw.sb("acs", [128, 2 * H], F32)
            nacs = fw.sb("nacs", [128, H], F32)
            ea = fw.sb("ea", [128, H], F32)
            wend = fw.sb("wend", [128, H], F32)
            cd = fw.sb("cd", [128, H], F32)
            E = fw.sb("E", [128, H, 128], F32)
            lt = fw.sb("lt", [128, Hg, 128], F32)
            M = fw.sb("M", [128, Hg, 128], BF16)
            cbm = fw.sb("cbm", [128, G, 128], F32)
            xdt = fw.sb("xdt", [128, 2048], BF16) if ssd else vt
            xe = fw.sb("xe", [128, 2048], BF16)
            yc = fw.sb("yc", [128, 2048], F32)
            tmp = fw.sb("tmp", [128, 512], F32)
            yfl = fw.sb("yfl", [128, 2048], F32)
            gt = fw.sb("gt", [128, 2048], F32)
            yb = fw.sb("yb", [128, 2048], BF16)
            yT = fw.sb("yT", [128, 16, 128], BF16)
            st6 = fw.sb("st6", [128, 4, 6], F32)
            mv = fw.sb("mv", [128, 4, 2], F32)
            ss = fw.sb("ss", [128, 4], F32)
            rstd = fw.sb("rstd", [128, 4], F32)
            xt = fw.sb("xt", [128, D], F32)
            t1 = fw.sb("t1", [128, D], F32)
            t2 = fw.sb("t2", [128, D], F32)
            st2 = fw.sb("st2", [128, 2, 6], F32)
            mv2 = fw.sb("mv2", [128, 2], F32)
            rs2 = fw.sb("rs2", [128, 1], F32)
            psm = fw.ps("psm", [128, 512], F32)
            pcb = fw.ps("pcb", [128, 512], F32)
            pbc = [fw.ps("pbc%d" % i, [128, 512], F32) for i in range(2)]
            pA = fw.ps("pA", [128, 1024], F32)
            pst = fw.ps("pst", [128, 512], F32)
            ptr = fw.ps("ptr", [128, 8, 128], BF16)

            qtv = self.QT.rearrange("g k p t -> p g k t")
            ktv = self.KT.rearrange("g k p t -> p g k t")

            def decay_prep(dirn):
                lad = la[:, dirn * 32:dirn * 32 + H] if ssd else la[:, 0:H]
                self.P(lambda e: e.matmul(psm[:, 0:H], lhsT=tri[:], rhs=lad, start=True, stop=True), [tri, la], [psm])
                self.P(lambda e: e.matmul(psm[:, H:2 * H], lhsT=ones[:], rhs=lad, start=True, stop=True), [ones, la], [psm])
                self.V(lambda e: e.tensor_copy(out=acs[:], in_=psm[:, 0:2 * H]), [psm], [acs])
                self.V(lambda e: e.tensor_scalar_mul(out=nacs[:], in0=acs[:, 0:H], scalar1=-1.0), [acs], [nacs])
                self.A(lambda e: e.activation(out=ea[:], in_=acs[:, 0:H], func=AF.Exp), [acs], [ea])
                self.V(lambda e: e.tensor_tensor(out=wend[:], in0=acs[:, H:2 * H], in1=acs[:, 0:H], op=ALU.subtract), [acs], [wend])
                self.A(lambda e: e.activation(out=wend[:], in_=wend[:], func=AF.Exp), [wend], [wend])
                self.A(lambda e: e.activation(out=cd[:], in_=acs[:, H:2 * H], func=AF.Exp), [acs], [cd])
                for g in range(G):
                    hs = slice(g * Hg, (g + 1) * Hg)
                    ladg = la[:, dirn * 32 + g * Hg:dirn * 32 + (g + 1) * Hg] if ssd else la[:, g:g + 1]
                    self.G(lambda e, ladg=ladg: e.tensor_tensor(out=lt[:], in0=bcast(tri[:].unsqueeze(1), [128, Hg, 128]),
                                                                in1=bcast(ladg.unsqueeze(2), [128, Hg, 128]), op=ALU.mult),
                           [tri, la], [lt])
                    for q in range((Hg + 3) // 4):
                        nh = min(4, Hg - q * 4)
                        pb = pbc[q % 2]
                        self.P(lambda e, q=q, nh=nh, pb=pb: e.matmul(pb[:, 0:nh * 128], lhsT=ones[:], rhs=lt[:, q * 4:q * 4 + nh, :],
                                                                     start=True, stop=True), [ones, lt], [pb])
                        for hh in range(nh):
                            h = g * Hg + q * 4 + hh
                            self.A(lambda e, h=h, hh=hh, pb=pb: e.activation(out=E[:, h, :], in_=pb[:, hh * 128:(hh + 1) * 128], func=AF.Exp,
                                                                            bias=nacs[:, h:h + 1], scale=1.0), [pb, nacs], [E])

            for dirn in range(2):
                order = list(range(NCH)) if dirn == 0 else [1, 0] + list(range(NCH - 1, 1, -1))
                self.ld(tri, tri[:], self.c_trif[dirn])
                self.V(lambda e: e.memset(S[:], 0.0), [], [S])
                self.G(lambda e: e.memset(Sb[:], 0.0), [], [Sb])
                if not ssd:
                    self.ld_row(la, la[:, 0:4], self.ret_decay[j, dirn:dirn + 1, :])
                    self.A(lambda e: e.activation(out=la[:, 0:4], in_=la[:, 0:4], func=AF.Exp, scale=-1.0), [la], [la])
                    self.A(lambda e: e.activation(out=la[:, 0:4], in_=la[:, 0:4], func=AF.Ln, bias=1.0), [la], [la])
                    self.V(lambda e: e.tensor_scalar_mul(out=la[:, 0:4], in0=la[:, 0:4], scalar1=-1.0), [la], [la])
                    decay_prep(dirn)
                for c in order:
                    v = 1 if c < NCTX else 0
                    cs_ = slice(c * 128, (c + 1) * 128)
                    self.ld(qt, qt[:].rearrange("p (g k) t -> p g k t", g=G), qtv[:, :, 0:KC, cs_])
                    self.ld(kt, kt[:].rearrange("p (g k) t -> p g k t", g=G), ktv[:, :, 0:KC, cs_])
                    self.ld(ktm, ktm[:], self.Ktm[cs_, 0:G * dk])
                    self.ld(vt, vt[:], self.Vtm[cs_, :])
                    if ssd:
                        self.ld(la, la[:], self.latm[cs_, :])
                        self.ld(dt, dt[:], self.dttm[cs_, :])
                        decay_prep(dirn)
                        self.V(lambda e: e.tensor_tensor(out=xdt[:].rearrange("p (h d) -> p h d", h=H), in0=vt[:].rearrange("p (h d) -> p h d", h=H),
                                                         in1=bcast(dt[:, dirn * 32:dirn * 32 + 32].unsqueeze(2), [128, H, dv]), op=ALU.mult),
                               [vt, dt], [xdt])
                    self.G(lambda e: e.tensor_tensor(out=xe[:].rearrange("p (h d) -> p h d", h=H), in0=xdt[:].rearrange("p (h d) -> p h d", h=H),
                                                     in1=bcast(wend[:].unsqueeze(2), [128, H, dv]), op=ALU.mult), [xdt, wend], [xe])
                    for g in range(G):
                        for kc in range(KC):
                            self.P(lambda e, g=g, kc=kc: e.matmul(pcb[:, g * 128:(g + 1) * 128], lhsT=kt[:, g * KC + kc, :], rhs=qt[:, g * KC + kc, :],
                                                                  start=(kc == 0), stop=(kc == KC - 1)), [kt, qt], [pcb])
                    self.V(lambda e: e.tensor_tensor(out=cbm[:], in0=pcb[:].rearrange("p (g l) -> p g l", g=G),
                                                     in1=bcast(tri[:].unsqueeze(1), [128, G, 128]), op=ALU.mult), [pcb, tri], [cbm])
                    for g in range(G):
                        gs = slice(g * 512, (g + 1) * 512)
                        self.V(lambda e, g=g: e.scalar_tensor_tensor(out=M[:], in0=E[:, g * Hg:(g + 1) * Hg, :], scalar=1.0,
                                                                     in1=bcast(cbm[:, g:g + 1, :], [128, Hg, 128]), op0=ALU.min, op1=ALU.mult),
                               [E, cbm], [M])
                        for hh in range(Hg):
                            h = g * Hg + hh
                            self.P(lambda e, h=h, hh=hh: e.matmul(pA[:, h * dv:(h + 1) * dv] if False else pA[:, (h * dv) % 512:(h * dv) % 512 + dv],
                                                                  lhsT=M[:, hh, :], rhs=xdt[:, h * dv:(h + 1) * dv], start=True, stop=True),
                                   [M, xdt], [pA])
                        for kc in range(KC):
                            self.P(lambda e, g=g, kc=kc, gs=gs: e.matmul(pA[:, 512:1024], lhsT=qt[:, g * KC + kc, :], rhs=Sb[:, kc, gs],
                                                                         start=(kc == 0), stop=(kc == KC - 1)), [qt, Sb], [pA])
                        self.V(lambda e, g=g: e.tensor_tensor(out=tmp[:].rearrange("p (h d) -> p h d", h=Hg),
                                                              in0=pA[:, 512:1024].rearrange("p (h d) -> p h d", h=Hg),
                                                              in1=bcast(ea[:, g * Hg:(g + 1) * Hg].unsqueeze(2), [128, Hg, dv]), op=ALU.mult),
                               [pA, ea], [tmp])
                        self.V(lambda e, gs=gs: e.tensor_tensor(out=yc[:, gs], in0=tmp[:], in1=pA[:, 0:512], op=ALU.add), [tmp, pA], [yc])
                        for kc in range(KC):
                            self.P(lambda e, g=g, kc=kc, gs=gs: e.matmul(pst[:], lhsT=ktm[:, g * dk + kc * 128:g * dk + (kc + 1) * 128], rhs=xe[:, gs],
                                                                         start=True, stop=True), [ktm, xe], [pst])
                            self.V(lambda e, g=g, kc=kc, gs=gs: e.tensor_tensor(out=S[:, kc, gs].rearrange("p (h d) -> p h d", h=Hg),
                                                                                in0=S[:, kc, gs].rearrange("p (h d) -> p h d", h=Hg),
                                                                                in1=bcast(cd[:, g * Hg:(g + 1) * Hg].unsqueeze(2), [128, Hg, dv]),
                                                                                op=ALU.mult), [S, cd], [S])
                            self.V(lambda e, kc=kc, gs=gs: e.tensor_tensor(out=S[:, kc, gs], in0=S[:, kc, gs], in1=pst[:], op=ALU.add), [S, pst], [S])
                            self.A(lambda e, kc=kc, gs=gs: e.copy(out=Sb[:, kc, gs], in_=S[:, kc, gs]), [S], [Sb])
                    if dirn == 0:
                        self.st(self.yf[cs_, :], yc, yc[:])
                        continue
                    self.ld(yfl, yfl[:], self.yf[cs_, :])
                    self.ld(gt, gt[:], self.gate[cs_, :])
                    self.V(lambda e: e.tensor_tensor(out=yc[:], in0=yc[:], in1=yfl[:], op=ALU.add), [yc, yfl], [yc])
                    if ssd:
                        self.G(lambda e: e.tensor_tensor(out=yfl[:].rearrange("p (h d) -> p h d", h=H), in0=vt[:].rearrange("p (h d) -> p h d", h=H),
                                                         in1=bcast(dsk[:].unsqueeze(2), [128, H, dv]), op=ALU.mult), [vt, dsk], [yfl])
                        self.V(lambda e: e.tensor_tensor(out=yc[:], in0=yc[:], in1=yfl[:], op=ALU.add), [yc, yfl], [yc])
                        self.A(lambda e: e.activation(out=gt[:], in_=gt[:], func=AF.Silu), [gt], [gt])
                        self.V(lambda e: e.tensor_tensor(out=yc[:], in0=yc[:], in1=gt[:], op=ALU.mult), [yc, gt], [yc])
                        for g in range(4):
                            self.V(lambda e, g=g: e.bn_stats(out=st6[:, g, :], in_=yc[:, g * 512:(g + 1) * 512]), [yc], [st6])
                            self.V(lambda e, g=g: e.bn_aggr(out=mv[:, g, :], in_=st6[:, g, :]), [st6], [mv])
                        self.V(lambda e: e.tensor_tensor(out=ss[:], in0=mv[:, :, 0], in1=mv[:, :, 0], op=ALU.mult), [mv], [ss])
                        self.V(lambda e: e.tensor_tensor(out=ss[:], in0=ss[:], in1=mv[:, :, 1], op=ALU.add), [ss, mv], [ss])
                        self.A(lambda e: e.activation(out=ss[:], in_=ss[:], func=AF.Sqrt, bias=EPS), [ss], [ss])
                        self.V(lambda e: e.reciprocal(out=rstd[:], in_=ss[:]), [ss], [rstd])
                        for g in range(4):
                            gs = slice(g * 512, (g + 1) * 512)
                            self.V(lambda e, g=g, gs=gs: e.scalar_tensor_tensor(out=yb[:, gs], in0=yc[:, gs], scalar=rstd[:, g:g + 1], in1=nw[:, gs],
                                                                                op0=ALU.mult, op1=ALU.mult), [yc, rstd, nw], [yb])
                    else:
                        for g in range(4):
                            self.V(lambda e, g=g: e.bn_stats(out=st6[:, g, :], in_=yc[:, g * 512:(g + 1) * 512]), [yc], [st6])
                            self.V(lambda e, g=g: e.bn_aggr(out=mv[:, g, :], in_=st6[:, g, :]), [st6], [mv])
                        self.A(lambda e: e.activation(out=ss[:], in_=mv[:, :, 1], func=AF.Sqrt, bias=EPS), [mv], [ss])
                        self.V(lambda e: e.reciprocal(out=rstd[:], in_=ss[:]), [ss], [rstd])
                        self.A(lambda e: e.activation(out=gt[:], in_=gt[:], func=AF.Silu), [gt], [gt])
                        for g in range(4):
                            gs = slice(g * 512, (g + 1) * 512)
                            self.V(lambda e, g=g, gs=gs: e.tensor_scalar(out=yc[:, gs], in0=yc[:, gs], scalar1=mv[:, g, 0:1], scalar2=rstd[:, g:g + 1],
                                                                         op0=ALU.subtract, op1=ALU.mult), [yc, mv, rstd], [yc])
                        self.G(lambda e: e.tensor_tensor(out=yc[:], in0=yc[:], in1=nw[:], op=ALU.mult), [yc, nw], [yc])
                        self.V(lambda e: e.tensor_tensor(out=yc[:], in0=yc[:], in1=nb_[:], op=ALU.add), [yc, nb_], [yc])
                        self.V(lambda e: e.tensor_tensor(out=yb[:], in0=yc[:], in1=gt[:], op=ALU.mult), [yc, gt], [yb])
                    for half in range(2):
                        for kk in range(8):
                            k = half * 8 + kk
                            self.P(lambda e, k=k, kk=kk: e.transpose(ptr[:, kk, :], yb[:, k * 128:(k + 1) * 128], identb[:]), [yb, identb], [ptr])
                        self.A(lambda e, half=half: e.copy(out=yT[:, half * 8:(half + 1) * 8, :], in_=ptr[:]), [ptr], [yT])
                    for nn in range(2):
                        for k in range(16):
                            self.P(lambda e, k=k, nn=nn: e.matmul(pA[:, nn * 512:(nn + 1) * 512], lhsT=yT[:, k, :], rhs=Wo[:, k, nn * 512:(nn + 1) * 512],
                                                                  start=(k == 0), stop=(k == 15)), [yT, Wo], [pA])
                    self.ld(xt, xt[:], xcur[cs_, :])
                    self.V(lambda e: e.tensor_tensor(out=t1[:], in0=pA[:], in1=g1[v][:], op=ALU.mult), [pA, g1[v]], [t1])
                    self.V(lambda e: e.scalar_tensor_tensor(out=t2[:], in0=xt[:], scalar=ALPHA, in1=t1[:], op0=ALU.mult, op1=ALU.add), [xt, t1], [t2])
                    self.layer_norm(t2, t1, st2, mv2, rs2, lng, lnb)
                    self.st(xnext[cs_, :], t1, t1[:])
                    self.G(lambda e: e.tensor_tensor(out=t2[:], in0=t1[:], in1=sc2[v][:], op=ALU.mult), [t1, sc2[v]], [t2])
                    self.V(lambda e: e.tensor_tensor(out=t2[:], in0=t2[:], in1=sh2[v][:], op=ALU.add), [t2, sh2[v]], [t2])
                    self.st(self.tok[cs_, :], t2, t2[:])
                if dirn == 0:
                    fw.barrier()

    def layer_norm(self, xin_t, out_t, st2, mv2, rs2, lng, lnb):
        for hf in range(2):
            self.V(lambda e, hf=hf: e.bn_stats(out=st2[:, hf, :], in_=xin_t[:, hf * 512:(hf + 1) * 512]), [xin_t], [st2])
        self.V(lambda e: e.bn_aggr(out=mv2[:], in_=st2[:].rearrange("p a b -> p (a b)")), [st2], [mv2])
        self.A(lambda e: e.activation(out=rs2[:], in_=mv2[:, 1:2], func=AF.Sqrt, bias=EPS), [mv2], [rs2])
        self.V(lambda e: e.reciprocal(out=rs2[:], in_=rs2[:]), [rs2], [rs2])
        self.V(lambda e: e.tensor_scalar(out=out_t[:], in0=xin_t[:], scalar1=mv2[:, 0:1], scalar2=rs2[:, 0:1],
                                         op0=ALU.subtract, op1=ALU.mult), [xin_t, mv2, rs2], [out_t])
        self.G(lambda e: e.tensor_tensor(out=out_t[:], in0=out_t[:], in1=lng[:], op=ALU.mult), [out_t, lng], [out_t])
        self.V(lambda e: e.tensor_tensor(out=out_t[:], in0=out_t[:], in1=lnb[:], op=ALU.add), [out_t, lnb], [out_t])


def host_consts():
    bf = ml_dtypes.bfloat16
    c = {}
    c["c_identb"] = np.eye(128, dtype=np.float32).astype(bf)
    c["c_identf"] = np.eye(128, dtype=np.float32)
    s = np.arange(128)
    c["c_trif"] = np.stack([(s[:, None] <= s[None, :]), (s[:, None] >= s[None, :])]).astype(np.float32)
    c["c_ones"] = np.ones((128, 128), np.float32)
    c["c_slow"] = (s[:, None] < s[None, :]).astype(np.float32).astype(bf)
    n_freq = 64
    inv_freq = (10000.0 ** (-np.arange(n_freq, dtype=np.float32) / np.float32(n_freq))).astype(np.float32)
    pos = np.arange(4096)
    rows = (pos // 64).astype(np.float32)
    cols = (pos % 64).astype(np.float32)
    ang = np.concatenate([rows[:, None] * inv_freq[None, :], cols[:, None] * inv_freq[None, :]], -1).astype(np.float32)
    cos = np.ones((T, 128), np.float32)
    sin = np.zeros((T, 128), np.float32)
    cos[256:] = np.cos(ang)
    sin[256:] = np.sin(ang)
    c["c_rope"] = np.ascontiguousarray(np.stack([cos.T, sin.T])).astype(np.float32)
    c["c_iota"] = np.broadcast_to(np.arange(512, dtype=np.float32)[None, :], (128, 512)).copy()
    c["c_pidx"] = (np.arange(12)[None, :] * 128 + np.arange(128)[:, None]).astype(np.float32)
    return c


def host_inputs(inp, b):
    f = np.float32
    m = {}
    m["xin"] = np.ascontiguousarray(np.concatenate([inp["ctx"][b], inp["x"][b]], 0)).astype(f)
    cv = np.stack([inp["c"][b].reshape(8, 128).T, inp["c_ctx"].reshape(8, 128).T], -1)
    m["cvec"] = np.ascontiguousarray(cv).astype(f)
    m["mod_w"] = inp["mod_w"]
    m["mod_b"] = inp["mod_b"]
    m["ssd_in_w"] = inp["ssd_in_w"]
    cw = inp["ssd_conv_w"]
    m["convw"] = np.ascontiguousarray(cw.reshape(2, 5, 24, 128).transpose(0, 3, 2, 1)).astype(f)
    m["convb"] = np.ascontiguousarray(inp["ssd_conv_b"].reshape(2, 24, 128).transpose(0, 2, 1)).astype(f)
    m["ssd_dt_bias"] = np.ascontiguousarray(inp["ssd_dt_bias"].reshape(2, 64))
    m["ssd_a_log"] = np.ascontiguousarray(inp["ssd_a_log"].reshape(2, 64))
    m["ssd_d_skip"] = inp["ssd_d_skip"]
    m["ssd_norm_w"] = inp["ssd_norm_w"]
    m["ssd_out_w"] = inp["ssd_out_w"]
    m["ret_in_w"] = inp["ret_in_w"]
    m["ret_decay_logit"] = inp["ret_decay_logit"]
    m["ret_gn_w"] = inp["ret_gn_w"]
    m["ret_gn_b"] = inp["ret_gn_b"]
    m["ret_out_w"] = inp["ret_out_w"]
    for k in ("ln_mix_g", "ln_mix_b", "ln_ffn_g", "ln_ffn_b"):
        m[k] = inp[k]
    m["moe_rw"] = np.ascontiguousarray(np.concatenate([inp["moe_group_w"], inp["moe_expert_w"]], -1)).astype(f)
    m["moe_rb"] = np.ascontiguousarray(np.concatenate([inp["moe_group_b"], inp["moe_expert_b"]], -1)).astype(f)
    m["moe_w_gate_up"] = inp["moe_w_gate_up"].reshape(DEPTH * 32 * 128, 8 * D)
    m["moe_w_down"] = inp["moe_w_down"].reshape(DEPTH * 32 * 128, 4 * D)
    return m


def _phase_moe(self, li, xcur, xnext, final):
    fw = self.fw
    NT = NCH
    with fw.scope():
        DI = fw.sb("DI", [128, 2 * NT], I32)
        Wt = fw.sb("Wt", [128, 2 * NT], F32)
        IGU = fw.sb("IGU", [128, NB], I32)
        with fw.scope():
            identf = fw.sb("identf", [128, 128], F32)
            self.ld(identf, identf[:], self.c_identf)
            slow = fw.sb("slow", [128, 128], BF16)
            self.ld(slow, slow[:], self.c_slow)
            onesf = fw.sb("onesf", [128, 128], F32)
            self.ld(onesf, onesf[:], self.c_ones)
            onesb = fw.sb("onesb", [128, 128], BF16)
            self.V(lambda e: e.tensor_copy(out=onesb[:], in_=onesf[:]), [onesf], [onesb])
            iota = fw.sb("iota", [128, 512], F32)
            self.ld(iota, iota[:], self.c_iota)
            pidx = fw.sb("pidx", [128, 12], F32)
            self.ld(pidx, pidx[:], self.c_pidx)
            rw = fw.sb("rw", [128, 8, 36], F32)
            self.ld(rw, rw[:], self.moe_rw[li].rearrange("(k p) e -> p k e", p=128))
            rb = fw.sb("rb", [128, 36], F32)
            self.ld_row(rb, rb[:], self.moe_rb[li:li + 1, :])
            OH = fw.sb("OH", [128, 2 * NT, 32], F32)
            SL = fw.sb("SL", [128, 2 * NT], F32)
            Acum = fw.sb("Acum", [128, 32], F32)
            Acb = fw.sb("Acb", [128, 32], BF16)
            self.V(lambda e: e.memset(Acum[:], 0.0), [], [Acum])
            self.V(lambda e: e.memset(Acb[:], 0.0), [], [Acb])
            tk = fw.sb("tk", [128, D], F32)
            tkT = fw.sb("tkT", [128, 8, 128], F32)
            L = fw.sb("L", [128, 36], F32)
            gmax = fw.sb("gmax", [128, 1], F32)
            ngmax = fw.sb("ngmax", [128, 1], F32)
            goh = fw.sb("goh", [128, 4], F32)
            pen = fw.sb("pen", [128, 4], F32)
            ex = fw.sb("ex", [128, 4], F32)
            gs = fw.sb("gs", [128, 1], F32)
            em = fw.sb("em", [128, 32], F32)
            em2 = fw.sb("em2", [128, 32], F32)
            m1 = fw.sb("m1", [128, 1], F32)
            m2 = fw.sb("m2", [128, 1], F32)
            dd = fw.sb("dd", [128, 1], F32)
            den = fw.sb("den", [128, 1], F32)
            A_ = fw.sb("A_", [128, 32], F32)
            Ab = fw.sb("Ab", [128, 32], BF16)
            Pp = fw.sb("Pp", [128, 32], F32)
            junk = fw.sb("junk", [128, 32], F32)
            pT = fw.ps("pT", [128, 8, 128], F32)
            plog = fw.ps("plog", [128, 512], F32)
            pP = fw.ps("pP", [128, 512], F32)
            for c in range(NT):
                cs_ = slice(c * 128, (c + 1) * 128)
                self.ld(tk, tk[:], self.tok[cs_, :])
                for k in range(8):
                    self.P(lambda e, k=k: e.transpose(pT[:, k, :], tk[:, k * 128:(k + 1) * 128], identf[:]), [tk, identf], [pT])
                self.A(lambda e: e.copy(out=tkT[:], in_=pT[:]), [pT], [tkT])
                for k in range(8):
                    self.P(lambda e, k=k: e.matmul(plog[:, 0:36], lhsT=tkT[:, k, :], rhs=rw[:, k, :], start=(k == 0), stop=(k == 7)),
                           [tkT, rw], [plog])
                self.V(lambda e: e.tensor_tensor(out=L[:], in0=plog[:, 0:36], in1=rb[:], op=ALU.add), [plog, rb], [L])
                self.V(lambda e: e.reduce_max(out=gmax[:], in_=L[:, 0:4], axis=AX.X), [L], [gmax])
                self.V(lambda e: e.tensor_scalar(out=goh[:], in0=L[:, 0:4], scalar1=gmax[:, 0:1], scalar2=None, op0=ALU.is_equal), [L, gmax], [goh])
                self.V(lambda e: e.tensor_scalar_mul(out=ngmax[:], in0=gmax[:], scalar1=-1.0), [gmax], [ngmax])
                self.A(lambda e: e.activation(out=ex[:], in_=L[:, 0:4], func=AF.Exp, bias=ngmax[:, 0:1], scale=1.0), [L, ngmax], [ex])
                self.V(lambda e: e.reduce_sum(out=gs[:], in_=ex[:], axis=AX.X), [ex], [gs])
                self.V(lambda e: e.reciprocal(out=gs[:], in_=gs[:]), [gs], [gs])
                self.V(lambda e: e.tensor_scalar(out=pen[:], in0=goh[:], scalar1=1e30, scalar2=-1e30, op0=ALU.mult, op1=ALU.add), [goh], [pen])
                self.V(lambda e: e.tensor_tensor(out=em[:].rearrange("p (g j) -> p g j", g=4), in0=L[:, 4:36].rearrange("p (g j) -> p g j", g=4),
                                                 in1=bcast(pen[:].unsqueeze(2), [128, 4, 8]), op=ALU.add), [L, pen], [em])
                self.V(lambda e: e.reduce_max(out=m1[:], in_=em[:], axis=AX.X), [em], [m1])
                o1 = OH[:, 2 * c, :]
                o2 = OH[:, 2 * c + 1, :]
                self.V(lambda e, o1=o1: e.tensor_scalar(out=o1, in0=em[:], scalar1=m1[:, 0:1], scalar2=None, op0=ALU.is_equal), [em, m1], [OH])
                self.V(lambda e, o1=o1: e.scalar_tensor_tensor(out=em2[:], in0=o1, scalar=-1e30, in1=em[:], op0=ALU.mult, op1=ALU.add), [OH, em], [em2])
                self.V(lambda e: e.reduce_max(out=m2[:], in_=em2[:], axis=AX.X), [em2], [m2])
                self.V(lambda e, o2=o2: e.tensor_scalar(out=o2, in0=em2[:], scalar1=m2[:, 0:1], scalar2=None, op0=ALU.is_equal), [em2, m2], [OH])
                self.V(lambda e: e.tensor_tensor(out=dd[:], in0=m2[:], in1=m1[:], op=ALU.subtract), [m1, m2], [dd])
                self.A(lambda e: e.activation(out=dd[:], in_=dd[:], func=AF.Exp), [dd], [dd])
                self.V(lambda e: e.tensor_scalar_add(out=den[:], in0=dd[:], scalar1=1.0), [dd], [den])
                self.V(lambda e: e.reciprocal(out=den[:], in_=den[:]), [den], [den])
                self.V(lambda e, c=c: e.tensor_tensor(out=Wt[:, 2 * c:2 * c + 1], in0=den[:], in1=gs[:], op=ALU.mult), [den, gs], [Wt])
                self.V(lambda e, c=c: e.tensor_tensor(out=Wt[:, 2 * c + 1:2 * c + 2], in0=Wt[:, 2 * c:2 * c + 1], in1=dd[:], op=ALU.mult), [Wt, dd], [Wt])
                self.V(lambda e, o1=o1, o2=o2: e.tensor_tensor(out=A_[:], in0=o1, in1=o2, op=ALU.add), [OH], [A_])
                self.V(lambda e: e.tensor_copy(out=Ab[:], in_=A_[:]), [A_], [Ab])
                self.P(lambda e: e.matmul(pP[:, 0:32], lhsT=slow[:], rhs=Ab[:], start=True, stop=False), [slow, Ab], [pP])
                self.P(lambda e: e.matmul(pP[:, 0:32], lhsT=onesb[:], rhs=Acb[:], start=False, stop=True), [onesb, Acb], [pP])
                self.V(lambda e: e.tensor_copy(out=Pp[:], in_=pP[:, 0:32]), [pP], [Pp])
                for k, ok in enumerate((o1, o2)):
                    self.V(lambda e, ok=ok: e.tensor_tensor(out=junk[:], in0=ok, in1=Pp[:], op=ALU.mult), [OH, Pp], [junk])
                    self.V(lambda e, c=c, k=k: e.reduce_sum(out=SL[:, 2 * c + k:2 * c + k + 1], in_=junk[:], axis=AX.X), [junk], [SL])
                self.V(lambda e: e.tensor_tensor(out=Acum[:], in0=Acum[:], in1=A_[:], op=ALU.add), [Acum, A_], [Acum])
                self.V(lambda e: e.tensor_copy(out=Acb[:], in_=Acum[:]), [Acum], [Acb])
            cnt = fw.sb("cnt", [128, 32], F32)
            self.P(lambda e: e.matmul(pP[:, 0:32], lhsT=onesb[:], rhs=Acb[:], start=True, stop=True), [onesb, Acb], [pP])
            self.V(lambda e: e.tensor_copy(out=cnt[:], in_=pP[:, 0:32]), [pP], [cnt])
            thr = fw.sb("thr", [128, 34], F32)
            self.V(lambda e: e.tensor_scalar_mul(out=thr[:], in0=iota[:, 0:34], scalar1=128.0), [iota], [thr])
            cmp = fw.sb("cmp", [128, 32, 34], F32)
            self.V(lambda e: e.tensor_tensor(out=cmp[:], in0=bcast(cnt[:].unsqueeze(2), [128, 32, 34]), in1=bcast(thr[:].unsqueeze(1), [128, 32, 34]),
                                             op=ALU.is_gt), [cnt, thr], [cmp])
            nblk = fw.sb("nblk", [128, 32], F32)
            self.V(lambda e: e.reduce_sum(out=nblk[:], in_=cmp[:], axis=AX.X), [cmp], [nblk])
            pa = fw.sb("pa", [128, 32], F32)
            pb_ = fw.sb("pb", [128, 32], F32)
            self.V(lambda e: e.tensor_copy(out=pa[:], in_=nblk[:]), [nblk], [pa])
            cur, oth = pa, pb_
            for sft in (1, 2, 4, 8, 16):
                self.V(lambda e, cur=cur, oth=oth: e.tensor_copy(out=oth[:], in_=cur[:]), [cur], [oth])
                self.V(lambda e, cur=cur, oth=oth, sft=sft: e.tensor_tensor(out=oth[:, sft:32], in0=cur[:, sft:32], in1=cur[:, 0:32 - sft], op=ALU.add),
                       [cur], [oth])
                cur, oth = oth, cur
            pend = cur
            pstart = fw.sb("pstart", [128, 32], F32)
            self.V(lambda e: e.tensor_tensor(out=pstart[:], in0=pend[:], in1=nblk[:], op=ALU.subtract), [pend, nblk], [pstart])
            self.V(lambda e: e.tensor_scalar_mul(out=pstart[:], in0=pstart[:], scalar1=128.0), [pstart], [pstart])
            big = fw.sb("big", [128, 2 * NT, 32], F32)
            self.V(lambda e: e.tensor_tensor(out=big[:], in0=OH[:], in1=bcast(pstart[:].unsqueeze(1), [128, 2 * NT, 32]), op=ALU.mult), [OH, pstart], [big])
            dst = fw.sb("dstf", [128, 2 * NT], F32)
            self.V(lambda e: e.reduce_sum(out=dst[:], in_=big[:], axis=AX.X), [big], [dst])
            self.V(lambda e: e.tensor_tensor(out=dst[:], in0=dst[:], in1=SL[:], op=ALU.add), [dst, SL], [dst])
            self.V(lambda e: e.tensor_copy(out=DI[:], in_=dst[:]), [dst], [DI])
            cmp2 = fw.sb("cmp2", [128, NB, 32], F32)
            self.V(lambda e: e.tensor_tensor(out=cmp2[:], in0=bcast(pend[:].unsqueeze(1), [128, NB, 32]), in1=bcast(iota[:, 0:NB].unsqueeze(2), [128, NB, 32]),
                                             op=ALU.is_le), [pend, iota], [cmp2])
            be = fw.sb("be", [128, NB], F32)
            self.V(lambda e: e.reduce_sum(out=be[:], in_=cmp2[:], axis=AX.X), [cmp2], [be])
            self.V(lambda e: e.tensor_scalar_min(out=be[:], in0=be[:], scalar1=31.0), [be], [be])
            same = fw.sb("same", [128, NB], F32)
            self.V(lambda e: e.memset(same[:], 0.0), [], [same])
            self.V(lambda e: e.tensor_tensor(out=same[:, 2:NB], in0=be[:, 2:NB], in1=be[:, 0:NB - 2], op=ALU.is_equal), [be], [same])
            self.V(lambda e: e.tensor_scalar(out=be[:], in0=be[:], scalar1=128.0, scalar2=float(li * 32 * 128), op0=ALU.mult, op1=ALU.add), [be], [be])
            self.V(lambda e: e.scalar_tensor_tensor(out=be[:], in0=same[:], scalar=1.0e6, in1=be[:], op0=ALU.mult, op1=ALU.add), [same, be], [be])
            self.V(lambda e: e.tensor_tensor(out=be[:], in0=be[:], in1=bcast(pidx[:, 0:1], [128, NB]), op=ALU.add), [be, pidx], [be])
            self.V(lambda e: e.tensor_copy(out=IGU[:], in_=be[:]), [be], [IGU])
        with fw.scope():
            tk = fw.sb("tk", [128, D], F32)
            tkb = fw.sb("tkb", [128, D], BF16)
            for c in range(NT):
                self.ld(tk, tk[:], self.tok[c * 128:(c + 1) * 128, :])
                self.V(lambda e: e.tensor_copy(out=tkb[:], in_=tk[:]), [tk], [tkb])
                for k in range(2):
                    col = 2 * c + k
                    self.fw.dma("gpsimd", lambda e, col=col: e.indirect_dma_start(
                        out=self.xbuf, out_offset=bass.IndirectOffsetOnAxis(ap=DI[:, col:col + 1], axis=0), in_=tkb[:, :], in_offset=None),
                        reads=[tkb, DI])
        with fw.scope():
            identb = fw.sb("identb", [128, 128], BF16)
            self.ld(identb, identb[:], self.c_identb)
            xb = [fw.sb("xb%d" % i, [128, D], BF16) for i in range(2)]
            xT = [fw.sb("xT%d" % i, [128, 8, 128], BF16) for i in range(2)]
            g32 = [fw.sb("g32_%d" % i, [128, 8, D], F32) for i in range(2)]
            d32 = [fw.sb("d32_%d" % i, [128, 4, D], F32) for i in range(2)]
            gbf = [fw.sb("gbf_%d" % i, [128, 8, D], BF16) for i in range(2)]
            dbf = [fw.sb("dbf_%d" % i, [128, 4, D], BF16) for i in range(2)]
            sg = fw.sb("sg", [128, 4, 128], F32)
            hT = fw.sb("hT", [128, 4, 128], BF16)
            ob = fw.sb("ob", [128, D], F32)
            ptr = fw.ps("ptr", [128, 8, 128], BF16)
            pH = [fw.ps("pH%d" % i, [128, 8, 128], F32) for i in range(2)]
            pO = fw.ps("pO", [128, D], F32)
            if getattr(self, "_bcreg", None) is None:
                self._bcreg = self.nc.gpsimd.alloc_register("bcreg")
                self.nc.gpsimd.reg_mov(self._bcreg, DEPTH * 32 * 128 - 1)
            bcreg = self._bcreg
            for b in range(NB):
                i = b % 2
                self.ld(xb[i], xb[i][:], self.xbuf[b * 128:(b + 1) * 128, :])
                idx = IGU[:, b:b + 1]
                self.fw.dma("gpsimd", lambda e, i=i, idx=idx: e.indirect_dma_start(
                    out=g32[i][:].rearrange("p k n -> p (k n)"), out_offset=None, in_=self.moe_wgu,
                    in_offset=bass.IndirectOffsetOnAxis(ap=idx, axis=0), bounds_check=bcreg, oob_is_err=False),
                    reads=[IGU], writes=[g32[i]])
                self.fw.dma("gpsimd", lambda e, i=i, idx=idx: e.indirect_dma_start(
                    out=d32[i][:].rearrange("p k n -> p (k n)"), out_offset=None, in_=self.moe_wd,
                    in_offset=bass.IndirectOffsetOnAxis(ap=idx, axis=0), bounds_check=bcreg, oob_is_err=False),
                    reads=[IGU], writes=[d32[i]])
                xbv = xb[i][:].rearrange("s (p k) -> s k p", k=8)
                for k in range(8):
                    self.P(lambda e, k=k, xbv=xbv: e.transpose(ptr[:, k, :], xbv[:, k, :], identb[:]), [xb[i], identb], [ptr])
                self.V(lambda e, i=i: e.tensor_copy(out=xT[i][:], in_=ptr[:]), [ptr], [xT[i]])
                for q in range(4):
                    sl = slice(q * 2, q * 2 + 2)
                    if q % 2 == 0:
                        self.A(lambda e, i=i, sl=sl: e.copy(out=gbf[i][:, sl, :], in_=g32[i][:, sl, :]), [g32[i]], [gbf[i]])
                    else:
                        self.V(lambda e, i=i, sl=sl: e.tensor_copy(out=gbf[i][:, sl, :], in_=g32[i][:, sl, :]), [g32[i]], [gbf[i]])
                self.A(lambda e, i=i: e.copy(out=dbf[i][:, 0:2, :], in_=d32[i][:, 0:2, :]), [d32[i]], [dbf[i]])
                self.V(lambda e, i=i: e.tensor_copy(out=dbf[i][:, 2:4, :], in_=d32[i][:, 2:4, :]), [d32[i]], [dbf[i]])
                ph = pH[i]
                for m in range(8):
                    tt, kh = m // 4, m % 4
                    for k in range(8):
                        lw = gbf[i][:, k, :].rearrange("d (two p k) -> d two k p", two=2, k=4)[:, tt, kh, :]
                        self.P(lambda e, i=i, m=m, k=k, ph=ph, lw=lw: e.matmul(ph[:, m, :], lhsT=lw, rhs=xT[i][:, k, :],
                                                                               start=(k == 0), stop=(k == 7)), [gbf[i], xT[i]], [ph])
                self.A(lambda e, ph=ph: e.activation(out=sg[:], in_=ph[:, 0:4, :], func=AF.Silu), [ph], [sg])
                self.V(lambda e, ph=ph: e.tensor_tensor(out=hT[:], in0=sg[:], in1=ph[:, 4:8, :], op=ALU.mult), [sg, ph], [hT])
                for nn in range(2):
                    for k in range(4):
                        self.P(lambda e, i=i, nn=nn, k=k: e.matmul(pO[:, nn * 512:(nn + 1) * 512], lhsT=hT[:, k, :], rhs=dbf[i][:, k, nn * 512:(nn + 1) * 512],
                                                                   start=(k == 0), stop=(k == 3)), [hT, dbf[i]], [pO])
                self.A(lambda e: e.copy(out=ob[:], in_=pO[:]), [pO], [ob])
                self.st(self.ybuf[b * 128:(b + 1) * 128, :], ob, ob[:])
        with fw.scope():
            g2 = []
            for v in range(2):
                t = fw.sb("g2_%d" % v, [128, D], F32)
                self.ld_row(t, t[:], self.modrow[v:v + 1, 5 * D:6 * D])
                g2.append(t)
            lng = fw.sb("lng", [128, D], F32)
            lnb = fw.sb("lnb", [128, D], F32)
            self.ld_row(lng, lng[:], self.ln_ffn_g[li:li + 1, :])
            self.ld_row(lnb, lnb[:], self.ln_ffn_b[li:li + 1, :])
            o1 = fw.sb("o1", [128, D], F32)
            o2 = fw.sb("o2", [128, D], F32)
            xt = fw.sb("xt", [128, D], F32)
            t1 = fw.sb("t1", [128, D], F32)
            st2 = fw.sb("st2", [128, 2, 6], F32)
            mv2 = fw.sb("mv2", [128, 2], F32)
            rs2 = fw.sb("rs2", [128, 1], F32)
            for c in range(NT):
                if final and c < NCTX:
                    continue
                v = 1 if c < NCTX else 0
                cs_ = slice(c * 128, (c + 1) * 128)
                for k, ot in enumerate((o1, o2)):
                    col = 2 * c + k
                    self.fw.dma("gpsimd", lambda e, col=col, ot=ot: e.indirect_dma_start(
                        out=ot[:, :], out_offset=None, in_=self.ybuf, in_offset=bass.IndirectOffsetOnAxis(ap=DI[:, col:col + 1], axis=0)),
                        reads=[DI], writes=[ot])
                self.ld(xt, xt[:], xcur[cs_, :])
                self.V(lambda e, c=c: e.tensor_scalar(out=o1[:], in0=o1[:], scalar1=Wt[:, 2 * c:2 * c + 1], scalar2=None, op0=ALU.mult), [o1, Wt], [o1])
                self.V(lambda e, c=c: e.scalar_tensor_tensor(out=o1[:], in0=o2[:], scalar=Wt[:, 2 * c + 1:2 * c + 2], in1=o1[:], op0=ALU.mult, op1=ALU.add),
                       [o2, Wt, o1], [o1])
                self.G(lambda e, v=v: e.tensor_tensor(out=o1[:], in0=o1[:], in1=g2[v][:], op=ALU.mult), [o1, g2[v]], [o1])
                self.V(lambda e: e.scalar_tensor_tensor(out=o2[:], in0=xt[:], scalar=ALPHA, in1=o1[:], op0=ALU.mult, op1=ALU.add), [xt, o1], [o2])
                self.layer_norm(o2, t1, st2, mv2, rs2, lng, lnb)
                if final:
                    self.st(self.yout[(c - NCTX) * 128:(c - NCTX + 1) * 128, :], t1, t1[:])
                else:
                    self.st(xnext[cs_, :], t1, t1[:])


Builder.phase_moe = _phase_moe


def build_program(nlayers=DEPTH):
    nc = bass.Bass("TRN2", target_bir_lowering=False)
    b = Builder(nc)
    b.declare()
    xcur = b.xin
    for li in range(nlayers):
        j = li // 2
        b.phase_mod(li)
        if li % 2 == 0:
            b.phase_in_ssd(li, j, xcur)
            b.phase_scan(li, j, True, xcur, b.xA)
        else:
            b.phase_in_ret(li, j, xcur)
            b.phase_scan(li, j, False, xcur, b.xA)
        b.phase_moe(li, b.xA, b.xB, final=(li == nlayers - 1))
        xcur = b.xB
    b.fw.barrier()
    b.fw.root.close()
    return nc


def kernel(**inputs):
    inp = {k: np.asarray(v) for k, v in inputs.items()}
    nc = build_program()
    consts = host_consts()
    maps = []
    for c in range(8):
        m = host_inputs(inp, c)
        m.update(consts)
        maps.append(m)
    res = run_bass_kernel_spmd(nc, maps, core_ids=list(range(8)))
    out = np.stack([np.asarray(res.results[c]["yout"]) for c in range(8)], 0)
    return out.astype(np.float32)


def _phase_scan2(self, li, j, ssd, xcur, xnext):
    fw = self.fw
    G = 4
    Hg = 8 if ssd else 1
    KC = 1 if ssd else 2
    H = G * Hg
    dv = 2048 // H
    dk = KC * 128
    with fw.scope():
        identb = fw.sb("identb", [128, 128], BF16)
        self.ld(identb, identb[:], self.c_identb)
        ones = fw.sb("ones", [128, 128], F32)
        self.ld(ones, ones[:], self.c_ones)
        tri = fw.sb("tri", [128, 128], F32)
        Wo = fw.sb("Wo", [128, 16, D], BF16)
        owv = (self.ssd_out_w if ssd else self.ret_out_w)[j].rearrange("(k p) n -> p k n", p=128)
        with fw.scope():
            wst = fw.sb("wstO", [128, 4, D], F32)
            for q in range(4):
                self.ld(wst, wst[:], owv[:, q * 4:(q + 1) * 4, :])
                self.G(lambda e, q=q: e.tensor_copy(out=Wo[:, q * 4:(q + 1) * 4, :], in_=wst[:]), [wst], [Wo])
        g1 = fw.sb("g1", [128, D], F32)
        sh2 = fw.sb("sh2", [128, D], F32)
        sc2 = fw.sb("sc2", [128, D], F32)

        def load_rows(v):
            self.ld_row(g1, g1[:], self.modrow[v:v + 1, 2 * D:3 * D])
            self.ld_row(sh2, sh2[:], self.modrow[v:v + 1, 3 * D:4 * D])
            self.ld_row(sc2, sc2[:], self.modrow[v:v + 1, 4 * D:5 * D])
            self.V(lambda e: e.tensor_scalar_add(out=sc2[:], in0=sc2[:], scalar1=1.0), [sc2], [sc2])

        lng = fw.sb("lng", [128, D], F32)
        lnb = fw.sb("lnb", [128, D], F32)
        self.ld_row(lng, lng[:], self.ln_mix_g[li:li + 1, :])
        self.ld_row(lnb, lnb[:], self.ln_mix_b[li:li + 1, :])
        nw = fw.sb("nw", [128, 2048], F32)
        self.ld_row(nw, nw[:], (self.ssd_norm_w if ssd else self.ret_gn_w)[j:j + 1, :])
        if ssd:
            dsk = fw.sb("dsk", [128, 32], F32)
            self.ld_row(dsk, dsk[:], self.ssd_d_skip[j:j + 1, :])
        else:
            nb_ = fw.sb("nb", [128, 2048], F32)
            self.ld_row(nb_, nb_[:], self.ret_gn_b[j:j + 1, :])
        S = fw.sb("S", [128, KC, 2048], F32)
        Sb = fw.sb("Sb", [128, KC, 2048], BF16)

        def dbl(name, shape, dt):
            return [fw.sb(name + "0", shape, dt), fw.sb(name + "1", shape, dt)]

        qt = dbl("qt", [128, G * KC, 128], BF16)
        kt = fw.sb("kt", [128, G * KC, 128], BF16)
        ktm = dbl("ktm", [128, G * dk], BF16)
        vt = dbl("vt", [128, 2048], BF16)
        xdt = dbl("xdt", [128, 2048], BF16) if ssd else vt
        xe = dbl("xe", [128, 2048], BF16)
        cbm = dbl("cbm", [128, G, 128], F32)
        la = fw.sb("la", [128, 64], F32)
        dt = fw.sb("dt", [128, 64], F32)
        acs = fw.sb("acs", [128, 2 * H], F32)
        nacs = fw.sb("nacs", [128, H], F32)
        wend = fw.sb("wend", [128, H], F32)
        if ssd:
            ea = dbl("ea", [128, H], F32)
            cd = dbl("cd", [128, H], F32)
            E = [[fw.sb("E%d_%d" % (p_, g), [128, Hg, 128], BF16) for g in range(G)] for p_ in range(2)]
        else:
            ea0 = fw.sb("ea", [128, H], F32)
            cd0 = fw.sb("cd", [128, H], F32)
            ea = [ea0, ea0]
            cd = [cd0, cd0]
            E0 = [fw.sb("E_%d" % g, [128, Hg, 128], F32) for g in range(G)]
            E = [E0, E0]
        lt = dbl("lt", [128, Hg, 128], F32)
        nlb = dbl("nlb", [128, Hg, 128], F32)
        nones = fw.sb("nones", [128, 128], F32)
        self.V(lambda e: e.memset(nones[:], -1.0), [], [nones])
        M = dbl("M", [128, Hg, 128], BF16)
        yc = fw.sb("yc", [128, 2048], F32)
        tmp = dbl("tmp", [128, 512], F32)
        yfl = fw.sb("yfl", [128, 2048], F32)
        gt = fw.sb("gt", [128, 2048], F32)
        yb = fw.sb("yb", [128, 2048], BF16)
        yT = fw.sb("yT", [128, 16, 128], BF16)
        st6 = fw.sb("st6", [128, 4, 6], F32)
        mv = fw.sb("mv", [128, 4, 2], F32)
        ss = fw.sb("ss", [128, 4], F32)
        rstd = fw.sb("rstd", [128, 4], F32)
        xt = fw.sb("xt", [128, D], F32)
        t1 = fw.sb("t1", [128, D], F32)
        t2 = fw.sb("t2", [128, D], F32)
        st2 = fw.sb("st2", [128, 2, 6], F32)
        mv2 = fw.sb("mv2", [128, 2], F32)
        rs2 = fw.sb("rs2", [128, 1], F32)
        psm = fw.ps("psm", [128, 512], F32)
        pcb = fw.ps("pcb", [128, 512], F32)
        pbc = [fw.ps("pbc%d" % i, [128, 512], F32) for i in range(2)]
        pyd = fw.ps("pyd", [128, 512], F32)
        pyo = fw.ps("pyo", [128, 512], F32)
        pst = fw.ps("pst", [128, 512], F32)
        ptr = fw.ps("ptr", [128, 8, 128], BF16)

        qtv = self.QT.rearrange("g k p t -> p g k t")
        ktv = self.KT.rearrange("g k p t -> p g k t")
        r3 = lambda ap, h: ap.rearrange("p (h d) -> p h d", h=h)

        def decay_prep(dirn, par):
            lad = la[:, dirn * 32:dirn * 32 + H] if ssd else la[:, 0:H]
            self.P(lambda e: e.matmul(psm[:, 0:H], lhsT=tri[:], rhs=lad, start=True, stop=True), [tri, la], [psm])
            self.P(lambda e: e.matmul(psm[:, H:2 * H], lhsT=ones[:], rhs=lad, start=True, stop=True), [ones, la], [psm])
            self.V(lambda e: e.tensor_copy(out=acs[:], in_=psm[:, 0:2 * H]), [psm], [acs])
            self.V(lambda e: e.tensor_scalar_mul(out=nacs[:], in0=acs[:, 0:H], scalar1=-1.0), [acs], [nacs])
            self.A(lambda e: e.activation(out=ea[par][:], in_=acs[:, 0:H], func=AF.Exp), [acs], [ea[par]])
            self.V(lambda e: e.tensor_tensor(out=wend[:], in0=acs[:, H:2 * H], in1=acs[:, 0:H], op=ALU.subtract), [acs], [wend])
            self.A(lambda e: e.activation(out=wend[:], in_=wend[:], func=AF.Exp), [wend], [wend])
            self.A(lambda e: e.activation(out=cd[par][:], in_=acs[:, H:2 * H], func=AF.Exp), [acs], [cd[par]])
            for g in range(G):
                ltg = lt[g % 2]
                nlg = nlb[g % 2]
                ladg = la[:, dirn * 32 + g * Hg:dirn * 32 + (g + 1) * Hg] if ssd else la[:, g:g + 1]
                self.G(lambda e, ladg=ladg, ltg=ltg: e.tensor_tensor(out=ltg[:], in0=bcast(tri[:].unsqueeze(1), [128, Hg, 128]),
                                                                     in1=bcast(ladg.unsqueeze(2), [128, Hg, 128]), op=ALU.mult), [tri, la], [ltg])
                self.G(lambda e, ladg=ladg, nlg=nlg: e.tensor_tensor(out=nlg[:], in0=bcast(nones[:].unsqueeze(1), [128, Hg, 128]),
                                                                     in1=bcast(ladg.unsqueeze(2), [128, Hg, 128]), op=ALU.mult), [nones, la], [nlg])
                Eg = E[par][g]
                for q in range((Hg + 3) // 4):
                    nh = min(4, Hg - q * 4)
                    pb = pbc[q % 2]
                    self.P(lambda e, q=q, nh=nh, pb=pb, ltg=ltg: e.matmul(pb[:, 0:nh * 128], lhsT=ones[:], rhs=ltg[:, q * 4:q * 4 + nh, :],
                                                                          start=True, stop=False), [ones, ltg], [pb])
                    self.P(lambda e, q=q, nh=nh, pb=pb, nlg=nlg: e.matmul(pb[:, 0:nh * 128], lhsT=tri[:], rhs=nlg[:, q * 4:q * 4 + nh, :],
                                                                          start=False, stop=True), [tri, nlg], [pb])
                    self.A(lambda e, q=q, nh=nh, pb=pb, Eg=Eg: e.activation(out=Eg[:, q * 4:q * 4 + nh, :].rearrange("p h l -> p (h l)"), in_=pb[:, 0:nh * 128],
                                                                            func=AF.Exp), [pb], [Eg])

        def stage1(c, dirn, par):
            cs_ = slice(c * 128, (c + 1) * 128)
            self.ld(qt[par], qt[par][:].rearrange("p (g k) t -> p g k t", g=G), qtv[:, :, 0:KC, cs_])
            self.ld(kt, kt[:].rearrange("p (g k) t -> p g k t", g=G), ktv[:, :, 0:KC, cs_])
            self.ld(ktm[par], ktm[par][:], self.Ktm[cs_, 0:G * dk])
            self.ld(vt[par], vt[par][:], self.Vtm[cs_, :])
            if ssd:
                self.ld(la, la[:], self.latm[cs_, :])
                self.ld(dt, dt[:], self.dttm[cs_, :])
                decay_prep(dirn, par)
                self.V(lambda e: e.tensor_tensor(out=r3(xdt[par][:], H), in0=r3(vt[par][:], H),
                                                 in1=bcast(dt[:, dirn * 32:dirn * 32 + 32].unsqueeze(2), [128, H, dv]), op=ALU.mult),
                       [vt[par], dt], [xdt[par]])
            self.G(lambda e: e.tensor_tensor(out=r3(xe[par][:], H), in0=r3(xdt[par][:], H),
                                             in1=bcast(wend[:].unsqueeze(2), [128, H, dv]), op=ALU.mult), [xdt[par], wend], [xe[par]])
            for g in range(G):
                for kc in range(KC):
                    self.P(lambda e, g=g, kc=kc: e.matmul(pcb[:, g * 128:(g + 1) * 128], lhsT=kt[:, g * KC + kc, :], rhs=qt[par][:, g * KC + kc, :],
                                                          start=(kc == 0), stop=(kc == KC - 1)), [kt, qt[par]], [pcb])
            self.V(lambda e: e.tensor_tensor(out=cbm[par][:], in0=pcb[:].rearrange("p (g l) -> p g l", g=G),
                                             in1=bcast(tri[:].unsqueeze(1), [128, G, 128]), op=ALU.mult), [pcb, tri], [cbm[par]])

        def emitM(g, par):
            Mg = M[g % 2]
            self.V(lambda e: e.scalar_tensor_tensor(out=Mg[:], in0=E[par][g][:], scalar=1.0,
                                                    in1=bcast(cbm[par][:, g:g + 1, :], [128, Hg, 128]), op0=ALU.min, op1=ALU.mult),
                   [E[par][g], cbm[par]], [Mg])

        def stage2(c, dirn, par, v):
            cs_ = slice(c * 128, (c + 1) * 128)
            emitM(0, par)
            for g in range(G):
                gs = slice(g * 512, (g + 1) * 512)
                if g + 1 < G:
                    emitM(g + 1, par)
                Mg = M[g % 2]
                tg = tmp[g % 2]
                for hh in range(Hg):
                    h = g * Hg + hh
                    self.P(lambda e, h=h, hh=hh, Mg=Mg: e.matmul(pyd[:, hh * dv:(hh + 1) * dv], lhsT=Mg[:, hh, :], rhs=xdt[par][:, h * dv:(h + 1) * dv],
                                                                 start=True, stop=True), [Mg, xdt[par]], [pyd])
                for kc in range(KC):
                    self.P(lambda e, g=g, kc=kc, gs=gs: e.matmul(pyo[:], lhsT=qt[par][:, g * KC + kc, :], rhs=Sb[:, kc, gs],
                                                                 start=(kc == 0), stop=(kc == KC - 1)), [qt[par], Sb], [pyo])
                self.V(lambda e, g=g, tg=tg: e.tensor_tensor(out=r3(tg[:], Hg), in0=r3(pyo[:], Hg),
                                                             in1=bcast(ea[par][:, g * Hg:(g + 1) * Hg].unsqueeze(2), [128, Hg, dv]), op=ALU.mult),
                       [pyo, ea[par]], [tg])
                self.V(lambda e, gs=gs, tg=tg: e.tensor_tensor(out=yc[:, gs], in0=tg[:], in1=pyd[:], op=ALU.add), [tg, pyd], [yc])
                for kc in range(KC):
                    self.P(lambda e, g=g, kc=kc, gs=gs: e.matmul(pst[:], lhsT=ktm[par][:, g * dk + kc * 128:g * dk + (kc + 1) * 128], rhs=xe[par][:, gs],
                                                                 start=True, stop=True), [ktm[par], xe[par]], [pst])
                    self.G(lambda e, g=g, kc=kc, gs=gs: e.tensor_tensor(out=r3(S[:, kc, gs], Hg), in0=r3(S[:, kc, gs], Hg),
                                                                        in1=bcast(cd[par][:, g * Hg:(g + 1) * Hg].unsqueeze(2), [128, Hg, dv]),
                                                                        op=ALU.mult), [S, cd[par]], [S])
                    self.V(lambda e, kc=kc, gs=gs: e.tensor_tensor(out=S[:, kc, gs], in0=S[:, kc, gs], in1=pst[:], op=ALU.add), [S, pst], [S])
                    self.A(lambda e, kc=kc, gs=gs: e.copy(out=Sb[:, kc, gs], in_=S[:, kc, gs]), [S], [Sb])
            if dirn == 0:
                deferred.append(lambda: self.st(self.yf[cs_, :], yc, yc[:]))
                return
            vtp = vt[par]
            self.ld(yfl, yfl[:], self.yf[cs_, :])
            self.ld(gt, gt[:], self.gate[cs_, :])
            self.V(lambda e: e.tensor_tensor(out=yc[:], in0=yc[:], in1=yfl[:], op=ALU.add), [yc, yfl], [yc])
            if ssd:
                self.G(lambda e: e.tensor_tensor(out=r3(yfl[:], H), in0=r3(vtp[:], H),
                                                 in1=bcast(dsk[:].unsqueeze(2), [128, H, dv]), op=ALU.mult), [vtp, dsk], [yfl])
                self.V(lambda e: e.tensor_tensor(out=yc[:], in0=yc[:], in1=yfl[:], op=ALU.add), [yc, yfl], [yc])
                self.A(lambda e: e.activation(out=gt[:], in_=gt[:], func=AF.Silu), [gt], [gt])
                self.V(lambda e: e.tensor_tensor(out=yc[:], in0=yc[:], in1=gt[:], op=ALU.mult), [yc, gt], [yc])
                for g in range(4):
                    self.V(lambda e, g=g: e.bn_stats(out=st6[:, g, :], in_=yc[:, g * 512:(g + 1) * 512]), [yc], [st6])
                    self.V(lambda e, g=g: e.bn_aggr(out=mv[:, g, :], in_=st6[:, g, :]), [st6], [mv])
                self.V(lambda e: e.tensor_tensor(out=ss[:], in0=mv[:, :, 0], in1=mv[:, :, 0], op=ALU.mult), [mv], [ss])
                self.V(lambda e: e.tensor_tensor(out=ss[:], in0=ss[:], in1=mv[:, :, 1], op=ALU.add), [ss, mv], [ss])
                self.A(lambda e: e.activation(out=ss[:], in_=ss[:], func=AF.Sqrt, bias=EPS), [ss], [ss])
                self.V(lambda e: e.reciprocal(out=rstd[:], in_=ss[:]), [ss], [rstd])
                for g in range(4):
                    gs = slice(g * 512, (g + 1) * 512)
                    self.V(lambda e, g=g, gs=gs: e.scalar_tensor_tensor(out=yb[:, gs], in0=yc[:, gs], scalar=rstd[:, g:g + 1], in1=nw[:, gs],
                                                                        op0=ALU.mult, op1=ALU.mult), [yc, rstd, nw], [yb])
            else:
                for g in range(4):
                    self.V(lambda e, g=g: e.bn_stats(out=st6[:, g, :], in_=yc[:, g * 512:(g + 1) * 512]), [yc], [st6])
                    self.V(lambda e, g=g: e.bn_aggr(out=mv[:, g, :], in_=st6[:, g, :]), [st6], [mv])
                self.A(lambda e: e.activation(out=ss[:], in_=mv[:, :, 1], func=AF.Sqrt, bias=EPS), [mv], [ss])
                self.V(lambda e: e.reciprocal(out=rstd[:], in_=ss[:]), [ss], [rstd])
                self.A(lambda e: e.activation(out=gt[:], in_=gt[:], func=AF.Silu), [gt], [gt])
                for g in range(4):
                    gs = slice(g * 512, (g + 1) * 512)
                    self.V(lambda e, g=g, gs=gs: e.tensor_scalar(out=yc[:, gs], in0=yc[:, gs], scalar1=mv[:, g, 0:1], scalar2=rstd[:, g:g + 1],
                                                                 op0=ALU.subtract, op1=ALU.mult), [yc, mv, rstd], [yc])
                self.G(lambda e: e.tensor_tensor(out=yc[:], in0=yc[:], in1=nw[:], op=ALU.mult), [yc, nw], [yc])
                self.V(lambda e: e.tensor_tensor(out=yc[:], in0=yc[:], in1=nb_[:], op=ALU.add), [yc, nb_], [yc])
                self.V(lambda e: e.tensor_tensor(out=yb[:], in0=yc[:], in1=gt[:], op=ALU.mult), [yc, gt], [yb])
            for half in range(2):
                for kk in range(8):
                    k = half * 8 + kk
                    self.P(lambda e, k=k, kk=kk: e.transpose(ptr[:, kk, :], yb[:, k * 128:(k + 1) * 128], identb[:]), [yb, identb], [ptr])
                self.A(lambda e, half=half: e.copy(out=yT[:, half * 8:(half + 1) * 8, :], in_=ptr[:]), [ptr], [yT])
            pos = (pyd, pyo)
            for nn in range(2):
                for k in range(16):
                    self.P(lambda e, k=k, nn=nn: e.matmul(pos[nn][:], lhsT=yT[:, k, :], rhs=Wo[:, k, nn * 512:(nn + 1) * 512],
                                                          start=(k == 0), stop=(k == 15)), [yT, Wo], [pos[nn]])
            self.ld(xt, xt[:], xcur[cs_, :])
            for nn in range(2):
                ns = slice(nn * 512, (nn + 1) * 512)
                self.V(lambda e, nn=nn, ns=ns: e.tensor_tensor(out=t1[:, ns], in0=pos[nn][:], in1=g1[:, ns], op=ALU.mult), [pos[nn], g1], [t1])
            self.V(lambda e: e.scalar_tensor_tensor(out=t2[:], in0=xt[:], scalar=ALPHA, in1=t1[:], op0=ALU.mult, op1=ALU.add), [xt, t1], [t2])
            self.layer_norm(t2, t1, st2, mv2, rs2, lng, lnb)
            self.G(lambda e: e.tensor_tensor(out=t2[:], in0=t1[:], in1=sc2[:], op=ALU.mult), [t1, sc2], [t2])
            self.V(lambda e: e.tensor_tensor(out=t2[:], in0=t2[:], in1=sh2[:], op=ALU.add), [t2, sh2], [t2])
            deferred.append(lambda: self.st(xnext[cs_, :], t1, t1[:]))
            deferred.append(lambda: self.st(self.tok[cs_, :], t2, t2[:]))

        deferred = []

        def flush():
            for f in deferred:
                f()
            del deferred[:]

        for dirn in range(2):
            order = list(range(NCH)) if dirn == 0 else [1, 0] + list(range(NCH - 1, 1, -1))
            self.ld(tri, tri[:], self.c_trif[dirn])
            self.V(lambda e: e.memset(S[:], 0.0), [], [S])
            self.G(lambda e: e.memset(Sb[:], 0.0), [], [Sb])
            if dirn == 1:
                load_rows(1)
            if not ssd:
                self.ld_row(la, la[:, 0:4], self.ret_decay[j, dirn:dirn + 1, :])
                self.A(lambda e: e.activation(out=la[:, 0:4], in_=la[:, 0:4], func=AF.Exp, scale=-1.0), [la], [la])
                self.A(lambda e: e.activation(out=la[:, 0:4], in_=la[:, 0:4], func=AF.Ln, bias=1.0), [la], [la])
                self.V(lambda e: e.tensor_scalar_mul(out=la[:, 0:4], in0=la[:, 0:4], scalar1=-1.0), [la], [la])
                decay_prep(dirn, 0)
            stage1(order[0], dirn, 0)
            for i, c in enumerate(order):
                if i + 1 < len(order):
                    stage1(order[i + 1], dirn, (i + 1) % 2)
                flush()
                if dirn == 1 and i == NCTX:
                    load_rows(0)
                stage2(c, dirn, i % 2, 1 if c < NCTX else 0)
            flush()
            if dirn == 0:
                fw.barrier()


Builder.phase_scan = _phase_scan2
```

```python
import numpy as np
import ml_dtypes
from contextlib import ExitStack, contextmanager
import concourse.bass as bass
import concourse.mybir as mybir
from concourse.bass_utils import run_bass_kernel_spmd

F32 = mybir.dt.float32
BF16 = mybir.dt.bfloat16
I32 = mybir.dt.int32
ALU = mybir.AluOpType
AF = mybir.ActivationFunctionType
AX = mybir.AxisListType

ENGS = ["tensor", "vector", "scalar", "gpsimd", "sync"]
EPOCH = 20000
NDMA_SEM = 24

D = 1024
T = 4352
NCH = 34
NCTX = 2
NB = 100
DEPTH = 4
ALPHA = (2.0 * DEPTH) ** 0.25
EPS = 1e-5


class Res:
    __slots__ = ("w", "r")

    def __init__(self):
        self.w = None
        self.r = {}


class Tl:
    def __init__(self, t):
        self.t = t
        self.r = Res()

    def __getitem__(self, k):
        return self.t[k]


class FW:
    def __init__(self, nc):
        self.nc = nc
        self.root = ExitStack()
        self.es = self.root
        self.cnt = {e: 0 for e in ENGS}
        self.sems = {}
        self.waited = {e: {} for e in ENGS}
        self.dma_i = {e: 0 for e in ENGS}
        self.dma_last = {}
        self.latest = {}
        self.uid = 0

    def sem(self, key):
        if key not in self.sems:
            self.sems[key] = self.root.enter_context(self.nc.semaphore("s_%s_%s" % key))
        return self.sems[key]

    def sb(self, name, shape, dt):
        self.uid += 1
        return Tl(self.es.enter_context(self.nc.sbuf_tensor("%s_%d" % (name, self.uid), list(shape), dt)))

    def ps(self, name, shape, dt):
        self.uid += 1
        return Tl(self.es.enter_context(self.nc.psum_tensor("%s_%d" % (name, self.uid), list(shape), dt)))

    @contextmanager
    def scope(self):
        old = self.es
        self.es = ExitStack()
        try:
            yield
        finally:
            self.barrier()
            self.es.close()
            self.es = old

    def barrier(self):
        for eng in ENGS:
            for key, val in list(self.latest.items()):
                self._wait(eng, (key, val))

    def _wait(self, eng, ev):
        if ev is None:
            return
        key, val = ev
        if self.waited[eng].get(key, 0) >= val:
            return
        self.waited[eng][key] = val
        getattr(self.nc, eng).wait_ge(self.sem(key), val)

    def _deps(self, eng, reads, writes):
        evs = []
        for r in reads:
            if r.w is not None:
                evs.append(r.w)
        for w in writes:
            if w.w is not None:
                evs.append(w.w)
            evs.extend(w.r.items())
        for ev in evs:
            if ev[0][0] == "tensor" and eng == "tensor":
                continue
            self._wait(eng, ev)

    def _record(self, ev, reads, writes):
        self.latest[ev[0]] = ev[1]
        for r in reads:
            if r.r.get(ev[0], 0) < ev[1]:
                r.r[ev[0]] = ev[1]
        for w in writes:
            w.w = ev
            w.r = {}

    def op(self, eng, fn, reads=(), writes=()):
        reads = [t.r for t in reads]
        writes = [t.r for t in writes]
        self._deps(eng, reads, writes)
        c = self.cnt[eng]
        key = (eng, c // EPOCH)
        val = c % EPOCH + 1
        self.cnt[eng] = c + 1
        fn(getattr(self.nc, eng)).then_inc(self.sem(key), 1)
        self._record((key, val), reads, writes)

    def dma(self, eng, fn, reads=(), writes=()):
        reads = [t.r for t in reads]
        writes = [t.r for t in writes]
        self._deps(eng, reads, writes)
        i = self.dma_i[eng]
        self.dma_i[eng] = i + 1
        key = ("d" + eng, i % NDMA_SEM)
        prev = self.dma_last.get(key, 0)
        if prev:
            self._wait(eng, (key, prev))
        val = prev + 16
        self.dma_last[key] = val
        fn(getattr(self.nc, eng)).then_inc(self.sem(key), 16)
        self._record((key, val), reads, writes)


def bcast(ap, shape):
    return ap.to_broadcast(list(shape))


class Builder:
    def __init__(self, nc, nlayers=DEPTH, stop=None):
        self.nc = nc
        self.fw = FW(nc)
        self.nlayers = nlayers
        self.stop = stop
        self.dram = {}
        self.debug = set()
        self.only = None

    def din(self, name, shape, dt):
        if self.only is not None and name not in self.only:
            return None
        a = self.nc.dram_tensor(name, list(shape), dt, kind="ExternalInput").ap()
        self.dram[name] = a
        return a

    def dscr(self, name, shape, dt):
        kind = "ExternalOutput" if name in self.debug else "Internal"
        a = self.nc.dram_tensor(name, list(shape), dt, kind=kind).ap()
        self.dram[name] = a
        return a

    def ld(self, tile, dst, src, eng="sync"):
        self.fw.dma(eng, lambda e: e.dma_start(out=dst, in_=src), writes=[tile])

    def st(self, dst, tile, src, eng="sync"):
        self.fw.dma(eng, lambda e: e.dma_start(out=dst, in_=src), reads=[tile])

    def V(self, fn, rd, wr):
        self.fw.op("vector", fn, rd, wr)

    def A(self, fn, rd, wr):
        self.fw.op("scalar", fn, rd, wr)

    def G(self, fn, rd, wr):
        self.fw.op("gpsimd", fn, rd, wr)

    def P(self, fn, rd, wr):
        self.fw.op("tensor", fn, rd, wr)

    def ld_row(self, tile, dst, src_row, n=128):
        self.ld(tile, dst, src_row.partition_broadcast(n))

    def declare(self):
        d = self.din
        self.xin = d("xin", [T, D], F32)
        self.cvec = d("cvec", [128, 8, 2], F32)
        self.mod_w = d("mod_w", [DEPTH, D, 6 * D], F32)
        self.mod_b = d("mod_b", [DEPTH, 6 * D], F32)
        self.ssd_in_w = d("ssd_in_w", [2, D, 5184], F32)
        self.convw = d("convw", [2, 128, 24, 5], F32)
        self.convb = d("convb", [2, 128, 24], F32)
        self.ssd_dt_bias = d("ssd_dt_bias", [2, 64], F32)
        self.ssd_a_log = d("ssd_a_log", [2, 64], F32)
        self.ssd_d_skip = d("ssd_d_skip", [2, 32], F32)
        self.ssd_norm_w = d("ssd_norm_w", [2, 2048], F32)
        self.ssd_out_w = d("ssd_out_w", [2, 2048, D], F32)
        self.ret_in_w = d("ret_in_w", [2, D, 6144], F32)
        self.ret_decay = d("ret_decay_logit", [2, 2, 4], F32)
        self.ret_gn_w = d("ret_gn_w", [2, 2048], F32)
        self.ret_gn_b = d("ret_gn_b", [2, 2048], F32)
        self.ret_out_w = d("ret_out_w", [2, 2048, D], F32)
        self.ln_mix_g = d("ln_mix_g", [DEPTH, D], F32)
        self.ln_mix_b = d("ln_mix_b", [DEPTH, D], F32)
        self.ln_ffn_g = d("ln_ffn_g", [DEPTH, D], F32)
        self.ln_ffn_b = d("ln_ffn_b", [DEPTH, D], F32)
        self.moe_rw = d("moe_rw", [DEPTH, D, 36], F32)
        self.moe_rb = d("moe_rb", [DEPTH, 36], F32)
        self.moe_wgu = d("moe_w_gate_up", [DEPTH * 32 * 128, 8 * D], F32)
        self.moe_wd = d("moe_w_down", [DEPTH * 32 * 128, 4 * D], F32)
        self.c_identb = d("c_identb", [128, 128], BF16)
        self.c_identf = d("c_identf", [128, 128], F32)
        self.c_trif = d("c_trif", [2, 128, 128], F32)
        self.c_ones = d("c_ones", [128, 128], F32)
        self.c_slow = d("c_slow", [128, 128], BF16)
        self.c_rope = d("c_rope", [2, 128, T], F32)
        self.c_iota = d("c_iota", [128, 512], F32)
        self.c_pidx = d("c_pidx", [128, 12], F32)
        self.yout = self.nc.dram_tensor("yout", [4096, D], F32, kind="ExternalOutput").ap()
        s = self.dscr
        self.xA = s("xA", [T, D], F32)
        self.xB = s("xB", [T, D], F32)
        self.modrow = s("modrow", [2, 6 * D], F32)
        self.QT = s("QT", [4, 2, 128, T], BF16)
        self.KT = s("KT", [4, 2, 128, T], BF16)
        self.Ktm = s("Ktm", [T, 1024], BF16)
        self.Vtm = s("Vtm", [T, 2048], BF16)
        self.gate = s("gate", [T, 2048], F32)
        self.latm = s("latm", [T, 64], F32)
        self.dttm = s("dttm", [T, 64], F32)
        self.yf = s("yf", [T, 2048], F32)
        self.tok = s("tok", [T, D], F32)
        self.xbuf = s("xbuf", [NB * 128, D], BF16)
        self.ybuf = s("ybuf", [NB * 128, D], F32)
        self.dbg = {}

    def phase_mod(self, li):
        fw = self.fw
        with fw.scope():
            cv = fw.sb("cv", [128, 8, 2], F32)
            sv = fw.sb("sv", [128, 8, 2], F32)
            mb = fw.sb("mb", [2, 6 * D], F32)
            mr = fw.sb("mr", [2, 6 * D], F32)
            wst = fw.sb("wst", [128, 8, 512], F32)
            pm = fw.ps("pm", [128, 512], F32)
            self.ld(cv, cv[:], self.cvec)
            self.A(lambda e: e.activation(out=sv[:], in_=cv[:], func=AF.Silu), [cv], [sv])
            self.ld(mb, mb[0:1, :], self.mod_b[li:li + 1, :])
            self.ld(mb, mb[1:2, :], self.mod_b[li:li + 1, :])
            wv = self.mod_w[li].rearrange("(k p) n -> p k n", p=128)
            for n in range(12):
                self.ld(wst, wst[:], wv[:, :, n * 512:(n + 1) * 512])
                for k in range(8):
                    self.P(lambda e, k=k: e.matmul(pm[0:2, :], lhsT=sv[:, k, :], rhs=wst[:, k, :],
                                                   start=(k == 0), stop=(k == 7)), [sv, wst], [pm])
                self.V(lambda e, n=n: e.tensor_tensor(out=mr[0:2, n * 512:(n + 1) * 512], in0=pm[0:2, :],
                                                      in1=mb[0:2, n * 512:(n + 1) * 512], op=ALU.add),
                       [pm, mb], [mr])
            self.st(self.modrow, mr, mr[0:2, :])

    def make_uT(self, xcur, uT, identb, ptr, sc1, sh1, xt, u32, ub, c):
        v = 1 if c < NCTX else 0
        self.ld(xt, xt[:], xcur[c * 128:(c + 1) * 128, :])
        self.V(lambda e: e.tensor_tensor(out=u32[:], in0=xt[:], in1=sc1[v][:], op=ALU.mult), [xt, sc1[v]], [u32])
        self.G(lambda e: e.tensor_tensor(out=ub[:], in0=u32[:], in1=sh1[v][:], op=ALU.add), [u32, sh1[v]], [ub])
        for k in range(8):
            self.P(lambda e, k=k: e.transpose(ptr[:, k, :], ub[:, k * 128:(k + 1) * 128], identb[:]),
                   [ub, identb], [ptr])

    def load_mod_rows(self, lo, names):
        out = {}
        for nm, idx in names:
            tl = []
            for v in range(2):
                t = self.fw.sb("row_%s%d" % (nm, v), [128, D], F32)
                self.ld_row(t, t[:], self.modrow[v:v + 1, idx * D:(idx + 1) * D])
                tl.append(t)
            out[nm] = tl
        return out

    def phase_in_ssd(self, li, j, xcur):
        fw = self.fw
        with fw.scope():
            identb = fw.sb("identb", [128, 128], BF16)
            self.ld(identb, identb[:], self.c_identb)
            rows = self.load_mod_rows(0, [("sh1", 0), ("sc1", 1)])
            sh1, sc1 = rows["sh1"], rows["sc1"]
            for v in range(2):
                self.V(lambda e, v=v: e.tensor_scalar_add(out=sc1[v][:], in0=sc1[v][:], scalar1=1.0), [sc1[v]], [sc1[v]])
            uT = fw.sb("uT", [128, 8, T], BF16)
            xt = fw.sb("xt", [128, D], F32)
            u32 = fw.sb("u32", [128, D], F32)
            ub = fw.sb("ub", [128, D], BF16)
            ptr = fw.ps("ptr", [128, 8, 128], BF16)
            for c in range(NCH):
                self.make_uT(xcur, uT, identb, ptr, sc1, sh1, xt, u32, ub, c)
                self.A(lambda e, c=c: e.copy(out=uT[:, :, c * 128:(c + 1) * 128], in_=ptr[:]), [ptr], [uT])
            cw = fw.sb("cw", [128, 24, 5], F32)
            cb = fw.sb("cb", [128, 24], F32)
            self.ld(cw, cw[:], self.convw[j])
            self.ld(cb, cb[:], self.convb[j])
            wst_ = [fw.sb("wstA%d" % i, [128, 8, 128], F32) for i in range(2)]
            wb_ = [fw.sb("wbA%d" % i, [128, 8, 128], BF16) for i in range(2)]
            raw_ = [fw.sb("raw%d" % i, [128, T], F32) for i in range(2)]
            o_single = fw.sb("o", [128, T], F32)
            o_ = [o_single, o_single]
            ob_ = [fw.sb("ob%d" % i, [128, T], BF16) for i in range(2)]
            pp = [fw.ps("pp%d" % i, [128, 512], F32) for i in range(2)]
            trs_ = [fw.sb("trs%d" % i, [128, 8, 128], BF16) for i in range(2)]
            wv = self.ssd_in_w[j].rearrange("(k p) n -> p k n", p=128)
            segs = [(0, 256)] + [(256 + i * 512, 256 + (i + 1) * 512) for i in range(8)]
            seqs = [(0, 256), (256, T)]
            for f in range(24):
                wst, wb, raw, o, ob = wst_[f % 2], wb_[f % 2], raw_[f % 2], o_[f % 2], ob_[f % 2]
                col0 = 2048 + f * 128
                self.ld(wst, wst[:], wv[:, :, col0:col0 + 128])
                self.G(lambda e, wb=wb, wst=wst: e.tensor_copy(out=wb[:], in_=wst[:]), [wst], [wb])
                for si, (a, b) in enumerate(segs):
                    p = pp[si % 2]
                    for k in range(8):
                        self.P(lambda e, k=k, a=a, b=b, p=p, wb=wb: e.matmul(p[:, 0:b - a], lhsT=wb[:, k, :], rhs=uT[:, k, a:b],
                                                                       start=(k == 0), stop=(k == 7)), [wb, uT], [p])
                    self.A(lambda e, a=a, b=b, p=p, raw=raw: e.copy(out=raw[:, a:b], in_=p[:, 0:b - a]), [p], [raw])
                self.A(lambda e, f=f, o=o, raw=raw: e.activation(out=o[:], in_=raw[:], func=AF.Identity,
                                                   bias=cb[:, f:f + 1], scale=cw[:, f, 2:3]), [raw, cw, cb], [o])
                for (a, b) in seqs:
                    for kk, off in ((0, -2), (1, -1), (3, 1), (4, 2)):
                        if off < 0:
                            osl = (a - off, b)
                            isl = (a, b + off)
                        else:
                            osl = (a, b - off)
                            isl = (a + off, b)
                        self.V(lambda e, f=f, kk=kk, osl=osl, isl=isl, o=o, raw=raw: e.scalar_tensor_tensor(
                            out=o[:, osl[0]:osl[1]], in0=raw[:, isl[0]:isl[1]], scalar=cw[:, f, kk:kk + 1],
                            in1=o[:, osl[0]:osl[1]], op0=ALU.mult, op1=ALU.add), [raw, cw, o], [o])
                self.A(lambda e, o=o, ob=ob: e.activation(out=ob[:], in_=o[:], func=AF.Silu), [o], [ob])
                if f >= 16:
                    g = (f - 16) % 4
                    dst = self.KT if f < 20 else self.QT
                    self.st(dst[g, 0], ob, ob[:])
                if f < 20:
                    dstm = self.Vtm if f < 16 else self.Ktm
                    fc = f if f < 16 else f - 16
                    dv = dstm.rearrange("(c p) f -> p c f", p=128)
                    for c0 in range(0, NCH, 8):
                        trs = trs_[(c0 // 8) % 2]
                        n = min(8, NCH - c0)
                        for cc in range(n):
                            self.P(lambda e, cc=cc, c0=c0, ob=ob: e.transpose(ptr[:, cc, :], ob[:, (c0 + cc) * 128:(c0 + cc + 1) * 128],
                                                                       identb[:]), [ob, identb], [ptr])
                        self.V(lambda e, n=n, trs=trs: e.tensor_copy(out=trs[:, 0:n, :], in_=ptr[:, 0:n, :]), [ptr], [trs])
                        self.st(dv[:, c0:c0 + n, fc * 128:(fc + 1) * 128], trs, trs[:, 0:n, :])
            wst2 = fw.sb("wst2", [128, 8, 512], F32)
            wb2 = fw.sb("wb2", [128, 8, 512], BF16)
            zt = fw.sb("zt", [128, 512], F32)
            for n in range(4):
                self.ld(wst2, wst2[:], wv[:, :, n * 512:(n + 1) * 512])
                self.G(lambda e: e.tensor_copy(out=wb2[:], in_=wst2[:]), [wst2], [wb2])
                for c in range(NCH):
                    p = pp[c % 2]
                    for k in range(8):
                        self.P(lambda e, k=k, c=c, p=p: e.matmul(p[:], lhsT=uT[:, k, c * 128:(c + 1) * 128], rhs=wb2[:, k, :],
                                                                 start=(k == 0), stop=(k == 7)), [uT, wb2], [p])
                    self.A(lambda e, p=p: e.copy(out=zt[:], in_=p[:]), [p], [zt])
                    self.st(self.gate[c * 128:(c + 1) * 128, n * 512:(n + 1) * 512], zt, zt[:])
            dtb = fw.sb("dtb", [128, 64], F32)
            nega = fw.sb("nega", [128, 64], F32)
            self.ld_row(dtb, dtb[:], self.ssd_dt_bias[j:j + 1, :])
            self.ld_row(nega, nega[:], self.ssd_a_log[j:j + 1, :])
            self.A(lambda e: e.activation(out=nega[:], in_=nega[:], func=AF.Exp), [nega], [nega])
            self.V(lambda e: e.tensor_scalar_mul(out=nega[:], in0=nega[:], scalar1=-1.0), [nega], [nega])
            self.ld(wst2, wst2[:, :, 0:64], wv[:, :, 5120:5184])
            self.G(lambda e: e.tensor_copy(out=wb2[:, :, 0:64], in_=wst2[:, :, 0:64]), [wst2], [wb2])
            d0 = fw.sb("d0", [128, 64], F32)
            d1 = fw.sb("d1", [128, 64], F32)
            d2 = fw.sb("d2", [128, 64], F32)
            for c in range(NCH):
                p = pp[c % 2]
                for k in range(8):
                    self.P(lambda e, k=k, c=c, p=p: e.matmul(p[:, 0:64], lhsT=uT[:, k, c * 128:(c + 1) * 128], rhs=wb2[:, k, 0:64],
                                                             start=(k == 0), stop=(k == 7)), [uT, wb2], [p])
                self.V(lambda e, p=p: e.tensor_tensor(out=d0[:], in0=p[:, 0:64], in1=dtb[:], op=ALU.add), [p, dtb], [d0])
                self.V(lambda e: e.tensor_scalar_mul(out=d1[:], in0=d0[:], scalar1=-1.0), [d0], [d1])
                self.V(lambda e: e.tensor_tensor(out=d1[:], in0=d1[:], in1=d0[:], op=ALU.max), [d0, d1], [d1])
                self.A(lambda e: e.activation(out=d1[:], in_=d1[:], func=AF.Exp, scale=-1.0), [d1], [d1])
                self.A(lambda e: e.activation(out=d1[:], in_=d1[:], func=AF.Ln, bias=1.0), [d1], [d1])
                self.V(lambda e: e.scalar_tensor_tensor(out=d2[:], in0=d0[:], scalar=0.0, in1=d1[:], op0=ALU.max, op1=ALU.add),
                       [d0, d1], [d2])
                self.st(self.dttm[c * 128:(c + 1) * 128, :], d2, d2[:])
                self.V(lambda e: e.tensor_tensor(out=d0[:], in0=d2[:], in1=nega[:], op=ALU.mult), [d2, nega], [d0])
                self.st(self.latm[c * 128:(c + 1) * 128, :], d0, d0[:])

    def phase_in_ret(self, li, j, xcur):
        fw = self.fw
        with fw.scope():
            identb = fw.sb("identb", [128, 128], BF16)
            self.ld(identb, identb[:], self.c_identb)
            rows = self.load_mod_rows(0, [("sh1", 0), ("sc1", 1)])
            sh1, sc1 = rows["sh1"], rows["sc1"]
            for v in range(2):
                self.V(lambda e, v=v: e.tensor_scalar_add(out=sc1[v][:], in0=sc1[v][:], scalar1=1.0), [sc1[v]], [sc1[v]])
            W = fw.sb("Wret", [128, 8, 6144], BF16)
            wst = fw.sb("wstR", [128, 8, 512], F32)
            wv = self.ret_in_w[j].rearrange("(k p) n -> p k n", p=128)
            for n in range(12):
                self.ld(wst, wst[:], wv[:, :, n * 512:(n + 1) * 512])
                eng = self.G if n % 2 == 0 else self.A
                if n % 2 == 0:
                    self.G(lambda e, n=n: e.tensor_copy(out=W[:, :, n * 512:(n + 1) * 512], in_=wst[:]), [wst], [W])
                else:
                    self.A(lambda e, n=n: e.copy(out=W[:, :, n * 512:(n + 1) * 512], in_=wst[:]), [wst], [W])
            uT = fw.sb("uTs", [128, 8, 512], BF16)
            xt = fw.sb("xt", [128, D], F32)
            u32 = fw.sb("u32", [128, D], F32)
            ub = fw.sb("ub", [128, D], BF16)
            ptr = fw.ps("ptr", [128, 8, 128], BF16)
            pp = [fw.ps("pp%d" % i, [128, 512], F32) for i in range(4)]
            cs = fw.sb("cs", [128, 512], F32)
            sn = fw.sb("sn", [128, 512], F32)
            r1_ = [fw.sb("r1%d" % i, [128, 512], F32) for i in range(2)]
            r2_ = [fw.sb("r2%d" % i, [128, 512], F32) for i in range(2)]
            ta_ = [fw.sb("ta%d" % i, [128, 512], F32) for i in range(2)]
            tb_ = [fw.sb("tb%d" % i, [128, 512], F32) for i in range(2)]
            o1_ = [fw.sb("o1%d" % i, [128, 512], BF16) for i in range(2)]
            o2_ = [fw.sb("o2%d" % i, [128, 512], BF16) for i in range(2)]
            trs_ = [fw.sb("trs%d" % i, [128, 8, 128], BF16) for i in range(2)]
            zt = fw.sb("zt", [128, 512], F32)
            vb = fw.sb("vb", [128, 512], BF16)
            segs = [(0, 256)] + [(256 + i * 512, 256 + (i + 1) * 512) for i in range(8)]
            ktv = self.Ktm.rearrange("(c p) f -> p c f", p=128)
            for (a, b) in segs:
                n = b - a
                nt = n // 128
                c0 = a // 128
                for ci in range(nt):
                    self.make_uT(xcur, uT, identb, ptr, sc1, sh1, xt, u32, ub, c0 + ci)
                    self.A(lambda e, ci=ci: e.copy(out=uT[:, :, ci * 128:(ci + 1) * 128], in_=ptr[:]), [ptr], [uT])
                self.ld(cs, cs[:, 0:n], self.c_rope[0, :, a:b])
                self.ld(sn, sn[:, 0:n], self.c_rope[1, :, a:b])
                for which in range(2):
                    for h in range(4):
                        ii = (which * 4 + h) % 2
                        r1, r2, ta, tb, o1, o2, trs = r1_[ii], r2_[ii], ta_[ii], tb_[ii], o1_[ii], o2_[ii], trs_[ii]
                        base = which * 1024 + h * 256
                        for half, (pt, rr) in enumerate(((pp[ii * 2], r1), (pp[ii * 2 + 1], r2))):
                            cb0 = base + half * 128
                            for k in range(8):
                                self.P(lambda e, k=k, cb0=cb0, pt=pt: e.matmul(pt[:, 0:n], lhsT=W[:, k, cb0:cb0 + 128], rhs=uT[:, k, 0:n],
                                                                               start=(k == 0), stop=(k == 7)), [W, uT], [pt])
                            sc = 1.0 if which == 0 else 0.0625
                            self.A(lambda e, pt=pt, rr=rr, sc=sc: e.activation(out=rr[:, 0:n], in_=pt[:, 0:n], func=AF.Copy, scale=sc),
                                   [pt], [rr])
                        self.V(lambda e, ta=ta, r1=r1: e.tensor_tensor(out=ta[:, 0:n], in0=r1[:, 0:n], in1=cs[:, 0:n], op=ALU.mult), [r1, cs], [ta])
                        self.G(lambda e, tb=tb, r2=r2: e.tensor_tensor(out=tb[:, 0:n], in0=r2[:, 0:n], in1=sn[:, 0:n], op=ALU.mult), [r2, sn], [tb])
                        self.V(lambda e, ta=ta, tb=tb, o1=o1: e.tensor_tensor(out=o1[:, 0:n], in0=ta[:, 0:n], in1=tb[:, 0:n], op=ALU.subtract), [ta, tb], [o1])
                        self.V(lambda e, ta=ta, r1=r1: e.tensor_tensor(out=ta[:, 0:n], in0=r1[:, 0:n], in1=sn[:, 0:n], op=ALU.mult), [r1, sn], [ta])
                        self.G(lambda e, tb=tb, r2=r2: e.tensor_tensor(out=tb[:, 0:n], in0=r2[:, 0:n], in1=cs[:, 0:n], op=ALU.mult), [r2, cs], [tb])
                        self.V(lambda e, ta=ta, tb=tb, o2=o2: e.tensor_tensor(out=o2[:, 0:n], in0=ta[:, 0:n], in1=tb[:, 0:n], op=ALU.add), [ta, tb], [o2])
                        dst = self.QT if which == 0 else self.KT
                        self.st(dst[h, 0, :, a:b], o1, o1[:, 0:n])
                        self.st(dst[h, 1, :, a:b], o2, o2[:, 0:n])
                        if which == 1:
                            for half, oo in enumerate((o1, o2)):
                                for ci in range(nt):
                                    self.P(lambda e, ci=ci, oo=oo, half=half: e.transpose(ptr[:, half * 4 + ci, :], oo[:, ci * 128:(ci + 1) * 128],
                                                                                          identb[:]), [oo, identb], [ptr])
                            self.V(lambda e, trs=trs: e.tensor_copy(out=trs[:], in_=ptr[:]), [ptr], [trs])
                            for half in range(2):
                                col = h * 256 + half * 128
                                self.st(ktv[:, c0:c0 + nt, col:col + 128], trs, trs[:, half * 4:half * 4 + nt, :])
                for ci in range(nt):
                    c = c0 + ci
                    for nn in range(8):
                        p = pp[2 + nn % 2]
                        colw = 2048 + nn * 512
                        for k in range(8):
                            self.P(lambda e, k=k, ci=ci, p=p, colw=colw: e.matmul(p[:], lhsT=uT[:, k, ci * 128:(ci + 1) * 128],
                                                                                  rhs=W[:, k, colw:colw + 512], start=(k == 0), stop=(k == 7)),
                                   [uT, W], [p])
                        if nn < 4:
                            self.A(lambda e, p=p: e.copy(out=vb[:], in_=p[:]), [p], [vb])
                            self.st(self.Vtm[c * 128:(c + 1) * 128, nn * 512:(nn + 1) * 512], vb, vb[:])
                        else:
                            self.V(lambda e, p=p: e.tensor_copy(out=zt[:], in_=p[:]), [p], [zt])
                            self.st(self.gate[c * 128:(c + 1) * 128, (nn - 4) * 512:(nn - 3) * 512], zt, zt[:])

    def phase_scan(self, li, j, ssd, xcur, xnext):
        fw = self.fw
        G = 4
        Hg = 8 if ssd else 1
        KC = 1 if ssd else 2
        H = G * Hg
        dv = 2048 // H
        dk = KC * 128
        with fw.scope():
            identb = fw.sb("identb", [128, 128], BF16)
            self.ld(identb, identb[:], self.c_identb)
            ones = fw.sb("ones", [128, 128], F32)
            self.ld(ones, ones[:], self.c_ones)
            tri = fw.sb("tri", [128, 128], F32)
            Wo = fw.sb("Wo", [128, 16, D], BF16)
            wst = fw.sb("wstO", [128, 4, D], F32)
            owv = (self.ssd_out_w if ssd else self.ret_out_w)[j].rearrange("(k p) n -> p k n", p=128)
            for q in range(4):
                self.ld(wst, wst[:], owv[:, q * 4:(q + 1) * 4, :])
                self.G(lambda e, q=q: e.tensor_copy(out=Wo[:, q * 4:(q + 1) * 4, :], in_=wst[:]), [wst], [Wo])
            rows = self.load_mod_rows(0, [("g1", 2), ("sh2", 3), ("sc2", 4)])
            g1, sh2, sc2 = rows["g1"], rows["sh2"], rows["sc2"]
            for v in range(2):
                self.V(lambda e, v=v: e.tensor_scalar_add(out=sc2[v][:], in0=sc2[v][:], scalar1=1.0), [sc2[v]], [sc2[v]])
            lng = fw.sb("lng", [128, D], F32)
            lnb = fw.sb("lnb", [128, D], F32)
            self.ld_row(lng, lng[:], self.ln_mix_g[li:li + 1, :])
            self.ld_row(lnb, lnb[:], self.ln_mix_b[li:li + 1, :])
            nw = fw.sb("nw", [128, 2048], F32)
            self.ld_row(nw, nw[:], (self.ssd_norm_w if ssd else self.ret_gn_w)[j:j + 1, :])
            if ssd:
                dsk = fw.sb("dsk", [128, 32], F32)
                self.ld_row(dsk, dsk[:], self.ssd_d_skip[j:j + 1, :])
            else:
                nb_ = fw.sb("nb", [128, 2048], F32)
                self.ld_row(nb_, nb_[:], self.ret_gn_b[j:j + 1, :])
            S = fw.sb("S", [128, KC, 2048], F32)
            Sb = fw.sb("Sb", [128, KC, 2048], BF16)
            qt = fw.sb("qt", [128, G * KC, 128], BF16)
            kt = fw.sb("kt", [128, G * KC, 128], BF16)
            ktm = fw.sb("ktm", [128, G * dk], BF16)
            vt = fw.sb("vt", [128, 2048], BF16)
            la = fw.sb("la", [128, 64], F32)
            dt = fw.sb("dt", [128, 64], F32)
            acs = fw.sb("acs", [128, 2 * H], F32)
            nacs = fw.sb("nacs", [128, H], F32)
            ea = fw.sb("ea", [128, H], F32)
            wend = fw.sb("wend", [128, H], F32)
            cd = fw.sb("cd", [128, H], F32)
            E = fw.sb("E", [128, H, 128], F32)
            lt = fw.sb("lt", [128, Hg, 128], F32)
            M = fw.sb("M", [128, Hg, 128], BF16)
            cbm = fw.sb("cbm", [128, G, 128], F32)
            xdt = fw.sb("xdt", [128, 2048], BF16) if ssd else vt
            xe = fw.sb("xe", [128, 2048], BF16)
            yc = fw.sb("yc", [128, 2048], F32)
            tmp = fw.sb("tmp", [128, 512], F32)
            yfl = fw.sb("yfl", [128, 2048], F32)
            gt = fw.sb("gt", [128, 2048], F32)
            yb = fw.sb("yb", [128, 2048], BF16)
            yT = fw.sb("yT", [128, 16, 128], BF16)
            st6 = fw.sb("st6", [128, 4, 6], F32)
            mv = fw.sb("mv", [128, 4, 2], F32)
            ss = fw.sb("ss", [128, 4], F32)
            rstd = fw.sb("rstd", [128, 4], F32)
            xt = fw.sb("xt", [128, D], F32)
            t1 = fw.sb("t1", [128, D], F32)
            t2 = fw.sb("t2", [128, D], F32)
            st2 = fw.sb("st2", [128, 2, 6], F32)
            mv2 = fw.sb("mv2", [128, 2], F32)
            rs2 = fw.sb("rs2", [128, 1], F32)
            psm = fw.ps("psm", [128, 512], F32)
            pcb = fw.ps("pcb", [128, 512], F32)
            pbc = [fw.ps("pbc%d" % i, [128, 512], F32) for i in range(2)]
            pA = fw.ps("pA", [128, 1024], F32)
            pst = fw.ps("pst", [128, 512], F32)
            ptr = fw.ps("ptr", [128, 8, 128], BF16)

            qtv = self.QT.rearrange("g k p t -> p g k t")
            ktv = self.KT.rearrange("g k p t -> p g k t")

            def decay_prep(dirn):
                lad = la[:, dirn * 32:dirn * 32 + H] if ssd else la[:, 0:H]
                self.P(lambda e: e.matmul(psm[:, 0:H], lhsT=tri[:], rhs=lad, start=True, stop=True), [tri, la], [psm])
                self.P(lambda e: e.matmul(psm[:, H:2 * H], lhsT=ones[:], rhs=lad, start=True, stop=True), [ones, la], [psm])
                self.V(lambda e: e.tensor_copy(out=acs[:], in_=psm[:, 0:2 * H]), [psm], [acs])
                self.V(lambda e: e.tensor_scalar_mul(out=nacs[:], in0=acs[:, 0:H], scalar1=-1.0), [acs], [nacs])
                self.A(lambda e: e.activation(out=ea[:], in_=acs[:, 0:H], func=AF.Exp), [acs], [ea])
                self.V(lambda e: e.tensor_tensor(out=wend[:], in0=acs[:, H:2 * H], in1=acs[:, 0:H], op=ALU.subtract), [acs], [wend])
                self.A(lambda e: e.activation(out=wend[:], in_=wend[:], func=AF.Exp), [wend], [wend])
                self.A(lambda e: e.activation(out=cd[:], in_=acs[:, H:2 * H], func=AF.Exp), [acs], [cd])
                for g in range(G):
                    hs = slice(g * Hg, (g + 1) * Hg)
                    ladg = la[:, dirn * 32 + g * Hg:dirn * 32 + (g + 1) * Hg] if ssd else la[:, g:g + 1]
                    self.G(lambda e, ladg=ladg: e.tensor_tensor(out=lt[:], in0=bcast(tri[:].unsqueeze(1), [128, Hg, 128]),
                                                                in1=bcast(ladg.unsqueeze(2), [128, Hg, 128]), op=ALU.mult),
                           [tri, la], [lt])
                    for q in range((Hg + 3) // 4):
                        nh = min(4, Hg - q * 4)
                        pb = pbc[q % 2]
                        self.P(lambda e, q=q, nh=nh, pb=pb: e.matmul(pb[:, 0:nh * 128], lhsT=ones[:], rhs=lt[:, q * 4:q * 4 + nh, :],
                                                                     start=True, stop=True), [ones, lt], [pb])
                        for hh in range(nh):
                            h = g * Hg + q * 4 + hh
                            self.A(lambda e, h=h, hh=hh, pb=pb: e.activation(out=E[:, h, :], in_=pb[:, hh * 128:(hh + 1) * 128], func=AF.Exp,
                                                                            bias=nacs[:, h:h + 1], scale=1.0), [pb, nacs], [E])

            for dirn in range(2):
                order = list(range(NCH)) if dirn == 0 else [1, 0] + list(range(NCH - 1, 1, -1))
                self.ld(tri, tri[:], self.c_trif[dirn])
                self.V(lambda e: e.memset(S[:], 0.0), [], [S])
                self.G(lambda e: e.memset(Sb[:], 0.0), [], [Sb])
                if not ssd:
                    self.ld_row(la, la[:, 0:4], self.ret_decay[j, dirn:dirn + 1, :])
                    self.A(lambda e: e.activation(out=la[:, 0:4], in_=la[:, 0:4], func=AF.Exp, scale=-1.0), [la], [la])
                    self.A(lambda e: e.activation(out=la[:, 0:4], in_=la[:, 0:4], func=AF.Ln, bias=1.0), [la], [la])
                    self.V(lambda e: e.tensor_scalar_mul(out=la[:, 0:4], in0=la[:, 0:4], scalar1=-1.0), [la], [la])
                    decay_prep(dirn)
                for c in order:
                    v = 1 if c < NCTX else 0
                    cs_ = slice(c * 128, (c + 1) * 128)
                    self.ld(qt, qt[:].rearrange("p (g k) t -> p g k t", g=G), qtv[:, :, 0:KC, cs_])
                    self.ld(kt, kt[:].rearrange("p (g k) t -> p g k t", g=G), ktv[:, :, 0:KC, cs_])
                    self.ld(ktm, ktm[:], self.Ktm[cs_, 0:G * dk])
                    self.ld(vt, vt[:], self.Vtm[cs_, :])
                    if ssd:
                        self.ld(la, la[:], self.latm[cs_, :])
                        self.ld(dt, dt[:], self.dttm[cs_, :])
                        decay_prep(dirn)
                        self.V(lambda e: e.tensor_tensor(out=xdt[:].rearrange("p (h d) -> p h d", h=H), in0=vt[:].rearrange("p (h d) -> p h d", h=H),
                                                         in1=bcast(dt[:, dirn * 32:dirn * 32 + 32].unsqueeze(2), [128, H, dv]), op=ALU.mult),
                               [vt, dt], [xdt])
                    self.G(lambda e: e.tensor_tensor(out=xe[:].rearrange("p (h d) -> p h d", h=H), in0=xdt[:].rearrange("p (h d) -> p h d", h=H),
                                                     in1=bcast(wend[:].unsqueeze(2), [128, H, dv]), op=ALU.mult), [xdt, wend], [xe])
                    for g in range(G):
                        for kc in range(KC):
                            self.P(lambda e, g=g, kc=kc: e.matmul(pcb[:, g * 128:(g + 1) * 128], lhsT=kt[:, g * KC + kc, :], rhs=qt[:, g * KC + kc, :],
                                                                  start=(kc == 0), stop=(kc == KC - 1)), [kt, qt], [pcb])
                    self.V(lambda e: e.tensor_tensor(out=cbm[:], in0=pcb[:].rearrange("p (g l) -> p g l", g=G),
                                                     in1=bcast(tri[:].unsqueeze(1), [128, G, 128]), op=ALU.mult), [pcb, tri], [cbm])
                    for g in range(G):
                        gs = slice(g * 512, (g + 1) * 512)
                        self.V(lambda e, g=g: e.scalar_tensor_tensor(out=M[:], in0=E[:, g * Hg:(g + 1) * Hg, :], scalar=1.0,
                                                                     in1=bcast(cbm[:, g:g + 1, :], [128, Hg, 128]), op0=ALU.min, op1=ALU.mult),
                               [E, cbm], [M])
                        for hh in range(Hg):
                            h = g * Hg + hh
                            self.P(lambda e, h=h, hh=hh: e.matmul(pA[:, h * dv:(h + 1) * dv] if False else pA[:, (h * dv) % 512:(h * dv) % 512 + dv],
                                                                  lhsT=M[:, hh, :], rhs=xdt[:, h * dv:(h + 1) * dv], start=True, stop=True),
                                   [M, xdt], [pA])
                        for kc in range(KC):
                            self.P(lambda e, g=g, kc=kc, gs=gs: e.matmul(pA[:, 512:1024], lhsT=qt[:, g * KC + kc, :], rhs=Sb[:, kc, gs],
                                                                         start=(kc == 0), stop=(kc == KC - 1)), [qt, Sb], [pA])
                        self.V(lambda e, g=g: e.tensor_tensor(out=tmp[:].rearrange("p (h d) -> p h d", h=Hg),
                                                              in0=pA[:, 512:1024].rearrange("p (h d) -> p h d", h=Hg),
                                                              in1=bcast(ea[:, g * Hg:(g + 1) * Hg].unsqueeze(2), [128, Hg, dv]), op=ALU.mult),
                               [pA, ea], [tmp])
                        self.V(lambda e, gs=gs: e.tensor_tensor(out=yc[:, gs], in0=tmp[:], in1=pA[:, 0:512], op=ALU.add), [tmp, pA], [yc])
                        for kc in range(KC):
                            self.P(lambda e, g=g, kc=kc, gs=gs: e.matmul(pst[:], lhsT=ktm[:, g * dk + kc * 128:g * dk + (kc + 1) * 128], rhs=xe[:, gs],
                                                                         start=True, stop=True), [ktm, xe], [pst])
                            self.V(lambda e, g=g, kc=kc, gs=gs: e.tensor_tensor(out=S[:, kc, gs].rearrange("p (h d) -> p h d", h=Hg),
                                                                                in0=S[:, kc, gs].rearrange("p (h d) -> p h d", h=Hg),
                                                                                in1=bcast(cd[:, g * Hg:(g + 1) * Hg].unsqueeze(2), [128, Hg, dv]),
                                                                                op=ALU.mult), [S, cd], [S])
                            self.V(lambda e, kc=kc, gs=gs: e.tensor_tensor(out=S[:, kc, gs], in0=S[:, kc, gs], in1=pst[:], op=ALU.add), [S, pst], [S])
                            self.A(lambda e, kc=kc, gs=gs: e.copy(out=Sb[:, kc, gs], in_=S[:, kc, gs]), [S], [Sb])
                    if dirn == 0:
                        self.st(self.yf[cs_, :], yc, yc[:])
                        continue
                    self.ld(yfl, yfl[:], self.yf[cs_, :])
                    self.ld(gt, gt[:], self.gate[cs_, :])
                    self.V(lambda e: e.tensor_tensor(out=yc[:], in0=yc[:], in1=yfl[:], op=ALU.add), [yc, yfl], [yc])
                    if ssd:
                        self.G(lambda e: e.tensor_tensor(out=yfl[:].rearrange("p (h d) -> p h d", h=H), in0=vt[:].rearrange("p (h d) -> p h d", h=H),
                                                         in1=bcast(dsk[:].unsqueeze(2), [128, H, dv]), op=ALU.mult), [vt, dsk], [yfl])
                        self.V(lambda e: e.tensor_tensor(out=yc[:], in0=yc[:], in1=yfl[:], op=ALU.add), [yc, yfl], [yc])
                        self.A(lambda e: e.activation(out=gt[:], in_=gt[:], func=AF.Silu), [gt], [gt])
                        self.V(lambda e: e.tensor_tensor(out=yc[:], in0=yc[:], in1=gt[:], op=ALU.mult), [yc, gt], [yc])
                        for g in range(4):
                            self.V(lambda e, g=g: e.bn_stats(out=st6[:, g, :], in_=yc[:, g * 512:(g + 1) * 512]), [yc], [st6])
                            self.V(lambda e, g=g: e.bn_aggr(out=mv[:, g, :], in_=st6[:, g, :]), [st6], [mv])
                        self.V(lambda e: e.tensor_tensor(out=ss[:], in0=mv[:, :, 0], in1=mv[:, :, 0], op=ALU.mult), [mv], [ss])
                        self.V(lambda e: e.tensor_tensor(out=ss[:], in0=ss[:], in1=mv[:, :, 1], op=ALU.add), [ss, mv], [ss])
                        self.A(lambda e: e.activation(out=ss[:], in_=ss[:], func=AF.Sqrt, bias=EPS), [ss], [ss])
                        self.V(lambda e: e.reciprocal(out=rstd[:], in_=ss[:]), [ss], [rstd])
                        for g in range(4):
                            gs = slice(g * 512, (g + 1) * 512)
                            self.V(lambda e, g=g, gs=gs: e.scalar_tensor_tensor(out=yb[:, gs], in0=yc[:, gs], scalar=rstd[:, g:g + 1], in1=nw[:, gs],
                                                                                op0=ALU.mult, op1=ALU.mult), [yc, rstd, nw], [yb])
                    else:
                        for g in range(4):
                            self.V(lambda e, g=g: e.bn_stats(out=st6[:, g, :], in_=yc[:, g * 512:(g + 1) * 512]), [yc], [st6])
                            self.V(lambda e, g=g: e.bn_aggr(out=mv[:, g, :], in_=st6[:, g, :]), [st6], [mv])
                        self.A(lambda e: e.activation(out=ss[:], in_=mv[:, :, 1], func=AF.Sqrt, bias=EPS), [mv], [ss])
                        self.V(lambda e: e.reciprocal(out=rstd[:], in_=ss[:]), [ss], [rstd])
                        self.A(lambda e: e.activation(out=gt[:], in_=gt[:], func=AF.Silu), [gt], [gt])
                        for g in range(4):
                            gs = slice(g * 512, (g + 1) * 512)
                            self.V(lambda e, g=g, gs=gs: e.tensor_scalar(out=yc[:, gs], in0=yc[:, gs], scalar1=mv[:, g, 0:1], scalar2=rstd[:, g:g + 1],
                                                                         op0=ALU.subtract, op1=ALU.mult), [yc, mv, rstd], [yc])
                        self.G(lambda e: e.tensor_tensor(out=yc[:], in0=yc[:], in1=nw[:], op=ALU.mult), [yc, nw], [yc])
                        self.V(lambda e: e.tensor_tensor(out=yc[:], in0=yc[:], in1=nb_[:], op=ALU.add), [yc, nb_], [yc])
                        self.V(lambda e: e.tensor_tensor(out=yb[:], in0=yc[:], in1=gt[:], op=ALU.mult), [yc, gt], [yb])
                    for half in range(2):
                        for kk in range(8):
                            k = half * 8 + kk
                            self.P(lambda e, k=k, kk=kk: e.transpose(ptr[:, kk, :], yb[:, k * 128:(k + 1) * 128], identb[:]), [yb, identb], [ptr])
                        self.A(lambda e, half=half: e.copy(out=yT[:, half * 8:(half + 1) * 8, :], in_=ptr[:]), [ptr], [yT])
                    for nn in range(2):
                        for k in range(16):
                            self.P(lambda e, k=k, nn=nn: e.matmul(pA[:, nn * 512:(nn + 1) * 512], lhsT=yT[:, k, :], rhs=Wo[:, k, nn * 512:(nn + 1) * 512],
                                                                  start=(k == 0), stop=(k == 15)), [yT, Wo], [pA])
                    self.ld(xt, xt[:], xcur[cs_, :])
                    self.V(lambda e: e.tensor_tensor(out=t1[:], in0=pA[:], in1=g1[v][:], op=ALU.mult), [pA, g1[v]], [t1])
                    self.V(lambda e: e.scalar_tensor_tensor(out=t2[:], in0=xt[:], scalar=ALPHA, in1=t1[:], op0=ALU.mult, op1=ALU.add), [xt, t1], [t2])
                    self.layer_norm(t2, t1, st2, mv2, rs2, lng, lnb)
                    self.st(xnext[cs_, :], t1, t1[:])
                    self.G(lambda e: e.tensor_tensor(out=t2[:], in0=t1[:], in1=sc2[v][:], op=ALU.mult), [t1, sc2[v]], [t2])
                    self.V(lambda e: e.tensor_tensor(out=t2[:], in0=t2[:], in1=sh2[v][:], op=ALU.add), [t2, sh2[v]], [t2])
                    self.st(self.tok[cs_, :], t2, t2[:])
                if dirn == 0:
                    fw.barrier()

    def layer_norm(self, xin_t, out_t, st2, mv2, rs2, lng, lnb):
        for hf in range(2):
            self.V(lambda e, hf=hf: e.bn_stats(out=st2[:, hf, :], in_=xin_t[:, hf * 512:(hf + 1) * 512]), [xin_t], [st2])
        self.V(lambda e: e.bn_aggr(out=mv2[:], in_=st2[:].rearrange("p a b -> p (a b)")), [st2], [mv2])
        self.A(lambda e: e.activation(out=rs2[:], in_=mv2[:, 1:2], func=AF.Sqrt, bias=EPS), [mv2], [rs2])
        self.V(lambda e: e.reciprocal(out=rs2[:], in_=rs2[:]), [rs2], [rs2])
        self.V(lambda e: e.tensor_scalar(out=out_t[:], in0=xin_t[:], scalar1=mv2[:, 0:1], scalar2=rs2[:, 0:1],
                                         op0=ALU.subtract, op1=ALU.mult), [xin_t, mv2, rs2], [out_t])
        self.G(lambda e: e.tensor_tensor(out=out_t[:], in0=out_t[:], in1=lng[:], op=ALU.mult), [out_t, lng], [out_t])
        self.V(lambda e: e.tensor_tensor(out=out_t[:], in0=out_t[:], in1=lnb[:], op=ALU.add), [out_t, lnb], [out_t])


def host_consts():
    bf = ml_dtypes.bfloat16
    c = {}
    c["c_identb"] = np.eye(128, dtype=np.float32).astype(bf)
    c["c_identf"] = np.eye(128, dtype=np.float32)
    s = np.arange(128)
    c["c_trif"] = np.stack([(s[:, None] <= s[None, :]), (s[:, None] >= s[None, :])]).astype(np.float32)
    c["c_ones"] = np.ones((128, 128), np.float32)
    c["c_slow"] = (s[:, None] < s[None, :]).astype(np.float32).astype(bf)
    n_freq = 64
    inv_freq = (10000.0 ** (-np.arange(n_freq, dtype=np.float32) / np.float32(n_freq))).astype(np.float32)
    pos = np.arange(4096)
    rows = (pos // 64).astype(np.float32)
    cols = (pos % 64).astype(np.float32)
    ang = np.concatenate([rows[:, None] * inv_freq[None, :], cols[:, None] * inv_freq[None, :]], -1).astype(np.float32)
    cos = np.ones((T, 128), np.float32)
    sin = np.zeros((T, 128), np.float32)
    cos[256:] = np.cos(ang)
    sin[256:] = np.sin(ang)
    c["c_rope"] = np.ascontiguousarray(np.stack([cos.T, sin.T])).astype(np.float32)
    c["c_iota"] = np.broadcast_to(np.arange(512, dtype=np.float32)[None, :], (128, 512)).copy()
    c["c_pidx"] = (np.arange(12)[None, :] * 128 + np.arange(128)[:, None]).astype(np.float32)
    return c


def host_inputs(inp, b):
    f = np.float32
    m = {}
    m["xin"] = np.ascontiguousarray(np.concatenate([inp["ctx"][b], inp["x"][b]], 0)).astype(f)
    cv = np.stack([inp["c"][b].reshape(8, 128).T, inp["c_ctx"].reshape(8, 128).T], -1)
    m["cvec"] = np.ascontiguousarray(cv).astype(f)
    m["mod_w"] = inp["mod_w"]
    m["mod_b"] = inp["mod_b"]
    m["ssd_in_w"] = inp["ssd_in_w"]
    cw = inp["ssd_conv_w"]
    m["convw"] = np.ascontiguousarray(cw.reshape(2, 5, 24, 128).transpose(0, 3, 2, 1)).astype(f)
    m["convb"] = np.ascontiguousarray(inp["ssd_conv_b"].reshape(2, 24, 128).transpose(0, 2, 1)).astype(f)
    m["ssd_dt_bias"] = np.ascontiguousarray(inp["ssd_dt_bias"].reshape(2, 64))
    m["ssd_a_log"] = np.ascontiguousarray(inp["ssd_a_log"].reshape(2, 64))
    m["ssd_d_skip"] = inp["ssd_d_skip"]
    m["ssd_norm_w"] = inp["ssd_norm_w"]
    m["ssd_out_w"] = inp["ssd_out_w"]
    m["ret_in_w"] = inp["ret_in_w"]
    m["ret_decay_logit"] = inp["ret_decay_logit"]
    m["ret_gn_w"] = inp["ret_gn_w"]
    m["ret_gn_b"] = inp["ret_gn_b"]
    m["ret_out_w"] = inp["ret_out_w"]
    for k in ("ln_mix_g", "ln_mix_b", "ln_ffn_g", "ln_ffn_b"):
        m[k] = inp[k]
    m["moe_rw"] = np.ascontiguousarray(np.concatenate([inp["moe_group_w"], inp["moe_expert_w"]], -1)).astype(f)
    m["moe_rb"] = np.ascontiguousarray(np.concatenate([inp["moe_group_b"], inp["moe_expert_b"]], -1)).astype(f)
    m["moe_w_gate_up"] = inp["moe_w_gate_up"].reshape(DEPTH * 32 * 128, 8 * D)
    m["moe_w_down"] = inp["moe_w_down"].reshape(DEPTH * 32 * 128, 4 * D)
    return m


def _phase_moe(self, li, xcur, xnext, final):
    fw = self.fw
    NT = NCH
    with fw.scope():
        DI = fw.sb("DI", [128, 2 * NT], I32)
        Wt = fw.sb("Wt", [128, 2 * NT], F32)
        IGU = fw.sb("IGU", [128, NB], I32)
        with fw.scope():
            identf = fw.sb("identf", [128, 128], F32)
            self.ld(identf, identf[:], self.c_identf)
            slow = fw.sb("slow", [128, 128], BF16)
            self.ld(slow, slow[:], self.c_slow)
            onesf = fw.sb("onesf", [128, 128], F32)
            self.ld(onesf, onesf[:], self.c_ones)
            onesb = fw.sb("onesb", [128, 128], BF16)
            self.V(lambda e: e.tensor_copy(out=onesb[:], in_=onesf[:]), [onesf], [onesb])
            iota = fw.sb("iota", [128, 512], F32)
            self.ld(iota, iota[:], self.c_iota)
            pidx = fw.sb("pidx", [128, 12], F32)
            self.ld(pidx, pidx[:], self.c_pidx)
            rw = fw.sb("rw", [128, 8, 36], F32)
            self.ld(rw, rw[:], self.moe_rw[li].rearrange("(k p) e -> p k e", p=128))
            rb = fw.sb("rb", [128, 36], F32)
            self.ld_row(rb, rb[:], self.moe_rb[li:li + 1, :])
            OH = fw.sb("OH", [128, 2 * NT, 32], F32)
            SL = fw.sb("SL", [128, 2 * NT], F32)
            Acum = fw.sb("Acum", [128, 32], F32)
            Acb = fw.sb("Acb", [128, 32], BF16)
            self.V(lambda e: e.memset(Acum[:], 0.0), [], [Acum])
            self.V(lambda e: e.memset(Acb[:], 0.0), [], [Acb])
            tk = fw.sb("tk", [128, D], F32)
            tkT = fw.sb("tkT", [128, 8, 128], F32)
            L = fw.sb("L", [128, 36], F32)
            gmax = fw.sb("gmax", [128, 1], F32)
            ngmax = fw.sb("ngmax", [128, 1], F32)
            goh = fw.sb("goh", [128, 4], F32)
            pen = fw.sb("pen", [128, 4], F32)
            ex = fw.sb("ex", [128, 4], F32)
            gs = fw.sb("gs", [128, 1], F32)
            em = fw.sb("em", [128, 32], F32)
            em2 = fw.sb("em2", [128, 32], F32)
            m1 = fw.sb("m1", [128, 1], F32)
            m2 = fw.sb("m2", [128, 1], F32)
            dd = fw.sb("dd", [128, 1], F32)
            den = fw.sb("den", [128, 1], F32)
            A_ = fw.sb("A_", [128, 32], F32)
            Ab = fw.sb("Ab", [128, 32], BF16)
            Pp = fw.sb("Pp", [128, 32], F32)
            junk = fw.sb("junk", [128, 32], F32)
            pT = fw.ps("pT", [128, 8, 128], F32)
            plog = fw.ps("plog", [128, 512], F32)
            pP = fw.ps("pP", [128, 512], F32)
            for c in range(NT):
                cs_ = slice(c * 128, (c + 1) * 128)
                self.ld(tk, tk[:], self.tok[cs_, :])
                for k in range(8):
                    self.P(lambda e, k=k: e.transpose(pT[:, k, :], tk[:, k * 128:(k + 1) * 128], identf[:]), [tk, identf], [pT])
                self.A(lambda e: e.copy(out=tkT[:], in_=pT[:]), [pT], [tkT])
                for k in range(8):
                    self.P(lambda e, k=k: e.matmul(plog[:, 0:36], lhsT=tkT[:, k, :], rhs=rw[:, k, :], start=(k == 0), stop=(k == 7)),
                           [tkT, rw], [plog])
                self.V(lambda e: e.tensor_tensor(out=L[:], in0=plog[:, 0:36], in1=rb[:], op=ALU.add), [plog, rb], [L])
                self.V(lambda e: e.reduce_max(out=gmax[:], in_=L[:, 0:4], axis=AX.X), [L], [gmax])
                self.V(lambda e: e.tensor_scalar(out=goh[:], in0=L[:, 0:4], scalar1=gmax[:, 0:1], scalar2=None, op0=ALU.is_equal), [L, gmax], [goh])
                self.V(lambda e: e.tensor_scalar_mul(out=ngmax[:], in0=gmax[:], scalar1=-1.0), [gmax], [ngmax])
                self.A(lambda e: e.activation(out=ex[:], in_=L[:, 0:4], func=AF.Exp, bias=ngmax[:, 0:1], scale=1.0), [L, ngmax], [ex])
                self.V(lambda e: e.reduce_sum(out=gs[:], in_=ex[:], axis=AX.X), [ex], [gs])
                self.V(lambda e: e.reciprocal(out=gs[:], in_=gs[:]), [gs], [gs])
                self.V(lambda e: e.tensor_scalar(out=pen[:], in0=goh[:], scalar1=1e30, scalar2=-1e30, op0=ALU.mult, op1=ALU.add), [goh], [pen])
                self.V(lambda e: e.tensor_tensor(out=em[:].rearrange("p (g j) -> p g j", g=4), in0=L[:, 4:36].rearrange("p (g j) -> p g j", g=4),
                                                 in1=bcast(pen[:].unsqueeze(2), [128, 4, 8]), op=ALU.add), [L, pen], [em])
                self.V(lambda e: e.reduce_max(out=m1[:], in_=em[:], axis=AX.X), [em], [m1])
                o1 = OH[:, 2 * c, :]
                o2 = OH[:, 2 * c + 1, :]
                self.V(lambda e, o1=o1: e.tensor_scalar(out=o1, in0=em[:], scalar1=m1[:, 0:1], scalar2=None, op0=ALU.is_equal), [em, m1], [OH])
                self.V(lambda e, o1=o1: e.scalar_tensor_tensor(out=em2[:], in0=o1, scalar=-1e30, in1=em[:], op0=ALU.mult, op1=ALU.add), [OH, em], [em2])
                self.V(lambda e: e.reduce_max(out=m2[:], in_=em2[:], axis=AX.X), [em2], [m2])
                self.V(lambda e, o2=o2: e.tensor_scalar(out=o2, in0=em2[:], scalar1=m2[:, 0:1], scalar2=None, op0=ALU.is_equal), [em2, m2], [OH])
                self.V(lambda e: e.tensor_tensor(out=dd[:], in0=m2[:], in1=m1[:], op=ALU.subtract), [m1, m2], [dd])
                self.A(lambda e: e.activation(out=dd[:], in_=dd[:], func=AF.Exp), [dd], [dd])
                self.V(lambda e: e.tensor_scalar_add(out=den[:], in0=dd[:], scalar1=1.0), [dd], [den])
                self.V(lambda e: e.reciprocal(out=den[:], in_=den[:]), [den], [den])
                self.V(lambda e, c=c: e.tensor_tensor(out=Wt[:, 2 * c:2 * c + 1], in0=den[:], in1=gs[:], op=ALU.mult), [den, gs], [Wt])
                self.V(lambda e, c=c: e.tensor_tensor(out=Wt[:, 2 * c + 1:2 * c + 2], in0=Wt[:, 2 * c:2 * c + 1], in1=dd[:], op=ALU.mult), [Wt, dd], [Wt])
                self.V(lambda e, o1=o1, o2=o2: e.tensor_tensor(out=A_[:], in0=o1, in1=o2, op=ALU.add), [OH], [A_])
                self.V(lambda e: e.tensor_copy(out=Ab[:], in_=A_[:]), [A_], [Ab])
                self.P(lambda e: e.matmul(pP[:, 0:32], lhsT=slow[:], rhs=Ab[:], start=True, stop=False), [slow, Ab], [pP])
                self.P(lambda e: e.matmul(pP[:, 0:32], lhsT=onesb[:], rhs=Acb[:], start=False, stop=True), [onesb, Acb], [pP])
                self.V(lambda e: e.tensor_copy(out=Pp[:], in_=pP[:, 0:32]), [pP], [Pp])
                for k, ok in enumerate((o1, o2)):
                    self.V(lambda e, ok=ok: e.tensor_tensor(out=junk[:], in0=ok, in1=Pp[:], op=ALU.mult), [OH, Pp], [junk])
                    self.V(lambda e, c=c, k=k: e.reduce_sum(out=SL[:, 2 * c + k:2 * c + k + 1], in_=junk[:], axis=AX.X), [junk], [SL])
                self.V(lambda e: e.tensor_tensor(out=Acum[:], in0=Acum[:], in1=A_[:], op=ALU.add), [Acum, A_], [Acum])
                self.V(lambda e: e.tensor_copy(out=Acb[:], in_=Acum[:]), [Acum], [Acb])
            cnt = fw.sb("cnt", [128, 32], F32)
            self.P(lambda e: e.matmul(pP[:, 0:32], lhsT=onesb[:], rhs=Acb[:], start=True, stop=True), [onesb, Acb], [pP])
            self.V(lambda e: e.tensor_copy(out=cnt[:], in_=pP[:, 0:32]), [pP], [cnt])
            thr = fw.sb("thr", [128, 34], F32)
            self.V(lambda e: e.tensor_scalar_mul(out=thr[:], in0=iota[:, 0:34], scalar1=128.0), [iota], [thr])
            cmp = fw.sb("cmp", [128, 32, 34], F32)
            self.V(lambda e: e.tensor_tensor(out=cmp[:], in0=bcast(cnt[:].unsqueeze(2), [128, 32, 34]), in1=bcast(thr[:].unsqueeze(1), [128, 32, 34]),
                                             op=ALU.is_gt), [cnt, thr], [cmp])
            nblk = fw.sb("nblk", [128, 32], F32)
            self.V(lambda e: e.reduce_sum(out=nblk[:], in_=cmp[:], axis=AX.X), [cmp], [nblk])
            pa = fw.sb("pa", [128, 32], F32)
            pb_ = fw.sb("pb", [128, 32], F32)
            self.V(lambda e: e.tensor_copy(out=pa[:], in_=nblk[:]), [nblk], [pa])
            cur, oth = pa, pb_
            for sft in (1, 2, 4, 8, 16):
                self.V(lambda e, cur=cur, oth=oth: e.tensor_copy(out=oth[:], in_=cur[:]), [cur], [oth])
                self.V(lambda e, cur=cur, oth=oth, sft=sft: e.tensor_tensor(out=oth[:, sft:32], in0=cur[:, sft:32], in1=cur[:, 0:32 - sft], op=ALU.add),
                       [cur], [oth])
                cur, oth = oth, cur
            pend = cur
            pstart = fw.sb("pstart", [128, 32], F32)
            self.V(lambda e: e.tensor_tensor(out=pstart[:], in0=pend[:], in1=nblk[:], op=ALU.subtract), [pend, nblk], [pstart])
            self.V(lambda e: e.tensor_scalar_mul(out=pstart[:], in0=pstart[:], scalar1=128.0), [pstart], [pstart])
            big = fw.sb("big", [128, 2 * NT, 32], F32)
            self.V(lambda e: e.tensor_tensor(out=big[:], in0=OH[:], in1=bcast(pstart[:].unsqueeze(1), [128, 2 * NT, 32]), op=ALU.mult), [OH, pstart], [big])
            dst = fw.sb("dstf", [128, 2 * NT], F32)
            self.V(lambda e: e.reduce_sum(out=dst[:], in_=big[:], axis=AX.X), [big], [dst])
            self.V(lambda e: e.tensor_tensor(out=dst[:], in0=dst[:], in1=SL[:], op=ALU.add), [dst, SL], [dst])
            self.V(lambda e: e.tensor_copy(out=DI[:], in_=dst[:]), [dst], [DI])
            cmp2 = fw.sb("cmp2", [128, NB, 32], F32)
            self.V(lambda e: e.tensor_tensor(out=cmp2[:], in0=bcast(pend[:].unsqueeze(1), [128, NB, 32]), in1=bcast(iota[:, 0:NB].unsqueeze(2), [128, NB, 32]),
                                             op=ALU.is_le), [pend, iota], [cmp2])
            be = fw.sb("be", [128, NB], F32)
            self.V(lambda e: e.reduce_sum(out=be[:], in_=cmp2[:], axis=AX.X), [cmp2], [be])
            self.V(lambda e: e.tensor_scalar_min(out=be[:], in0=be[:], scalar1=31.0), [be], [be])
            same = fw.sb("same", [128, NB], F32)
            self.V(lambda e: e.memset(same[:], 0.0), [], [same])
            self.V(lambda e: e.tensor_tensor(out=same[:, 2:NB], in0=be[:, 2:NB], in1=be[:, 0:NB - 2], op=ALU.is_equal), [be], [same])
            self.V(lambda e: e.tensor_scalar(out=be[:], in0=be[:], scalar1=128.0, scalar2=float(li * 32 * 128), op0=ALU.mult, op1=ALU.add), [be], [be])
            self.V(lambda e: e.scalar_tensor_tensor(out=be[:], in0=same[:], scalar=1.0e6, in1=be[:], op0=ALU.mult, op1=ALU.add), [same, be], [be])
            self.V(lambda e: e.tensor_tensor(out=be[:], in0=be[:], in1=bcast(pidx[:, 0:1], [128, NB]), op=ALU.add), [be, pidx], [be])
            self.V(lambda e: e.tensor_copy(out=IGU[:], in_=be[:]), [be], [IGU])
        with fw.scope():
            tk = fw.sb("tk", [128, D], F32)
            tkb = fw.sb("tkb", [128, D], BF16)
            for c in range(NT):
                self.ld(tk, tk[:], self.tok[c * 128:(c + 1) * 128, :])
                self.V(lambda e: e.tensor_copy(out=tkb[:], in_=tk[:]), [tk], [tkb])
                for k in range(2):
                    col = 2 * c + k
                    self.fw.dma("gpsimd", lambda e, col=col: e.indirect_dma_start(
                        out=self.xbuf, out_offset=bass.IndirectOffsetOnAxis(ap=DI[:, col:col + 1], axis=0), in_=tkb[:, :], in_offset=None),
                        reads=[tkb, DI])
        with fw.scope():
            identb = fw.sb("identb", [128, 128], BF16)
            self.ld(identb, identb[:], self.c_identb)
            xb = [fw.sb("xb%d" % i, [128, D], BF16) for i in range(2)]
            xT = [fw.sb("xT%d" % i, [128, 8, 128], BF16) for i in range(2)]
            g32 = [fw.sb("g32_%d" % i, [128, 8, D], F32) for i in range(2)]
            d32 = [fw.sb("d32_%d" % i, [128, 4, D], F32) for i in range(2)]
            gbf = [fw.sb("gbf_%d" % i, [128, 8, D], BF16) for i in range(2)]
            dbf = [fw.sb("dbf_%d" % i, [128, 4, D], BF16) for i in range(2)]
            sg = fw.sb("sg", [128, 4, 128], F32)
            hT = fw.sb("hT", [128, 4, 128], BF16)
            ob = fw.sb("ob", [128, D], F32)
            ptr = fw.ps("ptr", [128, 8, 128], BF16)
            pH = [fw.ps("pH%d" % i, [128, 8, 128], F32) for i in range(2)]
            pO = fw.ps("pO", [128, D], F32)
            if getattr(self, "_bcreg", None) is None:
                self._bcreg = self.nc.gpsimd.alloc_register("bcreg")
                self.nc.gpsimd.reg_mov(self._bcreg, DEPTH * 32 * 128 - 1)
            bcreg = self._bcreg
            for b in range(NB):
                i = b % 2
                self.ld(xb[i], xb[i][:], self.xbuf[b * 128:(b + 1) * 128, :])
                idx = IGU[:, b:b + 1]
                self.fw.dma("gpsimd", lambda e, i=i, idx=idx: e.indirect_dma_start(
                    out=g32[i][:].rearrange("p k n -> p (k n)"), out_offset=None, in_=self.moe_wgu,
                    in_offset=bass.IndirectOffsetOnAxis(ap=idx, axis=0), bounds_check=bcreg, oob_is_err=False),
                    reads=[IGU], writes=[g32[i]])
                self.fw.dma("gpsimd", lambda e, i=i, idx=idx: e.indirect_dma_start(
                    out=d32[i][:].rearrange("p k n -> p (k n)"), out_offset=None, in_=self.moe_wd,
                    in_offset=bass.IndirectOffsetOnAxis(ap=idx, axis=0), bounds_check=bcreg, oob_is_err=False),
                    reads=[IGU], writes=[d32[i]])
                xbv = xb[i][:].rearrange("s (p k) -> s k p", k=8)
                for k in range(8):
                    self.P(lambda e, k=k, xbv=xbv: e.transpose(ptr[:, k, :], xbv[:, k, :], identb[:]), [xb[i], identb], [ptr])
                self.V(lambda e, i=i: e.tensor_copy(out=xT[i][:], in_=ptr[:]), [ptr], [xT[i]])
                for q in range(4):
                    sl = slice(q * 2, q * 2 + 2)
                    if q % 2 == 0:
                        self.A(lambda e, i=i, sl=sl: e.copy(out=gbf[i][:, sl, :], in_=g32[i][:, sl, :]), [g32[i]], [gbf[i]])
                    else:
                        self.V(lambda e, i=i, sl=sl: e.tensor_copy(out=gbf[i][:, sl, :], in_=g32[i][:, sl, :]), [g32[i]], [gbf[i]])
                self.A(lambda e, i=i: e.copy(out=dbf[i][:, 0:2, :], in_=d32[i][:, 0:2, :]), [d32[i]], [dbf[i]])
                self.V(lambda e, i=i: e.tensor_copy(out=dbf[i][:, 2:4, :], in_=d32[i][:, 2:4, :]), [d32[i]], [dbf[i]])
                ph = pH[i]
                for m in range(8):
                    tt, kh = m // 4, m % 4
                    for k in range(8):
                        lw = gbf[i][:, k, :].rearrange("d (two p k) -> d two k p", two=2, k=4)[:, tt, kh, :]
                        self.P(lambda e, i=i, m=m, k=k, ph=ph, lw=lw: e.matmul(ph[:, m, :], lhsT=lw, rhs=xT[i][:, k, :],
                                                                               start=(k == 0), stop=(k == 7)), [gbf[i], xT[i]], [ph])
                self.A(lambda e, ph=ph: e.activation(out=sg[:], in_=ph[:, 0:4, :], func=AF.Silu), [ph], [sg])
                self.V(lambda e, ph=ph: e.tensor_tensor(out=hT[:], in0=sg[:], in1=ph[:, 4:8, :], op=ALU.mult), [sg, ph], [hT])
                for nn in range(2):
                    for k in range(4):
                        self.P(lambda e, i=i, nn=nn, k=k: e.matmul(pO[:, nn * 512:(nn + 1) * 512], lhsT=hT[:, k, :], rhs=dbf[i][:, k, nn * 512:(nn + 1) * 512],
                                                                   start=(k == 0), stop=(k == 3)), [hT, dbf[i]], [pO])
                self.A(lambda e: e.copy(out=ob[:], in_=pO[:]), [pO], [ob])
                self.st(self.ybuf[b * 128:(b + 1) * 128, :], ob, ob[:])
        with fw.scope():
            g2 = []
            for v in range(2):
                t = fw.sb("g2_%d" % v, [128, D], F32)
                self.ld_row(t, t[:], self.modrow[v:v + 1, 5 * D:6 * D])
                g2.append(t)
            lng = fw.sb("lng", [128, D], F32)
            lnb = fw.sb("lnb", [128, D], F32)
            self.ld_row(lng, lng[:], self.ln_ffn_g[li:li + 1, :])
            self.ld_row(lnb, lnb[:], self.ln_ffn_b[li:li + 1, :])
            o1_ = [fw.sb("o1%d" % i, [128, D], F32) for i in range(2)]
            o2_ = [fw.sb("o2%d" % i, [128, D], F32) for i in range(2)]
            xt_ = [fw.sb("xt%d" % i, [128, D], F32) for i in range(2)]
            t1_ = [fw.sb("t1%d" % i, [128, D], F32) for i in range(2)]
            st2 = fw.sb("st2", [128, 2, 6], F32)
            mv2 = fw.sb("mv2", [128, 2], F32)
            rs2 = fw.sb("rs2", [128, 1], F32)
            for c in range(NT):
                if final and c < NCTX:
                    continue
                v = 1 if c < NCTX else 0
                o1, o2, xt, t1 = o1_[c % 2], o2_[c % 2], xt_[c % 2], t1_[c % 2]
                cs_ = slice(c * 128, (c + 1) * 128)
                for k, ot in enumerate((o1, o2)):
                    col = 2 * c + k
                    self.fw.dma("gpsimd", lambda e, col=col, ot=ot: e.indirect_dma_start(
                        out=ot[:, :], out_offset=None, in_=self.ybuf, in_offset=bass.IndirectOffsetOnAxis(ap=DI[:, col:col + 1], axis=0)),
                        reads=[DI], writes=[ot])
                self.ld(xt, xt[:], xcur[cs_, :])
                self.V(lambda e, c=c, o1=o1: e.tensor_scalar(out=o1[:], in0=o1[:], scalar1=Wt[:, 2 * c:2 * c + 1], scalar2=None, op0=ALU.mult), [o1, Wt], [o1])
                self.V(lambda e, c=c, o1=o1, o2=o2: e.scalar_tensor_tensor(out=o1[:], in0=o2[:], scalar=Wt[:, 2 * c + 1:2 * c + 2], in1=o1[:], op0=ALU.mult, op1=ALU.add),
                       [o2, Wt, o1], [o1])
                self.G(lambda e, v=v, o1=o1: e.tensor_tensor(out=o1[:], in0=o1[:], in1=g2[v][:], op=ALU.mult), [o1, g2[v]], [o1])
                self.V(lambda e, o1=o1, o2=o2, xt=xt: e.scalar_tensor_tensor(out=o2[:], in0=xt[:], scalar=ALPHA, in1=o1[:], op0=ALU.mult, op1=ALU.add), [xt, o1], [o2])
                self.layer_norm(o2, t1, st2, mv2, rs2, lng, lnb)
                if final:
                    self.st(self.yout[(c - NCTX) * 128:(c - NCTX + 1) * 128, :], t1, t1[:])
                else:
                    self.st(xnext[cs_, :], t1, t1[:])


Builder.phase_moe = _phase_moe


def build_program(nlayers=DEPTH):
    nc = bass.Bass("TRN2", target_bir_lowering=False)
    b = Builder(nc)
    b.declare()
    xcur = b.xin
    for li in range(nlayers):
        j = li // 2
        b.phase_mod(li)
        if li % 2 == 0:
            b.phase_in_ssd(li, j, xcur)
            b.phase_scan(li, j, True, xcur, b.xA)
        else:
            b.phase_in_ret(li, j, xcur)
            b.phase_scan(li, j, False, xcur, b.xA)
        b.phase_moe(li, b.xA, b.xB, final=(li == nlayers - 1))
        xcur = b.xB
    b.fw.barrier()
    b.fw.root.close()
    return nc


def kernel(**inputs):
    inp = {k: np.asarray(v) for k, v in inputs.items()}
    nc = build_program()
    consts = host_consts()
    maps = []
    for c in range(8):
        m = host_inputs(inp, c)
        m.update(consts)
        maps.append(m)
    res = run_bass_kernel_spmd(nc, maps, core_ids=list(range(8)))
    out = np.stack([np.asarray(res.results[c]["yout"]) for c in range(8)], 0)
    return out.astype(np.float32)


def _phase_scan2(self, li, j, ssd, xcur, xnext):
    fw = self.fw
    G = 4
    Hg = 8 if ssd else 1
    KC = 1 if ssd else 2
    H = G * Hg
    dv = 2048 // H
    dk = KC * 128
    with fw.scope():
        identb = fw.sb("identb", [128, 128], BF16)
        self.ld(identb, identb[:], self.c_identb)
        ones = fw.sb("ones", [128, 128], F32)
        self.ld(ones, ones[:], self.c_ones)
        tri = fw.sb("tri", [128, 128], F32)
        Wo = fw.sb("Wo", [128, 16, D], BF16)
        owv = (self.ssd_out_w if ssd else self.ret_out_w)[j].rearrange("(k p) n -> p k n", p=128)
        with fw.scope():
            wst = fw.sb("wstO", [128, 4, D], F32)
            for q in range(4):
                self.ld(wst, wst[:], owv[:, q * 4:(q + 1) * 4, :])
                self.G(lambda e, q=q: e.tensor_copy(out=Wo[:, q * 4:(q + 1) * 4, :], in_=wst[:]), [wst], [Wo])
        g1 = fw.sb("g1", [128, D], F32)
        sh2 = fw.sb("sh2", [128, D], F32)
        sc2 = fw.sb("sc2", [128, D], F32)

        def load_rows(v):
            self.ld_row(g1, g1[:], self.modrow[v:v + 1, 2 * D:3 * D])
            self.ld_row(sh2, sh2[:], self.modrow[v:v + 1, 3 * D:4 * D])
            self.ld_row(sc2, sc2[:], self.modrow[v:v + 1, 4 * D:5 * D])
            self.V(lambda e: e.tensor_scalar_add(out=sc2[:], in0=sc2[:], scalar1=1.0), [sc2], [sc2])

        lng = fw.sb("lng", [128, D], F32)
        lnb = fw.sb("lnb", [128, D], F32)
        self.ld_row(lng, lng[:], self.ln_mix_g[li:li + 1, :])
        self.ld_row(lnb, lnb[:], self.ln_mix_b[li:li + 1, :])
        nw = fw.sb("nw", [128, 2048], F32)
        self.ld_row(nw, nw[:], (self.ssd_norm_w if ssd else self.ret_gn_w)[j:j + 1, :])
        if ssd:
            dsk = fw.sb("dsk", [128, 32], F32)
            self.ld_row(dsk, dsk[:], self.ssd_d_skip[j:j + 1, :])
        else:
            nb_ = fw.sb("nb", [128, 2048], F32)
            self.ld_row(nb_, nb_[:], self.ret_gn_b[j:j + 1, :])
        S = fw.sb("S", [128, KC, 2048], F32)
        Sb = fw.sb("Sb", [128, KC, 2048], BF16)

        def dbl(name, shape, dt):
            return [fw.sb(name + "0", shape, dt), fw.sb(name + "1", shape, dt)]

        qt = dbl("qt", [128, G * KC, 128], BF16)
        kt = fw.sb("kt", [128, G * KC, 128], BF16)
        ktm = dbl("ktm", [128, G * dk], BF16)
        vt = dbl("vt", [128, 2048], BF16)
        xdt = dbl("xdt", [128, 2048], BF16) if ssd else vt
        xe = dbl("xe", [128, 2048], BF16)
        cbm = dbl("cbm", [128, G, 128], F32)
        la = fw.sb("la", [128, 64], F32)
        dt = fw.sb("dt", [128, 64], F32)
        acs = fw.sb("acs", [128, 2 * H], F32)
        nacs = fw.sb("nacs", [128, H], F32)
        wend = fw.sb("wend", [128, H], F32)
        if ssd:
            ea = dbl("ea", [128, H], F32)
            cd = dbl("cd", [128, H], F32)
            E = [[fw.sb("E%d_%d" % (p_, g), [128, Hg, 128], BF16) for g in range(G)] for p_ in range(2)]
        else:
            ea0 = fw.sb("ea", [128, H], F32)
            cd0 = fw.sb("cd", [128, H], F32)
            ea = [ea0, ea0]
            cd = [cd0, cd0]
            E0 = [fw.sb("E_%d" % g, [128, Hg, 128], F32) for g in range(G)]
            E = [E0, E0]
        lt = dbl("lt", [128, Hg, 128], F32)
        nlb = dbl("nlb", [128, Hg, 128], F32)
        nones = fw.sb("nones", [128, 128], F32)
        self.V(lambda e: e.memset(nones[:], -1.0), [], [nones])
        M = dbl("M", [128, Hg, 128], BF16)
        yc = fw.sb("yc", [128, 2048], F32)
        tmp = dbl("tmp", [128, 512], F32)
        yfl = fw.sb("yfl", [128, 2048], F32)
        gt = fw.sb("gt", [128, 2048], F32)
        yb = fw.sb("yb", [128, 2048], BF16)
        yT = fw.sb("yT", [128, 16, 128], BF16)
        st6 = fw.sb("st6", [128, 4, 6], F32)
        mv = fw.sb("mv", [128, 4, 2], F32)
        ss = fw.sb("ss", [128, 4], F32)
        rstd = fw.sb("rstd", [128, 4], F32)
        xt = fw.sb("xt", [128, D], F32)
        t1 = fw.sb("t1", [128, D], F32)
        t2 = fw.sb("t2", [128, D], F32)
        st2 = fw.sb("st2", [128, 2, 6], F32)
        mv2 = fw.sb("mv2", [128, 2], F32)
        rs2 = fw.sb("rs2", [128, 1], F32)
        psm = fw.ps("psm", [128, 512], F32)
        pcb = fw.ps("pcb", [128, 512], F32)
        pbc = [fw.ps("pbc%d" % i, [128, 512], F32) for i in range(2)]
        pyd = fw.ps("pyd", [128, 512], F32)
        pyo = fw.ps("pyo", [128, 512], F32)
        pst = fw.ps("pst", [128, 512], F32)
        ptr = fw.ps("ptr", [128, 8, 128], BF16)

        qtv = self.QT.rearrange("g k p t -> p g k t")
        ktv = self.KT.rearrange("g k p t -> p g k t")
        r3 = lambda ap, h: ap.rearrange("p (h d) -> p h d", h=h)

        def decay_prep(dirn, par):
            lad = la[:, dirn * 32:dirn * 32 + H] if ssd else la[:, 0:H]
            self.P(lambda e: e.matmul(psm[:, 0:H], lhsT=tri[:], rhs=lad, start=True, stop=True), [tri, la], [psm])
            self.P(lambda e: e.matmul(psm[:, H:2 * H], lhsT=ones[:], rhs=lad, start=True, stop=True), [ones, la], [psm])
            self.V(lambda e: e.tensor_copy(out=acs[:], in_=psm[:, 0:2 * H]), [psm], [acs])
            self.V(lambda e: e.tensor_scalar_mul(out=nacs[:], in0=acs[:, 0:H], scalar1=-1.0), [acs], [nacs])
            self.A(lambda e: e.activation(out=ea[par][:], in_=acs[:, 0:H], func=AF.Exp), [acs], [ea[par]])
            self.V(lambda e: e.tensor_tensor(out=wend[:], in0=acs[:, H:2 * H], in1=acs[:, 0:H], op=ALU.subtract), [acs], [wend])
            self.A(lambda e: e.activation(out=wend[:], in_=wend[:], func=AF.Exp), [wend], [wend])
            self.A(lambda e: e.activation(out=cd[par][:], in_=acs[:, H:2 * H], func=AF.Exp), [acs], [cd[par]])
            for g in range(G):
                ltg = lt[g % 2]
                nlg = nlb[g % 2]
                ladg = la[:, dirn * 32 + g * Hg:dirn * 32 + (g + 1) * Hg] if ssd else la[:, g:g + 1]
                self.G(lambda e, ladg=ladg, ltg=ltg: e.tensor_tensor(out=ltg[:], in0=bcast(tri[:].unsqueeze(1), [128, Hg, 128]),
                                                                     in1=bcast(ladg.unsqueeze(2), [128, Hg, 128]), op=ALU.mult), [tri, la], [ltg])
                self.G(lambda e, ladg=ladg, nlg=nlg: e.tensor_tensor(out=nlg[:], in0=bcast(nones[:].unsqueeze(1), [128, Hg, 128]),
                                                                     in1=bcast(ladg.unsqueeze(2), [128, Hg, 128]), op=ALU.mult), [nones, la], [nlg])
                Eg = E[par][g]
                for q in range((Hg + 3) // 4):
                    nh = min(4, Hg - q * 4)
                    pb = pbc[q % 2]
                    self.P(lambda e, q=q, nh=nh, pb=pb, ltg=ltg: e.matmul(pb[:, 0:nh * 128], lhsT=ones[:], rhs=ltg[:, q * 4:q * 4 + nh, :],
                                                                          start=True, stop=False), [ones, ltg], [pb])
                    self.P(lambda e, q=q, nh=nh, pb=pb, nlg=nlg: e.matmul(pb[:, 0:nh * 128], lhsT=tri[:], rhs=nlg[:, q * 4:q * 4 + nh, :],
                                                                          start=False, stop=True), [tri, nlg], [pb])
                    self.A(lambda e, q=q, nh=nh, pb=pb, Eg=Eg: e.activation(out=Eg[:, q * 4:q * 4 + nh, :].rearrange("p h l -> p (h l)"), in_=pb[:, 0:nh * 128],
                                                                            func=AF.Exp), [pb], [Eg])

        def stage1(c, dirn, par):
            cs_ = slice(c * 128, (c + 1) * 128)
            self.ld(qt[par], qt[par][:].rearrange("p (g k) t -> p g k t", g=G), qtv[:, :, 0:KC, cs_])
            self.ld(kt, kt[:].rearrange("p (g k) t -> p g k t", g=G), ktv[:, :, 0:KC, cs_])
            self.ld(ktm[par], ktm[par][:], self.Ktm[cs_, 0:G * dk])
            self.ld(vt[par], vt[par][:], self.Vtm[cs_, :])
            if ssd:
                self.ld(la, la[:], self.latm[cs_, :])
                self.ld(dt, dt[:], self.dttm[cs_, :])
                decay_prep(dirn, par)
                self.V(lambda e: e.tensor_tensor(out=r3(xdt[par][:], H), in0=r3(vt[par][:], H),
                                                 in1=bcast(dt[:, dirn * 32:dirn * 32 + 32].unsqueeze(2), [128, H, dv]), op=ALU.mult),
                       [vt[par], dt], [xdt[par]])
            self.G(lambda e: e.tensor_tensor(out=r3(xe[par][:], H), in0=r3(xdt[par][:], H),
                                             in1=bcast(wend[:].unsqueeze(2), [128, H, dv]), op=ALU.mult), [xdt[par], wend], [xe[par]])
            for g in range(G):
                for kc in range(KC):
                    self.P(lambda e, g=g, kc=kc: e.matmul(pcb[:, g * 128:(g + 1) * 128], lhsT=kt[:, g * KC + kc, :], rhs=qt[par][:, g * KC + kc, :],
                                                          start=(kc == 0), stop=(kc == KC - 1)), [kt, qt[par]], [pcb])
            self.V(lambda e: e.tensor_tensor(out=cbm[par][:], in0=pcb[:].rearrange("p (g l) -> p g l", g=G),
                                             in1=bcast(tri[:].unsqueeze(1), [128, G, 128]), op=ALU.mult), [pcb, tri], [cbm[par]])

        def emitM(g, par):
            Mg = M[g % 2]
            self.V(lambda e: e.scalar_tensor_tensor(out=Mg[:], in0=E[par][g][:], scalar=1.0,
                                                    in1=bcast(cbm[par][:, g:g + 1, :], [128, Hg, 128]), op0=ALU.min, op1=ALU.mult),
                   [E[par][g], cbm[par]], [Mg])

        def stage2(c, dirn, par, v):
            cs_ = slice(c * 128, (c + 1) * 128)
            emitM(0, par)
            for g in range(G):
                gs = slice(g * 512, (g + 1) * 512)
                if g + 1 < G:
                    emitM(g + 1, par)
                Mg = M[g % 2]
                tg = tmp[g % 2]
                for hh in range(Hg):
                    h = g * Hg + hh
                    self.P(lambda e, h=h, hh=hh, Mg=Mg: e.matmul(pyd[:, hh * dv:(hh + 1) * dv], lhsT=Mg[:, hh, :], rhs=xdt[par][:, h * dv:(h + 1) * dv],
                                                                 start=True, stop=True), [Mg, xdt[par]], [pyd])
                for kc in range(KC):
                    self.P(lambda e, g=g, kc=kc, gs=gs: e.matmul(pyo[:], lhsT=qt[par][:, g * KC + kc, :], rhs=Sb[:, kc, gs],
                                                                 start=(kc == 0), stop=(kc == KC - 1)), [qt[par], Sb], [pyo])
                self.V(lambda e, g=g, tg=tg: e.tensor_tensor(out=r3(tg[:], Hg), in0=r3(pyo[:], Hg),
                                                             in1=bcast(ea[par][:, g * Hg:(g + 1) * Hg].unsqueeze(2), [128, Hg, dv]), op=ALU.mult),
                       [pyo, ea[par]], [tg])
                self.V(lambda e, gs=gs, tg=tg: e.tensor_tensor(out=yc[:, gs], in0=tg[:], in1=pyd[:], op=ALU.add), [tg, pyd], [yc])
                for kc in range(KC):
                    self.P(lambda e, g=g, kc=kc, gs=gs: e.matmul(pst[:], lhsT=ktm[par][:, g * dk + kc * 128:g * dk + (kc + 1) * 128], rhs=xe[par][:, gs],
                                                                 start=True, stop=True), [ktm[par], xe[par]], [pst])
                    self.G(lambda e, g=g, kc=kc, gs=gs: e.tensor_tensor(out=r3(S[:, kc, gs], Hg), in0=r3(S[:, kc, gs], Hg),
                                                                        in1=bcast(cd[par][:, g * Hg:(g + 1) * Hg].unsqueeze(2), [128, Hg, dv]),
                                                                        op=ALU.mult), [S, cd[par]], [S])
                    self.V(lambda e, kc=kc, gs=gs: e.tensor_tensor(out=S[:, kc, gs], in0=S[:, kc, gs], in1=pst[:], op=ALU.add), [S, pst], [S])
                    self.A(lambda e, kc=kc, gs=gs: e.copy(out=Sb[:, kc, gs], in_=S[:, kc, gs]), [S], [Sb])
            if dirn == 0:
                deferred.append(lambda: self.st(self.yf[cs_, :], yc, yc[:]))
                return
            vtp = vt[par]
            self.ld(yfl, yfl[:], self.yf[cs_, :])
            self.ld(gt, gt[:], self.gate[cs_, :])
            self.V(lambda e: e.tensor_tensor(out=yc[:], in0=yc[:], in1=yfl[:], op=ALU.add), [yc, yfl], [yc])
            if ssd:
                self.G(lambda e: e.tensor_tensor(out=r3(yfl[:], H), in0=r3(vtp[:], H),
                                                 in1=bcast(dsk[:].unsqueeze(2), [128, H, dv]), op=ALU.mult), [vtp, dsk], [yfl])
                self.V(lambda e: e.tensor_tensor(out=yc[:], in0=yc[:], in1=yfl[:], op=ALU.add), [yc, yfl], [yc])
                self.A(lambda e: e.activation(out=gt[:], in_=gt[:], func=AF.Silu), [gt], [gt])
                self.V(lambda e: e.tensor_tensor(out=yc[:], in0=yc[:], in1=gt[:], op=ALU.mult), [yc, gt], [yc])
                for g in range(4):
                    self.V(lambda e, g=g: e.bn_stats(out=st6[:, g, :], in_=yc[:, g * 512:(g + 1) * 512]), [yc], [st6])
                    self.V(lambda e, g=g: e.bn_aggr(out=mv[:, g, :], in_=st6[:, g, :]), [st6], [mv])
                self.V(lambda e: e.tensor_tensor(out=ss[:], in0=mv[:, :, 0], in1=mv[:, :, 0], op=ALU.mult), [mv], [ss])
                self.V(lambda e: e.tensor_tensor(out=ss[:], in0=ss[:], in1=mv[:, :, 1], op=ALU.add), [ss, mv], [ss])
                self.A(lambda e: e.activation(out=ss[:], in_=ss[:], func=AF.Sqrt, bias=EPS), [ss], [ss])
                self.V(lambda e: e.reciprocal(out=rstd[:], in_=ss[:]), [ss], [rstd])
                for g in range(4):
                    gs = slice(g * 512, (g + 1) * 512)
                    self.V(lambda e, g=g, gs=gs: e.scalar_tensor_tensor(out=yb[:, gs], in0=yc[:, gs], scalar=rstd[:, g:g + 1], in1=nw[:, gs],
                                                                        op0=ALU.mult, op1=ALU.mult), [yc, rstd, nw], [yb])
            else:
                for g in range(4):
                    self.V(lambda e, g=g: e.bn_stats(out=st6[:, g, :], in_=yc[:, g * 512:(g + 1) * 512]), [yc], [st6])
                    self.V(lambda e, g=g: e.bn_aggr(out=mv[:, g, :], in_=st6[:, g, :]), [st6], [mv])
                self.A(lambda e: e.activation(out=ss[:], in_=mv[:, :, 1], func=AF.Sqrt, bias=EPS), [mv], [ss])
                self.V(lambda e: e.reciprocal(out=rstd[:], in_=ss[:]), [ss], [rstd])
                self.A(lambda e: e.activation(out=gt[:], in_=gt[:], func=AF.Silu), [gt], [gt])
                for g in range(4):
                    gs = slice(g * 512, (g + 1) * 512)
                    self.V(lambda e, g=g, gs=gs: e.tensor_scalar(out=yc[:, gs], in0=yc[:, gs], scalar1=mv[:, g, 0:1], scalar2=rstd[:, g:g + 1],
                                                                 op0=ALU.subtract, op1=ALU.mult), [yc, mv, rstd], [yc])
                self.G(lambda e: e.tensor_tensor(out=yc[:], in0=yc[:], in1=nw[:], op=ALU.mult), [yc, nw], [yc])
                self.V(lambda e: e.tensor_tensor(out=yc[:], in0=yc[:], in1=nb_[:], op=ALU.add), [yc, nb_], [yc])
                self.V(lambda e: e.tensor_tensor(out=yb[:], in0=yc[:], in1=gt[:], op=ALU.mult), [yc, gt], [yb])
            for half in range(2):
                for kk in range(8):
                    k = half * 8 + kk
                    self.P(lambda e, k=k, kk=kk: e.transpose(ptr[:, kk, :], yb[:, k * 128:(k + 1) * 128], identb[:]), [yb, identb], [ptr])
                self.A(lambda e, half=half: e.copy(out=yT[:, half * 8:(half + 1) * 8, :], in_=ptr[:]), [ptr], [yT])
            pos = (pyd, pyo)
            for nn in range(2):
                for k in range(16):
                    self.P(lambda e, k=k, nn=nn: e.matmul(pos[nn][:], lhsT=yT[:, k, :], rhs=Wo[:, k, nn * 512:(nn + 1) * 512],
                                                          start=(k == 0), stop=(k == 15)), [yT, Wo], [pos[nn]])
            self.ld(xt, xt[:], xcur[cs_, :])
            for nn in range(2):
                ns = slice(nn * 512, (nn + 1) * 512)
                self.V(lambda e, nn=nn, ns=ns: e.tensor_tensor(out=t1[:, ns], in0=pos[nn][:], in1=g1[:, ns], op=ALU.mult), [pos[nn], g1], [t1])
            self.V(lambda e: e.scalar_tensor_tensor(out=t2[:], in0=xt[:], scalar=ALPHA, in1=t1[:], op0=ALU.mult, op1=ALU.add), [xt, t1], [t2])
            self.layer_norm(t2, t1, st2, mv2, rs2, lng, lnb)
            self.G(lambda e: e.tensor_tensor(out=t2[:], in0=t1[:], in1=sc2[:], op=ALU.mult), [t1, sc2], [t2])
            self.V(lambda e: e.tensor_tensor(out=t2[:], in0=t2[:], in1=sh2[:], op=ALU.add), [t2, sh2], [t2])
            deferred.append(lambda: self.st(xnext[cs_, :], t1, t1[:]))
            deferred.append(lambda: self.st(self.tok[cs_, :], t2, t2[:]))

        deferred = []

        def flush():
            for f in deferred:
                f()
            del deferred[:]

        for dirn in range(2):
            order = list(range(NCH)) if dirn == 0 else [1, 0] + list(range(NCH - 1, 1, -1))
            self.ld(tri, tri[:], self.c_trif[dirn])
            self.V(lambda e: e.memset(S[:], 0.0), [], [S])
            self.G(lambda e: e.memset(Sb[:], 0.0), [], [Sb])
            if dirn == 1:
                load_rows(1)
            if not ssd:
                self.ld_row(la, la[:, 0:4], self.ret_decay[j, dirn:dirn + 1, :])
                self.A(lambda e: e.activation(out=la[:, 0:4], in_=la[:, 0:4], func=AF.Exp, scale=-1.0), [la], [la])
                self.A(lambda e: e.activation(out=la[:, 0:4], in_=la[:, 0:4], func=AF.Ln, bias=1.0), [la], [la])
                self.V(lambda e: e.tensor_scalar_mul(out=la[:, 0:4], in0=la[:, 0:4], scalar1=-1.0), [la], [la])
                decay_prep(dirn, 0)
            stage1(order[0], dirn, 0)
            for i, c in enumerate(order):
                if i + 1 < len(order):
                    stage1(order[i + 1], dirn, (i + 1) % 2)
                flush()
                if dirn == 1 and i == NCTX:
                    load_rows(0)
                stage2(c, dirn, i % 2, 1 if c < NCTX else 0)
            flush()
            if dirn == 0:
                fw.barrier()


Builder.phase_scan = _phase_scan2
```

```python
import numpy as np
import ml_dtypes
from contextlib import ExitStack, contextmanager
import concourse.bass as bass
import concourse.mybir as mybir
from concourse.bass_utils import run_bass_kernel_spmd

F32 = mybir.dt.float32
BF16 = mybir.dt.bfloat16
I32 = mybir.dt.int32
ALU = mybir.AluOpType
AF = mybir.ActivationFunctionType
AX = mybir.AxisListType

ENGS = ["tensor", "vector", "scalar", "gpsimd", "sync"]
EPOCH = 20000
NDMA_SEM = 8

D = 1024
T = 4352
NCH = 34
NCTX = 2
NB = 100
DEPTH = 4
ALPHA = (2.0 * DEPTH) ** 0.25
EPS = 1e-5


class Res:
    __slots__ = ("w", "r")

    def __init__(self):
        self.w = None
        self.r = {}


class Tl:
    def __init__(self, t):
        self.t = t
        self.r = Res()

    def __getitem__(self, k):
        return self.t[k]


class FW:
    def __init__(self, nc):
        self.nc = nc
        self.root = ExitStack()
        self.es = self.root
        self.cnt = {e: 0 for e in ENGS}
        self.sems = {}
        self.waited = {e: {} for e in ENGS}
        self.dma_i = {e: 0 for e in ENGS}
        self.dma_last = {}
        self.latest = {}
        self.uid = 0

    def sem(self, key):
        if key not in self.sems:
            self.sems[key] = self.root.enter_context(self.nc.semaphore("s_%s_%s" % key))
        return self.sems[key]

    def sb(self, name, shape, dt):
        self.uid += 1
        return Tl(self.es.enter_context(self.nc.sbuf_tensor("%s_%d" % (name, self.uid), list(shape), dt)))

    def ps(self, name, shape, dt):
        self.uid += 1
        return Tl(self.es.enter_context(self.nc.psum_tensor("%s_%d" % (name, self.uid), list(shape), dt)))

    @contextmanager
    def scope(self):
        old = self.es
        self.es = ExitStack()
        try:
            yield
        finally:
            self.barrier()
            self.es.close()
            self.es = old

    def barrier(self):
        for eng in ENGS:
            for key, val in list(self.latest.items()):
                self._wait(eng, (key, val))

    def _wait(self, eng, ev):
        if ev is None:
            return
        key, val = ev
        if self.waited[eng].get(key, 0) >= val:
            return
        self.waited[eng][key] = val
        getattr(self.nc, eng).wait_ge(self.sem(key), val)

    def _deps(self, eng, reads, writes):
        evs = []
        for r in reads:
            if r.w is not None:
                evs.append(r.w)
        for w in writes:
            if w.w is not None:
                evs.append(w.w)
            evs.extend(w.r.items())
        for ev in evs:
            if ev[0][0] == "tensor" and eng == "tensor":
                continue
            self._wait(eng, ev)

    def _record(self, ev, reads, writes):
        self.latest[ev[0]] = ev[1]
        for r in reads:
            if r.r.get(ev[0], 0) < ev[1]:
                r.r[ev[0]] = ev[1]
        for w in writes:
            w.w = ev
            w.r = {}

    def op(self, eng, fn, reads=(), writes=()):
        reads = [t.r for t in reads]
        writes = [t.r for t in writes]
        self._deps(eng, reads, writes)
        c = self.cnt[eng]
        key = (eng, c // EPOCH)
        val = c % EPOCH + 1
        self.cnt[eng] = c + 1
        fn(getattr(self.nc, eng)).then_inc(self.sem(key), 1)
        self._record((key, val), reads, writes)

    def dma(self, eng, fn, reads=(), writes=()):
        reads = [t.r for t in reads]
        writes = [t.r for t in writes]
        self._deps(eng, reads, writes)
        i = self.dma_i[eng]
        self.dma_i[eng] = i + 1
        key = ("d" + eng, i % NDMA_SEM)
        prev = self.dma_last.get(key, 0)
        if prev:
            self._wait(eng, (key, prev))
        val = prev + 16
        self.dma_last[key] = val
        fn(getattr(self.nc, eng)).then_inc(self.sem(key), 16)
        self._record((key, val), reads, writes)


def bcast(ap, shape):
    return ap.to_broadcast(list(shape))


class Builder:
    def __init__(self, nc, nlayers=DEPTH, stop=None):
        self.nc = nc
        self.fw = FW(nc)
        self.nlayers = nlayers
        self.stop = stop
        self.dram = {}
        self.debug = set()
        self.only = None

    def din(self, name, shape, dt):
        if self.only is not None and name not in self.only:
            return None
        a = self.nc.dram_tensor(name, list(shape), dt, kind="ExternalInput").ap()
        self.dram[name] = a
        return a

    def dscr(self, name, shape, dt):
        kind = "ExternalOutput" if name in self.debug else "Internal"
        a = self.nc.dram_tensor(name, list(shape), dt, kind=kind).ap()
        self.dram[name] = a
        return a

    def ld(self, tile, dst, src, eng="sync"):
        self.fw.dma(eng, lambda e: e.dma_start(out=dst, in_=src), writes=[tile])

    def st(self, dst, tile, src, eng="sync"):
        self.fw.dma(eng, lambda e: e.dma_start(out=dst, in_=src), reads=[tile])

    def V(self, fn, rd, wr):
        self.fw.op("vector", fn, rd, wr)

    def A(self, fn, rd, wr):
        self.fw.op("scalar", fn, rd, wr)

    def G(self, fn, rd, wr):
        self.fw.op("gpsimd", fn, rd, wr)

    def P(self, fn, rd, wr):
        self.fw.op("tensor", fn, rd, wr)

    def ld_row(self, tile, dst, src_row, n=128):
        self.ld(tile, dst, src_row.partition_broadcast(n))

    def declare(self):
        d = self.din
        self.xin = d("xin", [T, D], F32)
        self.cvec = d("cvec", [128, 8, 2], F32)
        self.mod_w = d("mod_w", [DEPTH, D, 6 * D], F32)
        self.mod_b = d("mod_b", [DEPTH, 6 * D], F32)
        self.ssd_in_w = d("ssd_in_w", [2, D, 5184], F32)
        self.convw = d("convw", [2, 128, 24, 5], F32)
        self.convb = d("convb", [2, 128, 24], F32)
        self.ssd_dt_bias = d("ssd_dt_bias", [2, 64], F32)
        self.ssd_a_log = d("ssd_a_log", [2, 64], F32)
        self.ssd_d_skip = d("ssd_d_skip", [2, 32], F32)
        self.ssd_norm_w = d("ssd_norm_w", [2, 2048], F32)
        self.ssd_out_w = d("ssd_out_w", [2, 2048, D], F32)
        self.ret_in_w = d("ret_in_w", [2, D, 6144], F32)
        self.ret_decay = d("ret_decay_logit", [2, 2, 4], F32)
        self.ret_gn_w = d("ret_gn_w", [2, 2048], F32)
        self.ret_gn_b = d("ret_gn_b", [2, 2048], F32)
        self.ret_out_w = d("ret_out_w", [2, 2048, D], F32)
        self.ln_mix_g = d("ln_mix_g", [DEPTH, D], F32)
        self.ln_mix_b = d("ln_mix_b", [DEPTH, D], F32)
        self.ln_ffn_g = d("ln_ffn_g", [DEPTH, D], F32)
        self.ln_ffn_b = d("ln_ffn_b", [DEPTH, D], F32)
        self.moe_rw = d("moe_rw", [DEPTH, D, 36], F32)
        self.moe_rb = d("moe_rb", [DEPTH, 36], F32)
        self.moe_wgu = d("moe_w_gate_up", [DEPTH * 32 * 128, 8 * D], F32)
        self.moe_wd = d("moe_w_down", [DEPTH * 32 * 128, 4 * D], F32)
        self.c_identb = d("c_identb", [128, 128], BF16)
        self.c_identf = d("c_identf", [128, 128], F32)
        self.c_trif = d("c_trif", [2, 128, 128], F32)
        self.c_ones = d("c_ones", [128, 128], F32)
        self.c_slow = d("c_slow", [128, 128], BF16)
        self.c_rope = d("c_rope", [2, 128, T], F32)
        self.c_iota = d("c_iota", [128, 512], F32)
        self.c_pidx = d("c_pidx", [128, 12], F32)
        self.yout = self.nc.dram_tensor("yout", [4096, D], F32, kind="ExternalOutput").ap()
        s = self.dscr
        self.xA = s("xA", [T, D], F32)
        self.xB = s("xB", [T, D], F32)
        self.modrow = s("modrow", [2, 6 * D], F32)
        self.QT = s("QT", [4, 2, 128, T], BF16)
        self.KT = s("KT", [4, 2, 128, T], BF16)
        self.Ktm = s("Ktm", [T, 1024], BF16)
        self.Vtm = s("Vtm", [T, 2048], BF16)
        self.gate = s("gate", [T, 2048], F32)
        self.latm = s("latm", [T, 64], F32)
        self.dttm = s("dttm", [T, 64], F32)
        self.yf = s("yf", [T, 2048], F32)
        self.tok = s("tok", [T, D], F32)
        self.xbuf = s("xbuf", [NB * 128, D], BF16)
        self.ybuf = s("ybuf", [NB * 128, D], F32)
        self.dbg = {}

    def phase_mod(self, li):
        fw = self.fw
        with fw.scope():
            cv = fw.sb("cv", [128, 8, 2], F32)
            sv = fw.sb("sv", [128, 8, 2], F32)
            mb = fw.sb("mb", [2, 6 * D], F32)
            mr = fw.sb("mr", [2, 6 * D], F32)
            wst = fw.sb("wst", [128, 8, 512], F32)
            pm = fw.ps("pm", [128, 512], F32)
            self.ld(cv, cv[:], self.cvec)
            self.A(lambda e: e.activation(out=sv[:], in_=cv[:], func=AF.Silu), [cv], [sv])
            self.ld(mb, mb[0:1, :], self.mod_b[li:li + 1, :])
            self.ld(mb, mb[1:2, :], self.mod_b[li:li + 1, :])
            wv = self.mod_w[li].rearrange("(k p) n -> p k n", p=128)
            for n in range(12):
                self.ld(wst, wst[:], wv[:, :, n * 512:(n + 1) * 512])
                for k in range(8):
                    self.P(lambda e, k=k: e.matmul(pm[0:2, :], lhsT=sv[:, k, :], rhs=wst[:, k, :],
                                                   start=(k == 0), stop=(k == 7)), [sv, wst], [pm])
                self.V(lambda e, n=n: e.tensor_tensor(out=mr[0:2, n * 512:(n + 1) * 512], in0=pm[0:2, :],
                                                      in1=mb[0:2, n * 512:(n + 1) * 512], op=ALU.add),
                       [pm, mb], [mr])
            self.st(self.modrow, mr, mr[0:2, :])

    def make_uT(self, xcur, uT, identb, ptr, sc1, sh1, xt, u32, ub, c):
        v = 1 if c < NCTX else 0
        self.ld(xt, xt[:], xcur[c * 128:(c + 1) * 128, :])
        self.V(lambda e: e.tensor_tensor(out=u32[:], in0=xt[:], in1=sc1[v][:], op=ALU.mult), [xt, sc1[v]], [u32])
        self.G(lambda e: e.tensor_tensor(out=ub[:], in0=u32[:], in1=sh1[v][:], op=ALU.add), [u32, sh1[v]], [ub])
        for k in range(8):
            self.P(lambda e, k=k: e.transpose(ptr[:, k, :], ub[:, k * 128:(k + 1) * 128], identb[:]),
                   [ub, identb], [ptr])

    def load_mod_rows(self, lo, names):
        out = {}
        for nm, idx in names:
            tl = []
            for v in range(2):
                t = self.fw.sb("row_%s%d" % (nm, v), [128, D], F32)
                self.ld_row(t, t[:], self.modrow[v:v + 1, idx * D:(idx + 1) * D])
                tl.append(t)
            out[nm] = tl
        return out

    def phase_in_ssd(self, li, j, xcur):
        fw = self.fw
        with fw.scope():
            identb = fw.sb("identb", [128, 128], BF16)
            self.ld(identb, identb[:], self.c_identb)
            rows = self.load_mod_rows(0, [("sh1", 0), ("sc1", 1)])
            sh1, sc1 = rows["sh1"], rows["sc1"]
            for v in range(2):
                self.V(lambda e, v=v: e.tensor_scalar_add(out=sc1[v][:], in0=sc1[v][:], scalar1=1.0), [sc1[v]], [sc1[v]])
            uT = fw.sb("uT", [128, 8, T], BF16)
            xt = fw.sb("xt", [128, D], F32)
            u32 = fw.sb("u32", [128, D], F32)
            ub = fw.sb("ub", [128, D], BF16)
            ptr = fw.ps("ptr", [128, 8, 128], BF16)
            for c in range(NCH):
                self.make_uT(xcur, uT, identb, ptr, sc1, sh1, xt, u32, ub, c)
                self.A(lambda e, c=c: e.copy(out=uT[:, :, c * 128:(c + 1) * 128], in_=ptr[:]), [ptr], [uT])
            cw = fw.sb("cw", [128, 24, 5], F32)
            cb = fw.sb("cb", [128, 24], F32)
            self.ld(cw, cw[:], self.convw[j])
            self.ld(cb, cb[:], self.convb[j])
            wst_ = [fw.sb("wstA%d" % i, [128, 8, 128], F32) for i in range(2)]
            wb_ = [fw.sb("wbA%d" % i, [128, 8, 128], BF16) for i in range(2)]
            raw_ = [fw.sb("raw%d" % i, [128, T], F32) for i in range(2)]
            o_single = fw.sb("o", [128, T], F32)
            o_ = [o_single, o_single]
            ob_ = [fw.sb("ob%d" % i, [128, T], BF16) for i in range(2)]
            pp = [fw.ps("pp%d" % i, [128, 512], F32) for i in range(2)]
            trs_ = [fw.sb("trs%d" % i, [128, 8, 128], BF16) for i in range(2)]
            wv = self.ssd_in_w[j].rearrange("(k p) n -> p k n", p=128)
            segs = [(0, 256)] + [(256 + i * 512, 256 + (i + 1) * 512) for i in range(8)]
            seqs = [(0, 256), (256, T)]
            def stepA(f):
                wst, wb, raw, o, ob = wst_[f % 2], wb_[f % 2], raw_[f % 2], o_[f % 2], ob_[f % 2]
                col0 = 2048 + f * 128
                self.ld(wst, wst[:], wv[:, :, col0:col0 + 128])
                self.G(lambda e, wb=wb, wst=wst: e.tensor_copy(out=wb[:], in_=wst[:]), [wst], [wb])
                for si, (a, b) in enumerate(segs):
                    p = pp[si % 2]
                    for k in range(8):
                        self.P(lambda e, k=k, a=a, b=b, p=p, wb=wb: e.matmul(p[:, 0:b - a], lhsT=wb[:, k, :], rhs=uT[:, k, a:b],
                                                                       start=(k == 0), stop=(k == 7)), [wb, uT], [p])
                    self.A(lambda e, a=a, b=b, p=p, raw=raw: e.copy(out=raw[:, a:b], in_=p[:, 0:b - a]), [p], [raw])

            def stepB(f):
                wst, wb, raw, o, ob = wst_[f % 2], wb_[f % 2], raw_[f % 2], o_[f % 2], ob_[f % 2]
                self.A(lambda e, f=f, o=o, raw=raw: e.activation(out=o[:], in_=raw[:], func=AF.Identity,
                                                   bias=cb[:, f:f + 1], scale=cw[:, f, 2:3]), [raw, cw, cb], [o])
                for (a, b) in seqs:
                    for kk, off in ((0, -2), (1, -1), (3, 1), (4, 2)):
                        if off < 0:
                            osl = (a - off, b)
                            isl = (a, b + off)
                        else:
                            osl = (a, b - off)
                            isl = (a + off, b)
                        self.V(lambda e, f=f, kk=kk, osl=osl, isl=isl, o=o, raw=raw: e.scalar_tensor_tensor(
                            out=o[:, osl[0]:osl[1]], in0=raw[:, isl[0]:isl[1]], scalar=cw[:, f, kk:kk + 1],
                            in1=o[:, osl[0]:osl[1]], op0=ALU.mult, op1=ALU.add), [raw, cw, o], [o])
                self.A(lambda e, o=o, ob=ob: e.activation(out=ob[:], in_=o[:], func=AF.Silu), [o], [ob])
                if f >= 16:
                    g = (f - 16) % 4
                    dst = self.KT if f < 20 else self.QT
                    self.st(dst[g, 0], ob, ob[:])
                if f < 20:
                    dstm = self.Vtm if f < 16 else self.Ktm
                    fc = f if f < 16 else f - 16
                    dv = dstm.rearrange("(c p) f -> p c f", p=128)
                    for c0 in range(0, NCH, 8):
                        trs = trs_[(c0 // 8) % 2]
                        n = min(8, NCH - c0)
                        for cc in range(n):
                            self.P(lambda e, cc=cc, c0=c0, ob=ob: e.transpose(ptr[:, cc, :], ob[:, (c0 + cc) * 128:(c0 + cc + 1) * 128],
                                                                       identb[:]), [ob, identb], [ptr])
                        self.V(lambda e, n=n, trs=trs: e.tensor_copy(out=trs[:, 0:n, :], in_=ptr[:, 0:n, :]), [ptr], [trs])
                        self.st(dv[:, c0:c0 + n, fc * 128:(fc + 1) * 128], trs, trs[:, 0:n, :])

            stepA(0)
            for f in range(24):
                if f + 1 < 24:
                    stepA(f + 1)
                stepB(f)

            wst2 = fw.sb("wst2", [128, 8, 512], F32)
            wb2 = fw.sb("wb2", [128, 8, 512], BF16)
            zt = fw.sb("zt", [128, 512], F32)
            for n in range(4):
                self.ld(wst2, wst2[:], wv[:, :, n * 512:(n + 1) * 512])
                self.G(lambda e: e.tensor_copy(out=wb2[:], in_=wst2[:]), [wst2], [wb2])
                for c in range(NCH):
                    p = pp[c % 2]
                    for k in range(8):
                        self.P(lambda e, k=k, c=c, p=p: e.matmul(p[:], lhsT=uT[:, k, c * 128:(c + 1) * 128], rhs=wb2[:, k, :],
                                                                 start=(k == 0), stop=(k == 7)), [uT, wb2], [p])
                    self.A(lambda e, p=p: e.copy(out=zt[:], in_=p[:]), [p], [zt])
                    self.st(self.gate[c * 128:(c + 1) * 128, n * 512:(n + 1) * 512], zt, zt[:])
            dtb = fw.sb("dtb", [128, 64], F32)
            nega = fw.sb("nega", [128, 64], F32)
            self.ld_row(dtb, dtb[:], self.ssd_dt_bias[j:j + 1, :])
            self.ld_row(nega, nega[:], self.ssd_a_log[j:j + 1, :])
            self.A(lambda e: e.activation(out=nega[:], in_=nega[:], func=AF.Exp), [nega], [nega])
            self.V(lambda e: e.tensor_scalar_mul(out=nega[:], in0=nega[:], scalar1=-1.0), [nega], [nega])
            self.ld(wst2, wst2[:, :, 0:64], wv[:, :, 5120:5184])
            self.G(lambda e: e.tensor_copy(out=wb2[:, :, 0:64], in_=wst2[:, :, 0:64]), [wst2], [wb2])
            d0 = fw.sb("d0", [128, 64], F32)
            d1 = fw.sb("d1", [128, 64], F32)
            d2 = fw.sb("d2", [128, 64], F32)
            for c in range(NCH):
                p = pp[c % 2]
                for k in range(8):
                    self.P(lambda e, k=k, c=c, p=p: e.matmul(p[:, 0:64], lhsT=uT[:, k, c * 128:(c + 1) * 128], rhs=wb2[:, k, 0:64],
                                                             start=(k == 0), stop=(k == 7)), [uT, wb2], [p])
                self.V(lambda e, p=p: e.tensor_tensor(out=d0[:], in0=p[:, 0:64], in1=dtb[:], op=ALU.add), [p, dtb], [d0])
                self.V(lambda e: e.tensor_scalar_mul(out=d1[:], in0=d0[:], scalar1=-1.0), [d0], [d1])
                self.V(lambda e: e.tensor_tensor(out=d1[:], in0=d1[:], in1=d0[:], op=ALU.max), [d0, d1], [d1])
                self.A(lambda e: e.activation(out=d1[:], in_=d1[:], func=AF.Exp, scale=-1.0), [d1], [d1])
                self.A(lambda e: e.activation(out=d1[:], in_=d1[:], func=AF.Ln, bias=1.0), [d1], [d1])
                self.V(lambda e: e.scalar_tensor_tensor(out=d2[:], in0=d0[:], scalar=0.0, in1=d1[:], op0=ALU.max, op1=ALU.add),
                       [d0, d1], [d2])
                self.st(self.dttm[c * 128:(c + 1) * 128, :], d2, d2[:])
                self.V(lambda e: e.tensor_tensor(out=d0[:], in0=d2[:], in1=nega[:], op=ALU.mult), [d2, nega], [d0])
                self.st(self.latm[c * 128:(c + 1) * 128, :], d0, d0[:])

    def phase_in_ret(self, li, j, xcur):
        fw = self.fw
        with fw.scope():
            identb = fw.sb("identb", [128, 128], BF16)
            self.ld(identb, identb[:], self.c_identb)
            rows = self.load_mod_rows(0, [("sh1", 0), ("sc1", 1)])
            sh1, sc1 = rows["sh1"], rows["sc1"]
            for v in range(2):
                self.V(lambda e, v=v: e.tensor_scalar_add(out=sc1[v][:], in0=sc1[v][:], scalar1=1.0), [sc1[v]], [sc1[v]])
            W = fw.sb("Wret", [128, 8, 6144], BF16)
            wst = fw.sb("wstR", [128, 8, 512], F32)
            wv = self.ret_in_w[j].rearrange("(k p) n -> p k n", p=128)
            for n in range(12):
                self.ld(wst, wst[:], wv[:, :, n * 512:(n + 1) * 512])
                eng = self.G if n % 2 == 0 else self.A
                if n % 2 == 0:
                    self.G(lambda e, n=n: e.tensor_copy(out=W[:, :, n * 512:(n + 1) * 512], in_=wst[:]), [wst], [W])
                else:
                    self.A(lambda e, n=n: e.copy(out=W[:, :, n * 512:(n + 1) * 512], in_=wst[:]), [wst], [W])
            uT = fw.sb("uTs", [128, 8, 512], BF16)
            xt = fw.sb("xt", [128, D], F32)
            u32 = fw.sb("u32", [128, D], F32)
            ub = fw.sb("ub", [128, D], BF16)
            ptr = fw.ps("ptr", [128, 8, 128], BF16)
            pp = [fw.ps("pp%d" % i, [128, 512], F32) for i in range(4)]
            cs = fw.sb("cs", [128, 512], F32)
            sn = fw.sb("sn", [128, 512], F32)
            r1_ = [fw.sb("r1%d" % i, [128, 512], F32) for i in range(2)]
            r2_ = [fw.sb("r2%d" % i, [128, 512], F32) for i in range(2)]
            ta_ = [fw.sb("ta%d" % i, [128, 512], F32) for i in range(2)]
            tb_ = [fw.sb("tb%d" % i, [128, 512], F32) for i in range(2)]
            o1_ = [fw.sb("o1%d" % i, [128, 512], BF16) for i in range(2)]
            o2_ = [fw.sb("o2%d" % i, [128, 512], BF16) for i in range(2)]
            trs_ = [fw.sb("trs%d" % i, [128, 8, 128], BF16) for i in range(2)]
            zt = fw.sb("zt", [128, 512], F32)
            vb = fw.sb("vb", [128, 512], BF16)
            segs = [(0, 256)] + [(256 + i * 512, 256 + (i + 1) * 512) for i in range(8)]
            ktv = self.Ktm.rearrange("(c p) f -> p c f", p=128)
            for (a, b) in segs:
                n = b - a
                nt = n // 128
                c0 = a // 128
                for ci in range(nt):
                    self.make_uT(xcur, uT, identb, ptr, sc1, sh1, xt, u32, ub, c0 + ci)
                    self.A(lambda e, ci=ci: e.copy(out=uT[:, :, ci * 128:(ci + 1) * 128], in_=ptr[:]), [ptr], [uT])
                self.ld(cs, cs[:, 0:n], self.c_rope[0, :, a:b])
                self.ld(sn, sn[:, 0:n], self.c_rope[1, :, a:b])
                def qkA(which, h):
                    ii = (which * 4 + h) % 2
                    r1, r2, ta, tb, o1, o2, trs = r1_[ii], r2_[ii], ta_[ii], tb_[ii], o1_[ii], o2_[ii], trs_[ii]
                    base = which * 1024 + h * 256
                    for half, (pt, rr) in enumerate(((pp[ii * 2], r1), (pp[ii * 2 + 1], r2))):
                        cb0 = base + half * 128
                        for k in range(8):
                            self.P(lambda e, k=k, cb0=cb0, pt=pt: e.matmul(pt[:, 0:n], lhsT=W[:, k, cb0:cb0 + 128], rhs=uT[:, k, 0:n],
                                                                           start=(k == 0), stop=(k == 7)), [W, uT], [pt])
                        sc = 1.0 if which == 0 else 0.0625
                        self.A(lambda e, pt=pt, rr=rr, sc=sc: e.activation(out=rr[:, 0:n], in_=pt[:, 0:n], func=AF.Copy, scale=sc),
                               [pt], [rr])

                def qkB(which, h):
                    ii = (which * 4 + h) % 2
                    r1, r2, ta, tb, o1, o2, trs = r1_[ii], r2_[ii], ta_[ii], tb_[ii], o1_[ii], o2_[ii], trs_[ii]
                    base = which * 1024 + h * 256
                    self.V(lambda e, ta=ta, r1=r1: e.tensor_tensor(out=ta[:, 0:n], in0=r1[:, 0:n], in1=cs[:, 0:n], op=ALU.mult), [r1, cs], [ta])
                    self.G(lambda e, tb=tb, r2=r2: e.tensor_tensor(out=tb[:, 0:n], in0=r2[:, 0:n], in1=sn[:, 0:n], op=ALU.mult), [r2, sn], [tb])
                    self.V(lambda e, ta=ta, tb=tb, o1=o1: e.tensor_tensor(out=o1[:, 0:n], in0=ta[:, 0:n], in1=tb[:, 0:n], op=ALU.subtract), [ta, tb], [o1])
                    self.V(lambda e, ta=ta, r1=r1: e.tensor_tensor(out=ta[:, 0:n], in0=r1[:, 0:n], in1=sn[:, 0:n], op=ALU.mult), [r1, sn], [ta])
                    self.G(lambda e, tb=tb, r2=r2: e.tensor_tensor(out=tb[:, 0:n], in0=r2[:, 0:n], in1=cs[:, 0:n], op=ALU.mult), [r2, cs], [tb])
                    self.V(lambda e, ta=ta, tb=tb, o2=o2: e.tensor_tensor(out=o2[:, 0:n], in0=ta[:, 0:n], in1=tb[:, 0:n], op=ALU.add), [ta, tb], [o2])
                    dst = self.QT if which == 0 else self.KT
                    self.st(dst[h, 0, :, a:b], o1, o1[:, 0:n])
                    self.st(dst[h, 1, :, a:b], o2, o2[:, 0:n])
                    if which == 1:
                        for half, oo in enumerate((o1, o2)):
                            for ci in range(nt):
                                self.P(lambda e, ci=ci, oo=oo, half=half: e.transpose(ptr[:, half * 4 + ci, :], oo[:, ci * 128:(ci + 1) * 128],
                                                                                      identb[:]), [oo, identb], [ptr])
                        self.V(lambda e, trs=trs: e.tensor_copy(out=trs[:], in_=ptr[:]), [ptr], [trs])
                        for half in range(2):
                            col = h * 256 + half * 128
                            self.st(ktv[:, c0:c0 + nt, col:col + 128], trs, trs[:, half * 4:half * 4 + nt, :])

                wh = [(w_, h_) for w_ in range(2) for h_ in range(4)]
                qkA(*wh[0])
                for t_ in range(8):
                    if t_ + 1 < 8:
                        qkA(*wh[t_ + 1])
                    qkB(*wh[t_])
                for ci in range(nt):
                    c = c0 + ci
                    for nn in range(8):
                        p = pp[2 + nn % 2]
                        colw = 2048 + nn * 512
                        for k in range(8):
                            self.P(lambda e, k=k, ci=ci, p=p, colw=colw: e.matmul(p[:], lhsT=uT[:, k, ci * 128:(ci + 1) * 128],
                                                                                  rhs=W[:, k, colw:colw + 512], start=(k == 0), stop=(k == 7)),
                                   [uT, W], [p])
                        if nn < 4:
                            self.A(lambda e, p=p: e.copy(out=vb[:], in_=p[:]), [p], [vb])
                            self.st(self.Vtm[c * 128:(c + 1) * 128, nn * 512:(nn + 1) * 512], vb, vb[:])
                        else:
                            self.V(lambda e, p=p: e.tensor_copy(out=zt[:], in_=p[:]), [p], [zt])
                            self.st(self.gate[c * 128:(c + 1) * 128, (nn - 4) * 512:(nn - 3) * 512], zt, zt[:])

    def phase_scan(self, li, j, ssd, xcur, xnext):
        fw = self.fw
        G = 4
        Hg = 8 if ssd else 1
        KC = 1 if ssd else 2
        H = G * Hg
        dv = 2048 // H
        dk = KC * 128
        with fw.scope():
            identb = fw.sb("identb", [128, 128], BF16)
            self.ld(identb, identb[:], self.c_identb)
            ones = fw.sb("ones", [128, 128], F32)
            self.ld(ones, ones[:], self.c_ones)
            tri = fw.sb("tri", [128, 128], F32)
            Wo = fw.sb("Wo", [128, 16, D], BF16)
            wst = fw.sb("wstO", [128, 4, D], F32)
            owv = (self.ssd_out_w if ssd else self.ret_out_w)[j].rearrange("(k p) n -> p k n", p=128)
            for q in range(4):
                self.ld(wst, wst[:], owv[:, q * 4:(q + 1) * 4, :])
                self.G(lambda e, q=q: e.tensor_copy(out=Wo[:, q * 4:(q + 1) * 4, :], in_=wst[:]), [wst], [Wo])
            rows = self.load_mod_rows(0, [("g1", 2), ("sh2", 3), ("sc2", 4)])
            g1, sh2, sc2 = rows["g1"], rows["sh2"], rows["sc2"]
            for v in range(2):
                self.V(lambda e, v=v: e.tensor_scalar_add(out=sc2[v][:], in0=sc2[v][:], scalar1=1.0), [sc2[v]], [sc2[v]])
            lng = fw.sb("lng", [128, D], F32)
            lnb = fw.sb("lnb", [128, D], F32)
            self.ld_row(lng, lng[:], self.ln_mix_g[li:li + 1, :])
            self.ld_row(lnb, lnb[:], self.ln_mix_b[li:li + 1, :])
            nw = fw.sb("nw", [128, 2048], F32)
            self.ld_row(nw, nw[:], (self.ssd_norm_w if ssd else self.ret_gn_w)[j:j + 1, :])
            if ssd:
                dsk = fw.sb("dsk", [128, 32], F32)
                self.ld_row(dsk, dsk[:], self.ssd_d_skip[j:j + 1, :])
            else:
                nb_ = fw.sb("nb", [128, 2048], F32)
                self.ld_row(nb_, nb_[:], self.ret_gn_b[j:j + 1, :])
            S = fw.sb("S", [128, KC, 2048], F32)
            Sb = fw.sb("Sb", [128, KC, 2048], BF16)
            qt = fw.sb("qt", [128, G * KC, 128], BF16)
            kt = fw.sb("kt", [128, G * KC, 128], BF16)
            ktm = fw.sb("ktm", [128, G * dk], BF16)
            vt = fw.sb("vt", [128, 2048], BF16)
            la = fw.sb("la", [128, 64], F32)
            dt = fw.sb("dt", [128, 64], F32)
            acs = fw.sb("acs", [128, 2 * H], F32)
            nacs = fw.sb("nacs", [128, H], F32)
            ea = fw.sb("ea", [128, H], F32)
            wend = fw.sb("wend", [128, H], F32)
            cd = fw.sb("cd", [128, H], F32)
            E = fw.sb("E", [128, H, 128], F32)
            lt = fw.sb("lt", [128, Hg, 128], F32)
            M = fw.sb("M", [128, Hg, 128], BF16)
            cbm = fw.sb("cbm", [128, G, 128], F32)
            xdt = fw.sb("xdt", [128, 2048], BF16) if ssd else vt
            xe = fw.sb("xe", [128, 2048], BF16)
            yc = fw.sb("yc", [128, 2048], F32)
            tmp = fw.sb("tmp", [128, 512], F32)
            yfl = fw.sb("yfl", [128, 2048], F32)
            gt = fw.sb("gt", [128, 2048], F32)
            yb = fw.sb("yb", [128, 2048], BF16)
            yT = fw.sb("yT", [128, 16, 128], BF16)
            st6 = fw.sb("st6", [128, 4, 6], F32)
            mv = fw.sb("mv", [128, 4, 2], F32)
            ss = fw.sb("ss", [128, 4], F32)
            rstd = fw.sb("rstd", [128, 4], F32)
            xt = fw.sb("xt", [128, D], F32)
            t1 = fw.sb("t1", [128, D], F32)
            t2 = fw.sb("t2", [128, D], F32)
            st2 = fw.sb("st2", [128, 2, 6], F32)
            mv2 = fw.sb("mv2", [128, 2], F32)
            rs2 = fw.sb("rs2", [128, 1], F32)
            psm = fw.ps("psm", [128, 512], F32)
            pcb = fw.ps("pcb", [128, 512], F32)
            pbc = [fw.ps("pbc%d" % i, [128, 512], F32) for i in range(2)]
            pA = fw.ps("pA", [128, 1024], F32)
            pst = fw.ps("pst", [128, 512], F32)
            ptr = fw.ps("ptr", [128, 8, 128], BF16)

            qtv = self.QT.rearrange("g k p t -> p g k t")
            ktv = self.KT.rearrange("g k p t -> p g k t")

            def decay_prep(dirn):
                lad = la[:, dirn * 32:dirn * 32 + H] if ssd else la[:, 0:H]
                self.P(lambda e: e.matmul(psm[:, 0:H], lhsT=tri[:], rhs=lad, start=True, stop=True), [tri, la], [psm])
                self.P(lambda e: e.matmul(psm[:, H:2 * H], lhsT=ones[:], rhs=lad, start=True, stop=True), [ones, la], [psm])
                self.V(lambda e: e.tensor_copy(out=acs[:], in_=psm[:, 0:2 * H]), [psm], [acs])
                self.V(lambda e: e.tensor_scalar_mul(out=nacs[:], in0=acs[:, 0:H], scalar1=-1.0), [acs], [nacs])
                self.A(lambda e: e.activation(out=ea[:], in_=acs[:, 0:H], func=AF.Exp), [acs], [ea])
                self.V(lambda e: e.tensor_tensor(out=wend[:], in0=acs[:, H:2 * H], in1=acs[:, 0:H], op=ALU.subtract), [acs], [wend])
                self.A(lambda e: e.activation(out=wend[:], in_=wend[:], func=AF.Exp), [wend], [wend])
                self.A(lambda e: e.activation(out=cd[:], in_=acs[:, H:2 * H], func=AF.Exp), [acs], [cd])
                for g in range(G):
                    hs = slice(g * Hg, (g + 1) * Hg)
                    ladg = la[:, dirn * 32 + g * Hg:dirn * 32 + (g + 1) * Hg] if ssd else la[:, g:g + 1]
                    self.G(lambda e, ladg=ladg: e.tensor_tensor(out=lt[:], in0=bcast(tri[:].unsqueeze(1), [128, Hg, 128]),
                                                                in1=bcast(ladg.unsqueeze(2), [128, Hg, 128]), op=ALU.mult),
                           [tri, la], [lt])
                    for q in range((Hg + 3) // 4):
                        nh = min(4, Hg - q * 4)
                        pb = pbc[q % 2]
                        self.P(lambda e, q=q, nh=nh, pb=pb: e.matmul(pb[:, 0:nh * 128], lhsT=ones[:], rhs=lt[:, q * 4:q * 4 + nh, :],
                                                                     start=True, stop=True), [ones, lt], [pb])
                        for hh in range(nh):
                            h = g * Hg + q * 4 + hh
                            self.A(lambda e, h=h, hh=hh, pb=pb: e.activation(out=E[:, h, :], in_=pb[:, hh * 128:(hh + 1) * 128], func=AF.Exp,
                                                                            bias=nacs[:, h:h + 1], scale=1.0), [pb, nacs], [E])

            for dirn in range(2):
                order = list(range(NCH)) if dirn == 0 else [1, 0] + list(range(NCH - 1, 1, -1))
                self.ld(tri, tri[:], self.c_trif[dirn])
                self.V(lambda e: e.memset(S[:], 0.0), [], [S])
                self.G(lambda e: e.memset(Sb[:], 0.0), [], [Sb])
                if not ssd:
                    self.ld_row(la, la[:, 0:4], self.ret_decay[j, dirn:dirn + 1, :])
                    self.A(lambda e: e.activation(out=la[:, 0:4], in_=la[:, 0:4], func=AF.Exp, scale=-1.0), [la], [la])
                    self.A(lambda e: e.activation(out=la[:, 0:4], in_=la[:, 0:4], func=AF.Ln, bias=1.0), [la], [la])
                    self.V(lambda e: e.tensor_scalar_mul(out=la[:, 0:4], in0=la[:, 0:4], scalar1=-1.0), [la], [la])
                    decay_prep(dirn)
                for c in order:
                    v = 1 if c < NCTX else 0
                    cs_ = slice(c * 128, (c + 1) * 128)
                    self.ld(qt, qt[:].rearrange("p (g k) t -> p g k t", g=G), qtv[:, :, 0:KC, cs_])
                    self.ld(kt, kt[:].rearrange("p (g k) t -> p g k t", g=G), ktv[:, :, 0:KC, cs_])
                    self.ld(ktm, ktm[:], self.Ktm[cs_, 0:G * dk])
                    self.ld(vt, vt[:], self.Vtm[cs_, :])
                    if ssd:
                        self.ld(la, la[:], self.latm[cs_, :])
                        self.ld(dt, dt[:], self.dttm[cs_, :])
                        decay_prep(dirn)
                        self.V(lambda e: e.tensor_tensor(out=xdt[:].rearrange("p (h d) -> p h d", h=H), in0=vt[:].rearrange("p (h d) -> p h d", h=H),
                                                         in1=bcast(dt[:, dirn * 32:dirn * 32 + 32].unsqueeze(2), [128, H, dv]), op=ALU.mult),
                               [vt, dt], [xdt])
                    self.G(lambda e: e.tensor_tensor(out=xe[:].rearrange("p (h d) -> p h d", h=H), in0=xdt[:].rearrange("p (h d) -> p h d", h=H),
                                                     in1=bcast(wend[:].unsqueeze(2), [128, H, dv]), op=ALU.mult), [xdt, wend], [xe])
                    for g in range(G):
                        for kc in range(KC):
                            self.P(lambda e, g=g, kc=kc: e.matmul(pcb[:, g * 128:(g + 1) * 128], lhsT=kt[:, g * KC + kc, :], rhs=qt[:, g * KC + kc, :],
                                                                  start=(kc == 0), stop=(kc == KC - 1)), [kt, qt], [pcb])
                    self.V(lambda e: e.tensor_tensor(out=cbm[:], in0=pcb[:].rearrange("p (g l) -> p g l", g=G),
                                                     in1=bcast(tri[:].unsqueeze(1), [128, G, 128]), op=ALU.mult), [pcb, tri], [cbm])
                    for g in range(G):
                        gs = slice(g * 512, (g + 1) * 512)
                        self.V(lambda e, g=g: e.scalar_tensor_tensor(out=M[:], in0=E[:, g * Hg:(g + 1) * Hg, :], scalar=1.0,
                                                                     in1=bcast(cbm[:, g:g + 1, :], [128, Hg, 128]), op0=ALU.min, op1=ALU.mult),
                               [E, cbm], [M])
                        for hh in range(Hg):
                            h = g * Hg + hh
                            self.P(lambda e, h=h, hh=hh: e.matmul(pA[:, h * dv:(h + 1) * dv] if False else pA[:, (h * dv) % 512:(h * dv) % 512 + dv],
                                                                  lhsT=M[:, hh, :], rhs=xdt[:, h * dv:(h + 1) * dv], start=True, stop=True),
                                   [M, xdt], [pA])
                        for kc in range(KC):
                            self.P(lambda e, g=g, kc=kc, gs=gs: e.matmul(pA[:, 512:1024], lhsT=qt[:, g * KC + kc, :], rhs=Sb[:, kc, gs],
                                                                         start=(kc == 0), stop=(kc == KC - 1)), [qt, Sb], [pA])
                        self.V(lambda e, g=g: e.tensor_tensor(out=tmp[:].rearrange("p (h d) -> p h d", h=Hg),
                                                              in0=pA[:, 512:1024].rearrange("p (h d) -> p h d", h=Hg),
                                                              in1=bcast(ea[:, g * Hg:(g + 1) * Hg].unsqueeze(2), [128, Hg, dv]), op=ALU.mult),
                               [pA, ea], [tmp])
                        self.V(lambda e, gs=gs: e.tensor_tensor(out=yc[:, gs], in0=tmp[:], in1=pA[:, 0:512], op=ALU.add), [tmp, pA], [yc])
                        for kc in range(KC):
                            self.P(lambda e, g=g, kc=kc, gs=gs: e.matmul(pst[:], lhsT=ktm[:, g * dk + kc * 128:g * dk + (kc + 1) * 128], rhs=xe[:, gs],
                                                                         start=True, stop=True), [ktm, xe], [pst])
                            self.V(lambda e, g=g, kc=kc, gs=gs: e.tensor_tensor(out=S[:, kc, gs].rearrange("p (h d) -> p h d", h=Hg),
                                                                                in0=S[:, kc, gs].rearrange("p (h d) -> p h d", h=Hg),
                                                                                in1=bcast(cd[:, g * Hg:(g + 1) * Hg].unsqueeze(2), [128, Hg, dv]),
                                                                                op=ALU.mult), [S, cd], [S])
                            self.V(lambda e, kc=kc, gs=gs: e.tensor_tensor(out=S[:, kc, gs], in0=S[:, kc, gs], in1=pst[:], op=ALU.add), [S, pst], [S])
                            self.A(lambda e, kc=kc, gs=gs: e.copy(out=Sb[:, kc, gs], in_=S[:, kc, gs]), [S], [Sb])
                    if dirn == 0:
                        self.st(self.yf[cs_, :], yc, yc[:])
                        continue
                    self.ld(yfl, yfl[:], self.yf[cs_, :])
                    self.ld(gt, gt[:], self.gate[cs_, :])
                    self.V(lambda e: e.tensor_tensor(out=yc[:], in0=yc[:], in1=yfl[:], op=ALU.add), [yc, yfl], [yc])
                    if ssd:
                        self.G(lambda e: e.tensor_tensor(out=yfl[:].rearrange("p (h d) -> p h d", h=H), in0=vt[:].rearrange("p (h d) -> p h d", h=H),
                                                         in1=bcast(dsk[:].unsqueeze(2), [128, H, dv]), op=ALU.mult), [vt, dsk], [yfl])
                        self.V(lambda e: e.tensor_tensor(out=yc[:], in0=yc[:], in1=yfl[:], op=ALU.add), [yc, yfl], [yc])
                        self.A(lambda e: e.activation(out=gt[:], in_=gt[:], func=AF.Silu), [gt], [gt])
                        self.V(lambda e: e.tensor_tensor(out=yc[:], in0=yc[:], in1=gt[:], op=ALU.mult), [yc, gt], [yc])
                        for g in range(4):
                            self.V(lambda e, g=g: e.bn_stats(out=st6[:, g, :], in_=yc[:, g * 512:(g + 1) * 512]), [yc], [st6])
                            self.V(lambda e, g=g: e.bn_aggr(out=mv[:, g, :], in_=st6[:, g, :]), [st6], [mv])
                        self.V(lambda e: e.tensor_tensor(out=ss[:], in0=mv[:, :, 0], in1=mv[:, :, 0], op=ALU.mult), [mv], [ss])
                        self.V(lambda e: e.tensor_tensor(out=ss[:], in0=ss[:], in1=mv[:, :, 1], op=ALU.add), [ss, mv], [ss])
                        self.A(lambda e: e.activation(out=ss[:], in_=ss[:], func=AF.Sqrt, bias=EPS), [ss], [ss])
                        self.V(lambda e: e.reciprocal(out=rstd[:], in_=ss[:]), [ss], [rstd])
                        for g in range(4):
                            gs = slice(g * 512, (g + 1) * 512)
                            self.V(lambda e, g=g, gs=gs: e.scalar_tensor_tensor(out=yb[:, gs], in0=yc[:, gs], scalar=rstd[:, g:g + 1], in1=nw[:, gs],
                                                                                op0=ALU.mult, op1=ALU.mult), [yc, rstd, nw], [yb])
                    else:
                        for g in range(4):
                            self.V(lambda e, g=g: e.bn_stats(out=st6[:, g, :], in_=yc[:, g * 512:(g + 1) * 512]), [yc], [st6])
                            self.V(lambda e, g=g: e.bn_aggr(out=mv[:, g, :], in_=st6[:, g, :]), [st6], [mv])
                        self.A(lambda e: e.activation(out=ss[:], in_=mv[:, :, 1], func=AF.Sqrt, bias=EPS), [mv], [ss])
                        self.V(lambda e: e.reciprocal(out=rstd[:], in_=ss[:]), [ss], [rstd])
                        self.A(lambda e: e.activation(out=gt[:], in_=gt[:], func=AF.Silu), [gt], [gt])
                        for g in range(4):
                            gs = slice(g * 512, (g + 1) * 512)
                            self.V(lambda e, g=g, gs=gs: e.tensor_scalar(out=yc[:, gs], in0=yc[:, gs], scalar1=mv[:, g, 0:1], scalar2=rstd[:, g:g + 1],
                                                                         op0=ALU.subtract, op1=ALU.mult), [yc, mv, rstd], [yc])
                        self.G(lambda e: e.tensor_tensor(out=yc[:], in0=yc[:], in1=nw[:], op=ALU.mult), [yc, nw], [yc])
                        self.V(lambda e: e.tensor_tensor(out=yc[:], in0=yc[:], in1=nb_[:], op=ALU.add), [yc, nb_], [yc])
                        self.V(lambda e: e.tensor_tensor(out=yb[:], in0=yc[:], in1=gt[:], op=ALU.mult), [yc, gt], [yb])
                    for half in range(2):
                        for kk in range(8):
                            k = half * 8 + kk
                            self.P(lambda e, k=k, kk=kk: e.transpose(ptr[:, kk, :], yb[:, k * 128:(k + 1) * 128], identb[:]), [yb, identb], [ptr])
                        self.A(lambda e, half=half: e.copy(out=yT[:, half * 8:(half + 1) * 8, :], in_=ptr[:]), [ptr], [yT])
                    for nn in range(2):
                        for k in range(16):
                            self.P(lambda e, k=k, nn=nn: e.matmul(pA[:, nn * 512:(nn + 1) * 512], lhsT=yT[:, k, :], rhs=Wo[:, k, nn * 512:(nn + 1) * 512],
                                                                  start=(k == 0), stop=(k == 15)), [yT, Wo], [pA])
                    self.ld(xt, xt[:], xcur[cs_, :])
                    self.V(lambda e: e.tensor_tensor(out=t1[:], in0=pA[:], in1=g1[v][:], op=ALU.mult), [pA, g1[v]], [t1])
                    self.V(lambda e: e.scalar_tensor_tensor(out=t2[:], in0=xt[:], scalar=ALPHA, in1=t1[:], op0=ALU.mult, op1=ALU.add), [xt, t1], [t2])
                    self.layer_norm(t2, t1, st2, mv2, rs2, lng, lnb)
                    self.st(xnext[cs_, :], t1, t1[:])
                    self.G(lambda e: e.tensor_tensor(out=t2[:], in0=t1[:], in1=sc2[v][:], op=ALU.mult), [t1, sc2[v]], [t2])
                    self.V(lambda e: e.tensor_tensor(out=t2[:], in0=t2[:], in1=sh2[v][:], op=ALU.add), [t2, sh2[v]], [t2])
                    self.st(self.tok[cs_, :], t2, t2[:])
                if dirn == 0:
                    fw.barrier()

    def layer_norm(self, xin_t, out_t, st2, mv2, rs2, lng, lnb):
        for hf in range(2):
            self.V(lambda e, hf=hf: e.bn_stats(out=st2[:, hf, :], in_=xin_t[:, hf * 512:(hf + 1) * 512]), [xin_t], [st2])
        self.V(lambda e: e.bn_aggr(out=mv2[:], in_=st2[:].rearrange("p a b -> p (a b)")), [st2], [mv2])
        self.A(lambda e: e.activation(out=rs2[:], in_=mv2[:, 1:2], func=AF.Sqrt, bias=EPS), [mv2], [rs2])
        self.V(lambda e: e.reciprocal(out=rs2[:], in_=rs2[:]), [rs2], [rs2])
        self.V(lambda e: e.tensor_scalar(out=out_t[:], in0=xin_t[:], scalar1=mv2[:, 0:1], scalar2=rs2[:, 0:1],
                                         op0=ALU.subtract, op1=ALU.mult), [xin_t, mv2, rs2], [out_t])
        self.G(lambda e: e.tensor_tensor(out=out_t[:], in0=out_t[:], in1=lng[:], op=ALU.mult), [out_t, lng], [out_t])
        self.V(lambda e: e.tensor_tensor(out=out_t[:], in0=out_t[:], in1=lnb[:], op=ALU.add), [out_t, lnb], [out_t])


def host_consts():
    bf = ml_dtypes.bfloat16
    c = {}
    c["c_identb"] = np.eye(128, dtype=np.float32).astype(bf)
    c["c_identf"] = np.eye(128, dtype=np.float32)
    s = np.arange(128)
    c["c_trif"] = np.stack([(s[:, None] <= s[None, :]), (s[:, None] >= s[None, :])]).astype(np.float32)
    c["c_ones"] = np.ones((128, 128), np.float32)
    c["c_slow"] = (s[:, None] < s[None, :]).astype(np.float32).astype(bf)
    n_freq = 64
    inv_freq = (10000.0 ** (-np.arange(n_freq, dtype=np.float32) / np.float32(n_freq))).astype(np.float32)
    pos = np.arange(4096)
    rows = (pos // 64).astype(np.float32)
    cols = (pos % 64).astype(np.float32)
    ang = np.concatenate([rows[:, None] * inv_freq[None, :], cols[:, None] * inv_freq[None, :]], -1).astype(np.float32)
    cos = np.ones((T, 128), np.float32)
    sin = np.zeros((T, 128), np.float32)
    cos[256:] = np.cos(ang)
    sin[256:] = np.sin(ang)
    c["c_rope"] = np.ascontiguousarray(np.stack([cos.T, sin.T])).astype(np.float32)
    c["c_iota"] = np.broadcast_to(np.arange(512, dtype=np.float32)[None, :], (128, 512)).copy()
    c["c_pidx"] = (np.arange(12)[None, :] * 128 + np.arange(128)[:, None]).astype(np.float32)
    return c


def host_inputs(inp, b):
    f = np.float32
    m = {}
    m["xin"] = np.ascontiguousarray(np.concatenate([inp["ctx"][b], inp["x"][b]], 0)).astype(f)
    cv = np.stack([inp["c"][b].reshape(8, 128).T, inp["c_ctx"].reshape(8, 128).T], -1)
    m["cvec"] = np.ascontiguousarray(cv).astype(f)
    m["mod_w"] = inp["mod_w"]
    m["mod_b"] = inp["mod_b"]
    m["ssd_in_w"] = inp["ssd_in_w"]
    cw = inp["ssd_conv_w"]
    m["convw"] = np.ascontiguousarray(cw.reshape(2, 5, 24, 128).transpose(0, 3, 2, 1)).astype(f)
    m["convb"] = np.ascontiguousarray(inp["ssd_conv_b"].reshape(2, 24, 128).transpose(0, 2, 1)).astype(f)
    m["ssd_dt_bias"] = np.ascontiguousarray(inp["ssd_dt_bias"].reshape(2, 64))
    m["ssd_a_log"] = np.ascontiguousarray(inp["ssd_a_log"].reshape(2, 64))
    m["ssd_d_skip"] = inp["ssd_d_skip"]
    m["ssd_norm_w"] = inp["ssd_norm_w"]
    m["ssd_out_w"] = inp["ssd_out_w"]
    m["ret_in_w"] = inp["ret_in_w"]
    m["ret_decay_logit"] = inp["ret_decay_logit"]
    m["ret_gn_w"] = inp["ret_gn_w"]
    m["ret_gn_b"] = inp["ret_gn_b"]
    m["ret_out_w"] = inp["ret_out_w"]
    for k in ("ln_mix_g", "ln_mix_b", "ln_ffn_g", "ln_ffn_b"):
        m[k] = inp[k]
    m["moe_rw"] = np.ascontiguousarray(np.concatenate([inp["moe_group_w"], inp["moe_expert_w"]], -1)).astype(f)
    m["moe_rb"] = np.ascontiguousarray(np.concatenate([inp["moe_group_b"], inp["moe_expert_b"]], -1)).astype(f)
    m["moe_w_gate_up"] = inp["moe_w_gate_up"].reshape(DEPTH * 32 * 128, 8 * D)
    m["moe_w_down"] = inp["moe_w_down"].reshape(DEPTH * 32 * 128, 4 * D)
    return m


def _phase_moe(self, li, xcur, xnext, final):
    fw = self.fw
    NT = NCH
    with fw.scope():
        DI = fw.sb("DI", [128, 2 * NT], I32)
        Wt = fw.sb("Wt", [128, 2 * NT], F32)
        IGU = fw.sb("IGU", [128, NB], I32)
        with fw.scope():
            identf = fw.sb("identf", [128, 128], F32)
            self.ld(identf, identf[:], self.c_identf)
            slow = fw.sb("slow", [128, 128], BF16)
            self.ld(slow, slow[:], self.c_slow)
            onesf = fw.sb("onesf", [128, 128], F32)
            self.ld(onesf, onesf[:], self.c_ones)
            onesb = fw.sb("onesb", [128, 128], BF16)
            self.V(lambda e: e.tensor_copy(out=onesb[:], in_=onesf[:]), [onesf], [onesb])
            iota = fw.sb("iota", [128, 512], F32)
            self.ld(iota, iota[:], self.c_iota)
            pidx = fw.sb("pidx", [128, 12], F32)
            self.ld(pidx, pidx[:], self.c_pidx)
            rw = fw.sb("rw", [128, 8, 36], F32)
            self.ld(rw, rw[:], self.moe_rw[li].rearrange("(k p) e -> p k e", p=128))
            rb = fw.sb("rb", [128, 36], F32)
            self.ld_row(rb, rb[:], self.moe_rb[li:li + 1, :])
            OH = fw.sb("OH", [128, 2 * NT, 32], F32)
            SL = fw.sb("SL", [128, 2 * NT], F32)
            Acum = fw.sb("Acum", [128, 32], F32)
            Acb = fw.sb("Acb", [128, 32], BF16)
            self.V(lambda e: e.memset(Acum[:], 0.0), [], [Acum])
            self.V(lambda e: e.memset(Acb[:], 0.0), [], [Acb])
            tk = fw.sb("tk", [128, D], F32)
            tkT = fw.sb("tkT", [128, 8, 128], F32)
            L = fw.sb("L", [128, 36], F32)
            gmax = fw.sb("gmax", [128, 1], F32)
            ngmax = fw.sb("ngmax", [128, 1], F32)
            goh = fw.sb("goh", [128, 4], F32)
            pen = fw.sb("pen", [128, 4], F32)
            ex = fw.sb("ex", [128, 4], F32)
            gs = fw.sb("gs", [128, 1], F32)
            em = fw.sb("em", [128, 32], F32)
            em2 = fw.sb("em2", [128, 32], F32)
            m1 = fw.sb("m1", [128, 1], F32)
            m2 = fw.sb("m2", [128, 1], F32)
            dd = fw.sb("dd", [128, 1], F32)
            den = fw.sb("den", [128, 1], F32)
            A_ = fw.sb("A_", [128, 32], F32)
            Ab = fw.sb("Ab", [128, 32], BF16)
            Pp = fw.sb("Pp", [128, 32], F32)
            junk = fw.sb("junk", [128, 32], F32)
            pT = fw.ps("pT", [128, 8, 128], F32)
            plog = fw.ps("plog", [128, 512], F32)
            pP = fw.ps("pP", [128, 512], F32)
            for c in range(NT):
                cs_ = slice(c * 128, (c + 1) * 128)
                self.ld(tk, tk[:], self.tok[cs_, :])
                for k in range(8):
                    self.P(lambda e, k=k: e.transpose(pT[:, k, :], tk[:, k * 128:(k + 1) * 128], identf[:]), [tk, identf], [pT])
                self.A(lambda e: e.copy(out=tkT[:], in_=pT[:]), [pT], [tkT])
                for k in range(8):
                    self.P(lambda e, k=k: e.matmul(plog[:, 0:36], lhsT=tkT[:, k, :], rhs=rw[:, k, :], start=(k == 0), stop=(k == 7)),
                           [tkT, rw], [plog])
                self.V(lambda e: e.tensor_tensor(out=L[:], in0=plog[:, 0:36], in1=rb[:], op=ALU.add), [plog, rb], [L])
                self.V(lambda e: e.reduce_max(out=gmax[:], in_=L[:, 0:4], axis=AX.X), [L], [gmax])
                self.V(lambda e: e.tensor_scalar(out=goh[:], in0=L[:, 0:4], scalar1=gmax[:, 0:1], scalar2=None, op0=ALU.is_equal), [L, gmax], [goh])
                self.V(lambda e: e.tensor_scalar_mul(out=ngmax[:], in0=gmax[:], scalar1=-1.0), [gmax], [ngmax])
                self.A(lambda e: e.activation(out=ex[:], in_=L[:, 0:4], func=AF.Exp, bias=ngmax[:, 0:1], scale=1.0), [L, ngmax], [ex])
                self.V(lambda e: e.reduce_sum(out=gs[:], in_=ex[:], axis=AX.X), [ex], [gs])
                self.V(lambda e: e.reciprocal(out=gs[:], in_=gs[:]), [gs], [gs])
                self.V(lambda e: e.tensor_scalar(out=pen[:], in0=goh[:], scalar1=1e30, scalar2=-1e30, op0=ALU.mult, op1=ALU.add), [goh], [pen])
                self.V(lambda e: e.tensor_tensor(out=em[:].rearrange("p (g j) -> p g j", g=4), in0=L[:, 4:36].rearrange("p (g j) -> p g j", g=4),
                                                 in1=bcast(pen[:].unsqueeze(2), [128, 4, 8]), op=ALU.add), [L, pen], [em])
                self.V(lambda e: e.reduce_max(out=m1[:], in_=em[:], axis=AX.X), [em], [m1])
                o1 = OH[:, 2 * c, :]
                o2 = OH[:, 2 * c + 1, :]
                self.V(lambda e, o1=o1: e.tensor_scalar(out=o1, in0=em[:], scalar1=m1[:, 0:1], scalar2=None, op0=ALU.is_equal), [em, m1], [OH])
                self.V(lambda e, o1=o1: e.scalar_tensor_tensor(out=em2[:], in0=o1, scalar=-1e30, in1=em[:], op0=ALU.mult, op1=ALU.add), [OH, em], [em2])
                self.V(lambda e: e.reduce_max(out=m2[:], in_=em2[:], axis=AX.X), [em2], [m2])
                self.V(lambda e, o2=o2: e.tensor_scalar(out=o2, in0=em2[:], scalar1=m2[:, 0:1], scalar2=None, op0=ALU.is_equal), [em2, m2], [OH])
                self.V(lambda e: e.tensor_tensor(out=dd[:], in0=m2[:], in1=m1[:], op=ALU.subtract), [m1, m2], [dd])
                self.A(lambda e: e.activation(out=dd[:], in_=dd[:], func=AF.Exp), [dd], [dd])
                self.V(lambda e: e.tensor_scalar_add(out=den[:], in0=dd[:], scalar1=1.0), [dd], [den])
                self.V(lambda e: e.reciprocal(out=den[:], in_=den[:]), [den], [den])
                self.V(lambda e, c=c: e.tensor_tensor(out=Wt[:, 2 * c:2 * c + 1], in0=den[:], in1=gs[:], op=ALU.mult), [den, gs], [Wt])
                self.V(lambda e, c=c: e.tensor_tensor(out=Wt[:, 2 * c + 1:2 * c + 2], in0=Wt[:, 2 * c:2 * c + 1], in1=dd[:], op=ALU.mult), [Wt, dd], [Wt])
                self.V(lambda e, o1=o1, o2=o2: e.tensor_tensor(out=A_[:], in0=o1, in1=o2, op=ALU.add), [OH], [A_])
                self.V(lambda e: e.tensor_copy(out=Ab[:], in_=A_[:]), [A_], [Ab])
                self.P(lambda e: e.matmul(pP[:, 0:32], lhsT=slow[:], rhs=Ab[:], start=True, stop=False), [slow, Ab], [pP])
                self.P(lambda e: e.matmul(pP[:, 0:32], lhsT=onesb[:], rhs=Acb[:], start=False, stop=True), [onesb, Acb], [pP])
                self.V(lambda e: e.tensor_copy(out=Pp[:], in_=pP[:, 0:32]), [pP], [Pp])
                for k, ok in enumerate((o1, o2)):
                    self.V(lambda e, ok=ok: e.tensor_tensor(out=junk[:], in0=ok, in1=Pp[:], op=ALU.mult), [OH, Pp], [junk])
                    self.V(lambda e, c=c, k=k: e.reduce_sum(out=SL[:, 2 * c + k:2 * c + k + 1], in_=junk[:], axis=AX.X), [junk], [SL])
                self.V(lambda e: e.tensor_tensor(out=Acum[:], in0=Acum[:], in1=A_[:], op=ALU.add), [Acum, A_], [Acum])
                self.V(lambda e: e.tensor_copy(out=Acb[:], in_=Acum[:]), [Acum], [Acb])
            cnt = fw.sb("cnt", [128, 32], F32)
            self.P(lambda e: e.matmul(pP[:, 0:32], lhsT=onesb[:], rhs=Acb[:], start=True, stop=True), [onesb, Acb], [pP])
            self.V(lambda e: e.tensor_copy(out=cnt[:], in_=pP[:, 0:32]), [pP], [cnt])
            thr = fw.sb("thr", [128, 34], F32)
            self.V(lambda e: e.tensor_scalar_mul(out=thr[:], in0=iota[:, 0:34], scalar1=128.0), [iota], [thr])
            cmp = fw.sb("cmp", [128, 32, 34], F32)
            self.V(lambda e: e.tensor_tensor(out=cmp[:], in0=bcast(cnt[:].unsqueeze(2), [128, 32, 34]), in1=bcast(thr[:].unsqueeze(1), [128, 32, 34]),
                                             op=ALU.is_gt), [cnt, thr], [cmp])
            nblk = fw.sb("nblk", [128, 32], F32)
            self.V(lambda e: e.reduce_sum(out=nblk[:], in_=cmp[:], axis=AX.X), [cmp], [nblk])
            pa = fw.sb("pa", [128, 32], F32)
            pb_ = fw.sb("pb", [128, 32], F32)
            self.V(lambda e: e.tensor_copy(out=pa[:], in_=nblk[:]), [nblk], [pa])
            cur, oth = pa, pb_
            for sft in (1, 2, 4, 8, 16):
                self.V(lambda e, cur=cur, oth=oth: e.tensor_copy(out=oth[:], in_=cur[:]), [cur], [oth])
                self.V(lambda e, cur=cur, oth=oth, sft=sft: e.tensor_tensor(out=oth[:, sft:32], in0=cur[:, sft:32], in1=cur[:, 0:32 - sft], op=ALU.add),
                       [cur], [oth])
                cur, oth = oth, cur
            pend = cur
            pstart = fw.sb("pstart", [128, 32], F32)
            self.V(lambda e: e.tensor_tensor(out=pstart[:], in0=pend[:], in1=nblk[:], op=ALU.subtract), [pend, nblk], [pstart])
            self.V(lambda e: e.tensor_scalar_mul(out=pstart[:], in0=pstart[:], scalar1=128.0), [pstart], [pstart])
            big = fw.sb("big", [128, 2 * NT, 32], F32)
            self.V(lambda e: e.tensor_tensor(out=big[:], in0=OH[:], in1=bcast(pstart[:].unsqueeze(1), [128, 2 * NT, 32]), op=ALU.mult), [OH, pstart], [big])
            dst = fw.sb("dstf", [128, 2 * NT], F32)
            self.V(lambda e: e.reduce_sum(out=dst[:], in_=big[:], axis=AX.X), [big], [dst])
            self.V(lambda e: e.tensor_tensor(out=dst[:], in0=dst[:], in1=SL[:], op=ALU.add), [dst, SL], [dst])
            self.V(lambda e: e.tensor_copy(out=DI[:], in_=dst[:]), [dst], [DI])
            cmp2 = fw.sb("cmp2", [128, NB, 32], F32)
            self.V(lambda e: e.tensor_tensor(out=cmp2[:], in0=bcast(pend[:].unsqueeze(1), [128, NB, 32]), in1=bcast(iota[:, 0:NB].unsqueeze(2), [128, NB, 32]),
                                             op=ALU.is_le), [pend, iota], [cmp2])
            be = fw.sb("be", [128, NB], F32)
            self.V(lambda e: e.reduce_sum(out=be[:], in_=cmp2[:], axis=AX.X), [cmp2], [be])
            self.V(lambda e: e.tensor_scalar_min(out=be[:], in0=be[:], scalar1=31.0), [be], [be])
            same = fw.sb("same", [128, NB], F32)
            self.V(lambda e: e.memset(same[:], 0.0), [], [same])
            self.V(lambda e: e.tensor_tensor(out=same[:, 2:NB], in0=be[:, 2:NB], in1=be[:, 0:NB - 2], op=ALU.is_equal), [be], [same])
            self.V(lambda e: e.tensor_scalar(out=be[:], in0=be[:], scalar1=128.0, scalar2=float(li * 32 * 128), op0=ALU.mult, op1=ALU.add), [be], [be])
            self.V(lambda e: e.scalar_tensor_tensor(out=be[:], in0=same[:], scalar=1.0e6, in1=be[:], op0=ALU.mult, op1=ALU.add), [same, be], [be])
            self.V(lambda e: e.tensor_tensor(out=be[:], in0=be[:], in1=bcast(pidx[:, 0:1], [128, NB]), op=ALU.add), [be, pidx], [be])
            self.V(lambda e: e.tensor_copy(out=IGU[:], in_=be[:]), [be], [IGU])
        with fw.scope():
            tk = fw.sb("tk", [128, D], F32)
            tkb = fw.sb("tkb", [128, D], BF16)
            for c in range(NT):
                self.ld(tk, tk[:], self.tok[c * 128:(c + 1) * 128, :])
                self.V(lambda e: e.tensor_copy(out=tkb[:], in_=tk[:]), [tk], [tkb])
                for k in range(2):
                    col = 2 * c + k
                    self.fw.dma("gpsimd", lambda e, col=col: e.indirect_dma_start(
                        out=self.xbuf, out_offset=bass.IndirectOffsetOnAxis(ap=DI[:, col:col + 1], axis=0), in_=tkb[:, :], in_offset=None),
                        reads=[tkb, DI])
        with fw.scope():
            identb = fw.sb("identb", [128, 128], BF16)
            self.ld(identb, identb[:], self.c_identb)
            xb = [fw.sb("xb%d" % i, [128, D], BF16) for i in range(2)]
            xT = [fw.sb("xT%d" % i, [128, 8, 128], BF16) for i in range(2)]
            g32 = [fw.sb("g32_%d" % i, [128, 8, D], F32) for i in range(2)]
            d32 = [fw.sb("d32_%d" % i, [128, 4, D], F32) for i in range(2)]
            gbf = [fw.sb("gbf_%d" % i, [128, 8, D], BF16) for i in range(2)]
            dbf = [fw.sb("dbf_%d" % i, [128, 4, D], BF16) for i in range(2)]
            sg = fw.sb("sg", [128, 4, 128], F32)
            hT = fw.sb("hT", [128, 4, 128], BF16)
            ob = fw.sb("ob", [128, D], F32)
            ptr = fw.ps("ptr", [128, 8, 128], BF16)
            pH = [fw.ps("pH%d" % i, [128, 8, 128], F32) for i in range(2)]
            pO = fw.ps("pO", [128, D], F32)
            if getattr(self, "_bcreg", None) is None:
                self._bcreg = self.nc.gpsimd.alloc_register("bcreg")
                self.nc.gpsimd.reg_mov(self._bcreg, DEPTH * 32 * 128 - 1)
            bcreg = self._bcreg
            for b in range(NB):
                i = b % 2
                self.ld(xb[i], xb[i][:], self.xbuf[b * 128:(b + 1) * 128, :])
                idx = IGU[:, b:b + 1]
                self.fw.dma("gpsimd", lambda e, i=i, idx=idx: e.indirect_dma_start(
                    out=g32[i][:].rearrange("p k n -> p (k n)"), out_offset=None, in_=self.moe_wgu,
                    in_offset=bass.IndirectOffsetOnAxis(ap=idx, axis=0), bounds_check=bcreg, oob_is_err=False),
                    reads=[IGU], writes=[g32[i]])
                self.fw.dma("gpsimd", lambda e, i=i, idx=idx: e.indirect_dma_start(
                    out=d32[i][:].rearrange("p k n -> p (k n)"), out_offset=None, in_=self.moe_wd,
                    in_offset=bass.IndirectOffsetOnAxis(ap=idx, axis=0), bounds_check=bcreg, oob_is_err=False),
                    reads=[IGU], writes=[d32[i]])
                xbv = xb[i][:].rearrange("s (p k) -> s k p", k=8)
                for k in range(8):
                    self.P(lambda e, k=k, xbv=xbv: e.transpose(ptr[:, k, :], xbv[:, k, :], identb[:]), [xb[i], identb], [ptr])
                self.V(lambda e, i=i: e.tensor_copy(out=xT[i][:], in_=ptr[:]), [ptr], [xT[i]])
                for q in range(4):
                    sl = slice(q * 2, q * 2 + 2)
                    if q % 2 == 0:
                        self.A(lambda e, i=i, sl=sl: e.copy(out=gbf[i][:, sl, :], in_=g32[i][:, sl, :]), [g32[i]], [gbf[i]])
                    else:
                        self.V(lambda e, i=i, sl=sl: e.tensor_copy(out=gbf[i][:, sl, :], in_=g32[i][:, sl, :]), [g32[i]], [gbf[i]])
                self.A(lambda e, i=i: e.copy(out=dbf[i][:, 0:2, :], in_=d32[i][:, 0:2, :]), [d32[i]], [dbf[i]])
                self.V(lambda e, i=i: e.tensor_copy(out=dbf[i][:, 2:4, :], in_=d32[i][:, 2:4, :]), [d32[i]], [dbf[i]])
                ph = pH[i]
                for m in range(8):
                    tt, kh = m // 4, m % 4
                    for k in range(8):
                        lw = gbf[i][:, k, :].rearrange("d (two p k) -> d two k p", two=2, k=4)[:, tt, kh, :]
                        self.P(lambda e, i=i, m=m, k=k, ph=ph, lw=lw: e.matmul(ph[:, m, :], lhsT=lw, rhs=xT[i][:, k, :],
                                                                               start=(k == 0), stop=(k == 7)), [gbf[i], xT[i]], [ph])
                self.A(lambda e, ph=ph: e.activation(out=sg[:], in_=ph[:, 0:4, :], func=AF.Silu), [ph], [sg])
                self.V(lambda e, ph=ph: e.tensor_tensor(out=hT[:], in0=sg[:], in1=ph[:, 4:8, :], op=ALU.mult), [sg, ph], [hT])
                for nn in range(2):
                    for k in range(4):
                        self.P(lambda e, i=i, nn=nn, k=k: e.matmul(pO[:, nn * 512:(nn + 1) * 512], lhsT=hT[:, k, :], rhs=dbf[i][:, k, nn * 512:(nn + 1) * 512],
                                                                   start=(k == 0), stop=(k == 3)), [hT, dbf[i]], [pO])
                self.A(lambda e: e.copy(out=ob[:], in_=pO[:]), [pO], [ob])
                self.st(self.ybuf[b * 128:(b + 1) * 128, :], ob, ob[:])
        with fw.scope():
            g2 = []
            for v in range(2):
                t = fw.sb("g2_%d" % v, [128, D], F32)
                self.ld_row(t, t[:], self.modrow[v:v + 1, 5 * D:6 * D])
                g2.append(t)
            lng = fw.sb("lng", [128, D], F32)
            lnb = fw.sb("lnb", [128, D], F32)
            self.ld_row(lng, lng[:], self.ln_ffn_g[li:li + 1, :])
            self.ld_row(lnb, lnb[:], self.ln_ffn_b[li:li + 1, :])
            o1_ = [fw.sb("o1%d" % i, [128, D], F32) for i in range(2)]
            o2_ = [fw.sb("o2%d" % i, [128, D], F32) for i in range(2)]
            xt_ = [fw.sb("xt%d" % i, [128, D], F32) for i in range(2)]
            t1_ = [fw.sb("t1%d" % i, [128, D], F32) for i in range(2)]
            st2 = fw.sb("st2", [128, 2, 6], F32)
            mv2 = fw.sb("mv2", [128, 2], F32)
            rs2 = fw.sb("rs2", [128, 1], F32)
            for c in range(NT):
                if final and c < NCTX:
                    continue
                v = 1 if c < NCTX else 0
                o1, o2, xt, t1 = o1_[c % 2], o2_[c % 2], xt_[c % 2], t1_[c % 2]
                cs_ = slice(c * 128, (c + 1) * 128)
                for k, ot in enumerate((o1, o2)):
                    col = 2 * c + k
                    self.fw.dma("gpsimd", lambda e, col=col, ot=ot: e.indirect_dma_start(
                        out=ot[:, :], out_offset=None, in_=self.ybuf, in_offset=bass.IndirectOffsetOnAxis(ap=DI[:, col:col + 1], axis=0)),
                        reads=[DI], writes=[ot])
                self.ld(xt, xt[:], xcur[cs_, :])
                self.V(lambda e, c=c, o1=o1: e.tensor_scalar(out=o1[:], in0=o1[:], scalar1=Wt[:, 2 * c:2 * c + 1], scalar2=None, op0=ALU.mult), [o1, Wt], [o1])
                self.V(lambda e, c=c, o1=o1, o2=o2: e.scalar_tensor_tensor(out=o1[:], in0=o2[:], scalar=Wt[:, 2 * c + 1:2 * c + 2], in1=o1[:], op0=ALU.mult, op1=ALU.add),
                       [o2, Wt, o1], [o1])
                self.G(lambda e, v=v, o1=o1: e.tensor_tensor(out=o1[:], in0=o1[:], in1=g2[v][:], op=ALU.mult), [o1, g2[v]], [o1])
                self.V(lambda e, o1=o1, o2=o2, xt=xt: e.scalar_tensor_tensor(out=o2[:], in0=xt[:], scalar=ALPHA, in1=o1[:], op0=ALU.mult, op1=ALU.add), [xt, o1], [o2])
                self.layer_norm(o2, t1, st2, mv2, rs2, lng, lnb)
                if final:
                    self.st(self.yout[(c - NCTX) * 128:(c - NCTX + 1) * 128, :], t1, t1[:])
                else:
                    self.st(xnext[cs_, :], t1, t1[:])


Builder.phase_moe = _phase_moe


def build_program(nlayers=DEPTH):
    nc = bass.Bass("TRN2", target_bir_lowering=False)
    b = Builder(nc)
    b.declare()
    xcur = b.xin
    for li in range(nlayers):
        j = li // 2
        b.phase_mod(li)
        if li % 2 == 0:
            b.phase_in_ssd(li, j, xcur)
            b.phase_scan(li, j, True, xcur, b.xA)
        else:
            b.phase_in_ret(li, j, xcur)
            b.phase_scan(li, j, False, xcur, b.xA)
        b.phase_moe(li, b.xA, b.xB, final=(li == nlayers - 1))
        xcur = b.xB
    b.fw.barrier()
    b.fw.root.close()
    return nc


def kernel(**inputs):
    inp = {k: np.asarray(v) for k, v in inputs.items()}
    nc = build_program()
    consts = host_consts()
    maps = []
    for c in range(8):
        m = host_inputs(inp, c)
        m.update(consts)
        maps.append(m)
    res = run_bass_kernel_spmd(nc, maps, core_ids=list(range(8)))
    out = np.stack([np.asarray(res.results[c]["yout"]) for c in range(8)], 0)
    return out.astype(np.float32)


def _phase_scan2(self, li, j, ssd, xcur, xnext):
    fw = self.fw
    G = 4
    Hg = 8 if ssd else 1
    KC = 1 if ssd else 2
    H = G * Hg
    dv = 2048 // H
    dk = KC * 128
    with fw.scope():
        identb = fw.sb("identb", [128, 128], BF16)
        self.ld(identb, identb[:], self.c_identb)
        ones = fw.sb("ones", [128, 128], F32)
        self.ld(ones, ones[:], self.c_ones)
        tri = fw.sb("tri", [128, 128], F32)
        Wo = fw.sb("Wo", [128, 16, D], BF16)
        owv = (self.ssd_out_w if ssd else self.ret_out_w)[j].rearrange("(k p) n -> p k n", p=128)
        with fw.scope():
            wst = fw.sb("wstO", [128, 4, D], F32)
            for q in range(4):
                self.ld(wst, wst[:], owv[:, q * 4:(q + 1) * 4, :])
                self.G(lambda e, q=q: e.tensor_copy(out=Wo[:, q * 4:(q + 1) * 4, :], in_=wst[:]), [wst], [Wo])
        g1 = fw.sb("g1", [128, D], F32)
        sh2 = fw.sb("sh2", [128, D], F32)
        sc2 = fw.sb("sc2", [128, D], F32)

        def load_rows(v):
            self.ld_row(g1, g1[:], self.modrow[v:v + 1, 2 * D:3 * D])
            self.ld_row(sh2, sh2[:], self.modrow[v:v + 1, 3 * D:4 * D])
            self.ld_row(sc2, sc2[:], self.modrow[v:v + 1, 4 * D:5 * D])
            self.V(lambda e: e.tensor_scalar_add(out=sc2[:], in0=sc2[:], scalar1=1.0), [sc2], [sc2])

        lng = fw.sb("lng", [128, D], F32)
        lnb = fw.sb("lnb", [128, D], F32)
        self.ld_row(lng, lng[:], self.ln_mix_g[li:li + 1, :])
        self.ld_row(lnb, lnb[:], self.ln_mix_b[li:li + 1, :])
        nw = fw.sb("nw", [128, 2048], F32)
        self.ld_row(nw, nw[:], (self.ssd_norm_w if ssd else self.ret_gn_w)[j:j + 1, :])
        if ssd:
            dsk = fw.sb("dsk", [128, 32], F32)
            self.ld_row(dsk, dsk[:], self.ssd_d_skip[j:j + 1, :])
        else:
            nb_ = fw.sb("nb", [128, 2048], F32)
            self.ld_row(nb_, nb_[:], self.ret_gn_b[j:j + 1, :])
        S = fw.sb("S", [128, KC, 2048], F32)
        Sb = fw.sb("Sb", [128, KC, 2048], BF16)

        def dbl(name, shape, dt):
            return [fw.sb(name + "0", shape, dt), fw.sb(name + "1", shape, dt)]

        def tpl(name, shape, dt):
            return [fw.sb(name + str(i_), shape, dt) for i_ in range(3)]

        qt3 = tpl("qt", [128, G * KC, 128], BF16)
        kt2 = dbl("kt", [128, G * KC, 128], BF16)
        ktm3 = tpl("ktm", [128, G * dk], BF16)
        vt3 = tpl("vt", [128, 2048], BF16)
        xdt2 = dbl("xdt", [128, 2048], BF16) if ssd else None
        xe = dbl("xe", [128, 2048], BF16)
        cbm = dbl("cbm", [128, G, 128], F32)
        la2 = dbl("la", [128, 64], F32)
        dt2 = dbl("dt", [128, 64], F32)
        acs = fw.sb("acs", [128, 2 * H], F32)
        nacs = fw.sb("nacs", [128, H], F32)
        wend = fw.sb("wend", [128, H], F32)
        if ssd:
            ea = dbl("ea", [128, H], F32)
            cd = dbl("cd", [128, H], F32)
            E = [[fw.sb("E%d_%d" % (p_, g), [128, Hg, 128], BF16) for g in range(G)] for p_ in range(2)]
        else:
            ea0 = fw.sb("ea", [128, H], F32)
            cd0 = fw.sb("cd", [128, H], F32)
            ea = [ea0, ea0]
            cd = [cd0, cd0]
            E0 = [fw.sb("E_%d" % g, [128, Hg, 128], F32) for g in range(G)]
            E = [E0, E0]
        lt = dbl("lt", [128, Hg, 128], F32)
        nlb = dbl("nlb", [128, Hg, 128], F32)
        nones = fw.sb("nones", [128, 128], F32)
        self.V(lambda e: e.memset(nones[:], -1.0), [], [nones])
        M = dbl("M", [128, Hg, 128], BF16)
        yc = fw.sb("yc", [128, 2048], F32)
        tmp = dbl("tmp", [128, 512], F32)
        yfl = fw.sb("yfl", [128, 2048], F32)
        gt = fw.sb("gt", [128, 2048], F32)
        yb = fw.sb("yb", [128, 2048], BF16)
        yT = fw.sb("yT", [128, 16, 128], BF16)
        st6 = fw.sb("st6", [128, 4, 6], F32)
        mv = fw.sb("mv", [128, 4, 2], F32)
        ss = fw.sb("ss", [128, 4], F32)
        rstd = fw.sb("rstd", [128, 4], F32)
        xt = fw.sb("xt", [128, D], F32)
        t1 = fw.sb("t1", [128, D], F32)
        t2 = fw.sb("t2", [128, D], F32)
        st2 = fw.sb("st2", [128, 2, 6], F32)
        mv2 = fw.sb("mv2", [128, 2], F32)
        rs2 = fw.sb("rs2", [128, 1], F32)
        psm = fw.ps("psm", [128, 512], F32)
        pcb = fw.ps("pcb", [128, 512], F32)
        pbc = [fw.ps("pbc%d" % i, [128, 512], F32) for i in range(2)]
        pyd = fw.ps("pyd", [128, 512], F32)
        pyo = fw.ps("pyo", [128, 512], F32)
        pst = fw.ps("pst", [128, 512], F32)
        ptr = fw.ps("ptr", [128, 8, 128], BF16)

        qtv = self.QT.rearrange("g k p t -> p g k t")
        ktv = self.KT.rearrange("g k p t -> p g k t")
        r3 = lambda ap, h: ap.rearrange("p (h d) -> p h d", h=h)

        def decay_pre(dirn, i):
            par = i % 2
            la = la2[par]
            lad = la[:, dirn * 32:dirn * 32 + H] if ssd else la[:, 0:H]
            self.P(lambda e: e.matmul(psm[:, 0:H], lhsT=tri[:], rhs=lad, start=True, stop=True), [tri, la], [psm])
            self.P(lambda e: e.matmul(psm[:, H:2 * H], lhsT=ones[:], rhs=lad, start=True, stop=True), [ones, la], [psm])
            self.V(lambda e: e.tensor_copy(out=acs[:], in_=psm[:, 0:2 * H]), [psm], [acs])
            self.A(lambda e: e.activation(out=ea[par][:], in_=acs[:, 0:H], func=AF.Exp), [acs], [ea[par]])
            self.V(lambda e: e.tensor_tensor(out=wend[:], in0=acs[:, H:2 * H], in1=acs[:, 0:H], op=ALU.subtract), [acs], [wend])
            self.A(lambda e: e.activation(out=wend[:], in_=wend[:], func=AF.Exp), [wend], [wend])
            self.A(lambda e: e.activation(out=cd[par][:], in_=acs[:, H:2 * H], func=AF.Exp), [acs], [cd[par]])

        def decay_g(dirn, i, g):
            par = i % 2
            la = la2[par]
            ltg = lt[g % 2]
            nlg = nlb[g % 2]
            ladg = la[:, dirn * 32 + g * Hg:dirn * 32 + (g + 1) * Hg] if ssd else la[:, g:g + 1]
            self.G(lambda e: e.tensor_tensor(out=ltg[:], in0=bcast(tri[:].unsqueeze(1), [128, Hg, 128]),
                                             in1=bcast(ladg.unsqueeze(2), [128, Hg, 128]), op=ALU.mult), [tri, la], [ltg])
            self.G(lambda e: e.tensor_tensor(out=nlg[:], in0=bcast(nones[:].unsqueeze(1), [128, Hg, 128]),
                                             in1=bcast(ladg.unsqueeze(2), [128, Hg, 128]), op=ALU.mult), [nones, la], [nlg])
            Eg = E[par][g]
            for q in range((Hg + 3) // 4):
                nh = min(4, Hg - q * 4)
                pb = pbc[q % 2]
                self.P(lambda e: e.matmul(pb[:, 0:nh * 128], lhsT=ones[:], rhs=ltg[:, q * 4:q * 4 + nh, :], start=True, stop=False), [ones, ltg], [pb])
                self.P(lambda e: e.matmul(pb[:, 0:nh * 128], lhsT=tri[:], rhs=nlg[:, q * 4:q * 4 + nh, :], start=False, stop=True), [tri, nlg], [pb])
                self.A(lambda e: e.activation(out=Eg[:, q * 4:q * 4 + nh, :].rearrange("p h l -> p (h l)"), in_=pb[:, 0:nh * 128], func=AF.Exp), [pb], [Eg])

        def loads(c, i):
            cs_ = slice(c * 128, (c + 1) * 128)
            t3, p2 = i % 3, i % 2
            self.ld(qt3[t3], qt3[t3][:].rearrange("p (g k) t -> p g k t", g=G), qtv[:, :, 0:KC, cs_])
            self.ld(kt2[p2], kt2[p2][:].rearrange("p (g k) t -> p g k t", g=G), ktv[:, :, 0:KC, cs_])
            self.ld(ktm3[t3], ktm3[t3][:], self.Ktm[cs_, 0:G * dk])
            self.ld(vt3[t3], vt3[t3][:], self.Vtm[cs_, :])
            if ssd:
                self.ld(la2[p2], la2[p2][:], self.latm[cs_, :])
                self.ld(dt2[p2], dt2[p2][:], self.dttm[cs_, :])

        def XDT(i):
            return xdt2[i % 2] if ssd else vt3[i % 3]

        def stage1_pre(dirn, i):
            par = i % 2
            qt, kt, vt, dt = qt3[i % 3], kt2[par], vt3[i % 3], dt2[par]
            xdt = XDT(i)
            if ssd:
                decay_pre(dirn, i)
                self.V(lambda e: e.tensor_tensor(out=r3(xdt[:], H), in0=r3(vt[:], H),
                                                 in1=bcast(dt[:, dirn * 32:dirn * 32 + 32].unsqueeze(2), [128, H, dv]), op=ALU.mult),
                       [vt, dt], [xdt])
            self.G(lambda e: e.tensor_tensor(out=r3(xe[par][:], H), in0=r3(xdt[:], H),
                                             in1=bcast(wend[:].unsqueeze(2), [128, H, dv]), op=ALU.mult), [xdt, wend], [xe[par]])
            for g in range(G):
                for kc in range(KC):
                    self.P(lambda e, g=g, kc=kc: e.matmul(pcb[:, g * 128:(g + 1) * 128], lhsT=kt[:, g * KC + kc, :], rhs=qt[:, g * KC + kc, :],
                                                          start=(kc == 0), stop=(kc == KC - 1)), [kt, qt], [pcb])
            self.V(lambda e: e.tensor_tensor(out=cbm[par][:], in0=pcb[:].rearrange("p (g l) -> p g l", g=G),
                                             in1=bcast(tri[:].unsqueeze(1), [128, G, 128]), op=ALU.mult), [pcb, tri], [cbm[par]])

        def emitM(g, par):
            Mg = M[g % 2]
            self.V(lambda e: e.scalar_tensor_tensor(out=Mg[:], in0=E[par][g][:], scalar=1.0,
                                                    in1=bcast(cbm[par][:, g:g + 1, :], [128, Hg, 128]), op0=ALU.min, op1=ALU.mult),
                   [E[par][g], cbm[par]], [Mg])

        def s2g(i, g):
            par = i % 2
            qt, ktm = qt3[i % 3], ktm3[i % 3]
            xdt = XDT(i)
            gs = slice(g * 512, (g + 1) * 512)
            if g + 1 < G:
                emitM(g + 1, par)
            Mg = M[g % 2]
            tg = tmp[g % 2]
            for hh in range(Hg):
                h = g * Hg + hh
                self.P(lambda e, h=h, hh=hh: e.matmul(pyd[:, hh * dv:(hh + 1) * dv], lhsT=Mg[:, hh, :], rhs=xdt[:, h * dv:(h + 1) * dv],
                                                      start=True, stop=True), [Mg, xdt], [pyd])
            for kc in range(KC):
                self.P(lambda e, kc=kc: e.matmul(pyo[:], lhsT=qt[:, g * KC + kc, :], rhs=Sb[:, kc, gs],
                                                 start=(kc == 0), stop=(kc == KC - 1)), [qt, Sb], [pyo])
            self.V(lambda e: e.tensor_tensor(out=r3(tg[:], Hg), in0=r3(pyo[:], Hg),
                                             in1=bcast(ea[par][:, g * Hg:(g + 1) * Hg].unsqueeze(2), [128, Hg, dv]), op=ALU.mult),
                   [pyo, ea[par]], [tg])
            self.V(lambda e: e.tensor_tensor(out=yc[:, gs], in0=tg[:], in1=pyd[:], op=ALU.add), [tg, pyd], [yc])
            for kc in range(KC):
                self.P(lambda e, kc=kc: e.matmul(pst[:], lhsT=ktm[:, g * dk + kc * 128:g * dk + (kc + 1) * 128], rhs=xe[par][:, gs],
                                                 start=True, stop=True), [ktm, xe[par]], [pst])
                self.G(lambda e, kc=kc: e.tensor_tensor(out=r3(S[:, kc, gs], Hg), in0=r3(S[:, kc, gs], Hg),
                                                        in1=bcast(cd[par][:, g * Hg:(g + 1) * Hg].unsqueeze(2), [128, Hg, dv]),
                                                        op=ALU.mult), [S, cd[par]], [S])
                self.V(lambda e, kc=kc: e.tensor_tensor(out=S[:, kc, gs], in0=S[:, kc, gs], in1=pst[:], op=ALU.add), [S, pst], [S])
                self.A(lambda e, kc=kc: e.copy(out=Sb[:, kc, gs], in_=S[:, kc, gs]), [S], [Sb])

        def stage2_post(c, dirn, i):
            par = i % 2
            cs_ = slice(c * 128, (c + 1) * 128)
            if dirn == 0:
                deferred.append(lambda: self.st(self.yf[cs_, :], yc, yc[:]))
                return
            vtp = vt3[i % 3]
            self.ld(yfl, yfl[:], self.yf[cs_, :])
            self.ld(gt, gt[:], self.gate[cs_, :])
            self.V(lambda e: e.tensor_tensor(out=yc[:], in0=yc[:], in1=yfl[:], op=ALU.add), [yc, yfl], [yc])
            if ssd:
                self.G(lambda e: e.tensor_tensor(out=r3(yfl[:], H), in0=r3(vtp[:], H),
                                                 in1=bcast(dsk[:].unsqueeze(2), [128, H, dv]), op=ALU.mult), [vtp, dsk], [yfl])
                self.V(lambda e: e.tensor_tensor(out=yc[:], in0=yc[:], in1=yfl[:], op=ALU.add), [yc, yfl], [yc])
                self.A(lambda e: e.activation(out=gt[:], in_=gt[:], func=AF.Silu), [gt], [gt])
                self.V(lambda e: e.tensor_tensor(out=yc[:], in0=yc[:], in1=gt[:], op=ALU.mult), [yc, gt], [yc])
                for g in range(4):
                    self.V(lambda e, g=g: e.bn_stats(out=st6[:, g, :], in_=yc[:, g * 512:(g + 1) * 512]), [yc], [st6])
                    self.V(lambda e, g=g: e.bn_aggr(out=mv[:, g, :], in_=st6[:, g, :]), [st6], [mv])
                self.V(lambda e: e.tensor_tensor(out=ss[:], in0=mv[:, :, 0], in1=mv[:, :, 0], op=ALU.mult), [mv], [ss])
                self.V(lambda e: e.tensor_tensor(out=ss[:], in0=ss[:], in1=mv[:, :, 1], op=ALU.add), [ss, mv], [ss])
                self.A(lambda e: e.activation(out=ss[:], in_=ss[:], func=AF.Sqrt, bias=EPS), [ss], [ss])
                self.V(lambda e: e.reciprocal(out=rstd[:], in_=ss[:]), [ss], [rstd])
                for g in range(4):
                    gs = slice(g * 512, (g + 1) * 512)
                    self.V(lambda e, g=g, gs=gs: e.scalar_tensor_tensor(out=yb[:, gs], in0=yc[:, gs], scalar=rstd[:, g:g + 1], in1=nw[:, gs],
                                                                        op0=ALU.mult, op1=ALU.mult), [yc, rstd, nw], [yb])
            else:
                for g in range(4):
                    self.V(lambda e, g=g: e.bn_stats(out=st6[:, g, :], in_=yc[:, g * 512:(g + 1) * 512]), [yc], [st6])
                    self.V(lambda e, g=g: e.bn_aggr(out=mv[:, g, :], in_=st6[:, g, :]), [st6], [mv])
                self.A(lambda e: e.activation(out=ss[:], in_=mv[:, :, 1], func=AF.Sqrt, bias=EPS), [mv], [ss])
                self.V(lambda e: e.reciprocal(out=rstd[:], in_=ss[:]), [ss], [rstd])
                self.A(lambda e: e.activation(out=gt[:], in_=gt[:], func=AF.Silu), [gt], [gt])
                for g in range(4):
                    gs = slice(g * 512, (g + 1) * 512)
                    self.V(lambda e, g=g, gs=gs: e.tensor_scalar(out=yc[:, gs], in0=yc[:, gs], scalar1=mv[:, g, 0:1], scalar2=rstd[:, g:g + 1],
                                                                 op0=ALU.subtract, op1=ALU.mult), [yc, mv, rstd], [yc])
                self.G(lambda e: e.tensor_tensor(out=yc[:], in0=yc[:], in1=nw[:], op=ALU.mult), [yc, nw], [yc])
                self.V(lambda e: e.tensor_tensor(out=yc[:], in0=yc[:], in1=nb_[:], op=ALU.add), [yc, nb_], [yc])
                self.V(lambda e: e.tensor_tensor(out=yb[:], in0=yc[:], in1=gt[:], op=ALU.mult), [yc, gt], [yb])
            for half in range(2):
                for kk in range(8):
                    k = half * 8 + kk
                    self.P(lambda e, k=k, kk=kk: e.transpose(ptr[:, kk, :], yb[:, k * 128:(k + 1) * 128], identb[:]), [yb, identb], [ptr])
                self.A(lambda e, half=half: e.copy(out=yT[:, half * 8:(half + 1) * 8, :], in_=ptr[:]), [ptr], [yT])
            pos = (pyd, pyo)
            for nn in range(2):
                for k in range(16):
                    self.P(lambda e, k=k, nn=nn: e.matmul(pos[nn][:], lhsT=yT[:, k, :], rhs=Wo[:, k, nn * 512:(nn + 1) * 512],
                                                          start=(k == 0), stop=(k == 15)), [yT, Wo], [pos[nn]])
            self.ld(xt, xt[:], xcur[cs_, :])
            for nn in range(2):
                ns = slice(nn * 512, (nn + 1) * 512)
                self.V(lambda e, nn=nn, ns=ns: e.tensor_tensor(out=t1[:, ns], in0=pos[nn][:], in1=g1[:, ns], op=ALU.mult), [pos[nn], g1], [t1])
            self.V(lambda e: e.scalar_tensor_tensor(out=t2[:], in0=xt[:], scalar=ALPHA, in1=t1[:], op0=ALU.mult, op1=ALU.add), [xt, t1], [t2])
            self.layer_norm(t2, t1, st2, mv2, rs2, lng, lnb)
            self.G(lambda e: e.tensor_tensor(out=t2[:], in0=t1[:], in1=sc2[:], op=ALU.mult), [t1, sc2], [t2])
            self.V(lambda e: e.tensor_tensor(out=t2[:], in0=t2[:], in1=sh2[:], op=ALU.add), [t2, sh2], [t2])
            deferred.append(lambda: self.st(xnext[cs_, :], t1, t1[:]))
            deferred.append(lambda: self.st(self.tok[cs_, :], t2, t2[:]))

        deferred = []

        def flush():
            for f in deferred:
                f()
            del deferred[:]

        for dirn in range(2):
            order = list(range(NCH)) if dirn == 0 else [1, 0] + list(range(NCH - 1, 1, -1))
            self.ld(tri, tri[:], self.c_trif[dirn])
            self.V(lambda e: e.memset(S[:], 0.0), [], [S])
            self.G(lambda e: e.memset(Sb[:], 0.0), [], [Sb])
            if dirn == 1:
                load_rows(1)
            n_ = len(order)
            if not ssd:
                for la in la2:
                    self.ld_row(la, la[:, 0:4], self.ret_decay[j, dirn:dirn + 1, :])
                    self.A(lambda e: e.activation(out=la[:, 0:4], in_=la[:, 0:4], func=AF.Exp, scale=-1.0), [la], [la])
                    self.A(lambda e: e.activation(out=la[:, 0:4], in_=la[:, 0:4], func=AF.Ln, bias=1.0), [la], [la])
                    self.V(lambda e: e.tensor_scalar_mul(out=la[:, 0:4], in0=la[:, 0:4], scalar1=-1.0), [la], [la])
                decay_pre(dirn, 0)
                for g in range(G):
                    decay_g(dirn, 0, g)
            loads(order[0], 0)
            loads(order[1], 1)
            stage1_pre(dirn, 0)
            if ssd:
                for g in range(G):
                    decay_g(dirn, 0, g)
            for i, c in enumerate(order):
                if i + 2 < n_:
                    loads(order[i + 2], i + 2)
                flush()
                if i + 1 < n_:
                    stage1_pre(dirn, i + 1)
                if dirn == 1 and i == NCTX:
                    load_rows(0)
                emitM(0, i % 2)
                for g in range(G):
                    if ssd and i + 1 < n_:
                        decay_g(dirn, i + 1, g)
                    s2g(i, g)
                stage2_post(c, dirn, i)
            flush()
            if dirn == 0:
                fw.barrier()


Builder.phase_scan = _phase_scan2
```

```python
import numpy as np
import ml_dtypes
from contextlib import ExitStack, contextmanager
import concourse.bass as bass
import concourse.mybir as mybir
from concourse.bass_utils import run_bass_kernel_spmd

F32 = mybir.dt.float32
BF16 = mybir.dt.bfloat16
I32 = mybir.dt.int32
ALU = mybir.AluOpType
AF = mybir.ActivationFunctionType
AX = mybir.AxisListType

ENGS = ["tensor", "vector", "scalar", "gpsimd", "sync"]
EPOCH = 20000
NDMA_SEM = 8

D = 1024
T = 4352
NCH = 34
NCTX = 2
NB = 100
DEPTH = 4
ALPHA = (2.0 * DEPTH) ** 0.25
EPS = 1e-5


class Res:
    __slots__ = ("w", "r")

    def __init__(self):
        self.w = None
        self.r = {}


class Tl:
    def __init__(self, t):
        self.t = t
        self.r = Res()

    def __getitem__(self, k):
        return self.t[k]


class FW:
    def __init__(self, nc):
        self.nc = nc
        self.root = ExitStack()
        self.es = self.root
        self.cnt = {e: 0 for e in ENGS}
        self.sems = {}
        self.waited = {e: {} for e in ENGS}
        self.dma_i = {e: 0 for e in ENGS}
        self.dma_last = {}
        self.latest = {}
        self.uid = 0

    def sem(self, key):
        if key not in self.sems:
            self.sems[key] = self.root.enter_context(self.nc.semaphore("s_%s_%s" % key))
        return self.sems[key]

    def sb(self, name, shape, dt):
        self.uid += 1
        return Tl(self.es.enter_context(self.nc.sbuf_tensor("%s_%d" % (name, self.uid), list(shape), dt)))

    def ps(self, name, shape, dt):
        self.uid += 1
        return Tl(self.es.enter_context(self.nc.psum_tensor("%s_%d" % (name, self.uid), list(shape), dt)))

    @contextmanager
    def scope(self):
        old = self.es
        self.es = ExitStack()
        try:
            yield
        finally:
            self.barrier()
            self.es.close()
            self.es = old

    def barrier(self):
        for eng in ENGS:
            for key, val in list(self.latest.items()):
                self._wait(eng, (key, val))

    def _wait(self, eng, ev):
        if ev is None:
            return
        key, val = ev
        if self.waited[eng].get(key, 0) >= val:
            return
        self.waited[eng][key] = val
        getattr(self.nc, eng).wait_ge(self.sem(key), val)

    def _deps(self, eng, reads, writes):
        evs = []
        for r in reads:
            if r.w is not None:
                evs.append(r.w)
        for w in writes:
            if w.w is not None:
                evs.append(w.w)
            evs.extend(w.r.items())
        for ev in evs:
            if ev[0][0] == "tensor" and eng == "tensor":
                continue
            self._wait(eng, ev)

    def _record(self, ev, reads, writes):
        self.latest[ev[0]] = ev[1]
        for r in reads:
            if r.r.get(ev[0], 0) < ev[1]:
                r.r[ev[0]] = ev[1]
        for w in writes:
            w.w = ev
            w.r = {}

    def op(self, eng, fn, reads=(), writes=()):
        reads = [t.r for t in reads]
        writes = [t.r for t in writes]
        self._deps(eng, reads, writes)
        c = self.cnt[eng]
        key = (eng, c // EPOCH)
        val = c % EPOCH + 1
        self.cnt[eng] = c + 1
        fn(getattr(self.nc, eng)).then_inc(self.sem(key), 1)
        self._record((key, val), reads, writes)

    def dma(self, eng, fn, reads=(), writes=()):
        reads = [t.r for t in reads]
        writes = [t.r for t in writes]
        self._deps(eng, reads, writes)
        i = self.dma_i[eng]
        self.dma_i[eng] = i + 1
        key = ("d" + eng, i % NDMA_SEM)
        prev = self.dma_last.get(key, 0)
        if prev:
            self._wait(eng, (key, prev))
        val = prev + 16
        self.dma_last[key] = val
        fn(getattr(self.nc, eng)).then_inc(self.sem(key), 16)
        self._record((key, val), reads, writes)


def bcast(ap, shape):
    return ap.to_broadcast(list(shape))


class Builder:
    def __init__(self, nc, nlayers=DEPTH, stop=None):
        self.nc = nc
        self.fw = FW(nc)
        self.nlayers = nlayers
        self.stop = stop
        self.dram = {}
        self.debug = set()
        self.only = None

    def din(self, name, shape, dt):
        if self.only is not None and name not in self.only:
            return None
        a = self.nc.dram_tensor(name, list(shape), dt, kind="ExternalInput").ap()
        self.dram[name] = a
        return a

    def dscr(self, name, shape, dt):
        kind = "ExternalOutput" if name in self.debug else "Internal"
        a = self.nc.dram_tensor(name, list(shape), dt, kind=kind).ap()
        self.dram[name] = a
        return a

    def ld(self, tile, dst, src, eng="sync"):
        self.fw.dma(eng, lambda e: e.dma_start(out=dst, in_=src), writes=[tile])

    def st(self, dst, tile, src, eng="sync"):
        self.fw.dma(eng, lambda e: e.dma_start(out=dst, in_=src), reads=[tile])

    def V(self, fn, rd, wr):
        self.fw.op("vector", fn, rd, wr)

    def A(self, fn, rd, wr):
        self.fw.op("scalar", fn, rd, wr)

    def G(self, fn, rd, wr):
        self.fw.op("gpsimd", fn, rd, wr)

    def P(self, fn, rd, wr):
        self.fw.op("tensor", fn, rd, wr)

    def ld_row(self, tile, dst, src_row, n=128):
        self.ld(tile, dst, src_row.partition_broadcast(n))

    def declare(self):
        d = self.din
        self.xin = d("xin", [T, D], F32)
        self.cvec = d("cvec", [128, 8, 2], F32)
        self.mod_w = d("mod_w", [DEPTH, D, 6 * D], F32)
        self.mod_b = d("mod_b", [DEPTH, 6 * D], F32)
        self.ssd_in_w = d("ssd_in_w", [2, D, 5184], F32)
        self.convw = d("convw", [2, 128, 24, 5], F32)
        self.convb = d("convb", [2, 128, 24], F32)
        self.ssd_dt_bias = d("ssd_dt_bias", [2, 64], F32)
        self.ssd_a_log = d("ssd_a_log", [2, 64], F32)
        self.ssd_d_skip = d("ssd_d_skip", [2, 32], F32)
        self.ssd_norm_w = d("ssd_norm_w", [2, 2048], F32)
        self.ssd_out_w = d("ssd_out_w", [2, 2048, D], F32)
        self.ret_in_w = d("ret_in_w", [2, D, 6144], F32)
        self.ret_decay = d("ret_decay_logit", [2, 2, 4], F32)
        self.ret_gn_w = d("ret_gn_w", [2, 2048], F32)
        self.ret_gn_b = d("ret_gn_b", [2, 2048], F32)
        self.ret_out_w = d("ret_out_w", [2, 2048, D], F32)
        self.ln_mix_g = d("ln_mix_g", [DEPTH, D], F32)
        self.ln_mix_b = d("ln_mix_b", [DEPTH, D], F32)
        self.ln_ffn_g = d("ln_ffn_g", [DEPTH, D], F32)
        self.ln_ffn_b = d("ln_ffn_b", [DEPTH, D], F32)
        self.moe_rw = d("moe_rw", [DEPTH, D, 36], F32)
        self.moe_rb = d("moe_rb", [DEPTH, 36], F32)
        self.moe_wgu = d("moe_w_gate_up", [DEPTH * 32 * 128, 8 * D], F32)
        self.moe_wd = d("moe_w_down", [DEPTH * 32 * 128, 4 * D], F32)
        self.c_identb = d("c_identb", [128, 128], BF16)
        self.c_identf = d("c_identf", [128, 128], F32)
        self.c_trif = d("c_trif", [2, 128, 128], F32)
        self.c_ones = d("c_ones", [128, 128], F32)
        self.c_slow = d("c_slow", [128, 128], BF16)
        self.c_rope = d("c_rope", [2, 128, T], F32)
        self.c_iota = d("c_iota", [128, 512], F32)
        self.c_pidx = d("c_pidx", [128, 12], F32)
        self.yout = self.nc.dram_tensor("yout", [4096, D], F32, kind="ExternalOutput").ap()
        s = self.dscr
        self.xA = s("xA", [T, D], F32)
        self.xB = s("xB", [T, D], F32)
        self.modrow = s("modrow", [2, 6 * D], F32)
        self.QT = s("QT", [4, 2, 128, T], BF16)
        self.KT = s("KT", [4, 2, 128, T], BF16)
        self.Ktm = s("Ktm", [T, 1024], BF16)
        self.Vtm = s("Vtm", [T, 2048], BF16)
        self.gate = s("gate", [T, 2048], F32)
        self.latm = s("latm", [T, 64], F32)
        self.dttm = s("dttm", [T, 64], F32)
        self.yf = s("yf", [T, 2048], F32)
        self.tok = s("tok", [T, D], F32)
        self.xbuf = s("xbuf", [NB * 128, D], BF16)
        self.ybuf = s("ybuf", [NB * 128, D], F32)
        self.dbg = {}

    def phase_mod(self, li):
        fw = self.fw
        with fw.scope():
            cv = fw.sb("cv", [128, 8, 2], F32)
            sv = fw.sb("sv", [128, 8, 2], F32)
            mb = fw.sb("mb", [2, 6 * D], F32)
            mr = fw.sb("mr", [2, 6 * D], F32)
            wst = fw.sb("wst", [128, 8, 512], F32)
            pm = fw.ps("pm", [128, 512], F32)
            self.ld(cv, cv[:], self.cvec)
            self.A(lambda e: e.activation(out=sv[:], in_=cv[:], func=AF.Silu), [cv], [sv])
            self.ld(mb, mb[0:1, :], self.mod_b[li:li + 1, :])
            self.ld(mb, mb[1:2, :], self.mod_b[li:li + 1, :])
            wv = self.mod_w[li].rearrange("(k p) n -> p k n", p=128)
            for n in range(12):
                self.ld(wst, wst[:], wv[:, :, n * 512:(n + 1) * 512])
                for k in range(8):
                    self.P(lambda e, k=k: e.matmul(pm[0:2, :], lhsT=sv[:, k, :], rhs=wst[:, k, :],
                                                   start=(k == 0), stop=(k == 7)), [sv, wst], [pm])
                self.V(lambda e, n=n: e.tensor_tensor(out=mr[0:2, n * 512:(n + 1) * 512], in0=pm[0:2, :],
                                                      in1=mb[0:2, n * 512:(n + 1) * 512], op=ALU.add),
                       [pm, mb], [mr])
            self.st(self.modrow, mr, mr[0:2, :])

    def make_uT(self, xcur, uT, identb, ptr, sc1, sh1, xt, u32, ub, c):
        v = 1 if c < NCTX else 0
        self.ld(xt, xt[:], xcur[c * 128:(c + 1) * 128, :])
        self.V(lambda e: e.tensor_tensor(out=u32[:], in0=xt[:], in1=sc1[v][:], op=ALU.mult), [xt, sc1[v]], [u32])
        self.G(lambda e: e.tensor_tensor(out=ub[:], in0=u32[:], in1=sh1[v][:], op=ALU.add), [u32, sh1[v]], [ub])
        for k in range(8):
            self.P(lambda e, k=k: e.transpose(ptr[:, k, :], ub[:, k * 128:(k + 1) * 128], identb[:]),
                   [ub, identb], [ptr])

    def load_mod_rows(self, lo, names):
        out = {}
        for nm, idx in names:
            tl = []
            for v in range(2):
                t = self.fw.sb("row_%s%d" % (nm, v), [128, D], F32)
                self.ld_row(t, t[:], self.modrow[v:v + 1, idx * D:(idx + 1) * D])
                tl.append(t)
            out[nm] = tl
        return out

    def phase_in_ssd(self, li, j, xcur):
        fw = self.fw
        with fw.scope():
            identb = fw.sb("identb", [128, 128], BF16)
            self.ld(identb, identb[:], self.c_identb)
            rows = self.load_mod_rows(0, [("sh1", 0), ("sc1", 1)])
            sh1, sc1 = rows["sh1"], rows["sc1"]
            for v in range(2):
                self.V(lambda e, v=v: e.tensor_scalar_add(out=sc1[v][:], in0=sc1[v][:], scalar1=1.0), [sc1[v]], [sc1[v]])
            uT = fw.sb("uT", [128, 8, T], BF16)
            xt = fw.sb("xt", [128, D], F32)
            u32 = fw.sb("u32", [128, D], F32)
            ub = fw.sb("ub", [128, D], BF16)
            ptr = fw.ps("ptr", [128, 8, 128], BF16)
            for c in range(NCH):
                self.make_uT(xcur, uT, identb, ptr, sc1, sh1, xt, u32, ub, c)
                self.A(lambda e, c=c: e.copy(out=uT[:, :, c * 128:(c + 1) * 128], in_=ptr[:]), [ptr], [uT])
            cw = fw.sb("cw", [128, 24, 5], F32)
            cb = fw.sb("cb", [128, 24], F32)
            self.ld(cw, cw[:], self.convw[j])
            self.ld(cb, cb[:], self.convb[j])
            wst_ = [fw.sb("wstA%d" % i, [128, 8, 128], F32) for i in range(2)]
            wb_ = [fw.sb("wbA%d" % i, [128, 8, 128], BF16) for i in range(2)]
            raw_ = [fw.sb("raw%d" % i, [128, T], F32) for i in range(2)]
            o_single = fw.sb("o", [128, T], F32)
            o_ = [o_single, o_single]
            ob_ = [fw.sb("ob%d" % i, [128, T], BF16) for i in range(2)]
            pp = [fw.ps("pp%d" % i, [128, 512], F32) for i in range(2)]
            trs_ = [fw.sb("trs%d" % i, [128, 8, 128], BF16) for i in range(2)]
            wv = self.ssd_in_w[j].rearrange("(k p) n -> p k n", p=128)
            segs = [(0, 256)] + [(256 + i * 512, 256 + (i + 1) * 512) for i in range(8)]
            seqs = [(0, 256), (256, T)]
            def loadW(f):
                wst = wst_[f % 2]
                col0 = 2048 + f * 128
                self.ld(wst, wst[:], wv[:, :, col0:col0 + 128])

            def stepA(f):
                wst, wb, raw, o, ob = wst_[f % 2], wb_[f % 2], raw_[f % 2], o_[f % 2], ob_[f % 2]
                self.G(lambda e, wb=wb, wst=wst: e.tensor_copy(out=wb[:], in_=wst[:]), [wst], [wb])
                for si, (a, b) in enumerate(segs):
                    p = pp[si % 2]
                    for k in range(8):
                        self.P(lambda e, k=k, a=a, b=b, p=p, wb=wb: e.matmul(p[:, 0:b - a], lhsT=wb[:, k, :], rhs=uT[:, k, a:b],
                                                                       start=(k == 0), stop=(k == 7)), [wb, uT], [p])
                    self.A(lambda e, a=a, b=b, p=p, raw=raw: e.copy(out=raw[:, a:b], in_=p[:, 0:b - a]), [p], [raw])

            def stepB1(f):
                wst, wb, raw, o, ob = wst_[f % 2], wb_[f % 2], raw_[f % 2], o_[f % 2], ob_[f % 2]
                self.A(lambda e, f=f, o=o, raw=raw: e.activation(out=o[:], in_=raw[:], func=AF.Identity,
                                                   bias=cb[:, f:f + 1], scale=cw[:, f, 2:3]), [raw, cw, cb], [o])

            def stepB(f):
                wst, wb, raw, o, ob = wst_[f % 2], wb_[f % 2], raw_[f % 2], o_[f % 2], ob_[f % 2]
                for (a, b) in seqs:
                    for kk, off in ((0, -2), (1, -1), (3, 1), (4, 2)):
                        if off < 0:
                            osl = (a - off, b)
                            isl = (a, b + off)
                        else:
                            osl = (a, b - off)
                            isl = (a + off, b)
                        self.V(lambda e, f=f, kk=kk, osl=osl, isl=isl, o=o, raw=raw: e.scalar_tensor_tensor(
                            out=o[:, osl[0]:osl[1]], in0=raw[:, isl[0]:isl[1]], scalar=cw[:, f, kk:kk + 1],
                            in1=o[:, osl[0]:osl[1]], op0=ALU.mult, op1=ALU.add), [raw, cw, o], [o])
                self.A(lambda e, o=o, ob=ob: e.activation(out=ob[:], in_=o[:], func=AF.Silu), [o], [ob])
                if f >= 16:
                    g = (f - 16) % 4
                    dst = self.KT if f < 20 else self.QT
                    self.st(dst[g, 0], ob, ob[:])
                if f < 20:
                    dstm = self.Vtm if f < 16 else self.Ktm
                    fc = f if f < 16 else f - 16
                    dv = dstm.rearrange("(c p) f -> p c f", p=128)
                    for c0 in range(0, NCH, 8):
                        trs = trs_[(c0 // 8) % 2]
                        n = min(8, NCH - c0)
                        for cc in range(n):
                            self.P(lambda e, cc=cc, c0=c0, ob=ob: e.transpose(ptr[:, cc, :], ob[:, (c0 + cc) * 128:(c0 + cc + 1) * 128],
                                                                       identb[:]), [ob, identb], [ptr])
                        self.V(lambda e, n=n, trs=trs: e.tensor_copy(out=trs[:, 0:n, :], in_=ptr[:, 0:n, :]), [ptr], [trs])
                        self.st(dv[:, c0:c0 + n, fc * 128:(fc + 1) * 128], trs, trs[:, 0:n, :])

            loadW(0)
            loadW(1)
            stepA(0)
            for f in range(24):
                stepB1(f)
                if f + 1 < 24:
                    stepA(f + 1)
                if f + 2 < 24:
                    loadW(f + 2)
                stepB(f)

            wst2 = fw.sb("wst2", [128, 8, 512], F32)
            wb2 = fw.sb("wb2", [128, 8, 512], BF16)
            zt = fw.sb("zt", [128, 512], F32)
            for n in range(4):
                self.ld(wst2, wst2[:], wv[:, :, n * 512:(n + 1) * 512])
                self.G(lambda e: e.tensor_copy(out=wb2[:], in_=wst2[:]), [wst2], [wb2])
                for c in range(NCH):
                    p = pp[c % 2]
                    for k in range(8):
                        self.P(lambda e, k=k, c=c, p=p: e.matmul(p[:], lhsT=uT[:, k, c * 128:(c + 1) * 128], rhs=wb2[:, k, :],
                                                                 start=(k == 0), stop=(k == 7)), [uT, wb2], [p])
                    self.A(lambda e, p=p: e.copy(out=zt[:], in_=p[:]), [p], [zt])
                    self.st(self.gate[c * 128:(c + 1) * 128, n * 512:(n + 1) * 512], zt, zt[:])
            dtb = fw.sb("dtb", [128, 64], F32)
            nega = fw.sb("nega", [128, 64], F32)
            self.ld_row(dtb, dtb[:], self.ssd_dt_bias[j:j + 1, :])
            self.ld_row(nega, nega[:], self.ssd_a_log[j:j + 1, :])
            self.A(lambda e: e.activation(out=nega[:], in_=nega[:], func=AF.Exp), [nega], [nega])
            self.V(lambda e: e.tensor_scalar_mul(out=nega[:], in0=nega[:], scalar1=-1.0), [nega], [nega])
            self.ld(wst2, wst2[:, :, 0:64], wv[:, :, 5120:5184])
            self.G(lambda e: e.tensor_copy(out=wb2[:, :, 0:64], in_=wst2[:, :, 0:64]), [wst2], [wb2])
            d0 = fw.sb("d0", [128, 64], F32)
            d1 = fw.sb("d1", [128, 64], F32)
            d2 = fw.sb("d2", [128, 64], F32)
            for c in range(NCH):
                p = pp[c % 2]
                for k in range(8):
                    self.P(lambda e, k=k, c=c, p=p: e.matmul(p[:, 0:64], lhsT=uT[:, k, c * 128:(c + 1) * 128], rhs=wb2[:, k, 0:64],
                                                             start=(k == 0), stop=(k == 7)), [uT, wb2], [p])
                self.V(lambda e, p=p: e.tensor_tensor(out=d0[:], in0=p[:, 0:64], in1=dtb[:], op=ALU.add), [p, dtb], [d0])
                self.V(lambda e: e.tensor_scalar_mul(out=d1[:], in0=d0[:], scalar1=-1.0), [d0], [d1])
                self.V(lambda e: e.tensor_tensor(out=d1[:], in0=d1[:], in1=d0[:], op=ALU.max), [d0, d1], [d1])
                self.A(lambda e: e.activation(out=d1[:], in_=d1[:], func=AF.Exp, scale=-1.0), [d1], [d1])
                self.A(lambda e: e.activation(out=d1[:], in_=d1[:], func=AF.Ln, bias=1.0), [d1], [d1])
                self.V(lambda e: e.scalar_tensor_tensor(out=d2[:], in0=d0[:], scalar=0.0, in1=d1[:], op0=ALU.max, op1=ALU.add),
                       [d0, d1], [d2])
                self.st(self.dttm[c * 128:(c + 1) * 128, :], d2, d2[:])
                self.V(lambda e: e.tensor_tensor(out=d0[:], in0=d2[:], in1=nega[:], op=ALU.mult), [d2, nega], [d0])
                self.st(self.latm[c * 128:(c + 1) * 128, :], d0, d0[:])

    def phase_in_ret(self, li, j, xcur):
        fw = self.fw
        with fw.scope():
            identb = fw.sb("identb", [128, 128], BF16)
            self.ld(identb, identb[:], self.c_identb)
            rows = self.load_mod_rows(0, [("sh1", 0), ("sc1", 1)])
            sh1, sc1 = rows["sh1"], rows["sc1"]
            for v in range(2):
                self.V(lambda e, v=v: e.tensor_scalar_add(out=sc1[v][:], in0=sc1[v][:], scalar1=1.0), [sc1[v]], [sc1[v]])
            W = fw.sb("Wret", [128, 8, 6144], BF16)
            wst = fw.sb("wstR", [128, 8, 512], F32)
            wv = self.ret_in_w[j].rearrange("(k p) n -> p k n", p=128)
            for n in range(12):
                self.ld(wst, wst[:], wv[:, :, n * 512:(n + 1) * 512])
                eng = self.G if n % 2 == 0 else self.A
                if n % 2 == 0:
                    self.G(lambda e, n=n: e.tensor_copy(out=W[:, :, n * 512:(n + 1) * 512], in_=wst[:]), [wst], [W])
                else:
                    self.A(lambda e, n=n: e.copy(out=W[:, :, n * 512:(n + 1) * 512], in_=wst[:]), [wst], [W])
            uT = fw.sb("uTs", [128, 8, 512], BF16)
            xt = fw.sb("xt", [128, D], F32)
            u32 = fw.sb("u32", [128, D], F32)
            ub = fw.sb("ub", [128, D], BF16)
            ptr = fw.ps("ptr", [128, 8, 128], BF16)
            pp = [fw.ps("pp%d" % i, [128, 512], F32) for i in range(4)]
            cs = fw.sb("cs", [128, 512], F32)
            sn = fw.sb("sn", [128, 512], F32)
            r1_ = [fw.sb("r1%d" % i, [128, 512], F32) for i in range(2)]
            r2_ = [fw.sb("r2%d" % i, [128, 512], F32) for i in range(2)]
            ta_ = [fw.sb("ta%d" % i, [128, 512], F32) for i in range(2)]
            tb_ = [fw.sb("tb%d" % i, [128, 512], F32) for i in range(2)]
            o1_ = [fw.sb("o1%d" % i, [128, 512], BF16) for i in range(2)]
            o2_ = [fw.sb("o2%d" % i, [128, 512], BF16) for i in range(2)]
            trs_ = [fw.sb("trs%d" % i, [128, 8, 128], BF16) for i in range(2)]
            zt = fw.sb("zt", [128, 512], F32)
            vb = fw.sb("vb", [128, 512], BF16)
            segs = [(0, 256)] + [(256 + i * 512, 256 + (i + 1) * 512) for i in range(8)]
            ktv = self.Ktm.rearrange("(c p) f -> p c f", p=128)
            for (a, b) in segs:
                n = b - a
                nt = n // 128
                c0 = a // 128
                for ci in range(nt):
                    self.make_uT(xcur, uT, identb, ptr, sc1, sh1, xt, u32, ub, c0 + ci)
                    self.A(lambda e, ci=ci: e.copy(out=uT[:, :, ci * 128:(ci + 1) * 128], in_=ptr[:]), [ptr], [uT])
                self.ld(cs, cs[:, 0:n], self.c_rope[0, :, a:b])
                self.ld(sn, sn[:, 0:n], self.c_rope[1, :, a:b])
                def qkA(which, h):
                    ii = (which * 4 + h) % 2
                    r1, r2, ta, tb, o1, o2, trs = r1_[ii], r2_[ii], ta_[ii], tb_[ii], o1_[ii], o2_[ii], trs_[ii]
                    base = which * 1024 + h * 256
                    for half, (pt, rr) in enumerate(((pp[ii * 2], r1), (pp[ii * 2 + 1], r2))):
                        cb0 = base + half * 128
                        for k in range(8):
                            self.P(lambda e, k=k, cb0=cb0, pt=pt: e.matmul(pt[:, 0:n], lhsT=W[:, k, cb0:cb0 + 128], rhs=uT[:, k, 0:n],
                                                                           start=(k == 0), stop=(k == 7)), [W, uT], [pt])
                        sc = 1.0 if which == 0 else 0.0625
                        self.A(lambda e, pt=pt, rr=rr, sc=sc: e.activation(out=rr[:, 0:n], in_=pt[:, 0:n], func=AF.Copy, scale=sc),
                               [pt], [rr])

                def qkB(which, h):
                    ii = (which * 4 + h) % 2
                    r1, r2, ta, tb, o1, o2, trs = r1_[ii], r2_[ii], ta_[ii], tb_[ii], o1_[ii], o2_[ii], trs_[ii]
                    base = which * 1024 + h * 256
                    self.V(lambda e, ta=ta, r1=r1: e.tensor_tensor(out=ta[:, 0:n], in0=r1[:, 0:n], in1=cs[:, 0:n], op=ALU.mult), [r1, cs], [ta])
                    self.G(lambda e, tb=tb, r2=r2: e.tensor_tensor(out=tb[:, 0:n], in0=r2[:, 0:n], in1=sn[:, 0:n], op=ALU.mult), [r2, sn], [tb])
                    self.V(lambda e, ta=ta, tb=tb, o1=o1: e.tensor_tensor(out=o1[:, 0:n], in0=ta[:, 0:n], in1=tb[:, 0:n], op=ALU.subtract), [ta, tb], [o1])
                    self.V(lambda e, ta=ta, r1=r1: e.tensor_tensor(out=ta[:, 0:n], in0=r1[:, 0:n], in1=sn[:, 0:n], op=ALU.mult), [r1, sn], [ta])
                    self.G(lambda e, tb=tb, r2=r2: e.tensor_tensor(out=tb[:, 0:n], in0=r2[:, 0:n], in1=cs[:, 0:n], op=ALU.mult), [r2, cs], [tb])
                    self.V(lambda e, ta=ta, tb=tb, o2=o2: e.tensor_tensor(out=o2[:, 0:n], in0=ta[:, 0:n], in1=tb[:, 0:n], op=ALU.add), [ta, tb], [o2])
                    dst = self.QT if which == 0 else self.KT
                    self.st(dst[h, 0, :, a:b], o1, o1[:, 0:n])
                    self.st(dst[h, 1, :, a:b], o2, o2[:, 0:n])
                    if which == 1:
                        for half, oo in enumerate((o1, o2)):
                            for ci in range(nt):
                                self.P(lambda e, ci=ci, oo=oo, half=half: e.transpose(ptr[:, half * 4 + ci, :], oo[:, ci * 128:(ci + 1) * 128],
                                                                                      identb[:]), [oo, identb], [ptr])
                        self.V(lambda e, trs=trs: e.tensor_copy(out=trs[:], in_=ptr[:]), [ptr], [trs])
                        for half in range(2):
                            col = h * 256 + half * 128
                            self.st(ktv[:, c0:c0 + nt, col:col + 128], trs, trs[:, half * 4:half * 4 + nt, :])

                wh = [(w_, h_) for w_ in range(2) for h_ in range(4)]
                qkA(*wh[0])
                for t_ in range(8):
                    if t_ + 1 < 8:
                        qkA(*wh[t_ + 1])
                    qkB(*wh[t_])
                for ci in range(nt):
                    c = c0 + ci
                    for nn in range(8):
                        p = pp[2 + nn % 2]
                        colw = 2048 + nn * 512
                        for k in range(8):
                            self.P(lambda e, k=k, ci=ci, p=p, colw=colw: e.matmul(p[:], lhsT=uT[:, k, ci * 128:(ci + 1) * 128],
                                                                                  rhs=W[:, k, colw:colw + 512], start=(k == 0), stop=(k == 7)),
                                   [uT, W], [p])
                        if nn < 4:
                            self.A(lambda e, p=p: e.copy(out=vb[:], in_=p[:]), [p], [vb])
                            self.st(self.Vtm[c * 128:(c + 1) * 128, nn * 512:(nn + 1) * 512], vb, vb[:])
                        else:
                            self.V(lambda e, p=p: e.tensor_copy(out=zt[:], in_=p[:]), [p], [zt])
                            self.st(self.gate[c * 128:(c + 1) * 128, (nn - 4) * 512:(nn - 3) * 512], zt, zt[:])

    def phase_scan(self, li, j, ssd, xcur, xnext):
        fw = self.fw
        G = 4
        Hg = 8 if ssd else 1
        KC = 1 if ssd else 2
        H = G * Hg
        dv = 2048 // H
        dk = KC * 128
        with fw.scope():
            identb = fw.sb("identb", [128, 128], BF16)
            self.ld(identb, identb[:], self.c_identb)
            ones = fw.sb("ones", [128, 128], F32)
            self.ld(ones, ones[:], self.c_ones)
            tri = fw.sb("tri", [128, 128], F32)
            Wo = fw.sb("Wo", [128, 16, D], BF16)
            wst = fw.sb("wstO", [128, 4, D], F32)
            owv = (self.ssd_out_w if ssd else self.ret_out_w)[j].rearrange("(k p) n -> p k n", p=128)
            for q in range(4):
                self.ld(wst, wst[:], owv[:, q * 4:(q + 1) * 4, :])
                self.G(lambda e, q=q: e.tensor_copy(out=Wo[:, q * 4:(q + 1) * 4, :], in_=wst[:]), [wst], [Wo])
            rows = self.load_mod_rows(0, [("g1", 2), ("sh2", 3), ("sc2", 4)])
            g1, sh2, sc2 = rows["g1"], rows["sh2"], rows["sc2"]
            for v in range(2):
                self.V(lambda e, v=v: e.tensor_scalar_add(out=sc2[v][:], in0=sc2[v][:], scalar1=1.0), [sc2[v]], [sc2[v]])
            lng = fw.sb("lng", [128, D], F32)
            lnb = fw.sb("lnb", [128, D], F32)
            self.ld_row(lng, lng[:], self.ln_mix_g[li:li + 1, :])
            self.ld_row(lnb, lnb[:], self.ln_mix_b[li:li + 1, :])
            nw = fw.sb("nw", [128, 2048], F32)
            self.ld_row(nw, nw[:], (self.ssd_norm_w if ssd else self.ret_gn_w)[j:j + 1, :])
            if ssd:
                dsk = fw.sb("dsk", [128, 32], F32)
                self.ld_row(dsk, dsk[:], self.ssd_d_skip[j:j + 1, :])
            else:
                nb_ = fw.sb("nb", [128, 2048], F32)
                self.ld_row(nb_, nb_[:], self.ret_gn_b[j:j + 1, :])
            S = fw.sb("S", [128, KC, 2048], F32)
            Sb = fw.sb("Sb", [128, KC, 2048], BF16)
            qt = fw.sb("qt", [128, G * KC, 128], BF16)
            kt = fw.sb("kt", [128, G * KC, 128], BF16)
            ktm = fw.sb("ktm", [128, G * dk], BF16)
            vt = fw.sb("vt", [128, 2048], BF16)
            la = fw.sb("la", [128, 64], F32)
            dt = fw.sb("dt", [128, 64], F32)
            acs = fw.sb("acs", [128, 2 * H], F32)
            nacs = fw.sb("nacs", [128, H], F32)
            ea = fw.sb("ea", [128, H], F32)
            wend = fw.sb("wend", [128, H], F32)
            cd = fw.sb("cd", [128, H], F32)
            E = fw.sb("E", [128, H, 128], F32)
            lt = fw.sb("lt", [128, Hg, 128], F32)
            M = fw.sb("M", [128, Hg, 128], BF16)
            cbm = fw.sb("cbm", [128, G, 128], F32)
            xdt = fw.sb("xdt", [128, 2048], BF16) if ssd else vt
            xe = fw.sb("xe", [128, 2048], BF16)
            yc = fw.sb("yc", [128, 2048], F32)
            tmp = fw.sb("tmp", [128, 512], F32)
            yfl = fw.sb("yfl", [128, 2048], F32)
            gt = fw.sb("gt", [128, 2048], F32)
            yb = fw.sb("yb", [128, 2048], BF16)
            yT = fw.sb("yT", [128, 16, 128], BF16)
            st6 = fw.sb("st6", [128, 4, 6], F32)
            mv = fw.sb("mv", [128, 4, 2], F32)
            ss = fw.sb("ss", [128, 4], F32)
            rstd = fw.sb("rstd", [128, 4], F32)
            xt = fw.sb("xt", [128, D], F32)
            t1 = fw.sb("t1", [128, D], F32)
            t2 = fw.sb("t2", [128, D], F32)
            st2 = fw.sb("st2", [128, 2, 6], F32)
            mv2 = fw.sb("mv2", [128, 2], F32)
            rs2 = fw.sb("rs2", [128, 1], F32)
            psm = fw.ps("psm", [128, 512], F32)
            pcb = fw.ps("pcb", [128, 512], F32)
            pbc = [fw.ps("pbc%d" % i, [128, 512], F32) for i in range(2)]
            pA = fw.ps("pA", [128, 1024], F32)
            pst = fw.ps("pst", [128, 512], F32)
            ptr = fw.ps("ptr", [128, 8, 128], BF16)

            qtv = self.QT.rearrange("g k p t -> p g k t")
            ktv = self.KT.rearrange("g k p t -> p g k t")

            def decay_prep(dirn):
                lad = la[:, dirn * 32:dirn * 32 + H] if ssd else la[:, 0:H]
                self.P(lambda e: e.matmul(psm[:, 0:H], lhsT=tri[:], rhs=lad, start=True, stop=True), [tri, la], [psm])
                self.P(lambda e: e.matmul(psm[:, H:2 * H], lhsT=ones[:], rhs=lad, start=True, stop=True), [ones, la], [psm])
                self.V(lambda e: e.tensor_copy(out=acs[:], in_=psm[:, 0:2 * H]), [psm], [acs])
                self.V(lambda e: e.tensor_scalar_mul(out=nacs[:], in0=acs[:, 0:H], scalar1=-1.0), [acs], [nacs])
                self.A(lambda e: e.activation(out=ea[:], in_=acs[:, 0:H], func=AF.Exp), [acs], [ea])
                self.V(lambda e: e.tensor_tensor(out=wend[:], in0=acs[:, H:2 * H], in1=acs[:, 0:H], op=ALU.subtract), [acs], [wend])
                self.A(lambda e: e.activation(out=wend[:], in_=wend[:], func=AF.Exp), [wend], [wend])
                self.A(lambda e: e.activation(out=cd[:], in_=acs[:, H:2 * H], func=AF.Exp), [acs], [cd])
                for g in range(G):
                    hs = slice(g * Hg, (g + 1) * Hg)
                    ladg = la[:, dirn * 32 + g * Hg:dirn * 32 + (g + 1) * Hg] if ssd else la[:, g:g + 1]
                    self.G(lambda e, ladg=ladg: e.tensor_tensor(out=lt[:], in0=bcast(tri[:].unsqueeze(1), [128, Hg, 128]),
                                                                in1=bcast(ladg.unsqueeze(2), [128, Hg, 128]), op=ALU.mult),
                           [tri, la], [lt])
                    for q in range((Hg + 3) // 4):
                        nh = min(4, Hg - q * 4)
                        pb = pbc[q % 2]
                        self.P(lambda e, q=q, nh=nh, pb=pb: e.matmul(pb[:, 0:nh * 128], lhsT=ones[:], rhs=lt[:, q * 4:q * 4 + nh, :],
                                                                     start=True, stop=True), [ones, lt], [pb])
                        for hh in range(nh):
                            h = g * Hg + q * 4 + hh
                            self.A(lambda e, h=h, hh=hh, pb=pb: e.activation(out=E[:, h, :], in_=pb[:, hh * 128:(hh + 1) * 128], func=AF.Exp,
                                                                            bias=nacs[:, h:h + 1], scale=1.0), [pb, nacs], [E])

            for dirn in range(2):
                order = list(range(NCH)) if dirn == 0 else [1, 0] + list(range(NCH - 1, 1, -1))
                self.ld(tri, tri[:], self.c_trif[dirn])
                self.V(lambda e: e.memset(S[:], 0.0), [], [S])
                self.G(lambda e: e.memset(Sb[:], 0.0), [], [Sb])
                if not ssd:
                    self.ld_row(la, la[:, 0:4], self.ret_decay[j, dirn:dirn + 1, :])
                    self.A(lambda e: e.activation(out=la[:, 0:4], in_=la[:, 0:4], func=AF.Exp, scale=-1.0), [la], [la])
                    self.A(lambda e: e.activation(out=la[:, 0:4], in_=la[:, 0:4], func=AF.Ln, bias=1.0), [la], [la])
                    self.V(lambda e: e.tensor_scalar_mul(out=la[:, 0:4], in0=la[:, 0:4], scalar1=-1.0), [la], [la])
                    decay_prep(dirn)
                for c in order:
                    v = 1 if c < NCTX else 0
                    cs_ = slice(c * 128, (c + 1) * 128)
                    self.ld(qt, qt[:].rearrange("p (g k) t -> p g k t", g=G), qtv[:, :, 0:KC, cs_])
                    self.ld(kt, kt[:].rearrange("p (g k) t -> p g k t", g=G), ktv[:, :, 0:KC, cs_])
                    self.ld(ktm, ktm[:], self.Ktm[cs_, 0:G * dk])
                    self.ld(vt, vt[:], self.Vtm[cs_, :])
                    if ssd:
                        self.ld(la, la[:], self.latm[cs_, :])
                        self.ld(dt, dt[:], self.dttm[cs_, :])
                        decay_prep(dirn)
                        self.V(lambda e: e.tensor_tensor(out=xdt[:].rearrange("p (h d) -> p h d", h=H), in0=vt[:].rearrange("p (h d) -> p h d", h=H),
                                                         in1=bcast(dt[:, dirn * 32:dirn * 32 + 32].unsqueeze(2), [128, H, dv]), op=ALU.mult),
                               [vt, dt], [xdt])
                    self.G(lambda e: e.tensor_tensor(out=xe[:].rearrange("p (h d) -> p h d", h=H), in0=xdt[:].rearrange("p (h d) -> p h d", h=H),
                                                     in1=bcast(wend[:].unsqueeze(2), [128, H, dv]), op=ALU.mult), [xdt, wend], [xe])
                    for g in range(G):
                        for kc in range(KC):
                            self.P(lambda e, g=g, kc=kc: e.matmul(pcb[:, g * 128:(g + 1) * 128], lhsT=kt[:, g * KC + kc, :], rhs=qt[:, g * KC + kc, :],
                                                                  start=(kc == 0), stop=(kc == KC - 1)), [kt, qt], [pcb])
                    self.V(lambda e: e.tensor_tensor(out=cbm[:], in0=pcb[:].rearrange("p (g l) -> p g l", g=G),
                                                     in1=bcast(tri[:].unsqueeze(1), [128, G, 128]), op=ALU.mult), [pcb, tri], [cbm])
                    for g in range(G):
                        gs = slice(g * 512, (g + 1) * 512)
                        self.V(lambda e, g=g: e.scalar_tensor_tensor(out=M[:], in0=E[:, g * Hg:(g + 1) * Hg, :], scalar=1.0,
                                                                     in1=bcast(cbm[:, g:g + 1, :], [128, Hg, 128]), op0=ALU.min, op1=ALU.mult),
                               [E, cbm], [M])
                        for hh in range(Hg):
                            h = g * Hg + hh
                            self.P(lambda e, h=h, hh=hh: e.matmul(pA[:, h * dv:(h + 1) * dv] if False else pA[:, (h * dv) % 512:(h * dv) % 512 + dv],
                                                                  lhsT=M[:, hh, :], rhs=xdt[:, h * dv:(h + 1) * dv], start=True, stop=True),
                                   [M, xdt], [pA])
                        for kc in range(KC):
                            self.P(lambda e, g=g, kc=kc, gs=gs: e.matmul(pA[:, 512:1024], lhsT=qt[:, g * KC + kc, :], rhs=Sb[:, kc, gs],
                                                                         start=(kc == 0), stop=(kc == KC - 1)), [qt, Sb], [pA])
                        self.V(lambda e, g=g: e.tensor_tensor(out=tmp[:].rearrange("p (h d) -> p h d", h=Hg),
                                                              in0=pA[:, 512:1024].rearrange("p (h d) -> p h d", h=Hg),
                                                              in1=bcast(ea[:, g * Hg:(g + 1) * Hg].unsqueeze(2), [128, Hg, dv]), op=ALU.mult),
                               [pA, ea], [tmp])
                        self.V(lambda e, gs=gs: e.tensor_tensor(out=yc[:, gs], in0=tmp[:], in1=pA[:, 0:512], op=ALU.add), [tmp, pA], [yc])
                        for kc in range(KC):
                            self.P(lambda e, g=g, kc=kc, gs=gs: e.matmul(pst[:], lhsT=ktm[:, g * dk + kc * 128:g * dk + (kc + 1) * 128], rhs=xe[:, gs],
                                                                         start=True, stop=True), [ktm, xe], [pst])
                            self.V(lambda e, g=g, kc=kc, gs=gs: e.tensor_tensor(out=S[:, kc, gs].rearrange("p (h d) -> p h d", h=Hg),
                                                                                in0=S[:, kc, gs].rearrange("p (h d) -> p h d", h=Hg),
                                                                                in1=bcast(cd[:, g * Hg:(g + 1) * Hg].unsqueeze(2), [128, Hg, dv]),
                                                                                op=ALU.mult), [S, cd], [S])
                            self.V(lambda e, kc=kc, gs=gs: e.tensor_tensor(out=S[:, kc, gs], in0=S[:, kc, gs], in1=pst[:], op=ALU.add), [S, pst], [S])
                            self.A(lambda e, kc=kc, gs=gs: e.copy(out=Sb[:, kc, gs], in_=S[:, kc, gs]), [S], [Sb])
                    if dirn == 0:
                        self.st(self.yf[cs_, :], yc, yc[:])
                        continue
                    self.ld(yfl, yfl[:], self.yf[cs_, :])
                    self.ld(gt, gt[:], self.gate[cs_, :])
                    self.V(lambda e: e.tensor_tensor(out=yc[:], in0=yc[:], in1=yfl[:], op=ALU.add), [yc, yfl], [yc])
                    if ssd:
                        self.G(lambda e: e.tensor_tensor(out=yfl[:].rearrange("p (h d) -> p h d", h=H), in0=vt[:].rearrange("p (h d) -> p h d", h=H),
                                                         in1=bcast(dsk[:].unsqueeze(2), [128, H, dv]), op=ALU.mult), [vt, dsk], [yfl])
                        self.V(lambda e: e.tensor_tensor(out=yc[:], in0=yc[:], in1=yfl[:], op=ALU.add), [yc, yfl], [yc])
                        self.A(lambda e: e.activation(out=gt[:], in_=gt[:], func=AF.Silu), [gt], [gt])
                        self.V(lambda e: e.tensor_tensor(out=yc[:], in0=yc[:], in1=gt[:], op=ALU.mult), [yc, gt], [yc])
                        for g in range(4):
                            self.V(lambda e, g=g: e.bn_stats(out=st6[:, g, :], in_=yc[:, g * 512:(g + 1) * 512]), [yc], [st6])
                            self.V(lambda e, g=g: e.bn_aggr(out=mv[:, g, :], in_=st6[:, g, :]), [st6], [mv])
                        self.V(lambda e: e.tensor_tensor(out=ss[:], in0=mv[:, :, 0], in1=mv[:, :, 0], op=ALU.mult), [mv], [ss])
                        self.V(lambda e: e.tensor_tensor(out=ss[:], in0=ss[:], in1=mv[:, :, 1], op=ALU.add), [ss, mv], [ss])
                        self.A(lambda e: e.activation(out=ss[:], in_=ss[:], func=AF.Sqrt, bias=EPS), [ss], [ss])
                        self.V(lambda e: e.reciprocal(out=rstd[:], in_=ss[:]), [ss], [rstd])
                        for g in range(4):
                            gs = slice(g * 512, (g + 1) * 512)
                            self.V(lambda e, g=g, gs=gs: e.scalar_tensor_tensor(out=yb[:, gs], in0=yc[:, gs], scalar=rstd[:, g:g + 1], in1=nw[:, gs],
                                                                                op0=ALU.mult, op1=ALU.mult), [yc, rstd, nw], [yb])
                    else:
                        for g in range(4):
                            self.V(lambda e, g=g: e.bn_stats(out=st6[:, g, :], in_=yc[:, g * 512:(g + 1) * 512]), [yc], [st6])
                            self.V(lambda e, g=g: e.bn_aggr(out=mv[:, g, :], in_=st6[:, g, :]), [st6], [mv])
                        self.A(lambda e: e.activation(out=ss[:], in_=mv[:, :, 1], func=AF.Sqrt, bias=EPS), [mv], [ss])
                        self.V(lambda e: e.reciprocal(out=rstd[:], in_=ss[:]), [ss], [rstd])
                        self.A(lambda e: e.activation(out=gt[:], in_=gt[:], func=AF.Silu), [gt], [gt])
                        for g in range(4):
                            gs = slice(g * 512, (g + 1) * 512)
                            self.V(lambda e, g=g, gs=gs: e.tensor_scalar(out=yc[:, gs], in0=yc[:, gs], scalar1=mv[:, g, 0:1], scalar2=rstd[:, g:g + 1],
                                                                         op0=ALU.subtract, op1=ALU.mult), [yc, mv, rstd], [yc])
                        self.G(lambda e: e.tensor_tensor(out=yc[:], in0=yc[:], in1=nw[:], op=ALU.mult), [yc, nw], [yc])
                        self.V(lambda e: e.tensor_tensor(out=yc[:], in0=yc[:], in1=nb_[:], op=ALU.add), [yc, nb_], [yc])
                        self.V(lambda e: e.tensor_tensor(out=yb[:], in0=yc[:], in1=gt[:], op=ALU.mult), [yc, gt], [yb])
                    for half in range(2):
                        for kk in range(8):
                            k = half * 8 + kk
                            self.P(lambda e, k=k, kk=kk: e.transpose(ptr[:, kk, :], yb[:, k * 128:(k + 1) * 128], identb[:]), [yb, identb], [ptr])
                        self.A(lambda e, half=half: e.copy(out=yT[:, half * 8:(half + 1) * 8, :], in_=ptr[:]), [ptr], [yT])
                    for nn in range(2):
                        for k in range(16):
                            self.P(lambda e, k=k, nn=nn: e.matmul(pA[:, nn * 512:(nn + 1) * 512], lhsT=yT[:, k, :], rhs=Wo[:, k, nn * 512:(nn + 1) * 512],
                                                                  start=(k == 0), stop=(k == 15)), [yT, Wo], [pA])
                    self.ld(xt, xt[:], xcur[cs_, :])
                    self.V(lambda e: e.tensor_tensor(out=t1[:], in0=pA[:], in1=g1[v][:], op=ALU.mult), [pA, g1[v]], [t1])
                    self.V(lambda e: e.scalar_tensor_tensor(out=t2[:], in0=xt[:], scalar=ALPHA, in1=t1[:], op0=ALU.mult, op1=ALU.add), [xt, t1], [t2])
                    self.layer_norm(t2, t1, st2, mv2, rs2, lng, lnb)
                    self.st(xnext[cs_, :], t1, t1[:])
                    self.G(lambda e: e.tensor_tensor(out=t2[:], in0=t1[:], in1=sc2[v][:], op=ALU.mult), [t1, sc2[v]], [t2])
                    self.V(lambda e: e.tensor_tensor(out=t2[:], in0=t2[:], in1=sh2[v][:], op=ALU.add), [t2, sh2[v]], [t2])
                    self.st(self.tok[cs_, :], t2, t2[:])
                if dirn == 0:
                    fw.barrier()

    def layer_norm(self, xin_t, out_t, st2, mv2, rs2, lng, lnb):
        for hf in range(2):
            self.V(lambda e, hf=hf: e.bn_stats(out=st2[:, hf, :], in_=xin_t[:, hf * 512:(hf + 1) * 512]), [xin_t], [st2])
        self.V(lambda e: e.bn_aggr(out=mv2[:], in_=st2[:].rearrange("p a b -> p (a b)")), [st2], [mv2])
        self.A(lambda e: e.activation(out=rs2[:], in_=mv2[:, 1:2], func=AF.Sqrt, bias=EPS), [mv2], [rs2])
        self.V(lambda e: e.reciprocal(out=rs2[:], in_=rs2[:]), [rs2], [rs2])
        self.V(lambda e: e.tensor_scalar(out=out_t[:], in0=xin_t[:], scalar1=mv2[:, 0:1], scalar2=rs2[:, 0:1],
                                         op0=ALU.subtract, op1=ALU.mult), [xin_t, mv2, rs2], [out_t])
        self.G(lambda e: e.tensor_tensor(out=out_t[:], in0=out_t[:], in1=lng[:], op=ALU.mult), [out_t, lng], [out_t])
        self.V(lambda e: e.tensor_tensor(out=out_t[:], in0=out_t[:], in1=lnb[:], op=ALU.add), [out_t, lnb], [out_t])


def host_consts():
    bf = ml_dtypes.bfloat16
    c = {}
    c["c_identb"] = np.eye(128, dtype=np.float32).astype(bf)
    c["c_identf"] = np.eye(128, dtype=np.float32)
    s = np.arange(128)
    c["c_trif"] = np.stack([(s[:, None] <= s[None, :]), (s[:, None] >= s[None, :])]).astype(np.float32)
    c["c_ones"] = np.ones((128, 128), np.float32)
    c["c_slow"] = (s[:, None] < s[None, :]).astype(np.float32).astype(bf)
    n_freq = 64
    inv_freq = (10000.0 ** (-np.arange(n_freq, dtype=np.float32) / np.float32(n_freq))).astype(np.float32)
    pos = np.arange(4096)
    rows = (pos // 64).astype(np.float32)
    cols = (pos % 64).astype(np.float32)
    ang = np.concatenate([rows[:, None] * inv_freq[None, :], cols[:, None] * inv_freq[None, :]], -1).astype(np.float32)
    cos = np.ones((T, 128), np.float32)
    sin = np.zeros((T, 128), np.float32)
    cos[256:] = np.cos(ang)
    sin[256:] = np.sin(ang)
    c["c_rope"] = np.ascontiguousarray(np.stack([cos.T, sin.T])).astype(np.float32)
    c["c_iota"] = np.broadcast_to(np.arange(512, dtype=np.float32)[None, :], (128, 512)).copy()
    c["c_pidx"] = (np.arange(12)[None, :] * 128 + np.arange(128)[:, None]).astype(np.float32)
    return c


def host_inputs(inp, b):
    f = np.float32
    m = {}
    m["xin"] = np.ascontiguousarray(np.concatenate([inp["ctx"][b], inp["x"][b]], 0)).astype(f)
    cv = np.stack([inp["c"][b].reshape(8, 128).T, inp["c_ctx"].reshape(8, 128).T], -1)
    m["cvec"] = np.ascontiguousarray(cv).astype(f)
    m["mod_w"] = inp["mod_w"]
    m["mod_b"] = inp["mod_b"]
    m["ssd_in_w"] = inp["ssd_in_w"]
    cw = inp["ssd_conv_w"]
    m["convw"] = np.ascontiguousarray(cw.reshape(2, 5, 24, 128).transpose(0, 3, 2, 1)).astype(f)
    m["convb"] = np.ascontiguousarray(inp["ssd_conv_b"].reshape(2, 24, 128).transpose(0, 2, 1)).astype(f)
    m["ssd_dt_bias"] = np.ascontiguousarray(inp["ssd_dt_bias"].reshape(2, 64))
    m["ssd_a_log"] = np.ascontiguousarray(inp["ssd_a_log"].reshape(2, 64))
    m["ssd_d_skip"] = inp["ssd_d_skip"]
    m["ssd_norm_w"] = inp["ssd_norm_w"]
    m["ssd_out_w"] = inp["ssd_out_w"]
    m["ret_in_w"] = inp["ret_in_w"]
    m["ret_decay_logit"] = inp["ret_decay_logit"]
    m["ret_gn_w"] = inp["ret_gn_w"]
    m["ret_gn_b"] = inp["ret_gn_b"]
    m["ret_out_w"] = inp["ret_out_w"]
    for k in ("ln_mix_g", "ln_mix_b", "ln_ffn_g", "ln_ffn_b"):
        m[k] = inp[k]
    m["moe_rw"] = np.ascontiguousarray(np.concatenate([inp["moe_group_w"], inp["moe_expert_w"]], -1)).astype(f)
    m["moe_rb"] = np.ascontiguousarray(np.concatenate([inp["moe_group_b"], inp["moe_expert_b"]], -1)).astype(f)
    m["moe_w_gate_up"] = inp["moe_w_gate_up"].reshape(DEPTH * 32 * 128, 8 * D)
    m["moe_w_down"] = inp["moe_w_down"].reshape(DEPTH * 32 * 128, 4 * D)
    return m


def _phase_moe(self, li, xcur, xnext, final):
    fw = self.fw
    NT = NCH
    with fw.scope():
        DI = fw.sb("DI", [128, 2 * NT], I32)
        Wt = fw.sb("Wt", [128, 2 * NT], F32)
        IGU = fw.sb("IGU", [128, NB], I32)
        with fw.scope():
            identf = fw.sb("identf", [128, 128], F32)
            self.ld(identf, identf[:], self.c_identf)
            slow = fw.sb("slow", [128, 128], BF16)
            self.ld(slow, slow[:], self.c_slow)
            onesf = fw.sb("onesf", [128, 128], F32)
            self.ld(onesf, onesf[:], self.c_ones)
            onesb = fw.sb("onesb", [128, 128], BF16)
            self.V(lambda e: e.tensor_copy(out=onesb[:], in_=onesf[:]), [onesf], [onesb])
            iota = fw.sb("iota", [128, 512], F32)
            self.ld(iota, iota[:], self.c_iota)
            pidx = fw.sb("pidx", [128, 12], F32)
            self.ld(pidx, pidx[:], self.c_pidx)
            rw = fw.sb("rw", [128, 8, 36], F32)
            self.ld(rw, rw[:], self.moe_rw[li].rearrange("(k p) e -> p k e", p=128))
            rb = fw.sb("rb", [128, 36], F32)
            self.ld_row(rb, rb[:], self.moe_rb[li:li + 1, :])
            OH = fw.sb("OH", [128, 2 * NT, 32], F32)
            SL = fw.sb("SL", [128, 2 * NT], F32)
            Acum = fw.sb("Acum", [128, 32], F32)
            Acb = fw.sb("Acb", [128, 32], BF16)
            self.V(lambda e: e.memset(Acum[:], 0.0), [], [Acum])
            self.V(lambda e: e.memset(Acb[:], 0.0), [], [Acb])
            tk = fw.sb("tk", [128, D], F32)
            tkT = fw.sb("tkT", [128, 8, 128], F32)
            L = fw.sb("L", [128, 36], F32)
            gmax = fw.sb("gmax", [128, 1], F32)
            ngmax = fw.sb("ngmax", [128, 1], F32)
            goh = fw.sb("goh", [128, 4], F32)
            pen = fw.sb("pen", [128, 4], F32)
            ex = fw.sb("ex", [128, 4], F32)
            gs = fw.sb("gs", [128, 1], F32)
            em = fw.sb("em", [128, 32], F32)
            em2 = fw.sb("em2", [128, 32], F32)
            m1 = fw.sb("m1", [128, 1], F32)
            m2 = fw.sb("m2", [128, 1], F32)
            dd = fw.sb("dd", [128, 1], F32)
            den = fw.sb("den", [128, 1], F32)
            A_ = fw.sb("A_", [128, 32], F32)
            Ab = fw.sb("Ab", [128, 32], BF16)
            Pp = fw.sb("Pp", [128, 32], F32)
            junk = fw.sb("junk", [128, 32], F32)
            pT = fw.ps("pT", [128, 8, 128], F32)
            plog = fw.ps("plog", [128, 512], F32)
            pP = fw.ps("pP", [128, 512], F32)
            for c in range(NT):
                cs_ = slice(c * 128, (c + 1) * 128)
                self.ld(tk, tk[:], self.tok[cs_, :])
                for k in range(8):
                    self.P(lambda e, k=k: e.transpose(pT[:, k, :], tk[:, k * 128:(k + 1) * 128], identf[:]), [tk, identf], [pT])
                self.A(lambda e: e.copy(out=tkT[:], in_=pT[:]), [pT], [tkT])
                for k in range(8):
                    self.P(lambda e, k=k: e.matmul(plog[:, 0:36], lhsT=tkT[:, k, :], rhs=rw[:, k, :], start=(k == 0), stop=(k == 7)),
                           [tkT, rw], [plog])
                self.V(lambda e: e.tensor_tensor(out=L[:], in0=plog[:, 0:36], in1=rb[:], op=ALU.add), [plog, rb], [L])
                self.V(lambda e: e.reduce_max(out=gmax[:], in_=L[:, 0:4], axis=AX.X), [L], [gmax])
                self.V(lambda e: e.tensor_scalar(out=goh[:], in0=L[:, 0:4], scalar1=gmax[:, 0:1], scalar2=None, op0=ALU.is_equal), [L, gmax], [goh])
                self.V(lambda e: e.tensor_scalar_mul(out=ngmax[:], in0=gmax[:], scalar1=-1.0), [gmax], [ngmax])
                self.A(lambda e: e.activation(out=ex[:], in_=L[:, 0:4], func=AF.Exp, bias=ngmax[:, 0:1], scale=1.0), [L, ngmax], [ex])
                self.V(lambda e: e.reduce_sum(out=gs[:], in_=ex[:], axis=AX.X), [ex], [gs])
                self.V(lambda e: e.reciprocal(out=gs[:], in_=gs[:]), [gs], [gs])
                self.V(lambda e: e.tensor_scalar(out=pen[:], in0=goh[:], scalar1=1e30, scalar2=-1e30, op0=ALU.mult, op1=ALU.add), [goh], [pen])
                self.V(lambda e: e.tensor_tensor(out=em[:].rearrange("p (g j) -> p g j", g=4), in0=L[:, 4:36].rearrange("p (g j) -> p g j", g=4),
                                                 in1=bcast(pen[:].unsqueeze(2), [128, 4, 8]), op=ALU.add), [L, pen], [em])
                self.V(lambda e: e.reduce_max(out=m1[:], in_=em[:], axis=AX.X), [em], [m1])
                o1 = OH[:, 2 * c, :]
                o2 = OH[:, 2 * c + 1, :]
                self.V(lambda e, o1=o1: e.tensor_scalar(out=o1, in0=em[:], scalar1=m1[:, 0:1], scalar2=None, op0=ALU.is_equal), [em, m1], [OH])
                self.V(lambda e, o1=o1: e.scalar_tensor_tensor(out=em2[:], in0=o1, scalar=-1e30, in1=em[:], op0=ALU.mult, op1=ALU.add), [OH, em], [em2])
                self.V(lambda e: e.reduce_max(out=m2[:], in_=em2[:], axis=AX.X), [em2], [m2])
                self.V(lambda e, o2=o2: e.tensor_scalar(out=o2, in0=em2[:], scalar1=m2[:, 0:1], scalar2=None, op0=ALU.is_equal), [em2, m2], [OH])
                self.V(lambda e: e.tensor_tensor(out=dd[:], in0=m2[:], in1=m1[:], op=ALU.subtract), [m1, m2], [dd])
                self.A(lambda e: e.activation(out=dd[:], in_=dd[:], func=AF.Exp), [dd], [dd])
                self.V(lambda e: e.tensor_scalar_add(out=den[:], in0=dd[:], scalar1=1.0), [dd], [den])
                self.V(lambda e: e.reciprocal(out=den[:], in_=den[:]), [den], [den])
                self.V(lambda e, c=c: e.tensor_tensor(out=Wt[:, 2 * c:2 * c + 1], in0=den[:], in1=gs[:], op=ALU.mult), [den, gs], [Wt])
                self.V(lambda e, c=c: e.tensor_tensor(out=Wt[:, 2 * c + 1:2 * c + 2], in0=Wt[:, 2 * c:2 * c + 1], in1=dd[:], op=ALU.mult), [Wt, dd], [Wt])
                self.V(lambda e, o1=o1, o2=o2: e.tensor_tensor(out=A_[:], in0=o1, in1=o2, op=ALU.add), [OH], [A_])
                self.V(lambda e: e.tensor_copy(out=Ab[:], in_=A_[:]), [A_], [Ab])
                self.P(lambda e: e.matmul(pP[:, 0:32], lhsT=slow[:], rhs=Ab[:], start=True, stop=False), [slow, Ab], [pP])
                self.P(lambda e: e.matmul(pP[:, 0:32], lhsT=onesb[:], rhs=Acb[:], start=False, stop=True), [onesb, Acb], [pP])
                self.V(lambda e: e.tensor_copy(out=Pp[:], in_=pP[:, 0:32]), [pP], [Pp])
                for k, ok in enumerate((o1, o2)):
                    self.V(lambda e, ok=ok: e.tensor_tensor(out=junk[:], in0=ok, in1=Pp[:], op=ALU.mult), [OH, Pp], [junk])
                    self.V(lambda e, c=c, k=k: e.reduce_sum(out=SL[:, 2 * c + k:2 * c + k + 1], in_=junk[:], axis=AX.X), [junk], [SL])
                self.V(lambda e: e.tensor_tensor(out=Acum[:], in0=Acum[:], in1=A_[:], op=ALU.add), [Acum, A_], [Acum])
                self.V(lambda e: e.tensor_copy(out=Acb[:], in_=Acum[:]), [Acum], [Acb])
            cnt = fw.sb("cnt", [128, 32], F32)
            self.P(lambda e: e.matmul(pP[:, 0:32], lhsT=onesb[:], rhs=Acb[:], start=True, stop=True), [onesb, Acb], [pP])
            self.V(lambda e: e.tensor_copy(out=cnt[:], in_=pP[:, 0:32]), [pP], [cnt])
            thr = fw.sb("thr", [128, 34], F32)
            self.V(lambda e: e.tensor_scalar_mul(out=thr[:], in0=iota[:, 0:34], scalar1=128.0), [iota], [thr])
            cmp = fw.sb("cmp", [128, 32, 34], F32)
            self.V(lambda e: e.tensor_tensor(out=cmp[:], in0=bcast(cnt[:].unsqueeze(2), [128, 32, 34]), in1=bcast(thr[:].unsqueeze(1), [128, 32, 34]),
                                             op=ALU.is_gt), [cnt, thr], [cmp])
            nblk = fw.sb("nblk", [128, 32], F32)
            self.V(lambda e: e.reduce_sum(out=nblk[:], in_=cmp[:], axis=AX.X), [cmp], [nblk])
            pa = fw.sb("pa", [128, 32], F32)
            pb_ = fw.sb("pb", [128, 32], F32)
            self.V(lambda e: e.tensor_copy(out=pa[:], in_=nblk[:]), [nblk], [pa])
            cur, oth = pa, pb_
            for sft in (1, 2, 4, 8, 16):
                self.V(lambda e, cur=cur, oth=oth: e.tensor_copy(out=oth[:], in_=cur[:]), [cur], [oth])
                self.V(lambda e, cur=cur, oth=oth, sft=sft: e.tensor_tensor(out=oth[:, sft:32], in0=cur[:, sft:32], in1=cur[:, 0:32 - sft], op=ALU.add),
                       [cur], [oth])
                cur, oth = oth, cur
            pend = cur
            pstart = fw.sb("pstart", [128, 32], F32)
            self.V(lambda e: e.tensor_tensor(out=pstart[:], in0=pend[:], in1=nblk[:], op=ALU.subtract), [pend, nblk], [pstart])
            self.V(lambda e: e.tensor_scalar_mul(out=pstart[:], in0=pstart[:], scalar1=128.0), [pstart], [pstart])
            big = fw.sb("big", [128, 2 * NT, 32], F32)
            self.V(lambda e: e.tensor_tensor(out=big[:], in0=OH[:], in1=bcast(pstart[:].unsqueeze(1), [128, 2 * NT, 32]), op=ALU.mult), [OH, pstart], [big])
            dst = fw.sb("dstf", [128, 2 * NT], F32)
            self.V(lambda e: e.reduce_sum(out=dst[:], in_=big[:], axis=AX.X), [big], [dst])
            self.V(lambda e: e.tensor_tensor(out=dst[:], in0=dst[:], in1=SL[:], op=ALU.add), [dst, SL], [dst])
            self.V(lambda e: e.tensor_copy(out=DI[:], in_=dst[:]), [dst], [DI])
            cmp2 = fw.sb("cmp2", [128, NB, 32], F32)
            self.V(lambda e: e.tensor_tensor(out=cmp2[:], in0=bcast(pend[:].unsqueeze(1), [128, NB, 32]), in1=bcast(iota[:, 0:NB].unsqueeze(2), [128, NB, 32]),
                                             op=ALU.is_le), [pend, iota], [cmp2])
            be = fw.sb("be", [128, NB], F32)
            self.V(lambda e: e.reduce_sum(out=be[:], in_=cmp2[:], axis=AX.X), [cmp2], [be])
            self.V(lambda e: e.tensor_scalar_min(out=be[:], in0=be[:], scalar1=31.0), [be], [be])
            same = fw.sb("same", [128, NB], F32)
            self.V(lambda e: e.memset(same[:], 0.0), [], [same])
            self.V(lambda e: e.tensor_tensor(out=same[:, 2:NB], in0=be[:, 2:NB], in1=be[:, 0:NB - 2], op=ALU.is_equal), [be], [same])
            self.V(lambda e: e.tensor_scalar(out=be[:], in0=be[:], scalar1=128.0, scalar2=float(li * 32 * 128), op0=ALU.mult, op1=ALU.add), [be], [be])
            self.V(lambda e: e.scalar_tensor_tensor(out=be[:], in0=same[:], scalar=1.0e6, in1=be[:], op0=ALU.mult, op1=ALU.add), [same, be], [be])
            self.V(lambda e: e.tensor_tensor(out=be[:], in0=be[:], in1=bcast(pidx[:, 0:1], [128, NB]), op=ALU.add), [be, pidx], [be])
            self.V(lambda e: e.tensor_copy(out=IGU[:], in_=be[:]), [be], [IGU])
        with fw.scope():
            tk = fw.sb("tk", [128, D], F32)
            tkb = fw.sb("tkb", [128, D], BF16)
            for c in range(NT):
                self.ld(tk, tk[:], self.tok[c * 128:(c + 1) * 128, :])
                self.V(lambda e: e.tensor_copy(out=tkb[:], in_=tk[:]), [tk], [tkb])
                for k in range(2):
                    col = 2 * c + k
                    self.fw.dma("gpsimd", lambda e, col=col: e.indirect_dma_start(
                        out=self.xbuf, out_offset=bass.IndirectOffsetOnAxis(ap=DI[:, col:col + 1], axis=0), in_=tkb[:, :], in_offset=None),
                        reads=[tkb, DI])
        with fw.scope():
            identb = fw.sb("identb", [128, 128], BF16)
            self.ld(identb, identb[:], self.c_identb)
            xb = [fw.sb("xb%d" % i, [128, D], BF16) for i in range(2)]
            xT = [fw.sb("xT%d" % i, [128, 8, 128], BF16) for i in range(2)]
            g32 = [fw.sb("g32_%d" % i, [128, 8, D], F32) for i in range(2)]
            d32 = [fw.sb("d32_%d" % i, [128, 4, D], F32) for i in range(2)]
            gbf = [fw.sb("gbf_%d" % i, [128, 8, D], BF16) for i in range(2)]
            dbf = [fw.sb("dbf_%d" % i, [128, 4, D], BF16) for i in range(2)]
            sg = fw.sb("sg", [128, 4, 128], F32)
            hT = fw.sb("hT", [128, 4, 128], BF16)
            ob = fw.sb("ob", [128, D], F32)
            ptr = fw.ps("ptr", [128, 8, 128], BF16)
            pH = [fw.ps("pH%d" % i, [128, 8, 128], F32) for i in range(2)]
            pO = fw.ps("pO", [128, D], F32)
            if getattr(self, "_bcreg", None) is None:
                self._bcreg = self.nc.gpsimd.alloc_register("bcreg")
                self.nc.gpsimd.reg_mov(self._bcreg, DEPTH * 32 * 128 - 1)
            bcreg = self._bcreg
            for b in range(NB):
                i = b % 2
                self.ld(xb[i], xb[i][:], self.xbuf[b * 128:(b + 1) * 128, :])
                idx = IGU[:, b:b + 1]
                self.fw.dma("gpsimd", lambda e, i=i, idx=idx: e.indirect_dma_start(
                    out=g32[i][:].rearrange("p k n -> p (k n)"), out_offset=None, in_=self.moe_wgu,
                    in_offset=bass.IndirectOffsetOnAxis(ap=idx, axis=0), bounds_check=bcreg, oob_is_err=False),
                    reads=[IGU], writes=[g32[i]])
                self.fw.dma("gpsimd", lambda e, i=i, idx=idx: e.indirect_dma_start(
                    out=d32[i][:].rearrange("p k n -> p (k n)"), out_offset=None, in_=self.moe_wd,
                    in_offset=bass.IndirectOffsetOnAxis(ap=idx, axis=0), bounds_check=bcreg, oob_is_err=False),
                    reads=[IGU], writes=[d32[i]])
                xbv = xb[i][:].rearrange("s (p k) -> s k p", k=8)
                for k in range(8):
                    self.P(lambda e, k=k, xbv=xbv: e.transpose(ptr[:, k, :], xbv[:, k, :], identb[:]), [xb[i], identb], [ptr])
                self.V(lambda e, i=i: e.tensor_copy(out=xT[i][:], in_=ptr[:]), [ptr], [xT[i]])
                for q in range(4):
                    sl = slice(q * 2, q * 2 + 2)
                    if q % 2 == 0:
                        self.A(lambda e, i=i, sl=sl: e.copy(out=gbf[i][:, sl, :], in_=g32[i][:, sl, :]), [g32[i]], [gbf[i]])
                    else:
                        self.V(lambda e, i=i, sl=sl: e.tensor_copy(out=gbf[i][:, sl, :], in_=g32[i][:, sl, :]), [g32[i]], [gbf[i]])
                self.A(lambda e, i=i: e.copy(out=dbf[i][:, 0:2, :], in_=d32[i][:, 0:2, :]), [d32[i]], [dbf[i]])
                self.V(lambda e, i=i: e.tensor_copy(out=dbf[i][:, 2:4, :], in_=d32[i][:, 2:4, :]), [d32[i]], [dbf[i]])
                ph = pH[i]
                for m in range(8):
                    tt, kh = m // 4, m % 4
                    for k in range(8):
                        lw = gbf[i][:, k, :].rearrange("d (two p k) -> d two k p", two=2, k=4)[:, tt, kh, :]
                        self.P(lambda e, i=i, m=m, k=k, ph=ph, lw=lw: e.matmul(ph[:, m, :], lhsT=lw, rhs=xT[i][:, k, :],
                                                                               start=(k == 0), stop=(k == 7)), [gbf[i], xT[i]], [ph])
                self.A(lambda e, ph=ph: e.activation(out=sg[:], in_=ph[:, 0:4, :], func=AF.Silu), [ph], [sg])
                self.V(lambda e, ph=ph: e.tensor_tensor(out=hT[:], in0=sg[:], in1=ph[:, 4:8, :], op=ALU.mult), [sg, ph], [hT])
                for nn in range(2):
                    for k in range(4):
                        self.P(lambda e, i=i, nn=nn, k=k: e.matmul(pO[:, nn * 512:(nn + 1) * 512], lhsT=hT[:, k, :], rhs=dbf[i][:, k, nn * 512:(nn + 1) * 512],
                                                                   start=(k == 0), stop=(k == 3)), [hT, dbf[i]], [pO])
                self.A(lambda e: e.copy(out=ob[:], in_=pO[:]), [pO], [ob])
                self.st(self.ybuf[b * 128:(b + 1) * 128, :], ob, ob[:])
        with fw.scope():
            g2 = []
            for v in range(2):
                t = fw.sb("g2_%d" % v, [128, D], F32)
                self.ld_row(t, t[:], self.modrow[v:v + 1, 5 * D:6 * D])
                g2.append(t)
            lng = fw.sb("lng", [128, D], F32)
            lnb = fw.sb("lnb", [128, D], F32)
            self.ld_row(lng, lng[:], self.ln_ffn_g[li:li + 1, :])
            self.ld_row(lnb, lnb[:], self.ln_ffn_b[li:li + 1, :])
            o1_ = [fw.sb("o1%d" % i, [128, D], F32) for i in range(2)]
            o2_ = [fw.sb("o2%d" % i, [128, D], F32) for i in range(2)]
            xt_ = [fw.sb("xt%d" % i, [128, D], F32) for i in range(2)]
            t1_ = [fw.sb("t1%d" % i, [128, D], F32) for i in range(2)]
            st2 = fw.sb("st2", [128, 2, 6], F32)
            mv2 = fw.sb("mv2", [128, 2], F32)
            rs2 = fw.sb("rs2", [128, 1], F32)
            for c in range(NT):
                if final and c < NCTX:
                    continue
                v = 1 if c < NCTX else 0
                o1, o2, xt, t1 = o1_[c % 2], o2_[c % 2], xt_[c % 2], t1_[c % 2]
                cs_ = slice(c * 128, (c + 1) * 128)
                for k, ot in enumerate((o1, o2)):
                    col = 2 * c + k
                    self.fw.dma("gpsimd", lambda e, col=col, ot=ot: e.indirect_dma_start(
                        out=ot[:, :], out_offset=None, in_=self.ybuf, in_offset=bass.IndirectOffsetOnAxis(ap=DI[:, col:col + 1], axis=0)),
                        reads=[DI], writes=[ot])
                self.ld(xt, xt[:], xcur[cs_, :])
                self.V(lambda e, c=c, o1=o1: e.tensor_scalar(out=o1[:], in0=o1[:], scalar1=Wt[:, 2 * c:2 * c + 1], scalar2=None, op0=ALU.mult), [o1, Wt], [o1])
                self.V(lambda e, c=c, o1=o1, o2=o2: e.scalar_tensor_tensor(out=o1[:], in0=o2[:], scalar=Wt[:, 2 * c + 1:2 * c + 2], in1=o1[:], op0=ALU.mult, op1=ALU.add),
                       [o2, Wt, o1], [o1])
                self.G(lambda e, v=v, o1=o1: e.tensor_tensor(out=o1[:], in0=o1[:], in1=g2[v][:], op=ALU.mult), [o1, g2[v]], [o1])
                self.V(lambda e, o1=o1, o2=o2, xt=xt: e.scalar_tensor_tensor(out=o2[:], in0=xt[:], scalar=ALPHA, in1=o1[:], op0=ALU.mult, op1=ALU.add), [xt, o1], [o2])
                self.layer_norm(o2, t1, st2, mv2, rs2, lng, lnb)
                if final:
                    self.st(self.yout[(c - NCTX) * 128:(c - NCTX + 1) * 128, :], t1, t1[:])
                else:
                    self.st(xnext[cs_, :], t1, t1[:])


Builder.phase_moe = _phase_moe


def build_program(nlayers=DEPTH):
    nc = bass.Bass("TRN2", target_bir_lowering=False)
    b = Builder(nc)
    b.declare()
    xcur = b.xin
    for li in range(nlayers):
        j = li // 2
        b.phase_mod(li)
        if li % 2 == 0:
            b.phase_in_ssd(li, j, xcur)
            b.phase_scan(li, j, True, xcur, b.xA)
        else:
            b.phase_in_ret(li, j, xcur)
            b.phase_scan(li, j, False, xcur, b.xA)
        b.phase_moe(li, b.xA, b.xB, final=(li == nlayers - 1))
        xcur = b.xB
    b.fw.barrier()
    b.fw.root.close()
    return nc


def kernel(**inputs):
    inp = {k: np.asarray(v) for k, v in inputs.items()}
    nc = build_program()
    consts = host_consts()
    maps = []
    for c in range(8):
        m = host_inputs(inp, c)
        m.update(consts)
        maps.append(m)
    res = run_bass_kernel_spmd(nc, maps, core_ids=list(range(8)))
    out = np.stack([np.asarray(res.results[c]["yout"]) for c in range(8)], 0)
    return out.astype(np.float32)


def _phase_scan2(self, li, j, ssd, xcur, xnext):
    fw = self.fw
    G = 4
    Hg = 8 if ssd else 1
    KC = 1 if ssd else 2
    H = G * Hg
    dv = 2048 // H
    dk = KC * 128
    with fw.scope():
        identb = fw.sb("identb", [128, 128], BF16)
        self.ld(identb, identb[:], self.c_identb)
        ones = fw.sb("ones", [128, 128], F32)
        self.ld(ones, ones[:], self.c_ones)
        tri = fw.sb("tri", [128, 128], F32)
        Wo = fw.sb("Wo", [128, 16, D], BF16)
        owv = (self.ssd_out_w if ssd else self.ret_out_w)[j].rearrange("(k p) n -> p k n", p=128)
        with fw.scope():
            wst = fw.sb("wstO", [128, 4, D], F32)
            for q in range(4):
                self.ld(wst, wst[:], owv[:, q * 4:(q + 1) * 4, :])
                self.G(lambda e, q=q: e.tensor_copy(out=Wo[:, q * 4:(q + 1) * 4, :], in_=wst[:]), [wst], [Wo])
        g1 = fw.sb("g1", [128, D], F32)
        sh2 = fw.sb("sh2", [128, D], F32)
        sc2 = fw.sb("sc2", [128, D], F32)

        def load_rows(v):
            self.ld_row(g1, g1[:], self.modrow[v:v + 1, 2 * D:3 * D])
            self.ld_row(sh2, sh2[:], self.modrow[v:v + 1, 3 * D:4 * D])
            self.ld_row(sc2, sc2[:], self.modrow[v:v + 1, 4 * D:5 * D])
            self.V(lambda e: e.tensor_scalar_add(out=sc2[:], in0=sc2[:], scalar1=1.0), [sc2], [sc2])

        lng = fw.sb("lng", [128, D], F32)
        lnb = fw.sb("lnb", [128, D], F32)
        self.ld_row(lng, lng[:], self.ln_mix_g[li:li + 1, :])
        self.ld_row(lnb, lnb[:], self.ln_mix_b[li:li + 1, :])
        nw = fw.sb("nw", [128, 2048], F32)
        self.ld_row(nw, nw[:], (self.ssd_norm_w if ssd else self.ret_gn_w)[j:j + 1, :])
        if ssd:
            dsk = fw.sb("dsk", [128, 32], F32)
            self.ld_row(dsk, dsk[:], self.ssd_d_skip[j:j + 1, :])
        else:
            nb_ = fw.sb("nb", [128, 2048], F32)
            self.ld_row(nb_, nb_[:], self.ret_gn_b[j:j + 1, :])
        S = fw.sb("S", [128, KC, 2048], F32)
        Sb = fw.sb("Sb", [128, KC, 2048], BF16)

        def dbl(name, shape, dt):
            return [fw.sb(name + "0", shape, dt), fw.sb(name + "1", shape, dt)]

        def tpl(name, shape, dt):
            return [fw.sb(name + str(i_), shape, dt) for i_ in range(3)]

        qt3 = tpl("qt", [128, G * KC, 128], BF16)
        kt2 = dbl("kt", [128, G * KC, 128], BF16)
        ktm3 = tpl("ktm", [128, G * dk], BF16)
        vt3 = tpl("vt", [128, 2048], BF16)
        xdt2 = dbl("xdt", [128, 2048], BF16) if ssd else None
        xe = dbl("xe", [128, 2048], BF16)
        cbm = dbl("cbm", [128, G, 128], F32)
        la2 = dbl("la", [128, 64], F32)
        dt2 = dbl("dt", [128, 64], F32)
        acs = fw.sb("acs", [128, 2 * H], F32)
        nacs = fw.sb("nacs", [128, H], F32)
        wend = fw.sb("wend", [128, H], F32)
        if ssd:
            ea = dbl("ea", [128, H], F32)
            cd = dbl("cd", [128, H], F32)
            E = [[fw.sb("E%d_%d" % (p_, g), [128, Hg, 128], BF16) for g in range(G)] for p_ in range(2)]
        else:
            ea0 = fw.sb("ea", [128, H], F32)
            cd0 = fw.sb("cd", [128, H], F32)
            ea = [ea0, ea0]
            cd = [cd0, cd0]
            E0 = [fw.sb("E_%d" % g, [128, Hg, 128], F32) for g in range(G)]
            E = [E0, E0]
        lt = dbl("lt", [128, Hg, 128], F32)
        nlb = dbl("nlb", [128, Hg, 128], F32)
        nones = fw.sb("nones", [128, 128], F32)
        self.V(lambda e: e.memset(nones[:], -1.0), [], [nones])
        M = dbl("M", [128, Hg, 128], BF16)
        yc = fw.sb("yc", [128, 2048], F32)
        tmp = dbl("tmp", [128, 512], F32)
        yfl = fw.sb("yfl", [128, 2048], F32)
        gt = fw.sb("gt", [128, 2048], F32)
        yb = fw.sb("yb", [128, 2048], BF16)
        yT = fw.sb("yT", [128, 16, 128], BF16)
        st6 = fw.sb("st6", [128, 4, 6], F32)
        mv = fw.sb("mv", [128, 4, 2], F32)
        ss = fw.sb("ss", [128, 4], F32)
        rstd = fw.sb("rstd", [128, 4], F32)
        xt = fw.sb("xt", [128, D], F32)
        t1 = fw.sb("t1", [128, D], F32)
        t2 = fw.sb("t2", [128, D], F32)
        st2 = fw.sb("st2", [128, 2, 6], F32)
        mv2 = fw.sb("mv2", [128, 2], F32)
        rs2 = fw.sb("rs2", [128, 1], F32)
        psm = fw.ps("psm", [128, 512], F32)
        pcb = fw.ps("pcb", [128, 512], F32)
        pbc = [fw.ps("pbc%d" % i, [128, 512], F32) for i in range(2)]
        pyd = fw.ps("pyd", [128, 512], F32)
        pyo = fw.ps("pyo", [128, 512], F32)
        pst = fw.ps("pst", [128, 512], F32)
        ptr = fw.ps("ptr", [128, 8, 128], BF16)

        qtv = self.QT.rearrange("g k p t -> p g k t")
        ktv = self.KT.rearrange("g k p t -> p g k t")
        r3 = lambda ap, h: ap.rearrange("p (h d) -> p h d", h=h)

        def decay_pre(dirn, i):
            par = i % 2
            la = la2[par]
            lad = la[:, dirn * 32:dirn * 32 + H] if ssd else la[:, 0:H]
            self.P(lambda e: e.matmul(psm[:, 0:H], lhsT=tri[:], rhs=lad, start=True, stop=True), [tri, la], [psm])
            self.P(lambda e: e.matmul(psm[:, H:2 * H], lhsT=ones[:], rhs=lad, start=True, stop=True), [ones, la], [psm])
            self.V(lambda e: e.tensor_copy(out=acs[:], in_=psm[:, 0:2 * H]), [psm], [acs])
            self.A(lambda e: e.activation(out=ea[par][:], in_=acs[:, 0:H], func=AF.Exp), [acs], [ea[par]])
            self.V(lambda e: e.tensor_tensor(out=wend[:], in0=acs[:, H:2 * H], in1=acs[:, 0:H], op=ALU.subtract), [acs], [wend])
            self.A(lambda e: e.activation(out=wend[:], in_=wend[:], func=AF.Exp), [wend], [wend])
            self.A(lambda e: e.activation(out=cd[par][:], in_=acs[:, H:2 * H], func=AF.Exp), [acs], [cd[par]])

        def decay_g(dirn, i, g):
            par = i % 2
            la = la2[par]
            ltg = lt[g % 2]
            nlg = nlb[g % 2]
            ladg = la[:, dirn * 32 + g * Hg:dirn * 32 + (g + 1) * Hg] if ssd else la[:, g:g + 1]
            self.G(lambda e: e.tensor_tensor(out=ltg[:], in0=bcast(tri[:].unsqueeze(1), [128, Hg, 128]),
                                             in1=bcast(ladg.unsqueeze(2), [128, Hg, 128]), op=ALU.mult), [tri, la], [ltg])
            self.G(lambda e: e.tensor_tensor(out=nlg[:], in0=bcast(nones[:].unsqueeze(1), [128, Hg, 128]),
                                             in1=bcast(ladg.unsqueeze(2), [128, Hg, 128]), op=ALU.mult), [nones, la], [nlg])
            Eg = E[par][g]
            for q in range((Hg + 3) // 4):
                nh = min(4, Hg - q * 4)
                pb = pbc[q % 2]
                self.P(lambda e: e.matmul(pb[:, 0:nh * 128], lhsT=ones[:], rhs=ltg[:, q * 4:q * 4 + nh, :], start=True, stop=False), [ones, ltg], [pb])
                self.P(lambda e: e.matmul(pb[:, 0:nh * 128], lhsT=tri[:], rhs=nlg[:, q * 4:q * 4 + nh, :], start=False, stop=True), [tri, nlg], [pb])
                self.A(lambda e: e.activation(out=Eg[:, q * 4:q * 4 + nh, :].rearrange("p h l -> p (h l)"), in_=pb[:, 0:nh * 128], func=AF.Exp), [pb], [Eg])

        def loads(c, i):
            cs_ = slice(c * 128, (c + 1) * 128)
            t3, p2 = i % 3, i % 2
            self.ld(qt3[t3], qt3[t3][:].rearrange("p (g k) t -> p g k t", g=G), qtv[:, :, 0:KC, cs_])
            self.ld(kt2[p2], kt2[p2][:].rearrange("p (g k) t -> p g k t", g=G), ktv[:, :, 0:KC, cs_])
            self.ld(ktm3[t3], ktm3[t3][:], self.Ktm[cs_, 0:G * dk])
            self.ld(vt3[t3], vt3[t3][:], self.Vtm[cs_, :])
            if ssd:
                self.ld(la2[p2], la2[p2][:], self.latm[cs_, :])
                self.ld(dt2[p2], dt2[p2][:], self.dttm[cs_, :])

        def XDT(i):
            return xdt2[i % 2] if ssd else vt3[i % 3]

        def stage1_pre(dirn, i):
            par = i % 2
            qt, kt, vt, dt = qt3[i % 3], kt2[par], vt3[i % 3], dt2[par]
            xdt = XDT(i)
            if ssd:
                decay_pre(dirn, i)
                self.V(lambda e: e.tensor_tensor(out=r3(xdt[:], H), in0=r3(vt[:], H),
                                                 in1=bcast(dt[:, dirn * 32:dirn * 32 + 32].unsqueeze(2), [128, H, dv]), op=ALU.mult),
                       [vt, dt], [xdt])
            self.G(lambda e: e.tensor_tensor(out=r3(xe[par][:], H), in0=r3(xdt[:], H),
                                             in1=bcast(wend[:].unsqueeze(2), [128, H, dv]), op=ALU.mult), [xdt, wend], [xe[par]])
            for g in range(G):
                for kc in range(KC):
                    self.P(lambda e, g=g, kc=kc: e.matmul(pcb[:, g * 128:(g + 1) * 128], lhsT=kt[:, g * KC + kc, :], rhs=qt[:, g * KC + kc, :],
                                                          start=(kc == 0), stop=(kc == KC - 1)), [kt, qt], [pcb])
            self.V(lambda e: e.tensor_tensor(out=cbm[par][:], in0=pcb[:].rearrange("p (g l) -> p g l", g=G),
                                             in1=bcast(tri[:].unsqueeze(1), [128, G, 128]), op=ALU.mult), [pcb, tri], [cbm[par]])

        def emitM(g, par):
            Mg = M[g % 2]
            self.V(lambda e: e.scalar_tensor_tensor(out=Mg[:], in0=E[par][g][:], scalar=1.0,
                                                    in1=bcast(cbm[par][:, g:g + 1, :], [128, Hg, 128]), op0=ALU.min, op1=ALU.mult),
                   [E[par][g], cbm[par]], [Mg])

        def s2g(i, g):
            par = i % 2
            qt, ktm = qt3[i % 3], ktm3[i % 3]
            xdt = XDT(i)
            gs = slice(g * 512, (g + 1) * 512)
            if g + 1 < G:
                emitM(g + 1, par)
            Mg = M[g % 2]
            tg = tmp[g % 2]
            for hh in range(Hg):
                h = g * Hg + hh
                self.P(lambda e, h=h, hh=hh: e.matmul(pyd[:, hh * dv:(hh + 1) * dv], lhsT=Mg[:, hh, :], rhs=xdt[:, h * dv:(h + 1) * dv],
                                                      start=True, stop=True), [Mg, xdt], [pyd])
            for kc in range(KC):
                self.P(lambda e, kc=kc: e.matmul(pyo[:], lhsT=qt[:, g * KC + kc, :], rhs=Sb[:, kc, gs],
                                                 start=(kc == 0), stop=(kc == KC - 1)), [qt, Sb], [pyo])
            self.V(lambda e: e.tensor_tensor(out=r3(tg[:], Hg), in0=r3(pyo[:], Hg),
                                             in1=bcast(ea[par][:, g * Hg:(g + 1) * Hg].unsqueeze(2), [128, Hg, dv]), op=ALU.mult),
                   [pyo, ea[par]], [tg])
            self.V(lambda e: e.tensor_tensor(out=yc[:, gs], in0=tg[:], in1=pyd[:], op=ALU.add), [tg, pyd], [yc])
            for kc in range(KC):
                self.P(lambda e, kc=kc: e.matmul(pst[:], lhsT=ktm[:, g * dk + kc * 128:g * dk + (kc + 1) * 128], rhs=xe[par][:, gs],
                                                 start=True, stop=True), [ktm, xe[par]], [pst])
                self.G(lambda e, kc=kc: e.tensor_tensor(out=r3(S[:, kc, gs], Hg), in0=r3(S[:, kc, gs], Hg),
                                                        in1=bcast(cd[par][:, g * Hg:(g + 1) * Hg].unsqueeze(2), [128, Hg, dv]),
                                                        op=ALU.mult), [S, cd[par]], [S])
                self.V(lambda e, kc=kc: e.tensor_tensor(out=S[:, kc, gs], in0=S[:, kc, gs], in1=pst[:], op=ALU.add), [S, pst], [S])
                self.A(lambda e, kc=kc: e.copy(out=Sb[:, kc, gs], in_=S[:, kc, gs]), [S], [Sb])

        def stage2_post(c, dirn, i):
            par = i % 2
            cs_ = slice(c * 128, (c + 1) * 128)
            if dirn == 0:
                deferred.append(lambda: self.st(self.yf[cs_, :], yc, yc[:]))
                return
            vtp = vt3[i % 3]
            self.ld(yfl, yfl[:], self.yf[cs_, :])
            self.ld(gt, gt[:], self.gate[cs_, :])
            self.V(lambda e: e.tensor_tensor(out=yc[:], in0=yc[:], in1=yfl[:], op=ALU.add), [yc, yfl], [yc])
            if ssd:
                self.G(lambda e: e.tensor_tensor(out=r3(yfl[:], H), in0=r3(vtp[:], H),
                                                 in1=bcast(dsk[:].unsqueeze(2), [128, H, dv]), op=ALU.mult), [vtp, dsk], [yfl])
                self.V(lambda e: e.tensor_tensor(out=yc[:], in0=yc[:], in1=yfl[:], op=ALU.add), [yc, yfl], [yc])
                self.A(lambda e: e.activation(out=gt[:], in_=gt[:], func=AF.Silu), [gt], [gt])
                self.V(lambda e: e.tensor_tensor(out=yc[:], in0=yc[:], in1=gt[:], op=ALU.mult), [yc, gt], [yc])
                for g in range(4):
                    self.V(lambda e, g=g: e.bn_stats(out=st6[:, g, :], in_=yc[:, g * 512:(g + 1) * 512]), [yc], [st6])
                    self.V(lambda e, g=g: e.bn_aggr(out=mv[:, g, :], in_=st6[:, g, :]), [st6], [mv])
                self.V(lambda e: e.tensor_tensor(out=ss[:], in0=mv[:, :, 0], in1=mv[:, :, 0], op=ALU.mult), [mv], [ss])
                self.V(lambda e: e.tensor_tensor(out=ss[:], in0=ss[:], in1=mv[:, :, 1], op=ALU.add), [ss, mv], [ss])
                self.A(lambda e: e.activation(out=ss[:], in_=ss[:], func=AF.Sqrt, bias=EPS), [ss], [ss])
                self.V(lambda e: e.reciprocal(out=rstd[:], in_=ss[:]), [ss], [rstd])
                for g in range(4):
                    gs = slice(g * 512, (g + 1) * 512)
                    self.V(lambda e, g=g, gs=gs: e.scalar_tensor_tensor(out=yb[:, gs], in0=yc[:, gs], scalar=rstd[:, g:g + 1], in1=nw[:, gs],
                                                                        op0=ALU.mult, op1=ALU.mult), [yc, rstd, nw], [yb])
            else:
                for g in range(4):
                    self.V(lambda e, g=g: e.bn_stats(out=st6[:, g, :], in_=yc[:, g * 512:(g + 1) * 512]), [yc], [st6])
                    self.V(lambda e, g=g: e.bn_aggr(out=mv[:, g, :], in_=st6[:, g, :]), [st6], [mv])
                self.A(lambda e: e.activation(out=ss[:], in_=mv[:, :, 1], func=AF.Sqrt, bias=EPS), [mv], [ss])
                self.V(lambda e: e.reciprocal(out=rstd[:], in_=ss[:]), [ss], [rstd])
                self.A(lambda e: e.activation(out=gt[:], in_=gt[:], func=AF.Silu), [gt], [gt])
                for g in range(4):
                    gs = slice(g * 512, (g + 1) * 512)
                    self.V(lambda e, g=g, gs=gs: e.tensor_scalar(out=yc[:, gs], in0=yc[:, gs], scalar1=mv[:, g, 0:1], scalar2=rstd[:, g:g + 1],
                                                                 op0=ALU.subtract, op1=ALU.mult), [yc, mv, rstd], [yc])
                self.G(lambda e: e.tensor_tensor(out=yc[:], in0=yc[:], in1=nw[:], op=ALU.mult), [yc, nw], [yc])
                self.V(lambda e: e.tensor_tensor(out=yc[:], in0=yc[:], in1=nb_[:], op=ALU.add), [yc, nb_], [yc])
                self.V(lambda e: e.tensor_tensor(out=yb[:], in0=yc[:], in1=gt[:], op=ALU.mult), [yc, gt], [yb])
            for half in range(2):
                for kk in range(8):
                    k = half * 8 + kk
                    self.P(lambda e, k=k, kk=kk: e.transpose(ptr[:, kk, :], yb[:, k * 128:(k + 1) * 128], identb[:]), [yb, identb], [ptr])
                self.A(lambda e, half=half: e.copy(out=yT[:, half * 8:(half + 1) * 8, :], in_=ptr[:]), [ptr], [yT])
            pos = (pyd, pyo)
            for nn in range(2):
                for k in range(16):
                    self.P(lambda e, k=k, nn=nn: e.matmul(pos[nn][:], lhsT=yT[:, k, :], rhs=Wo[:, k, nn * 512:(nn + 1) * 512],
                                                          start=(k == 0), stop=(k == 15)), [yT, Wo], [pos[nn]])
            self.ld(xt, xt[:], xcur[cs_, :])
            for nn in range(2):
                ns = slice(nn * 512, (nn + 1) * 512)
                self.V(lambda e, nn=nn, ns=ns: e.tensor_tensor(out=t1[:, ns], in0=pos[nn][:], in1=g1[:, ns], op=ALU.mult), [pos[nn], g1], [t1])
            self.V(lambda e: e.scalar_tensor_tensor(out=t2[:], in0=xt[:], scalar=ALPHA, in1=t1[:], op0=ALU.mult, op1=ALU.add), [xt, t1], [t2])
            self.layer_norm(t2, t1, st2, mv2, rs2, lng, lnb)
            self.G(lambda e: e.tensor_tensor(out=t2[:], in0=t1[:], in1=sc2[:], op=ALU.mult), [t1, sc2], [t2])
            self.V(lambda e: e.tensor_tensor(out=t2[:], in0=t2[:], in1=sh2[:], op=ALU.add), [t2, sh2], [t2])
            deferred.append(lambda: self.st(xnext[cs_, :], t1, t1[:]))
            deferred.append(lambda: self.st(self.tok[cs_, :], t2, t2[:]))

        deferred = []

        def flush():
            for f in deferred:
                f()
            del deferred[:]

        for dirn in range(2):
            order = list(range(NCH)) if dirn == 0 else [1, 0] + list(range(NCH - 1, 1, -1))
            self.ld(tri, tri[:], self.c_trif[dirn])
            self.V(lambda e: e.memset(S[:], 0.0), [], [S])
            self.G(lambda e: e.memset(Sb[:], 0.0), [], [Sb])
            if dirn == 1:
                load_rows(1)
            n_ = len(order)
            if not ssd:
                for la in la2:
                    self.ld_row(la, la[:, 0:4], self.ret_decay[j, dirn:dirn + 1, :])
                    self.A(lambda e: e.activation(out=la[:, 0:4], in_=la[:, 0:4], func=AF.Exp, scale=-1.0), [la], [la])
                    self.A(lambda e: e.activation(out=la[:, 0:4], in_=la[:, 0:4], func=AF.Ln, bias=1.0), [la], [la])
                    self.V(lambda e: e.tensor_scalar_mul(out=la[:, 0:4], in0=la[:, 0:4], scalar1=-1.0), [la], [la])
                decay_pre(dirn, 0)
                for g in range(G):
                    decay_g(dirn, 0, g)
            loads(order[0], 0)
            loads(order[1], 1)
            stage1_pre(dirn, 0)
            if ssd:
                for g in range(G):
                    decay_g(dirn, 0, g)
            for i, c in enumerate(order):
                if i + 2 < n_:
                    loads(order[i + 2], i + 2)
                flush()
                if i + 1 < n_:
                    stage1_pre(dirn, i + 1)
                if dirn == 1 and i == NCTX:
                    load_rows(0)
                emitM(0, i % 2)
                for g in range(G):
                    if ssd and i + 1 < n_:
                        decay_g(dirn, i + 1, g)
                    s2g(i, g)
                stage2_post(c, dirn, i)
            flush()
            if dirn == 0:
                fw.barrier()


Builder.phase_scan = _phase_scan2
```

```python
import numpy as np
import ml_dtypes
from contextlib import ExitStack, contextmanager
import concourse.bass as bass
import concourse.mybir as mybir
from concourse.bass_utils import run_bass_kernel_spmd

F32 = mybir.dt.float32
BF16 = mybir.dt.bfloat16
I32 = mybir.dt.int32
ALU = mybir.AluOpType
AF = mybir.ActivationFunctionType
AX = mybir.AxisListType

ENGS = ["tensor", "vector", "scalar", "gpsimd", "sync"]
EPOCH = 20000
NDMA_SEM = 8

D = 1024
T = 4352
NCH = 34
NCTX = 2
NB = 100
DEPTH = 4
ALPHA = (2.0 * DEPTH) ** 0.25
EPS = 1e-5


class Res:
    __slots__ = ("w", "r")

    def __init__(self):
        self.w = None
        self.r = {}


class Tl:
    def __init__(self, t):
        self.t = t
        self.r = Res()

    def __getitem__(self, k):
        return self.t[k]


class FW:
    def __init__(self, nc):
        self.nc = nc
        self.root = ExitStack()
        self.es = self.root
        self.cnt = {e: 0 for e in ENGS}
        self.sems = {}
        self.waited = {e: {} for e in ENGS}
        self.dma_i = {e: 0 for e in ENGS}
        self.dma_last = {}
        self.latest = {}
        self.uid = 0

    def sem(self, key):
        if key not in self.sems:
            self.sems[key] = self.root.enter_context(self.nc.semaphore("s_%s_%s" % key))
        return self.sems[key]

    def sb(self, name, shape, dt):
        self.uid += 1
        return Tl(self.es.enter_context(self.nc.sbuf_tensor("%s_%d" % (name, self.uid), list(shape), dt)))

    def ps(self, name, shape, dt):
        self.uid += 1
        return Tl(self.es.enter_context(self.nc.psum_tensor("%s_%d" % (name, self.uid), list(shape), dt)))

    @contextmanager
    def scope(self):
        old = self.es
        self.es = ExitStack()
        try:
            yield
        finally:
            self.barrier()
            self.es.close()
            self.es = old

    def barrier(self):
        for eng in ENGS:
            for key, val in list(self.latest.items()):
                self._wait(eng, (key, val))

    def _wait(self, eng, ev):
        if ev is None:
            return
        key, val = ev
        if self.waited[eng].get(key, 0) >= val:
            return
        self.waited[eng][key] = val
        getattr(self.nc, eng).wait_ge(self.sem(key), val)

    def _deps(self, eng, reads, writes):
        evs = []
        for r in reads:
            if r.w is not None:
                evs.append(r.w)
        for w in writes:
            if w.w is not None:
                evs.append(w.w)
            evs.extend(w.r.items())
        for ev in evs:
            if ev[0][0] == "tensor" and eng == "tensor":
                continue
            self._wait(eng, ev)

    def _record(self, ev, reads, writes):
        self.latest[ev[0]] = ev[1]
        for r in reads:
            if r.r.get(ev[0], 0) < ev[1]:
                r.r[ev[0]] = ev[1]
        for w in writes:
            w.w = ev
            w.r = {}

    def op(self, eng, fn, reads=(), writes=()):
        reads = [t.r for t in reads]
        writes = [t.r for t in writes]
        self._deps(eng, reads, writes)
        c = self.cnt[eng]
        key = (eng, c // EPOCH)
        val = c % EPOCH + 1
        self.cnt[eng] = c + 1
        fn(getattr(self.nc, eng)).then_inc(self.sem(key), 1)
        self._record((key, val), reads, writes)

    def dma(self, eng, fn, reads=(), writes=()):
        reads = [t.r for t in reads]
        writes = [t.r for t in writes]
        self._deps(eng, reads, writes)
        i = self.dma_i[eng]
        self.dma_i[eng] = i + 1
        key = ("d" + eng, i % NDMA_SEM)
        prev = self.dma_last.get(key, 0)
        if prev:
            self._wait(eng, (key, prev))
        val = prev + 16
        self.dma_last[key] = val
        fn(getattr(self.nc, eng)).then_inc(self.sem(key), 16)
        self._record((key, val), reads, writes)


def bcast(ap, shape):
    return ap.to_broadcast(list(shape))


class Builder:
    def __init__(self, nc, nlayers=DEPTH, stop=None):
        self.nc = nc
        self.fw = FW(nc)
        self.nlayers = nlayers
        self.stop = stop
        self.dram = {}
        self.debug = set()
        self.only = None

    def din(self, name, shape, dt):
        if self.only is not None and name not in self.only:
            return None
        a = self.nc.dram_tensor(name, list(shape), dt, kind="ExternalInput").ap()
        self.dram[name] = a
        return a

    def dscr(self, name, shape, dt):
        kind = "ExternalOutput" if name in self.debug else "Internal"
        a = self.nc.dram_tensor(name, list(shape), dt, kind=kind).ap()
        self.dram[name] = a
        return a

    def ld(self, tile, dst, src, eng="sync"):
        self.fw.dma(eng, lambda e: e.dma_start(out=dst, in_=src), writes=[tile])

    def st(self, dst, tile, src, eng="sync"):
        self.fw.dma(eng, lambda e: e.dma_start(out=dst, in_=src), reads=[tile])

    def V(self, fn, rd, wr):
        self.fw.op("vector", fn, rd, wr)

    def A(self, fn, rd, wr):
        self.fw.op("scalar", fn, rd, wr)

    def G(self, fn, rd, wr):
        self.fw.op("gpsimd", fn, rd, wr)

    def P(self, fn, rd, wr):
        self.fw.op("tensor", fn, rd, wr)

    def ld_row(self, tile, dst, src_row, n=128):
        self.ld(tile, dst, src_row.partition_broadcast(n))

    def declare(self):
        d = self.din
        self.xin = d("xin", [T, D], F32)
        self.cvec = d("cvec", [128, 8, 2], F32)
        self.mod_w = d("mod_w", [DEPTH, D, 6 * D], F32)
        self.mod_b = d("mod_b", [DEPTH, 6 * D], F32)
        self.ssd_in_w = d("ssd_in_w", [2, D, 5184], F32)
        self.convw = d("convw", [2, 128, 24, 5], F32)
        self.convb = d("convb", [2, 128, 24], F32)
        self.ssd_dt_bias = d("ssd_dt_bias", [2, 64], F32)
        self.ssd_a_log = d("ssd_a_log", [2, 64], F32)
        self.ssd_d_skip = d("ssd_d_skip", [2, 32], F32)
        self.ssd_norm_w = d("ssd_norm_w", [2, 2048], F32)
        self.ssd_out_w = d("ssd_out_w", [2, 2048, D], F32)
        self.ret_in_w = d("ret_in_w", [2, D, 6144], F32)
        self.ret_decay = d("ret_decay_logit", [2, 2, 4], F32)
        self.ret_gn_w = d("ret_gn_w", [2, 2048], F32)
        self.ret_gn_b = d("ret_gn_b", [2, 2048], F32)
        self.ret_out_w = d("ret_out_w", [2, 2048, D], F32)
        self.ln_mix_g = d("ln_mix_g", [DEPTH, D], F32)
        self.ln_mix_b = d("ln_mix_b", [DEPTH, D], F32)
        self.ln_ffn_g = d("ln_ffn_g", [DEPTH, D], F32)
        self.ln_ffn_b = d("ln_ffn_b", [DEPTH, D], F32)
        self.moe_rw = d("moe_rw", [DEPTH, D, 36], F32)
        self.moe_rb = d("moe_rb", [DEPTH, 36], F32)
        self.moe_wgu = d("moe_w_gate_up", [DEPTH * 32 * 128, 8 * D], F32)
        self.moe_wd = d("moe_w_down", [DEPTH * 32 * 128, 4 * D], F32)
        self.c_identb = d("c_identb", [128, 128], BF16)
        self.c_identf = d("c_identf", [128, 128], F32)
        self.c_trif = d("c_trif", [2, 128, 128], F32)
        self.c_ones = d("c_ones", [128, 128], F32)
        self.c_slow = d("c_slow", [128, 128], BF16)
        self.c_rope = d("c_rope", [2, 128, T], F32)
        self.c_iota = d("c_iota", [128, 512], F32)
        self.c_pidx = d("c_pidx", [128, 12], F32)
        self.yout = self.nc.dram_tensor("yout", [4096, D], F32, kind="ExternalOutput").ap()
        s = self.dscr
        self.xA = s("xA", [T, D], F32)
        self.xB = s("xB", [T, D], F32)
        self.modrow = s("modrow", [2, 6 * D], F32)
        self.QT = s("QT", [4, 2, 128, T], BF16)
        self.KT = s("KT", [4, 2, 128, T], BF16)
        self.Ktm = s("Ktm", [T, 1024], BF16)
        self.Vtm = s("Vtm", [T, 2048], BF16)
        self.gate = s("gate", [T, 2048], F32)
        self.latm = s("latm", [T, 64], F32)
        self.dttm = s("dttm", [T, 64], F32)
        self.yf = s("yf", [T, 2048], F32)
        self.tok = s("tok", [T, D], F32)
        self.xbuf = s("xbuf", [NB * 128, D], BF16)
        self.ybuf = s("ybuf", [NB * 128, D], F32)
        self.dbg = {}

    def phase_mod(self, li):
        fw = self.fw
        with fw.scope():
            cv = fw.sb("cv", [128, 8, 2], F32)
            sv = fw.sb("sv", [128, 8, 2], F32)
            mb = fw.sb("mb", [2, 6 * D], F32)
            mr = fw.sb("mr", [2, 6 * D], F32)
            wst = fw.sb("wst", [128, 8, 512], F32)
            pm = fw.ps("pm", [128, 512], F32)
            self.ld(cv, cv[:], self.cvec)
            self.A(lambda e: e.activation(out=sv[:], in_=cv[:], func=AF.Silu), [cv], [sv])
            self.ld(mb, mb[0:1, :], self.mod_b[li:li + 1, :])
            self.ld(mb, mb[1:2, :], self.mod_b[li:li + 1, :])
            wv = self.mod_w[li].rearrange("(k p) n -> p k n", p=128)
            for n in range(12):
                self.ld(wst, wst[:], wv[:, :, n * 512:(n + 1) * 512])
                for k in range(8):
                    self.P(lambda e, k=k: e.matmul(pm[0:2, :], lhsT=sv[:, k, :], rhs=wst[:, k, :],
                                                   start=(k == 0), stop=(k == 7)), [sv, wst], [pm])
                self.V(lambda e, n=n: e.tensor_tensor(out=mr[0:2, n * 512:(n + 1) * 512], in0=pm[0:2, :],
                                                      in1=mb[0:2, n * 512:(n + 1) * 512], op=ALU.add),
                       [pm, mb], [mr])
            self.st(self.modrow, mr, mr[0:2, :])

    def make_uT(self, xcur, uT, identb, ptr, sc1, sh1, xt, u32, ub, c):
        v = 1 if c < NCTX else 0
        self.ld(xt, xt[:], xcur[c * 128:(c + 1) * 128, :])
        self.V(lambda e: e.tensor_tensor(out=u32[:], in0=xt[:], in1=sc1[v][:], op=ALU.mult), [xt, sc1[v]], [u32])
        self.G(lambda e: e.tensor_tensor(out=ub[:], in0=u32[:], in1=sh1[v][:], op=ALU.add), [u32, sh1[v]], [ub])
        for k in range(8):
            self.P(lambda e, k=k: e.transpose(ptr[:, k, :], ub[:, k * 128:(k + 1) * 128], identb[:]),
                   [ub, identb], [ptr])

    def load_mod_rows(self, lo, names):
        out = {}
        for nm, idx in names:
            tl = []
            for v in range(2):
                t = self.fw.sb("row_%s%d" % (nm, v), [128, D], F32)
                self.ld_row(t, t[:], self.modrow[v:v + 1, idx * D:(idx + 1) * D])
                tl.append(t)
            out[nm] = tl
        return out

    def phase_in_ssd(self, li, j, xcur):
        fw = self.fw
        with fw.scope():
            identb = fw.sb("identb", [128, 128], BF16)
            self.ld(identb, identb[:], self.c_identb)
            rows = self.load_mod_rows(0, [("sh1", 0), ("sc1", 1)])
            sh1, sc1 = rows["sh1"], rows["sc1"]
            for v in range(2):
                self.V(lambda e, v=v: e.tensor_scalar_add(out=sc1[v][:], in0=sc1[v][:], scalar1=1.0), [sc1[v]], [sc1[v]])
            uT = fw.sb("uT", [128, 8, T], BF16)
            xt = fw.sb("xt", [128, D], F32)
            u32 = fw.sb("u32", [128, D], F32)
            ub = fw.sb("ub", [128, D], BF16)
            ptr = fw.ps("ptr", [128, 8, 128], BF16)
            for c in range(NCH):
                self.make_uT(xcur, uT, identb, ptr, sc1, sh1, xt, u32, ub, c)
                self.A(lambda e, c=c: e.copy(out=uT[:, :, c * 128:(c + 1) * 128], in_=ptr[:]), [ptr], [uT])
            cw = fw.sb("cw", [128, 24, 5], F32)
            cb = fw.sb("cb", [128, 24], F32)
            self.ld(cw, cw[:], self.convw[j])
            self.ld(cb, cb[:], self.convb[j])
            wst_ = [fw.sb("wstA%d" % i, [128, 8, 128], F32) for i in range(2)]
            wb_ = [fw.sb("wbA%d" % i, [128, 8, 128], BF16) for i in range(2)]
            raw_ = [fw.sb("raw%d" % i, [128, T], F32) for i in range(2)]
            o_single = fw.sb("o", [128, T], F32)
            o_ = [o_single, o_single]
            ob_ = [fw.sb("ob%d" % i, [128, T], BF16) for i in range(2)]
            pp = [fw.ps("pp%d" % i, [128, 512], F32) for i in range(2)]
            trs_ = [fw.sb("trs%d" % i, [128, 8, 128], BF16) for i in range(2)]
            wv = self.ssd_in_w[j].rearrange("(k p) n -> p k n", p=128)
            segs = [(0, 256)] + [(256 + i * 512, 256 + (i + 1) * 512) for i in range(8)]
            seqs = [(0, 256), (256, T)]
            def loadW(f):
                wst = wst_[f % 2]
                col0 = 2048 + f * 128
                self.ld(wst, wst[:], wv[:, :, col0:col0 + 128])

            def stepA(f):
                wst, wb, raw, o, ob = wst_[f % 2], wb_[f % 2], raw_[f % 2], o_[f % 2], ob_[f % 2]
                self.G(lambda e, wb=wb, wst=wst: e.tensor_copy(out=wb[:], in_=wst[:]), [wst], [wb])
                for si, (a, b) in enumerate(segs):
                    p = pp[si % 2]
                    for k in range(8):
                        self.P(lambda e, k=k, a=a, b=b, p=p, wb=wb: e.matmul(p[:, 0:b - a], lhsT=wb[:, k, :], rhs=uT[:, k, a:b],
                                                                       start=(k == 0), stop=(k == 7)), [wb, uT], [p])
                    self.A(lambda e, a=a, b=b, p=p, raw=raw: e.copy(out=raw[:, a:b], in_=p[:, 0:b - a]), [p], [raw])

            def stepB1(f):
                wst, wb, raw, o, ob = wst_[f % 2], wb_[f % 2], raw_[f % 2], o_[f % 2], ob_[f % 2]
                self.A(lambda e, f=f, o=o, raw=raw: e.activation(out=o[:], in_=raw[:], func=AF.Identity,
                                                   bias=cb[:, f:f + 1], scale=cw[:, f, 2:3]), [raw, cw, cb], [o])

            def stepB(f):
                wst, wb, raw, o, ob = wst_[f % 2], wb_[f % 2], raw_[f % 2], o_[f % 2], ob_[f % 2]
                for (a, b) in seqs:
                    for kk, off in ((0, -2), (1, -1), (3, 1), (4, 2)):
                        if off < 0:
                            osl = (a - off, b)
                            isl = (a, b + off)
                        else:
                            osl = (a, b - off)
                            isl = (a + off, b)
                        self.V(lambda e, f=f, kk=kk, osl=osl, isl=isl, o=o, raw=raw: e.scalar_tensor_tensor(
                            out=o[:, osl[0]:osl[1]], in0=raw[:, isl[0]:isl[1]], scalar=cw[:, f, kk:kk + 1],
                            in1=o[:, osl[0]:osl[1]], op0=ALU.mult, op1=ALU.add), [raw, cw, o], [o])
                self.A(lambda e, o=o, ob=ob: e.activation(out=ob[:], in_=o[:], func=AF.Silu), [o], [ob])
                if f >= 16:
                    g = (f - 16) % 4
                    dst = self.KT if f < 20 else self.QT
                    self.st(dst[g, 0], ob, ob[:])
                if f < 20:
                    dstm = self.Vtm if f < 16 else self.Ktm
                    fc = f if f < 16 else f - 16
                    dv = dstm.rearrange("(c p) f -> p c f", p=128)
                    for c0 in range(0, NCH, 8):
                        trs = trs_[(c0 // 8) % 2]
                        n = min(8, NCH - c0)
                        for cc in range(n):
                            self.P(lambda e, cc=cc, c0=c0, ob=ob: e.transpose(ptr[:, cc, :], ob[:, (c0 + cc) * 128:(c0 + cc + 1) * 128],
                                                                       identb[:]), [ob, identb], [ptr])
                        self.V(lambda e, n=n, trs=trs: e.tensor_copy(out=trs[:, 0:n, :], in_=ptr[:, 0:n, :]), [ptr], [trs])
                        self.st(dv[:, c0:c0 + n, fc * 128:(fc + 1) * 128], trs, trs[:, 0:n, :])

            loadW(0)
            loadW(1)
            stepA(0)
            for f in range(24):
                stepB1(f)
                if f + 1 < 24:
                    stepA(f + 1)
                if f + 2 < 24:
                    loadW(f + 2)
                stepB(f)

            wst2 = fw.sb("wst2", [128, 8, 512], F32)
            wb2 = fw.sb("wb2", [128, 8, 512], BF16)
            zt = fw.sb("zt", [128, 512], F32)
            for n in range(4):
                self.ld(wst2, wst2[:], wv[:, :, n * 512:(n + 1) * 512])
                self.G(lambda e: e.tensor_copy(out=wb2[:], in_=wst2[:]), [wst2], [wb2])
                for c in range(NCH):
                    p = pp[c % 2]
                    for k in range(8):
                        self.P(lambda e, k=k, c=c, p=p: e.matmul(p[:], lhsT=uT[:, k, c * 128:(c + 1) * 128], rhs=wb2[:, k, :],
                                                                 start=(k == 0), stop=(k == 7)), [uT, wb2], [p])
                    self.A(lambda e, p=p: e.copy(out=zt[:], in_=p[:]), [p], [zt])
                    self.st(self.gate[c * 128:(c + 1) * 128, n * 512:(n + 1) * 512], zt, zt[:])
            dtb = fw.sb("dtb", [128, 64], F32)
            nega = fw.sb("nega", [128, 64], F32)
            self.ld_row(dtb, dtb[:], self.ssd_dt_bias[j:j + 1, :])
            self.ld_row(nega, nega[:], self.ssd_a_log[j:j + 1, :])
            self.A(lambda e: e.activation(out=nega[:], in_=nega[:], func=AF.Exp), [nega], [nega])
            self.V(lambda e: e.tensor_scalar_mul(out=nega[:], in0=nega[:], scalar1=-1.0), [nega], [nega])
            self.ld(wst2, wst2[:, :, 0:64], wv[:, :, 5120:5184])
            self.G(lambda e: e.tensor_copy(out=wb2[:, :, 0:64], in_=wst2[:, :, 0:64]), [wst2], [wb2])
            d0 = fw.sb("d0", [128, 64], F32)
            d1 = fw.sb("d1", [128, 64], F32)
            d2 = fw.sb("d2", [128, 64], F32)
            for c in range(NCH):
                p = pp[c % 2]
                for k in range(8):
                    self.P(lambda e, k=k, c=c, p=p: e.matmul(p[:, 0:64], lhsT=uT[:, k, c * 128:(c + 1) * 128], rhs=wb2[:, k, 0:64],
                                                             start=(k == 0), stop=(k == 7)), [uT, wb2], [p])
                self.V(lambda e, p=p: e.tensor_tensor(out=d0[:], in0=p[:, 0:64], in1=dtb[:], op=ALU.add), [p, dtb], [d0])
                self.V(lambda e: e.tensor_scalar_mul(out=d1[:], in0=d0[:], scalar1=-1.0), [d0], [d1])
                self.V(lambda e: e.tensor_tensor(out=d1[:], in0=d1[:], in1=d0[:], op=ALU.max), [d0, d1], [d1])
                self.A(lambda e: e.activation(out=d1[:], in_=d1[:], func=AF.Exp, scale=-1.0), [d1], [d1])
                self.A(lambda e: e.activation(out=d1[:], in_=d1[:], func=AF.Ln, bias=1.0), [d1], [d1])
                self.V(lambda e: e.scalar_tensor_tensor(out=d2[:], in0=d0[:], scalar=0.0, in1=d1[:], op0=ALU.max, op1=ALU.add),
                       [d0, d1], [d2])
                self.st(self.dttm[c * 128:(c + 1) * 128, :], d2, d2[:])
                self.V(lambda e: e.tensor_tensor(out=d0[:], in0=d2[:], in1=nega[:], op=ALU.mult), [d2, nega], [d0])
                self.st(self.latm[c * 128:(c + 1) * 128, :], d0, d0[:])

    def phase_in_ret(self, li, j, xcur):
        fw = self.fw
        with fw.scope():
            identb = fw.sb("identb", [128, 128], BF16)
            self.ld(identb, identb[:], self.c_identb)
            rows = self.load_mod_rows(0, [("sh1", 0), ("sc1", 1)])
            sh1, sc1 = rows["sh1"], rows["sc1"]
            for v in range(2):
                self.V(lambda e, v=v: e.tensor_scalar_add(out=sc1[v][:], in0=sc1[v][:], scalar1=1.0), [sc1[v]], [sc1[v]])
            W = fw.sb("Wret", [128, 8, 6144], BF16)
            wst = fw.sb("wstR", [128, 8, 512], F32)
            wv = self.ret_in_w[j].rearrange("(k p) n -> p k n", p=128)
            for n in range(12):
                self.ld(wst, wst[:], wv[:, :, n * 512:(n + 1) * 512])
                eng = self.G if n % 2 == 0 else self.A
                if n % 2 == 0:
                    self.G(lambda e, n=n: e.tensor_copy(out=W[:, :, n * 512:(n + 1) * 512], in_=wst[:]), [wst], [W])
                else:
                    self.A(lambda e, n=n: e.copy(out=W[:, :, n * 512:(n + 1) * 512], in_=wst[:]), [wst], [W])
            uT = fw.sb("uTs", [128, 8, 512], BF16)
            xt = fw.sb("xt", [128, D], F32)
            u32 = fw.sb("u32", [128, D], F32)
            ub = fw.sb("ub", [128, D], BF16)
            ptr = fw.ps("ptr", [128, 8, 128], BF16)
            pp = [fw.ps("pp%d" % i, [128, 512], F32) for i in range(4)]
            cs = fw.sb("cs", [128, 512], F32)
            sn = fw.sb("sn", [128, 512], F32)
            r1_ = [fw.sb("r1%d" % i, [128, 512], F32) for i in range(2)]
            r2_ = [fw.sb("r2%d" % i, [128, 512], F32) for i in range(2)]
            ta_ = [fw.sb("ta%d" % i, [128, 512], F32) for i in range(2)]
            tb_ = [fw.sb("tb%d" % i, [128, 512], F32) for i in range(2)]
            o1_ = [fw.sb("o1%d" % i, [128, 512], BF16) for i in range(2)]
            o2_ = [fw.sb("o2%d" % i, [128, 512], BF16) for i in range(2)]
            trs_ = [fw.sb("trs%d" % i, [128, 8, 128], BF16) for i in range(2)]
            zt = fw.sb("zt", [128, 512], F32)
            vb = fw.sb("vb", [128, 512], BF16)
            segs = [(0, 256)] + [(256 + i * 512, 256 + (i + 1) * 512) for i in range(8)]
            ktv = self.Ktm.rearrange("(c p) f -> p c f", p=128)
            for (a, b) in segs:
                n = b - a
                nt = n // 128
                c0 = a // 128
                for ci in range(nt):
                    self.make_uT(xcur, uT, identb, ptr, sc1, sh1, xt, u32, ub, c0 + ci)
                    self.A(lambda e, ci=ci: e.copy(out=uT[:, :, ci * 128:(ci + 1) * 128], in_=ptr[:]), [ptr], [uT])
                self.ld(cs, cs[:, 0:n], self.c_rope[0, :, a:b])
                self.ld(sn, sn[:, 0:n], self.c_rope[1, :, a:b])
                def qkA(which, h):
                    ii = (which * 4 + h) % 2
                    r1, r2, ta, tb, o1, o2, trs = r1_[ii], r2_[ii], ta_[ii], tb_[ii], o1_[ii], o2_[ii], trs_[ii]
                    base = which * 1024 + h * 256
                    for half, (pt, rr) in enumerate(((pp[ii * 2], r1), (pp[ii * 2 + 1], r2))):
                        cb0 = base + half * 128
                        for k in range(8):
                            self.P(lambda e, k=k, cb0=cb0, pt=pt: e.matmul(pt[:, 0:n], lhsT=W[:, k, cb0:cb0 + 128], rhs=uT[:, k, 0:n],
                                                                           start=(k == 0), stop=(k == 7)), [W, uT], [pt])
                        sc = 1.0 if which == 0 else 0.0625
                        self.A(lambda e, pt=pt, rr=rr, sc=sc: e.activation(out=rr[:, 0:n], in_=pt[:, 0:n], func=AF.Copy, scale=sc),
                               [pt], [rr])

                def qkB(which, h):
                    ii = (which * 4 + h) % 2
                    r1, r2, ta, tb, o1, o2, trs = r1_[ii], r2_[ii], ta_[ii], tb_[ii], o1_[ii], o2_[ii], trs_[ii]
                    base = which * 1024 + h * 256
                    self.V(lambda e, ta=ta, r1=r1: e.tensor_tensor(out=ta[:, 0:n], in0=r1[:, 0:n], in1=cs[:, 0:n], op=ALU.mult), [r1, cs], [ta])
                    self.G(lambda e, tb=tb, r2=r2: e.tensor_tensor(out=tb[:, 0:n], in0=r2[:, 0:n], in1=sn[:, 0:n], op=ALU.mult), [r2, sn], [tb])
                    self.V(lambda e, ta=ta, tb=tb, o1=o1: e.tensor_tensor(out=o1[:, 0:n], in0=ta[:, 0:n], in1=tb[:, 0:n], op=ALU.subtract), [ta, tb], [o1])
                    self.V(lambda e, ta=ta, r1=r1: e.tensor_tensor(out=ta[:, 0:n], in0=r1[:, 0:n], in1=sn[:, 0:n], op=ALU.mult), [r1, sn], [ta])
                    self.G(lambda e, tb=tb, r2=r2: e.tensor_tensor(out=tb[:, 0:n], in0=r2[:, 0:n], in1=cs[:, 0:n], op=ALU.mult), [r2, cs], [tb])
                    self.V(lambda e, ta=ta, tb=tb, o2=o2: e.tensor_tensor(out=o2[:, 0:n], in0=ta[:, 0:n], in1=tb[:, 0:n], op=ALU.add), [ta, tb], [o2])
                    dst = self.QT if which == 0 else self.KT
                    self.st(dst[h, 0, :, a:b], o1, o1[:, 0:n])
                    self.st(dst[h, 1, :, a:b], o2, o2[:, 0:n])
                    if which == 1:
                        for half, oo in enumerate((o1, o2)):
                            for ci in range(nt):
                                self.P(lambda e, ci=ci, oo=oo, half=half: e.transpose(ptr[:, half * 4 + ci, :], oo[:, ci * 128:(ci + 1) * 128],
                                                                                      identb[:]), [oo, identb], [ptr])
                        self.V(lambda e, trs=trs: e.tensor_copy(out=trs[:], in_=ptr[:]), [ptr], [trs])
                        for half in range(2):
                            col = h * 256 + half * 128
                            self.st(ktv[:, c0:c0 + nt, col:col + 128], trs, trs[:, half * 4:half * 4 + nt, :])

                wh = [(w_, h_) for w_ in range(2) for h_ in range(4)]
                qkA(*wh[0])
                for t_ in range(8):
                    if t_ + 1 < 8:
                        qkA(*wh[t_ + 1])
                    qkB(*wh[t_])
                for ci in range(nt):
                    c = c0 + ci
                    for nn in range(8):
                        p = pp[2 + nn % 2]
                        colw = 2048 + nn * 512
                        for k in range(8):
                            self.P(lambda e, k=k, ci=ci, p=p, colw=colw: e.matmul(p[:], lhsT=uT[:, k, ci * 128:(ci + 1) * 128],
                                                                                  rhs=W[:, k, colw:colw + 512], start=(k == 0), stop=(k == 7)),
                                   [uT, W], [p])
                        if nn < 4:
                            self.A(lambda e, p=p: e.copy(out=vb[:], in_=p[:]), [p], [vb])
                            self.st(self.Vtm[c * 128:(c + 1) * 128, nn * 512:(nn + 1) * 512], vb, vb[:])
                        else:
                            self.V(lambda e, p=p: e.tensor_copy(out=zt[:], in_=p[:]), [p], [zt])
                            self.st(self.gate[c * 128:(c + 1) * 128, (nn - 4) * 512:(nn - 3) * 512], zt, zt[:])

    def phase_scan(self, li, j, ssd, xcur, xnext):
        fw = self.fw
        G = 4
        Hg = 8 if ssd else 1
        KC = 1 if ssd else 2
        H = G * Hg
        dv = 2048 // H
        dk = KC * 128
        with fw.scope():
            identb = fw.sb("identb", [128, 128], BF16)
            self.ld(identb, identb[:], self.c_identb)
            ones = fw.sb("ones", [128, 128], F32)
            self.ld(ones, ones[:], self.c_ones)
            tri = fw.sb("tri", [128, 128], F32)
            Wo = fw.sb("Wo", [128, 16, D], BF16)
            wst = fw.sb("wstO", [128, 4, D], F32)
            owv = (self.ssd_out_w if ssd else self.ret_out_w)[j].rearrange("(k p) n -> p k n", p=128)
            for q in range(4):
                self.ld(wst, wst[:], owv[:, q * 4:(q + 1) * 4, :])
                self.G(lambda e, q=q: e.tensor_copy(out=Wo[:, q * 4:(q + 1) * 4, :], in_=wst[:]), [wst], [Wo])
            rows = self.load_mod_rows(0, [("g1", 2), ("sh2", 3), ("sc2", 4)])
            g1, sh2, sc2 = rows["g1"], rows["sh2"], rows["sc2"]
            for v in range(2):
                self.V(lambda e, v=v: e.tensor_scalar_add(out=sc2[v][:], in0=sc2[v][:], scalar1=1.0), [sc2[v]], [sc2[v]])
            lng = fw.sb("lng", [128, D], F32)
            lnb = fw.sb("lnb", [128, D], F32)
            self.ld_row(lng, lng[:], self.ln_mix_g[li:li + 1, :])
            self.ld_row(lnb, lnb[:], self.ln_mix_b[li:li + 1, :])
            nw = fw.sb("nw", [128, 2048], F32)
            self.ld_row(nw, nw[:], (self.ssd_norm_w if ssd else self.ret_gn_w)[j:j + 1, :])
            if ssd:
                dsk = fw.sb("dsk", [128, 32], F32)
                self.ld_row(dsk, dsk[:], self.ssd_d_skip[j:j + 1, :])
            else:
                nb_ = fw.sb("nb", [128, 2048], F32)
                self.ld_row(nb_, nb_[:], self.ret_gn_b[j:j + 1, :])
            S = fw.sb("S", [128, KC, 2048], F32)
            Sb = fw.sb("Sb", [128, KC, 2048], BF16)
            qt = fw.sb("qt", [128, G * KC, 128], BF16)
            kt = fw.sb("kt", [128, G * KC, 128], BF16)
            ktm = fw.sb("ktm", [128, G * dk], BF16)
            vt = fw.sb("vt", [128, 2048], BF16)
            la = fw.sb("la", [128, 64], F32)
            dt = fw.sb("dt", [128, 64], F32)
            acs = fw.sb("acs", [128, 2 * H], F32)
            nacs = fw.sb("nacs", [128, H], F32)
            ea = fw.sb("ea", [128, H], F32)
            wend = fw.sb("wend", [128, H], F32)
            cd = fw.sb("cd", [128, H], F32)
            E = fw.sb("E", [128, H, 128], F32)
            lt = fw.sb("lt", [128, Hg, 128], F32)
            M = fw.sb("M", [128, Hg, 128], BF16)
            cbm = fw.sb("cbm", [128, G, 128], F32)
            xdt = fw.sb("xdt", [128, 2048], BF16) if ssd else vt
            xe = fw.sb("xe", [128, 2048], BF16)
            yc = fw.sb("yc", [128, 2048], F32)
            tmp = fw.sb("tmp", [128, 512], F32)
            yfl = fw.sb("yfl", [128, 2048], F32)
            gt = fw.sb("gt", [128, 2048], F32)
            yb = fw.sb("yb", [128, 2048], BF16)
            yT = fw.sb("yT", [128, 16, 128], BF16)
            st6 = fw.sb("st6", [128, 4, 6], F32)
            mv = fw.sb("mv", [128, 4, 2], F32)
            ss = fw.sb("ss", [128, 4], F32)
            rstd = fw.sb("rstd", [128, 4], F32)
            xt = fw.sb("xt", [128, D], F32)
            t1 = fw.sb("t1", [128, D], F32)
            t2 = fw.sb("t2", [128, D], F32)
            st2 = fw.sb("st2", [128, 2, 6], F32)
            mv2 = fw.sb("mv2", [128, 2], F32)
            rs2 = fw.sb("rs2", [128, 1], F32)
            psm = fw.ps("psm", [128, 512], F32)
            pcb = fw.ps("pcb", [128, 512], F32)
            pbc = [fw.ps("pbc%d" % i, [128, 512], F32) for i in range(2)]
            pA = fw.ps("pA", [128, 1024], F32)
            pst = fw.ps("pst", [128, 512], F32)
            ptr = fw.ps("ptr", [128, 8, 128], BF16)

            qtv = self.QT.rearrange("g k p t -> p g k t")
            ktv = self.KT.rearrange("g k p t -> p g k t")

            def decay_prep(dirn):
                lad = la[:, dirn * 32:dirn * 32 + H] if ssd else la[:, 0:H]
                self.P(lambda e: e.matmul(psm[:, 0:H], lhsT=tri[:], rhs=lad, start=True, stop=True), [tri, la], [psm])
                self.P(lambda e: e.matmul(psm[:, H:2 * H], lhsT=ones[:], rhs=lad, start=True, stop=True), [ones, la], [psm])
                self.V(lambda e: e.tensor_copy(out=acs[:], in_=psm[:, 0:2 * H]), [psm], [acs])
                self.V(lambda e: e.tensor_scalar_mul(out=nacs[:], in0=acs[:, 0:H], scalar1=-1.0), [acs], [nacs])
                self.A(lambda e: e.activation(out=ea[:], in_=acs[:, 0:H], func=AF.Exp), [acs], [ea])
                self.V(lambda e: e.tensor_tensor(out=wend[:], in0=acs[:, H:2 * H], in1=acs[:, 0:H], op=ALU.subtract), [acs], [wend])
                self.A(lambda e: e.activation(out=wend[:], in_=wend[:], func=AF.Exp), [wend], [wend])
                self.A(lambda e: e.activation(out=cd[:], in_=acs[:, H:2 * H], func=AF.Exp), [acs], [cd])
                for g in range(G):
                    hs = slice(g * Hg, (g + 1) * Hg)
                    ladg = la[:, dirn * 32 + g * Hg:dirn * 32 + (g + 1) * Hg] if ssd else la[:, g:g + 1]
                    self.G(lambda e, ladg=ladg: e.tensor_tensor(out=lt[:], in0=bcast(tri[:].unsqueeze(1), [128, Hg, 128]),
                                                                in1=bcast(ladg.unsqueeze(2), [128, Hg, 128]), op=ALU.mult),
                           [tri, la], [lt])
                    for q in range((Hg + 3) // 4):
                        nh = min(4, Hg - q * 4)
                        pb = pbc[q % 2]
                        self.P(lambda e, q=q, nh=nh, pb=pb: e.matmul(pb[:, 0:nh * 128], lhsT=ones[:], rhs=lt[:, q * 4:q * 4 + nh, :],
                                                                     start=True, stop=True), [ones, lt], [pb])
                        for hh in range(nh):
                            h = g * Hg + q * 4 + hh
                            self.A(lambda e, h=h, hh=hh, pb=pb: e.activation(out=E[:, h, :], in_=pb[:, hh * 128:(hh + 1) * 128], func=AF.Exp,
                                                                            bias=nacs[:, h:h + 1], scale=1.0), [pb, nacs], [E])

            for dirn in range(2):
                order = list(range(NCH)) if dirn == 0 else [1, 0] + list(range(NCH - 1, 1, -1))
                self.ld(tri, tri[:], self.c_trif[dirn])
                self.V(lambda e: e.memset(S[:], 0.0), [], [S])
                self.G(lambda e: e.memset(Sb[:], 0.0), [], [Sb])
                if not ssd:
                    self.ld_row(la, la[:, 0:4], self.ret_decay[j, dirn:dirn + 1, :])
                    self.A(lambda e: e.activation(out=la[:, 0:4], in_=la[:, 0:4], func=AF.Exp, scale=-1.0), [la], [la])
                    self.A(lambda e: e.activation(out=la[:, 0:4], in_=la[:, 0:4], func=AF.Ln, bias=1.0), [la], [la])
                    self.V(lambda e: e.tensor_scalar_mul(out=la[:, 0:4], in0=la[:, 0:4], scalar1=-1.0), [la], [la])
                    decay_prep(dirn)
                for c in order:
                    v = 1 if c < NCTX else 0
                    cs_ = slice(c * 128, (c + 1) * 128)
                    self.ld(qt, qt[:].rearrange("p (g k) t -> p g k t", g=G), qtv[:, :, 0:KC, cs_])
                    self.ld(kt, kt[:].rearrange("p (g k) t -> p g k t", g=G), ktv[:, :, 0:KC, cs_])
                    self.ld(ktm, ktm[:], self.Ktm[cs_, 0:G * dk])
                    self.ld(vt, vt[:], self.Vtm[cs_, :])
                    if ssd:
                        self.ld(la, la[:], self.latm[cs_, :])
                        self.ld(dt, dt[:], self.dttm[cs_, :])
                        decay_prep(dirn)
                        self.V(lambda e: e.tensor_tensor(out=xdt[:].rearrange("p (h d) -> p h d", h=H), in0=vt[:].rearrange("p (h d) -> p h d", h=H),
                                                         in1=bcast(dt[:, dirn * 32:dirn * 32 + 32].unsqueeze(2), [128, H, dv]), op=ALU.mult),
                               [vt, dt], [xdt])
                    self.G(lambda e: e.tensor_tensor(out=xe[:].rearrange("p (h d) -> p h d", h=H), in0=xdt[:].rearrange("p (h d) -> p h d", h=H),
                                                     in1=bcast(wend[:].unsqueeze(2), [128, H, dv]), op=ALU.mult), [xdt, wend], [xe])
                    for g in range(G):
                        for kc in range(KC):
                            self.P(lambda e, g=g, kc=kc: e.matmul(pcb[:, g * 128:(g + 1) * 128], lhsT=kt[:, g * KC + kc, :], rhs=qt[:, g * KC + kc, :],
                                                                  start=(kc == 0), stop=(kc == KC - 1)), [kt, qt], [pcb])
                    self.V(lambda e: e.tensor_tensor(out=cbm[:], in0=pcb[:].rearrange("p (g l) -> p g l", g=G),
                                                     in1=bcast(tri[:].unsqueeze(1), [128, G, 128]), op=ALU.mult), [pcb, tri], [cbm])
                    for g in range(G):
                        gs = slice(g * 512, (g + 1) * 512)
                        self.V(lambda e, g=g: e.scalar_tensor_tensor(out=M[:], in0=E[:, g * Hg:(g + 1) * Hg, :], scalar=1.0,
                                                                     in1=bcast(cbm[:, g:g + 1, :], [128, Hg, 128]), op0=ALU.min, op1=ALU.mult),
                               [E, cbm], [M])
                        for hh in range(Hg):
                            h = g * Hg + hh
                            self.P(lambda e, h=h, hh=hh: e.matmul(pA[:, h * dv:(h + 1) * dv] if False else pA[:, (h * dv) % 512:(h * dv) % 512 + dv],
                                                                  lhsT=M[:, hh, :], rhs=xdt[:, h * dv:(h + 1) * dv], start=True, stop=True),
                                   [M, xdt], [pA])
                        for kc in range(KC):
                            self.P(lambda e, g=g, kc=kc, gs=gs: e.matmul(pA[:, 512:1024], lhsT=qt[:, g * KC + kc, :], rhs=Sb[:, kc, gs],
                                                                         start=(kc == 0), stop=(kc == KC - 1)), [qt, Sb], [pA])
                        self.V(lambda e, g=g: e.tensor_tensor(out=tmp[:].rearrange("p (h d) -> p h d", h=Hg),
                                                              in0=pA[:, 512:1024].rearrange("p (h d) -> p h d", h=Hg),
                                                              in1=bcast(ea[:, g * Hg:(g + 1) * Hg].unsqueeze(2), [128, Hg, dv]), op=ALU.mult),
                               [pA, ea], [tmp])
                        self.V(lambda e, gs=gs: e.tensor_tensor(out=yc[:, gs], in0=tmp[:], in1=pA[:, 0:512], op=ALU.add), [tmp, pA], [yc])
                        for kc in range(KC):
                            self.P(lambda e, g=g, kc=kc, gs=gs: e.matmul(pst[:], lhsT=ktm[:, g * dk + kc * 128:g * dk + (kc + 1) * 128], rhs=xe[:, gs],
                                                                         start=True, stop=True), [ktm, xe], [pst])
                            self.V(lambda e, g=g, kc=kc, gs=gs: e.tensor_tensor(out=S[:, kc, gs].rearrange("p (h d) -> p h d", h=Hg),
                                                                                in0=S[:, kc, gs].rearrange("p (h d) -> p h d", h=Hg),
                                                                                in1=bcast(cd[:, g * Hg:(g + 1) * Hg].unsqueeze(2), [128, Hg, dv]),
                                                                                op=ALU.mult), [S, cd], [S])
                            self.V(lambda e, kc=kc, gs=gs: e.tensor_tensor(out=S[:, kc, gs], in0=S[:, kc, gs], in1=pst[:], op=ALU.add), [S, pst], [S])
                            self.A(lambda e, kc=kc, gs=gs: e.copy(out=Sb[:, kc, gs], in_=S[:, kc, gs]), [S], [Sb])
                    if dirn == 0:
                        self.st(self.yf[cs_, :], yc, yc[:])
                        continue
                    self.ld(yfl, yfl[:], self.yf[cs_, :])
                    self.ld(gt, gt[:], self.gate[cs_, :])
                    self.V(lambda e: e.tensor_tensor(out=yc[:], in0=yc[:], in1=yfl[:], op=ALU.add), [yc, yfl], [yc])
                    if ssd:
                        self.G(lambda e: e.tensor_tensor(out=yfl[:].rearrange("p (h d) -> p h d", h=H), in0=vt[:].rearrange("p (h d) -> p h d", h=H),
                                                         in1=bcast(dsk[:].unsqueeze(2), [128, H, dv]), op=ALU.mult), [vt, dsk], [yfl])
                        self.V(lambda e: e.tensor_tensor(out=yc[:], in0=yc[:], in1=yfl[:], op=ALU.add), [yc, yfl], [yc])
                        self.A(lambda e: e.activation(out=gt[:], in_=gt[:], func=AF.Silu), [gt], [gt])
                        self.V(lambda e: e.tensor_tensor(out=yc[:], in0=yc[:], in1=gt[:], op=ALU.mult), [yc, gt], [yc])
                        for g in range(4):
                            self.V(lambda e, g=g: e.bn_stats(out=st6[:, g, :], in_=yc[:, g * 512:(g + 1) * 512]), [yc], [st6])
                            self.V(lambda e, g=g: e.bn_aggr(out=mv[:, g, :], in_=st6[:, g, :]), [st6], [mv])
                        self.V(lambda e: e.tensor_tensor(out=ss[:], in0=mv[:, :, 0], in1=mv[:, :, 0], op=ALU.mult), [mv], [ss])
                        self.V(lambda e: e.tensor_tensor(out=ss[:], in0=ss[:], in1=mv[:, :, 1], op=ALU.add), [ss, mv], [ss])
                        self.A(lambda e: e.activation(out=ss[:], in_=ss[:], func=AF.Sqrt, bias=EPS), [ss], [ss])
                        self.V(lambda e: e.reciprocal(out=rstd[:], in_=ss[:]), [ss], [rstd])
                        for g in range(4):
                            gs = slice(g * 512, (g + 1) * 512)
                            self.V(lambda e, g=g, gs=gs: e.scalar_tensor_tensor(out=yb[:, gs], in0=yc[:, gs], scalar=rstd[:, g:g + 1], in1=nw[:, gs],
                                                                                op0=ALU.mult, op1=ALU.mult), [yc, rstd, nw], [yb])
                    else:
                        for g in range(4):
                            self.V(lambda e, g=g: e.bn_stats(out=st6[:, g, :], in_=yc[:, g * 512:(g + 1) * 512]), [yc], [st6])
                            self.V(lambda e, g=g: e.bn_aggr(out=mv[:, g, :], in_=st6[:, g, :]), [st6], [mv])
                        self.A(lambda e: e.activation(out=ss[:], in_=mv[:, :, 1], func=AF.Sqrt, bias=EPS), [mv], [ss])
                        self.V(lambda e: e.reciprocal(out=rstd[:], in_=ss[:]), [ss], [rstd])
                        self.A(lambda e: e.activation(out=gt[:], in_=gt[:], func=AF.Silu), [gt], [gt])
                        for g in range(4):
                            gs = slice(g * 512, (g + 1) * 512)
                            self.V(lambda e, g=g, gs=gs: e.tensor_scalar(out=yc[:, gs], in0=yc[:, gs], scalar1=mv[:, g, 0:1], scalar2=rstd[:, g:g + 1],
                                                                         op0=ALU.subtract, op1=ALU.mult), [yc, mv, rstd], [yc])
                        self.G(lambda e: e.tensor_tensor(out=yc[:], in0=yc[:], in1=nw[:], op=ALU.mult), [yc, nw], [yc])
                        self.V(lambda e: e.tensor_tensor(out=yc[:], in0=yc[:], in1=nb_[:], op=ALU.add), [yc, nb_], [yc])
                        self.V(lambda e: e.tensor_tensor(out=yb[:], in0=yc[:], in1=gt[:], op=ALU.mult), [yc, gt], [yb])
                    for half in range(2):
                        for kk in range(8):
                            k = half * 8 + kk
                            self.P(lambda e, k=k, kk=kk: e.transpose(ptr[:, kk, :], yb[:, k * 128:(k + 1) * 128], identb[:]), [yb, identb], [ptr])
                        self.A(lambda e, half=half: e.copy(out=yT[:, half * 8:(half + 1) * 8, :], in_=ptr[:]), [ptr], [yT])
                    for nn in range(2):
                        for k in range(16):
                            self.P(lambda e, k=k, nn=nn: e.matmul(pA[:, nn * 512:(nn + 1) * 512], lhsT=yT[:, k, :], rhs=Wo[:, k, nn * 512:(nn + 1) * 512],
                                                                  start=(k == 0), stop=(k == 15)), [yT, Wo], [pA])
                    self.ld(xt, xt[:], xcur[cs_, :])
                    self.V(lambda e: e.tensor_tensor(out=t1[:], in0=pA[:], in1=g1[v][:], op=ALU.mult), [pA, g1[v]], [t1])
                    self.V(lambda e: e.scalar_tensor_tensor(out=t2[:], in0=xt[:], scalar=ALPHA, in1=t1[:], op0=ALU.mult, op1=ALU.add), [xt, t1], [t2])
                    self.layer_norm(t2, t1, st2, mv2, rs2, lng, lnb)
                    self.st(xnext[cs_, :], t1, t1[:])
                    self.G(lambda e: e.tensor_tensor(out=t2[:], in0=t1[:], in1=sc2[v][:], op=ALU.mult), [t1, sc2[v]], [t2])
                    self.V(lambda e: e.tensor_tensor(out=t2[:], in0=t2[:], in1=sh2[v][:], op=ALU.add), [t2, sh2[v]], [t2])
                    self.st(self.tok[cs_, :], t2, t2[:])
                if dirn == 0:
                    fw.barrier()

    def layer_norm(self, xin_t, out_t, st2, mv2, rs2, lng, lnb):
        for hf in range(2):
            self.V(lambda e, hf=hf: e.bn_stats(out=st2[:, hf, :], in_=xin_t[:, hf * 512:(hf + 1) * 512]), [xin_t], [st2])
        self.V(lambda e: e.bn_aggr(out=mv2[:], in_=st2[:].rearrange("p a b -> p (a b)")), [st2], [mv2])
        self.A(lambda e: e.activation(out=rs2[:], in_=mv2[:, 1:2], func=AF.Sqrt, bias=EPS), [mv2], [rs2])
        self.V(lambda e: e.reciprocal(out=rs2[:], in_=rs2[:]), [rs2], [rs2])
        self.V(lambda e: e.tensor_scalar(out=out_t[:], in0=xin_t[:], scalar1=mv2[:, 0:1], scalar2=rs2[:, 0:1],
                                         op0=ALU.subtract, op1=ALU.mult), [xin_t, mv2, rs2], [out_t])
        self.G(lambda e: e.tensor_tensor(out=out_t[:], in0=out_t[:], in1=lng[:], op=ALU.mult), [out_t, lng], [out_t])
        self.V(lambda e: e.tensor_tensor(out=out_t[:], in0=out_t[:], in1=lnb[:], op=ALU.add), [out_t, lnb], [out_t])


def host_consts():
    bf = ml_dtypes.bfloat16
    c = {}
    c["c_identb"] = np.eye(128, dtype=np.float32).astype(bf)
    c["c_identf"] = np.eye(128, dtype=np.float32)
    s = np.arange(128)
    c["c_trif"] = np.stack([(s[:, None] <= s[None, :]), (s[:, None] >= s[None, :])]).astype(np.float32)
    c["c_ones"] = np.ones((128, 128), np.float32)
    c["c_slow"] = (s[:, None] < s[None, :]).astype(np.float32).astype(bf)
    n_freq = 64
    inv_freq = (10000.0 ** (-np.arange(n_freq, dtype=np.float32) / np.float32(n_freq))).astype(np.float32)
    pos = np.arange(4096)
    rows = (pos // 64).astype(np.float32)
    cols = (pos % 64).astype(np.float32)
    ang = np.concatenate([rows[:, None] * inv_freq[None, :], cols[:, None] * inv_freq[None, :]], -1).astype(np.float32)
    cos = np.ones((T, 128), np.float32)
    sin = np.zeros((T, 128), np.float32)
    cos[256:] = np.cos(ang)
    sin[256:] = np.sin(ang)
    c["c_rope"] = np.ascontiguousarray(np.stack([cos.T, sin.T])).astype(np.float32)
    c["c_iota"] = np.broadcast_to(np.arange(512, dtype=np.float32)[None, :], (128, 512)).copy()
    c["c_pidx"] = (np.arange(12)[None, :] * 128 + np.arange(128)[:, None]).astype(np.float32)
    return c


def host_inputs(inp, b):
    f = np.float32
    m = {}
    m["xin"] = np.ascontiguousarray(np.concatenate([inp["ctx"][b], inp["x"][b]], 0)).astype(f)
    cv = np.stack([inp["c"][b].reshape(8, 128).T, inp["c_ctx"].reshape(8, 128).T], -1)
    m["cvec"] = np.ascontiguousarray(cv).astype(f)
    m["mod_w"] = inp["mod_w"]
    m["mod_b"] = inp["mod_b"]
    m["ssd_in_w"] = inp["ssd_in_w"]
    cw = inp["ssd_conv_w"]
    m["convw"] = np.ascontiguousarray(cw.reshape(2, 5, 24, 128).transpose(0, 3, 2, 1)).astype(f)
    m["convb"] = np.ascontiguousarray(inp["ssd_conv_b"].reshape(2, 24, 128).transpose(0, 2, 1)).astype(f)
    m["ssd_dt_bias"] = np.ascontiguousarray(inp["ssd_dt_bias"].reshape(2, 64))
    m["ssd_a_log"] = np.ascontiguousarray(inp["ssd_a_log"].reshape(2, 64))
    m["ssd_d_skip"] = inp["ssd_d_skip"]
    m["ssd_norm_w"] = inp["ssd_norm_w"]
    m["ssd_out_w"] = inp["ssd_out_w"]
    m["ret_in_w"] = inp["ret_in_w"]
    m["ret_decay_logit"] = inp["ret_decay_logit"]
    m["ret_gn_w"] = inp["ret_gn_w"]
    m["ret_gn_b"] = inp["ret_gn_b"]
    m["ret_out_w"] = inp["ret_out_w"]
    for k in ("ln_mix_g", "ln_mix_b", "ln_ffn_g", "ln_ffn_b"):
        m[k] = inp[k]
    m["moe_rw"] = np.ascontiguousarray(np.concatenate([inp["moe_group_w"], inp["moe_expert_w"]], -1)).astype(f)
    m["moe_rb"] = np.ascontiguousarray(np.concatenate([inp["moe_group_b"], inp["moe_expert_b"]], -1)).astype(f)
    m["moe_w_gate_up"] = inp["moe_w_gate_up"].reshape(DEPTH * 32 * 128, 8 * D)
    m["moe_w_down"] = inp["moe_w_down"].reshape(DEPTH * 32 * 128, 4 * D)
    return m


def _phase_moe(self, li, xcur, xnext, final):
    fw = self.fw
    NT = NCH
    with fw.scope():
        DI = fw.sb("DI", [128, 2 * NT], I32)
        Wt = fw.sb("Wt", [128, 2 * NT], F32)
        IGU = fw.sb("IGU", [128, NB], I32)
        with fw.scope():
            identf = fw.sb("identf", [128, 128], F32)
            self.ld(identf, identf[:], self.c_identf)
            slow = fw.sb("slow", [128, 128], BF16)
            self.ld(slow, slow[:], self.c_slow)
            onesf = fw.sb("onesf", [128, 128], F32)
            self.ld(onesf, onesf[:], self.c_ones)
            onesb = fw.sb("onesb", [128, 128], BF16)
            self.V(lambda e: e.tensor_copy(out=onesb[:], in_=onesf[:]), [onesf], [onesb])
            iota = fw.sb("iota", [128, 512], F32)
            self.ld(iota, iota[:], self.c_iota)
            pidx = fw.sb("pidx", [128, 12], F32)
            self.ld(pidx, pidx[:], self.c_pidx)
            rw = fw.sb("rw", [128, 8, 36], F32)
            self.ld(rw, rw[:], self.moe_rw[li].rearrange("(k p) e -> p k e", p=128))
            rb = fw.sb("rb", [128, 36], F32)
            self.ld_row(rb, rb[:], self.moe_rb[li:li + 1, :])
            OH = fw.sb("OH", [128, 2 * NT, 32], F32)
            SL = fw.sb("SL", [128, 2 * NT], F32)
            Acum = fw.sb("Acum", [128, 32], F32)
            Acb = fw.sb("Acb", [128, 32], BF16)
            self.V(lambda e: e.memset(Acum[:], 0.0), [], [Acum])
            self.V(lambda e: e.memset(Acb[:], 0.0), [], [Acb])
            tk = fw.sb("tk", [128, D], F32)
            tkT = fw.sb("tkT", [128, 8, 128], F32)
            L = fw.sb("L", [128, 36], F32)
            gmax = fw.sb("gmax", [128, 1], F32)
            ngmax = fw.sb("ngmax", [128, 1], F32)
            goh = fw.sb("goh", [128, 4], F32)
            pen = fw.sb("pen", [128, 4], F32)
            ex = fw.sb("ex", [128, 4], F32)
            gs = fw.sb("gs", [128, 1], F32)
            em = fw.sb("em", [128, 32], F32)
            em2 = fw.sb("em2", [128, 32], F32)
            m1 = fw.sb("m1", [128, 1], F32)
            m2 = fw.sb("m2", [128, 1], F32)
            dd = fw.sb("dd", [128, 1], F32)
            den = fw.sb("den", [128, 1], F32)
            A_ = fw.sb("A_", [128, 32], F32)
            Ab = fw.sb("Ab", [128, 32], BF16)
            Pp = fw.sb("Pp", [128, 32], F32)
            junk = fw.sb("junk", [128, 32], F32)
            pT = fw.ps("pT", [128, 8, 128], F32)
            plog = fw.ps("plog", [128, 512], F32)
            pP = fw.ps("pP", [128, 512], F32)
            for c in range(NT):
                cs_ = slice(c * 128, (c + 1) * 128)
                self.ld(tk, tk[:], self.tok[cs_, :])
                for k in range(8):
                    self.P(lambda e, k=k: e.transpose(pT[:, k, :], tk[:, k * 128:(k + 1) * 128], identf[:]), [tk, identf], [pT])
                self.A(lambda e: e.copy(out=tkT[:], in_=pT[:]), [pT], [tkT])
                for k in range(8):
                    self.P(lambda e, k=k: e.matmul(plog[:, 0:36], lhsT=tkT[:, k, :], rhs=rw[:, k, :], start=(k == 0), stop=(k == 7)),
                           [tkT, rw], [plog])
                self.V(lambda e: e.tensor_tensor(out=L[:], in0=plog[:, 0:36], in1=rb[:], op=ALU.add), [plog, rb], [L])
                self.V(lambda e: e.reduce_max(out=gmax[:], in_=L[:, 0:4], axis=AX.X), [L], [gmax])
                self.V(lambda e: e.tensor_scalar(out=goh[:], in0=L[:, 0:4], scalar1=gmax[:, 0:1], scalar2=None, op0=ALU.is_equal), [L, gmax], [goh])
                self.V(lambda e: e.tensor_scalar_mul(out=ngmax[:], in0=gmax[:], scalar1=-1.0), [gmax], [ngmax])
                self.A(lambda e: e.activation(out=ex[:], in_=L[:, 0:4], func=AF.Exp, bias=ngmax[:, 0:1], scale=1.0), [L, ngmax], [ex])
                self.V(lambda e: e.reduce_sum(out=gs[:], in_=ex[:], axis=AX.X), [ex], [gs])
                self.V(lambda e: e.reciprocal(out=gs[:], in_=gs[:]), [gs], [gs])
                self.V(lambda e: e.tensor_scalar(out=pen[:], in0=goh[:], scalar1=1e30, scalar2=-1e30, op0=ALU.mult, op1=ALU.add), [goh], [pen])
                self.V(lambda e: e.tensor_tensor(out=em[:].rearrange("p (g j) -> p g j", g=4), in0=L[:, 4:36].rearrange("p (g j) -> p g j", g=4),
                                                 in1=bcast(pen[:].unsqueeze(2), [128, 4, 8]), op=ALU.add), [L, pen], [em])
                self.V(lambda e: e.reduce_max(out=m1[:], in_=em[:], axis=AX.X), [em], [m1])
                o1 = OH[:, 2 * c, :]
                o2 = OH[:, 2 * c + 1, :]
                self.V(lambda e, o1=o1: e.tensor_scalar(out=o1, in0=em[:], scalar1=m1[:, 0:1], scalar2=None, op0=ALU.is_equal), [em, m1], [OH])
                self.V(lambda e, o1=o1: e.scalar_tensor_tensor(out=em2[:], in0=o1, scalar=-1e30, in1=em[:], op0=ALU.mult, op1=ALU.add), [OH, em], [em2])
                self.V(lambda e: e.reduce_max(out=m2[:], in_=em2[:], axis=AX.X), [em2], [m2])
                self.V(lambda e, o2=o2: e.tensor_scalar(out=o2, in0=em2[:], scalar1=m2[:, 0:1], scalar2=None, op0=ALU.is_equal), [em2, m2], [OH])
                self.V(lambda e: e.tensor_tensor(out=dd[:], in0=m2[:], in1=m1[:], op=ALU.subtract), [m1, m2], [dd])
                self.A(lambda e: e.activation(out=dd[:], in_=dd[:], func=AF.Exp), [dd], [dd])
                self.V(lambda e: e.tensor_scalar_add(out=den[:], in0=dd[:], scalar1=1.0), [dd], [den])
                self.V(lambda e: e.reciprocal(out=den[:], in_=den[:]), [den], [den])
                self.V(lambda e, c=c: e.tensor_tensor(out=Wt[:, 2 * c:2 * c + 1], in0=den[:], in1=gs[:], op=ALU.mult), [den, gs], [Wt])
                self.V(lambda e, c=c: e.tensor_tensor(out=Wt[:, 2 * c + 1:2 * c + 2], in0=Wt[:, 2 * c:2 * c + 1], in1=dd[:], op=ALU.mult), [Wt, dd], [Wt])
                self.V(lambda e, o1=o1, o2=o2: e.tensor_tensor(out=A_[:], in0=o1, in1=o2, op=ALU.add), [OH], [A_])
                self.V(lambda e: e.tensor_copy(out=Ab[:], in_=A_[:]), [A_], [Ab])
                self.P(lambda e: e.matmul(pP[:, 0:32], lhsT=slow[:], rhs=Ab[:], start=True, stop=False), [slow, Ab], [pP])
                self.P(lambda e: e.matmul(pP[:, 0:32], lhsT=onesb[:], rhs=Acb[:], start=False, stop=True), [onesb, Acb], [pP])
                self.V(lambda e: e.tensor_copy(out=Pp[:], in_=pP[:, 0:32]), [pP], [Pp])
                for k, ok in enumerate((o1, o2)):
                    self.V(lambda e, ok=ok: e.tensor_tensor(out=junk[:], in0=ok, in1=Pp[:], op=ALU.mult), [OH, Pp], [junk])
                    self.V(lambda e, c=c, k=k: e.reduce_sum(out=SL[:, 2 * c + k:2 * c + k + 1], in_=junk[:], axis=AX.X), [junk], [SL])
                self.V(lambda e: e.tensor_tensor(out=Acum[:], in0=Acum[:], in1=A_[:], op=ALU.add), [Acum, A_], [Acum])
                self.V(lambda e: e.tensor_copy(out=Acb[:], in_=Acum[:]), [Acum], [Acb])
            cnt = fw.sb("cnt", [128, 32], F32)
            self.P(lambda e: e.matmul(pP[:, 0:32], lhsT=onesb[:], rhs=Acb[:], start=True, stop=True), [onesb, Acb], [pP])
            self.V(lambda e: e.tensor_copy(out=cnt[:], in_=pP[:, 0:32]), [pP], [cnt])
            thr = fw.sb("thr", [128, 34], F32)
            self.V(lambda e: e.tensor_scalar_mul(out=thr[:], in0=iota[:, 0:34], scalar1=128.0), [iota], [thr])
            cmp = fw.sb("cmp", [128, 32, 34], F32)
            self.V(lambda e: e.tensor_tensor(out=cmp[:], in0=bcast(cnt[:].unsqueeze(2), [128, 32, 34]), in1=bcast(thr[:].unsqueeze(1), [128, 32, 34]),
                                             op=ALU.is_gt), [cnt, thr], [cmp])
            nblk = fw.sb("nblk", [128, 32], F32)
            self.V(lambda e: e.reduce_sum(out=nblk[:], in_=cmp[:], axis=AX.X), [cmp], [nblk])
            pa = fw.sb("pa", [128, 32], F32)
            pb_ = fw.sb("pb", [128, 32], F32)
            self.V(lambda e: e.tensor_copy(out=pa[:], in_=nblk[:]), [nblk], [pa])
            cur, oth = pa, pb_
            for sft in (1, 2, 4, 8, 16):
                self.V(lambda e, cur=cur, oth=oth: e.tensor_copy(out=oth[:], in_=cur[:]), [cur], [oth])
                self.V(lambda e, cur=cur, oth=oth, sft=sft: e.tensor_tensor(out=oth[:, sft:32], in0=cur[:, sft:32], in1=cur[:, 0:32 - sft], op=ALU.add),
                       [cur], [oth])
                cur, oth = oth, cur
            pend = cur
            pstart = fw.sb("pstart", [128, 32], F32)
            self.V(lambda e: e.tensor_tensor(out=pstart[:], in0=pend[:], in1=nblk[:], op=ALU.subtract), [pend, nblk], [pstart])
            self.V(lambda e: e.tensor_scalar_mul(out=pstart[:], in0=pstart[:], scalar1=128.0), [pstart], [pstart])
            big = fw.sb("big", [128, 2 * NT, 32], F32)
            self.V(lambda e: e.tensor_tensor(out=big[:], in0=OH[:], in1=bcast(pstart[:].unsqueeze(1), [128, 2 * NT, 32]), op=ALU.mult), [OH, pstart], [big])
            dst = fw.sb("dstf", [128, 2 * NT], F32)
            self.V(lambda e: e.reduce_sum(out=dst[:], in_=big[:], axis=AX.X), [big], [dst])
            self.V(lambda e: e.tensor_tensor(out=dst[:], in0=dst[:], in1=SL[:], op=ALU.add), [dst, SL], [dst])
            self.V(lambda e: e.tensor_copy(out=DI[:], in_=dst[:]), [dst], [DI])
            cmp2 = fw.sb("cmp2", [128, NB, 32], F32)
            self.V(lambda e: e.tensor_tensor(out=cmp2[:], in0=bcast(pend[:].unsqueeze(1), [128, NB, 32]), in1=bcast(iota[:, 0:NB].unsqueeze(2), [128, NB, 32]),
                                             op=ALU.is_le), [pend, iota], [cmp2])
            be = fw.sb("be", [128, NB], F32)
            self.V(lambda e: e.reduce_sum(out=be[:], in_=cmp2[:], axis=AX.X), [cmp2], [be])
            self.V(lambda e: e.tensor_scalar_min(out=be[:], in0=be[:], scalar1=31.0), [be], [be])
            same = fw.sb("same", [128, NB], F32)
            self.V(lambda e: e.memset(same[:], 0.0), [], [same])
            self.V(lambda e: e.tensor_tensor(out=same[:, 2:NB], in0=be[:, 2:NB], in1=be[:, 0:NB - 2], op=ALU.is_equal), [be], [same])
            self.V(lambda e: e.tensor_scalar(out=be[:], in0=be[:], scalar1=128.0, scalar2=float(li * 32 * 128), op0=ALU.mult, op1=ALU.add), [be], [be])
            self.V(lambda e: e.scalar_tensor_tensor(out=be[:], in0=same[:], scalar=1.0e6, in1=be[:], op0=ALU.mult, op1=ALU.add), [same, be], [be])
            self.V(lambda e: e.tensor_tensor(out=be[:], in0=be[:], in1=bcast(pidx[:, 0:1], [128, NB]), op=ALU.add), [be, pidx], [be])
            self.V(lambda e: e.tensor_copy(out=IGU[:], in_=be[:]), [be], [IGU])
        with fw.scope():
            tk = fw.sb("tk", [128, D], F32)
            tkb = fw.sb("tkb", [128, D], BF16)
            for c in range(NT):
                self.ld(tk, tk[:], self.tok[c * 128:(c + 1) * 128, :])
                self.V(lambda e: e.tensor_copy(out=tkb[:], in_=tk[:]), [tk], [tkb])
                for k in range(2):
                    col = 2 * c + k
                    self.fw.dma("gpsimd", lambda e, col=col: e.indirect_dma_start(
                        out=self.xbuf, out_offset=bass.IndirectOffsetOnAxis(ap=DI[:, col:col + 1], axis=0), in_=tkb[:, :], in_offset=None),
                        reads=[tkb, DI])
        with fw.scope():
            identb = fw.sb("identb", [128, 128], BF16)
            self.ld(identb, identb[:], self.c_identb)
            xb = [fw.sb("xb%d" % i, [128, D], BF16) for i in range(2)]
            xT = [fw.sb("xT%d" % i, [128, 8, 128], BF16) for i in range(2)]
            g32 = [fw.sb("g32_%d" % i, [128, 8, D], F32) for i in range(2)]
            d32 = [fw.sb("d32_%d" % i, [128, 4, D], F32) for i in range(2)]
            gbf = [fw.sb("gbf_%d" % i, [128, 8, D], BF16) for i in range(2)]
            dbf = [fw.sb("dbf_%d" % i, [128, 4, D], BF16) for i in range(2)]
            sg = fw.sb("sg", [128, 4, 128], F32)
            hT = fw.sb("hT", [128, 4, 128], BF16)
            ob = fw.sb("ob", [128, D], F32)
            ptr = fw.ps("ptr", [128, 8, 128], BF16)
            pH = [fw.ps("pH%d" % i, [128, 8, 128], F32) for i in range(2)]
            pO = fw.ps("pO", [128, D], F32)
            if getattr(self, "_bcreg", None) is None:
                self._bcreg = self.nc.gpsimd.alloc_register("bcreg")
                self.nc.gpsimd.reg_mov(self._bcreg, DEPTH * 32 * 128 - 1)
            bcreg = self._bcreg
            for b in range(NB):
                i = b % 2
                self.ld(xb[i], xb[i][:], self.xbuf[b * 128:(b + 1) * 128, :])
                idx = IGU[:, b:b + 1]
                self.fw.dma("gpsimd", lambda e, i=i, idx=idx: e.indirect_dma_start(
                    out=g32[i][:].rearrange("p k n -> p (k n)"), out_offset=None, in_=self.moe_wgu,
                    in_offset=bass.IndirectOffsetOnAxis(ap=idx, axis=0), bounds_check=bcreg, oob_is_err=False),
                    reads=[IGU], writes=[g32[i]])
                self.fw.dma("gpsimd", lambda e, i=i, idx=idx: e.indirect_dma_start(
                    out=d32[i][:].rearrange("p k n -> p (k n)"), out_offset=None, in_=self.moe_wd,
                    in_offset=bass.IndirectOffsetOnAxis(ap=idx, axis=0), bounds_check=bcreg, oob_is_err=False),
                    reads=[IGU], writes=[d32[i]])
                xbv = xb[i][:].rearrange("s (p k) -> s k p", k=8)
                for k in range(8):
                    self.P(lambda e, k=k, xbv=xbv: e.transpose(ptr[:, k, :], xbv[:, k, :], identb[:]), [xb[i], identb], [ptr])
                self.V(lambda e, i=i: e.tensor_copy(out=xT[i][:], in_=ptr[:]), [ptr], [xT[i]])
                for q in range(4):
                    sl = slice(q * 2, q * 2 + 2)
                    if q % 2 == 0:
                        self.A(lambda e, i=i, sl=sl: e.copy(out=gbf[i][:, sl, :], in_=g32[i][:, sl, :]), [g32[i]], [gbf[i]])
                    else:
                        self.V(lambda e, i=i, sl=sl: e.tensor_copy(out=gbf[i][:, sl, :], in_=g32[i][:, sl, :]), [g32[i]], [gbf[i]])
                self.A(lambda e, i=i: e.copy(out=dbf[i][:, 0:2, :], in_=d32[i][:, 0:2, :]), [d32[i]], [dbf[i]])
                self.V(lambda e, i=i: e.tensor_copy(out=dbf[i][:, 2:4, :], in_=d32[i][:, 2:4, :]), [d32[i]], [dbf[i]])
                ph = pH[i]
                for m in range(8):
                    tt, kh = m // 4, m % 4
                    for k in range(8):
                        lw = gbf[i][:, k, :].rearrange("d (two p k) -> d two k p", two=2, k=4)[:, tt, kh, :]
                        self.P(lambda e, i=i, m=m, k=k, ph=ph, lw=lw: e.matmul(ph[:, m, :], lhsT=lw, rhs=xT[i][:, k, :],
                                                                               start=(k == 0), stop=(k == 7)), [gbf[i], xT[i]], [ph])
                self.A(lambda e, ph=ph: e.activation(out=sg[:], in_=ph[:, 0:4, :], func=AF.Silu), [ph], [sg])
                self.V(lambda e, ph=ph: e.tensor_tensor(out=hT[:], in0=sg[:], in1=ph[:, 4:8, :], op=ALU.mult), [sg, ph], [hT])
                for nn in range(2):
                    for k in range(4):
                        self.P(lambda e, i=i, nn=nn, k=k: e.matmul(pO[:, nn * 512:(nn + 1) * 512], lhsT=hT[:, k, :], rhs=dbf[i][:, k, nn * 512:(nn + 1) * 512],
                                                                   start=(k == 0), stop=(k == 3)), [hT, dbf[i]], [pO])
                self.A(lambda e: e.copy(out=ob[:], in_=pO[:]), [pO], [ob])
                self.st(self.ybuf[b * 128:(b + 1) * 128, :], ob, ob[:])
        with fw.scope():
            g2 = []
            for v in range(2):
                t = fw.sb("g2_%d" % v, [128, D], F32)
                self.ld_row(t, t[:], self.modrow[v:v + 1, 5 * D:6 * D])
                g2.append(t)
            lng = fw.sb("lng", [128, D], F32)
            lnb = fw.sb("lnb", [128, D], F32)
            self.ld_row(lng, lng[:], self.ln_ffn_g[li:li + 1, :])
            self.ld_row(lnb, lnb[:], self.ln_ffn_b[li:li + 1, :])
            o1_ = [fw.sb("o1%d" % i, [128, D], F32) for i in range(2)]
            o2_ = [fw.sb("o2%d" % i, [128, D], F32) for i in range(2)]
            xt_ = [fw.sb("xt%d" % i, [128, D], F32) for i in range(2)]
            t1_ = [fw.sb("t1%d" % i, [128, D], F32) for i in range(2)]
            st2 = fw.sb("st2", [128, 2, 6], F32)
            mv2 = fw.sb("mv2", [128, 2], F32)
            rs2 = fw.sb("rs2", [128, 1], F32)
            for c in range(NT):
                if final and c < NCTX:
                    continue
                v = 1 if c < NCTX else 0
                o1, o2, xt, t1 = o1_[c % 2], o2_[c % 2], xt_[c % 2], t1_[c % 2]
                cs_ = slice(c * 128, (c + 1) * 128)
                for k, ot in enumerate((o1, o2)):
                    col = 2 * c + k
                    self.fw.dma("gpsimd", lambda e, col=col, ot=ot: e.indirect_dma_start(
                        out=ot[:, :], out_offset=None, in_=self.ybuf, in_offset=bass.IndirectOffsetOnAxis(ap=DI[:, col:col + 1], axis=0)),
                        reads=[DI], writes=[ot])
                self.ld(xt, xt[:], xcur[cs_, :])
                self.V(lambda e, c=c, o1=o1: e.tensor_scalar(out=o1[:], in0=o1[:], scalar1=Wt[:, 2 * c:2 * c + 1], scalar2=None, op0=ALU.mult), [o1, Wt], [o1])
                self.V(lambda e, c=c, o1=o1, o2=o2: e.scalar_tensor_tensor(out=o1[:], in0=o2[:], scalar=Wt[:, 2 * c + 1:2 * c + 2], in1=o1[:], op0=ALU.mult, op1=ALU.add),
                       [o2, Wt, o1], [o1])
                self.G(lambda e, v=v, o1=o1: e.tensor_tensor(out=o1[:], in0=o1[:], in1=g2[v][:], op=ALU.mult), [o1, g2[v]], [o1])
                self.V(lambda e, o1=o1, o2=o2, xt=xt: e.scalar_tensor_tensor(out=o2[:], in0=xt[:], scalar=ALPHA, in1=o1[:], op0=ALU.mult, op1=ALU.add), [xt, o1], [o2])
                self.layer_norm(o2, t1, st2, mv2, rs2, lng, lnb)
                if final:
                    self.st(self.yout[(c - NCTX) * 128:(c - NCTX + 1) * 128, :], t1, t1[:])
                else:
                    self.st(xnext[cs_, :], t1, t1[:])


Builder.phase_moe = _phase_moe


def build_program(nlayers=DEPTH):
    nc = bass.Bass("TRN2", target_bir_lowering=False)
    b = Builder(nc)
    b.declare()
    xcur = b.xin
    for li in range(nlayers):
        j = li // 2
        b.phase_mod(li)
        if li % 2 == 0:
            b.phase_in_ssd(li, j, xcur)
            b.phase_scan(li, j, True, xcur, b.xA)
        else:
            b.phase_in_ret(li, j, xcur)
            b.phase_scan(li, j, False, xcur, b.xA)
        b.phase_moe(li, b.xA, b.xB, final=(li == nlayers - 1))
        xcur = b.xB
    b.fw.barrier()
    b.fw.root.close()
    return nc


def kernel(**inputs):
    inp = {k: np.asarray(v) for k, v in inputs.items()}
    nc = build_program()
    consts = host_consts()
    maps = []
    for c in range(8):
        m = host_inputs(inp, c)
        m.update(consts)
        maps.append(m)
    res = run_bass_kernel_spmd(nc, maps, core_ids=list(range(8)))
    out = np.stack([np.asarray(res.results[c]["yout"]) for c in range(8)], 0)
    return out.astype(np.float32)


def _phase_scan2(self, li, j, ssd, xcur, xnext):
    fw = self.fw
    G = 4
    Hg = 8 if ssd else 1
    KC = 1 if ssd else 2
    H = G * Hg
    dv = 2048 // H
    dk = KC * 128
    with fw.scope():
        identb = fw.sb("identb", [128, 128], BF16)
        self.ld(identb, identb[:], self.c_identb)
        ones = fw.sb("ones", [128, 128], F32)
        self.ld(ones, ones[:], self.c_ones)
        tri = fw.sb("tri", [128, 128], F32)
        Wo = fw.sb("Wo", [128, 16, D], BF16)
        owv = (self.ssd_out_w if ssd else self.ret_out_w)[j].rearrange("(k p) n -> p k n", p=128)
        with fw.scope():
            wst = fw.sb("wstO", [128, 4, D], F32)
            for q in range(4):
                self.ld(wst, wst[:], owv[:, q * 4:(q + 1) * 4, :])
                self.G(lambda e, q=q: e.tensor_copy(out=Wo[:, q * 4:(q + 1) * 4, :], in_=wst[:]), [wst], [Wo])
        g1 = fw.sb("g1", [128, D], F32)
        sh2 = fw.sb("sh2", [128, D], F32)
        sc2 = fw.sb("sc2", [128, D], F32)

        def load_rows(v):
            self.ld_row(g1, g1[:], self.modrow[v:v + 1, 2 * D:3 * D])
            self.ld_row(sh2, sh2[:], self.modrow[v:v + 1, 3 * D:4 * D])
            self.ld_row(sc2, sc2[:], self.modrow[v:v + 1, 4 * D:5 * D])
            self.V(lambda e: e.tensor_scalar_add(out=sc2[:], in0=sc2[:], scalar1=1.0), [sc2], [sc2])

        lng = fw.sb("lng", [128, D], F32)
        lnb = fw.sb("lnb", [128, D], F32)
        self.ld_row(lng, lng[:], self.ln_mix_g[li:li + 1, :])
        self.ld_row(lnb, lnb[:], self.ln_mix_b[li:li + 1, :])
        nw = fw.sb("nw", [128, 2048], F32)
        self.ld_row(nw, nw[:], (self.ssd_norm_w if ssd else self.ret_gn_w)[j:j + 1, :])
        if ssd:
            dsk = fw.sb("dsk", [128, 32], F32)
            self.ld_row(dsk, dsk[:], self.ssd_d_skip[j:j + 1, :])
        else:
            nb_ = fw.sb("nb", [128, 2048], F32)
            self.ld_row(nb_, nb_[:], self.ret_gn_b[j:j + 1, :])
        S = fw.sb("S", [128, KC, 2048], F32)
        Sb = fw.sb("Sb", [128, KC, 2048], BF16)

        def dbl(name, shape, dt):
            return [fw.sb(name + "0", shape, dt), fw.sb(name + "1", shape, dt)]

        def tpl(name, shape, dt):
            return [fw.sb(name + str(i_), shape, dt) for i_ in range(3)]

        qt3 = tpl("qt", [128, G * KC, 128], BF16)
        kt2 = dbl("kt", [128, G * KC, 128], BF16)
        ktm3 = tpl("ktm", [128, G * dk], BF16)
        vt3 = tpl("vt", [128, 2048], BF16)
        xdt2 = dbl("xdt", [128, 2048], BF16) if ssd else None
        xe = dbl("xe", [128, 2048], BF16)
        cbm = dbl("cbm", [128, G, 128], F32)
        la2 = dbl("la", [128, 64], F32)
        dt2 = dbl("dt", [128, 64], F32)
        acs = fw.sb("acs", [128, 2 * H], F32)
        wend = fw.sb("wend", [128, H], F32)
        if ssd:
            ea = dbl("ea", [128, H], F32)
            cd = dbl("cd", [128, H], F32)
            E = [[fw.sb("E%d_%d" % (p_, g), [128, Hg, 128], BF16) for g in range(G)] for p_ in range(2)]
        else:
            ea0 = fw.sb("ea", [128, H], F32)
            cd0 = fw.sb("cd", [128, H], F32)
            ea = [ea0, ea0]
            cd = [cd0, cd0]
            E0 = [fw.sb("E_%d" % g, [128, Hg, 128], F32) for g in range(G)]
            E = [E0, E0]
        lt = dbl("lt", [128, Hg, 128], F32)
        nlb = dbl("nlb", [128, Hg, 128], F32)
        nones = fw.sb("nones", [128, 128], F32)
        self.V(lambda e: e.memset(nones[:], -1.0), [], [nones])
        M = dbl("M", [128, Hg, 128], BF16)
        ycs = dbl("yc", [128, 2048], F32)
        tmp0 = fw.sb("tmp", [128, 512], F32)
        tmp = [tmp0, tmp0]
        yfl = fw.sb("yfl", [128, 2048], F32)
        gt = fw.sb("gt", [128, 2048], F32)
        yb = fw.sb("yb", [128, 2048], BF16)
        yT = fw.sb("yT", [128, 16, 128], BF16)
        st6 = fw.sb("st6", [128, 4, 6], F32)
        mv = fw.sb("mv", [128, 4, 2], F32)
        ss = fw.sb("ss", [128, 4], F32)
        rstd = fw.sb("rstd", [128, 4], F32)
        xt = fw.sb("xt", [128, D], F32)
        t1 = fw.sb("t1", [128, D], F32)
        t2 = fw.sb("t2", [128, D], F32)
        st2 = fw.sb("st2", [128, 2, 6], F32)
        mv2 = fw.sb("mv2", [128, 2], F32)
        rs2 = fw.sb("rs2", [128, 1], F32)
        psm = fw.ps("psm", [128, 512], F32)
        pcb = fw.ps("pcb", [128, 512], F32)
        pbc = [fw.ps("pbc%d" % i, [128, 512], F32) for i in range(2)]
        pyd = fw.ps("pyd", [128, 512], F32)
        pyo = fw.ps("pyo", [128, 512], F32)
        pst = fw.ps("pst", [128, 512], F32)
        ptr = fw.ps("ptr", [128, 8, 128], BF16)

        qtv = self.QT.rearrange("g k p t -> p g k t")
        ktv = self.KT.rearrange("g k p t -> p g k t")
        r3 = lambda ap, h: ap.rearrange("p (h d) -> p h d", h=h)

        def decay_pre(dirn, i):
            par = i % 2
            la = la2[par]
            lad = la[:, dirn * 32:dirn * 32 + H] if ssd else la[:, 0:H]
            self.P(lambda e: e.matmul(psm[:, 0:H], lhsT=tri[:], rhs=lad, start=True, stop=True), [tri, la], [psm])
            self.P(lambda e: e.matmul(psm[:, H:2 * H], lhsT=ones[:], rhs=lad, start=True, stop=True), [ones, la], [psm])
            self.V(lambda e: e.tensor_copy(out=acs[:], in_=psm[:, 0:2 * H]), [psm], [acs])
            self.A(lambda e: e.activation(out=ea[par][:], in_=acs[:, 0:H], func=AF.Exp), [acs], [ea[par]])
            self.V(lambda e: e.tensor_tensor(out=wend[:], in0=acs[:, H:2 * H], in1=acs[:, 0:H], op=ALU.subtract), [acs], [wend])
            self.A(lambda e: e.activation(out=wend[:], in_=wend[:], func=AF.Exp), [wend], [wend])
            self.A(lambda e: e.activation(out=cd[par][:], in_=acs[:, H:2 * H], func=AF.Exp), [acs], [cd[par]])

        def decay_g(dirn, i, g):
            par = i % 2
            la = la2[par]
            ltg = lt[g % 2]
            nlg = nlb[g % 2]
            ladg = la[:, dirn * 32 + g * Hg:dirn * 32 + (g + 1) * Hg] if ssd else la[:, g:g + 1]
            self.G(lambda e: e.tensor_tensor(out=ltg[:], in0=bcast(tri[:].unsqueeze(1), [128, Hg, 128]),
                                             in1=bcast(ladg.unsqueeze(2), [128, Hg, 128]), op=ALU.mult), [tri, la], [ltg])
            self.G(lambda e: e.tensor_tensor(out=nlg[:], in0=bcast(nones[:].unsqueeze(1), [128, Hg, 128]),
                                             in1=bcast(ladg.unsqueeze(2), [128, Hg, 128]), op=ALU.mult), [nones, la], [nlg])
            Eg = E[par][g]
            for q in range((Hg + 3) // 4):
                nh = min(4, Hg - q * 4)
                pb = pbc[q % 2]
                self.P(lambda e: e.matmul(pb[:, 0:nh * 128], lhsT=ones[:], rhs=ltg[:, q * 4:q * 4 + nh, :], start=True, stop=False), [ones, ltg], [pb])
                self.P(lambda e: e.matmul(pb[:, 0:nh * 128], lhsT=tri[:], rhs=nlg[:, q * 4:q * 4 + nh, :], start=False, stop=True), [tri, nlg], [pb])
                self.A(lambda e: e.activation(out=Eg[:, q * 4:q * 4 + nh, :].rearrange("p h l -> p (h l)"), in_=pb[:, 0:nh * 128], func=AF.Exp), [pb], [Eg])

        def loads(c, i):
            cs_ = slice(c * 128, (c + 1) * 128)
            t3, p2 = i % 3, i % 2
            self.ld(qt3[t3], qt3[t3][:].rearrange("p (g k) t -> p g k t", g=G), qtv[:, :, 0:KC, cs_])
            self.ld(kt2[p2], kt2[p2][:].rearrange("p (g k) t -> p g k t", g=G), ktv[:, :, 0:KC, cs_])
            self.ld(ktm3[t3], ktm3[t3][:], self.Ktm[cs_, 0:G * dk])
            self.ld(vt3[t3], vt3[t3][:], self.Vtm[cs_, :])
            if ssd:
                self.ld(la2[p2], la2[p2][:], self.latm[cs_, :])
                self.ld(dt2[p2], dt2[p2][:], self.dttm[cs_, :])

        def XDT(i):
            return xdt2[i % 2] if ssd else vt3[i % 3]

        def stage1_pre(dirn, i):
            par = i % 2
            qt, kt, vt, dt = qt3[i % 3], kt2[par], vt3[i % 3], dt2[par]
            xdt = XDT(i)
            if ssd:
                decay_pre(dirn, i)
                self.V(lambda e: e.tensor_tensor(out=r3(xdt[:], H), in0=r3(vt[:], H),
                                                 in1=bcast(dt[:, dirn * 32:dirn * 32 + 32].unsqueeze(2), [128, H, dv]), op=ALU.mult),
                       [vt, dt], [xdt])
            self.G(lambda e: e.tensor_tensor(out=r3(xe[par][:], H), in0=r3(xdt[:], H),
                                             in1=bcast(wend[:].unsqueeze(2), [128, H, dv]), op=ALU.mult), [xdt, wend], [xe[par]])
            for g in range(G):
                for kc in range(KC):
                    self.P(lambda e, g=g, kc=kc: e.matmul(pcb[:, g * 128:(g + 1) * 128], lhsT=kt[:, g * KC + kc, :], rhs=qt[:, g * KC + kc, :],
                                                          start=(kc == 0), stop=(kc == KC - 1)), [kt, qt], [pcb])
            self.V(lambda e: e.tensor_tensor(out=cbm[par][:], in0=pcb[:].rearrange("p (g l) -> p g l", g=G),
                                             in1=bcast(tri[:].unsqueeze(1), [128, G, 128]), op=ALU.mult), [pcb, tri], [cbm[par]])

        def emitM(g, par):
            Mg = M[g % 2]
            self.V(lambda e: e.scalar_tensor_tensor(out=Mg[:], in0=E[par][g][:], scalar=1.0,
                                                    in1=bcast(cbm[par][:, g:g + 1, :], [128, Hg, 128]), op0=ALU.min, op1=ALU.mult),
                   [E[par][g], cbm[par]], [Mg])

        def s2g(i, g):
            par = i % 2
            qt, ktm = qt3[i % 3], ktm3[i % 3]
            xdt = XDT(i)
            gs = slice(g * 512, (g + 1) * 512)
            yc = ycs[par]
            if g + 1 < G:
                emitM(g + 1, par)
            Mg = M[g % 2]
            tg = tmp[g % 2]
            for hh in range(Hg):
                h = g * Hg + hh
                self.P(lambda e, h=h, hh=hh: e.matmul(pyd[:, hh * dv:(hh + 1) * dv], lhsT=Mg[:, hh, :], rhs=xdt[:, h * dv:(h + 1) * dv],
                                                      start=True, stop=True), [Mg, xdt], [pyd])
            for kc in range(KC):
                self.P(lambda e, kc=kc: e.matmul(pyo[:], lhsT=qt[:, g * KC + kc, :], rhs=Sb[:, kc, gs],
                                                 start=(kc == 0), stop=(kc == KC - 1)), [qt, Sb], [pyo])
            self.V(lambda e: e.tensor_tensor(out=r3(tg[:], Hg), in0=r3(pyo[:], Hg),
                                             in1=bcast(ea[par][:, g * Hg:(g + 1) * Hg].unsqueeze(2), [128, Hg, dv]), op=ALU.mult),
                   [pyo, ea[par]], [tg])
            self.V(lambda e: e.tensor_tensor(out=yc[:, gs], in0=tg[:], in1=pyd[:], op=ALU.add), [tg, pyd], [yc])
            for kc in range(KC):
                self.P(lambda e, kc=kc: e.matmul(pst[:], lhsT=ktm[:, g * dk + kc * 128:g * dk + (kc + 1) * 128], rhs=xe[par][:, gs],
                                                 start=True, stop=True), [ktm, xe[par]], [pst])
                self.G(lambda e, kc=kc: e.tensor_tensor(out=r3(S[:, kc, gs], Hg), in0=r3(S[:, kc, gs], Hg),
                                                        in1=bcast(cd[par][:, g * Hg:(g + 1) * Hg].unsqueeze(2), [128, Hg, dv]),
                                                        op=ALU.mult), [S, cd[par]], [S])
                self.V(lambda e, kc=kc: e.tensor_tensor(out=S[:, kc, gs], in0=S[:, kc, gs], in1=pst[:], op=ALU.add), [S, pst], [S])
                self.A(lambda e, kc=kc: e.copy(out=Sb[:, kc, gs], in_=S[:, kc, gs]), [S], [Sb])

        def stage2_post(c, dirn, i):
            par = i % 2
            yc = ycs[par]
            cs_ = slice(c * 128, (c + 1) * 128)
            if dirn == 0:
                deferred.append(lambda: self.st(self.yf[cs_, :], yc, yc[:]))
                return
            self.ld(yfl, yfl[:], self.yf[cs_, :])
            self.ld(gt, gt[:], self.gate[cs_, :])
            self.V(lambda e: e.tensor_tensor(out=yc[:], in0=yc[:], in1=yfl[:], op=ALU.add), [yc, yfl], [yc])
            self.A(lambda e: e.activation(out=gt[:], in_=gt[:], func=AF.Silu), [gt], [gt])
            yield
            if ssd:
                self.V(lambda e: e.tensor_tensor(out=yc[:], in0=yc[:], in1=gt[:], op=ALU.mult), [yc, gt], [yc])
            for g in range(4):
                self.V(lambda e, g=g: e.bn_stats(out=st6[:, g, :], in_=yc[:, g * 512:(g + 1) * 512]), [yc], [st6])
                self.V(lambda e, g=g: e.bn_aggr(out=mv[:, g, :], in_=st6[:, g, :]), [st6], [mv])
            yield
            if ssd:
                self.V(lambda e: e.tensor_tensor(out=ss[:], in0=mv[:, :, 0], in1=mv[:, :, 0], op=ALU.mult), [mv], [ss])
                self.V(lambda e: e.tensor_tensor(out=ss[:], in0=ss[:], in1=mv[:, :, 1], op=ALU.add), [ss, mv], [ss])
                self.A(lambda e: e.activation(out=ss[:], in_=ss[:], func=AF.Sqrt, bias=EPS), [ss], [ss])
                self.V(lambda e: e.reciprocal(out=rstd[:], in_=ss[:]), [ss], [rstd])
                yield
                for g in range(4):
                    gs = slice(g * 512, (g + 1) * 512)
                    self.V(lambda e, g=g, gs=gs: e.scalar_tensor_tensor(out=yb[:, gs], in0=yc[:, gs], scalar=rstd[:, g:g + 1], in1=nw[:, gs],
                                                                        op0=ALU.mult, op1=ALU.mult), [yc, rstd, nw], [yb])
            else:
                self.A(lambda e: e.activation(out=ss[:], in_=mv[:, :, 1], func=AF.Sqrt, bias=EPS), [mv], [ss])
                self.V(lambda e: e.reciprocal(out=rstd[:], in_=ss[:]), [ss], [rstd])
                yield
                for g in range(4):
                    gs = slice(g * 512, (g + 1) * 512)
                    self.V(lambda e, g=g, gs=gs: e.tensor_scalar(out=yc[:, gs], in0=yc[:, gs], scalar1=mv[:, g, 0:1], scalar2=rstd[:, g:g + 1],
                                                                 op0=ALU.subtract, op1=ALU.mult), [yc, mv, rstd], [yc])
                self.G(lambda e: e.tensor_tensor(out=yc[:], in0=yc[:], in1=nw[:], op=ALU.mult), [yc, nw], [yc])
                self.V(lambda e: e.tensor_tensor(out=yc[:], in0=yc[:], in1=nb_[:], op=ALU.add), [yc, nb_], [yc])
                self.V(lambda e: e.tensor_tensor(out=yb[:], in0=yc[:], in1=gt[:], op=ALU.mult), [yc, gt], [yb])
            yield
            for half in range(2):
                for kk in range(8):
                    k = half * 8 + kk
                    self.P(lambda e, k=k, kk=kk: e.transpose(ptr[:, kk, :], yb[:, k * 128:(k + 1) * 128], identb[:]), [yb, identb], [ptr])
                self.A(lambda e, half=half: e.copy(out=yT[:, half * 8:(half + 1) * 8, :], in_=ptr[:]), [ptr], [yT])
                yield
            pos = (pyd, pyo)
            self.ld(xt, xt[:], xcur[cs_, :])
            for nn in range(2):
                for k in range(16):
                    self.P(lambda e, k=k, nn=nn: e.matmul(pos[nn][:], lhsT=yT[:, k, :], rhs=Wo[:, k, nn * 512:(nn + 1) * 512],
                                                          start=(k == 0), stop=(k == 15)), [yT, Wo], [pos[nn]])
                ns = slice(nn * 512, (nn + 1) * 512)
                self.V(lambda e, nn=nn, ns=ns: e.tensor_tensor(out=t1[:, ns], in0=pos[nn][:], in1=g1[:, ns], op=ALU.mult), [pos[nn], g1], [t1])
            yield
            self.V(lambda e: e.scalar_tensor_tensor(out=t2[:], in0=xt[:], scalar=ALPHA, in1=t1[:], op0=ALU.mult, op1=ALU.add), [xt, t1], [t2])
            self.layer_norm(t2, t1, st2, mv2, rs2, lng, lnb)
            yield
            self.G(lambda e: e.tensor_tensor(out=t2[:], in0=t1[:], in1=sc2[:], op=ALU.mult), [t1, sc2], [t2])
            self.V(lambda e: e.tensor_tensor(out=t2[:], in0=t2[:], in1=sh2[:], op=ALU.add), [t2, sh2], [t2])
            deferred.append(lambda: self.st(xnext[cs_, :], t1, t1[:]))
            deferred.append(lambda: self.st(self.tok[cs_, :], t2, t2[:]))

        deferred = []

        def flush():
            for f in deferred:
                f()
            del deferred[:]

        for dirn in range(2):
            order = list(range(NCH)) if dirn == 0 else [1, 0] + list(range(NCH - 1, 1, -1))
            self.ld(tri, tri[:], self.c_trif[dirn])
            self.V(lambda e: e.memset(S[:], 0.0), [], [S])
            self.G(lambda e: e.memset(Sb[:], 0.0), [], [Sb])
            if dirn == 1:
                load_rows(1)
            n_ = len(order)
            if not ssd:
                for la in la2:
                    self.ld_row(la, la[:, 0:4], self.ret_decay[j, dirn:dirn + 1, :])
                    self.A(lambda e: e.activation(out=la[:, 0:4], in_=la[:, 0:4], func=AF.Exp, scale=-1.0), [la], [la])
                    self.A(lambda e: e.activation(out=la[:, 0:4], in_=la[:, 0:4], func=AF.Ln, bias=1.0), [la], [la])
                    self.V(lambda e: e.tensor_scalar_mul(out=la[:, 0:4], in0=la[:, 0:4], scalar1=-1.0), [la], [la])
                decay_pre(dirn, 0)
                for g in range(G):
                    decay_g(dirn, 0, g)
            loads(order[0], 0)
            loads(order[1], 1)
            stage1_pre(dirn, 0)
            if ssd:
                for g in range(G):
                    decay_g(dirn, 0, g)
            pend = None

            def step_post():
                nonlocal pend
                if pend is not None:
                    try:
                        next(pend)
                    except StopIteration:
                        pend = None

            for i, c in enumerate(order):
                if i + 2 < n_:
                    loads(order[i + 2], i + 2)
                flush()
                if dirn == 1 and i == NCTX + 1:
                    load_rows(0)
                if i + 1 < n_:
                    stage1_pre(dirn, i + 1)
                step_post()
                emitM(0, i % 2)
                for g in range(G):
                    if ssd and i + 1 < n_:
                        decay_g(dirn, i + 1, g)
                    step_post()
                    s2g(i, g)
                    step_post()
                while pend is not None:
                    step_post()
                if dirn == 1 and ssd:
                    par = i % 2
                    self.G(lambda e: e.tensor_tensor(out=r3(xe[par][:], H), in0=r3(vt3[i % 3][:], H),
                                                     in1=bcast(dsk[:].unsqueeze(2), [128, H, dv]), op=ALU.mult), [vt3[i % 3], dsk], [xe[par]])
                    self.V(lambda e: e.tensor_tensor(out=ycs[par][:], in0=ycs[par][:], in1=xe[par][:], op=ALU.add), [ycs[par], xe[par]], [ycs[par]])
                pend = stage2_post(c, dirn, i)
                if dirn == 0:
                    for _ in pend:
                        pass
                    pend = None
            flush()
            while pend is not None:
                step_post()
            flush()
            if dirn == 0:
                fw.barrier()


Builder.phase_scan = _phase_scan2
```

```python
import numpy as np
import ml_dtypes
from contextlib import ExitStack, contextmanager
import concourse.bass as bass
import concourse.mybir as mybir
from concourse.bass_utils import run_bass_kernel_spmd

F32 = mybir.dt.float32
BF16 = mybir.dt.bfloat16
I32 = mybir.dt.int32
ALU = mybir.AluOpType
AF = mybir.ActivationFunctionType
AX = mybir.AxisListType

ENGS = ["tensor", "vector", "scalar", "gpsimd", "sync"]
EPOCH = 20000
NDMA_SEM = 8

D = 1024
T = 4352
NCH = 34
NCTX = 2
NB = 100
DEPTH = 4
ALPHA = (2.0 * DEPTH) ** 0.25
EPS = 1e-5


class Res:
    __slots__ = ("w", "r")

    def __init__(self):
        self.w = None
        self.r = {}


class Tl:
    def __init__(self, t):
        self.t = t
        self.r = Res()

    def __getitem__(self, k):
        return self.t[k]


class FW:
    def __init__(self, nc):
        self.nc = nc
        self.root = ExitStack()
        self.es = self.root
        self.cnt = {e: 0 for e in ENGS}
        self.sems = {}
        self.waited = {e: {} for e in ENGS}
        self.dma_i = {e: 0 for e in ENGS}
        self.dma_last = {}
        self.latest = {}
        self.uid = 0

    def sem(self, key):
        if key not in self.sems:
            self.sems[key] = self.root.enter_context(self.nc.semaphore("s_%s_%s" % key))
        return self.sems[key]

    def sb(self, name, shape, dt):
        self.uid += 1
        return Tl(self.es.enter_context(self.nc.sbuf_tensor("%s_%d" % (name, self.uid), list(shape), dt)))

    def ps(self, name, shape, dt):
        self.uid += 1
        return Tl(self.es.enter_context(self.nc.psum_tensor("%s_%d" % (name, self.uid), list(shape), dt)))

    @contextmanager
    def scope(self):
        old = self.es
        self.es = ExitStack()
        try:
            yield
        finally:
            self.barrier()
            self.es.close()
            self.es = old

    def barrier(self):
        for eng in ENGS:
            for key, val in list(self.latest.items()):
                self._wait(eng, (key, val))

    def _wait(self, eng, ev):
        if ev is None:
            return
        key, val = ev
        if self.waited[eng].get(key, 0) >= val:
            return
        self.waited[eng][key] = val
        getattr(self.nc, eng).wait_ge(self.sem(key), val)

    def _deps(self, eng, reads, writes):
        evs = []
        for r in reads:
            if r.w is not None:
                evs.append(r.w)
        for w in writes:
            if w.w is not None:
                evs.append(w.w)
            evs.extend(w.r.items())
        for ev in evs:
            if ev[0][0] == "tensor" and eng == "tensor":
                continue
            self._wait(eng, ev)

    def _record(self, ev, reads, writes):
        self.latest[ev[0]] = ev[1]
        for r in reads:
            if r.r.get(ev[0], 0) < ev[1]:
                r.r[ev[0]] = ev[1]
        for w in writes:
            w.w = ev
            w.r = {}

    def op(self, eng, fn, reads=(), writes=()):
        reads = [t.r for t in reads]
        writes = [t.r for t in writes]
        self._deps(eng, reads, writes)
        c = self.cnt[eng]
        key = (eng, c // EPOCH)
        val = c % EPOCH + 1
        self.cnt[eng] = c + 1
        fn(getattr(self.nc, eng)).then_inc(self.sem(key), 1)
        self._record((key, val), reads, writes)

    def dma(self, eng, fn, reads=(), writes=()):
        reads = [t.r for t in reads]
        writes = [t.r for t in writes]
        self._deps(eng, reads, writes)
        i = self.dma_i[eng]
        self.dma_i[eng] = i + 1
        key = ("d" + eng, i % NDMA_SEM)
        prev = self.dma_last.get(key, 0)
        if prev:
            self._wait(eng, (key, prev))
        val = prev + 16
        self.dma_last[key] = val
        fn(getattr(self.nc, eng)).then_inc(self.sem(key), 16)
        self._record((key, val), reads, writes)


def bcast(ap, shape):
    return ap.to_broadcast(list(shape))


class Builder:
    def __init__(self, nc, nlayers=DEPTH, stop=None):
        self.nc = nc
        self.fw = FW(nc)
        self.nlayers = nlayers
        self.stop = stop
        self.dram = {}
        self.debug = set()
        self.only = None

    def din(self, name, shape, dt):
        if self.only is not None and name not in self.only:
            return None
        a = self.nc.dram_tensor(name, list(shape), dt, kind="ExternalInput").ap()
        self.dram[name] = a
        return a

    def dscr(self, name, shape, dt):
        kind = "ExternalOutput" if name in self.debug else "Internal"
        a = self.nc.dram_tensor(name, list(shape), dt, kind=kind).ap()
        self.dram[name] = a
        return a

    def ld(self, tile, dst, src, eng="sync"):
        self.fw.dma(eng, lambda e: e.dma_start(out=dst, in_=src), writes=[tile])

    def st(self, dst, tile, src, eng="sync"):
        self.fw.dma(eng, lambda e: e.dma_start(out=dst, in_=src), reads=[tile])

    def V(self, fn, rd, wr):
        self.fw.op("vector", fn, rd, wr)

    def A(self, fn, rd, wr):
        self.fw.op("scalar", fn, rd, wr)

    def G(self, fn, rd, wr):
        self.fw.op("gpsimd", fn, rd, wr)

    def P(self, fn, rd, wr):
        self.fw.op("tensor", fn, rd, wr)

    def ld_row(self, tile, dst, src_row, n=128):
        self.ld(tile, dst, src_row.partition_broadcast(n))

    def declare(self):
        d = self.din
        self.xin = d("xin", [T, D], F32)
        self.cvec = d("cvec", [128, 8, 2], F32)
        self.mod_w = d("mod_w", [DEPTH, D, 6 * D], F32)
        self.mod_b = d("mod_b", [DEPTH, 6 * D], F32)
        self.ssd_in_w = d("ssd_in_w", [2, D, 5184], F32)
        self.convw = d("convw", [2, 128, 24, 5], F32)
        self.convb = d("convb", [2, 128, 24], F32)
        self.ssd_dt_bias = d("ssd_dt_bias", [2, 64], F32)
        self.ssd_a_log = d("ssd_a_log", [2, 64], F32)
        self.ssd_d_skip = d("ssd_d_skip", [2, 32], F32)
        self.ssd_norm_w = d("ssd_norm_w", [2, 2048], F32)
        self.ssd_out_w = d("ssd_out_w", [2, 2048, D], F32)
        self.ret_in_w = d("ret_in_w", [2, D, 6144], F32)
        self.ret_decay = d("ret_decay_logit", [2, 2, 4], F32)
        self.ret_gn_w = d("ret_gn_w", [2, 2048], F32)
        self.ret_gn_b = d("ret_gn_b", [2, 2048], F32)
        self.ret_out_w = d("ret_out_w", [2, 2048, D], F32)
        self.ln_mix_g = d("ln_mix_g", [DEPTH, D], F32)
        self.ln_mix_b = d("ln_mix_b", [DEPTH, D], F32)
        self.ln_ffn_g = d("ln_ffn_g", [DEPTH, D], F32)
        self.ln_ffn_b = d("ln_ffn_b", [DEPTH, D], F32)
        self.moe_rw = d("moe_rw", [DEPTH, D, 36], F32)
        self.moe_rb = d("moe_rb", [DEPTH, 36], F32)
        self.moe_wgu = d("moe_w_gate_up", [DEPTH * 32 * 128, 8 * D], F32)
        self.moe_wd = d("moe_w_down", [DEPTH * 32 * 128, 4 * D], F32)
        self.c_identb = d("c_identb", [128, 128], BF16)
        self.c_identf = d("c_identf", [128, 128], F32)
        self.c_trif = d("c_trif", [2, 128, 128], F32)
        self.c_ones = d("c_ones", [128, 128], F32)
        self.c_slow = d("c_slow", [128, 128], BF16)
        self.c_rope = d("c_rope", [2, 128, T], F32)
        self.c_iota = d("c_iota", [128, 512], F32)
        self.c_pidx = d("c_pidx", [128, 12], F32)
        self.yout = self.nc.dram_tensor("yout", [4096, D], F32, kind="ExternalOutput").ap()
        s = self.dscr
        self.xA = s("xA", [T, D], F32)
        self.xB = s("xB", [T, D], F32)
        self.modrow = s("modrow", [2, 6 * D], F32)
        self.QT = s("QT", [4, 2, 128, T], BF16)
        self.KT = s("KT", [4, 2, 128, T], BF16)
        self.Ktm = s("Ktm", [T, 1024], BF16)
        self.Vtm = s("Vtm", [T, 2048], BF16)
        self.gate = s("gate", [T, 2048], F32)
        self.latm = s("latm", [T, 64], F32)
        self.dttm = s("dttm", [T, 64], F32)
        self.yf = s("yf", [T, 2048], F32)
        self.tok = s("tok", [T, D], F32)
        self.xbuf = s("xbuf", [NB * 128, D], BF16)
        self.ybuf = s("ybuf", [NB * 128, D], F32)
        self.dbg = {}

    def phase_mod(self, li):
        fw = self.fw
        with fw.scope():
            cv = fw.sb("cv", [128, 8, 2], F32)
            sv = fw.sb("sv", [128, 8, 2], F32)
            mb = fw.sb("mb", [2, 6 * D], F32)
            mr = fw.sb("mr", [2, 6 * D], F32)
            wst = fw.sb("wst", [128, 8, 512], F32)
            pm = fw.ps("pm", [128, 512], F32)
            self.ld(cv, cv[:], self.cvec)
            self.A(lambda e: e.activation(out=sv[:], in_=cv[:], func=AF.Silu), [cv], [sv])
            self.ld(mb, mb[0:1, :], self.mod_b[li:li + 1, :])
            self.ld(mb, mb[1:2, :], self.mod_b[li:li + 1, :])
            wv = self.mod_w[li].rearrange("(k p) n -> p k n", p=128)
            for n in range(12):
                self.ld(wst, wst[:], wv[:, :, n * 512:(n + 1) * 512])
                for k in range(8):
                    self.P(lambda e, k=k: e.matmul(pm[0:2, :], lhsT=sv[:, k, :], rhs=wst[:, k, :],
                                                   start=(k == 0), stop=(k == 7)), [sv, wst], [pm])
                self.V(lambda e, n=n: e.tensor_tensor(out=mr[0:2, n * 512:(n + 1) * 512], in0=pm[0:2, :],
                                                      in1=mb[0:2, n * 512:(n + 1) * 512], op=ALU.add),
                       [pm, mb], [mr])
            self.st(self.modrow, mr, mr[0:2, :])

    def make_uT(self, xcur, uT, identb, ptr, sc1, sh1, xt, u32, ub, c):
        v = 1 if c < NCTX else 0
        self.ld(xt, xt[:], xcur[c * 128:(c + 1) * 128, :])
        self.V(lambda e: e.tensor_tensor(out=u32[:], in0=xt[:], in1=sc1[v][:], op=ALU.mult), [xt, sc1[v]], [u32])
        self.G(lambda e: e.tensor_tensor(out=ub[:], in0=u32[:], in1=sh1[v][:], op=ALU.add), [u32, sh1[v]], [ub])
        for k in range(8):
            self.P(lambda e, k=k: e.transpose(ptr[:, k, :], ub[:, k * 128:(k + 1) * 128], identb[:]),
                   [ub, identb], [ptr])

    def load_mod_rows(self, lo, names):
        out = {}
        for nm, idx in names:
            tl = []
            for v in range(2):
                t = self.fw.sb("row_%s%d" % (nm, v), [128, D], F32)
                self.ld_row(t, t[:], self.modrow[v:v + 1, idx * D:(idx + 1) * D])
                tl.append(t)
            out[nm] = tl
        return out

    def phase_in_ssd(self, li, j, xcur):
        fw = self.fw
        with fw.scope():
            identb = fw.sb("identb", [128, 128], BF16)
            self.ld(identb, identb[:], self.c_identb)
            rows = self.load_mod_rows(0, [("sh1", 0), ("sc1", 1)])
            sh1, sc1 = rows["sh1"], rows["sc1"]
            for v in range(2):
                self.V(lambda e, v=v: e.tensor_scalar_add(out=sc1[v][:], in0=sc1[v][:], scalar1=1.0), [sc1[v]], [sc1[v]])
            uT = fw.sb("uT", [128, 8, T], BF16)
            xt = fw.sb("xt", [128, D], F32)
            u32 = fw.sb("u32", [128, D], F32)
            ub = fw.sb("ub", [128, D], BF16)
            ptr = fw.ps("ptr", [128, 8, 128], BF16)
            for c in range(NCH):
                self.make_uT(xcur, uT, identb, ptr, sc1, sh1, xt, u32, ub, c)
                self.A(lambda e, c=c: e.copy(out=uT[:, :, c * 128:(c + 1) * 128], in_=ptr[:]), [ptr], [uT])
            cw = fw.sb("cw", [128, 24, 5], F32)
            cb = fw.sb("cb", [128, 24], F32)
            self.ld(cw, cw[:], self.convw[j])
            self.ld(cb, cb[:], self.convb[j])
            wst_ = [fw.sb("wstA%d" % i, [128, 8, 128], F32) for i in range(2)]
            wb_ = [fw.sb("wbA%d" % i, [128, 8, 128], BF16) for i in range(2)]
            raw_ = [fw.sb("raw%d" % i, [128, T], F32) for i in range(2)]
            o_single = fw.sb("o", [128, T], F32)
            o_ = [o_single, o_single]
            ob_ = [fw.sb("ob%d" % i, [128, T], BF16) for i in range(2)]
            pp = [fw.ps("pp%d" % i, [128, 512], F32) for i in range(2)]
            trs_ = [fw.sb("trs%d" % i, [128, 8, 128], BF16) for i in range(2)]
            wv = self.ssd_in_w[j].rearrange("(k p) n -> p k n", p=128)
            segs = [(0, 256)] + [(256 + i * 512, 256 + (i + 1) * 512) for i in range(8)]
            seqs = [(0, 256), (256, T)]
            def loadW(f):
                wst = wst_[f % 2]
                col0 = 2048 + f * 128
                self.ld(wst, wst[:], wv[:, :, col0:col0 + 128])

            def stepA(f):
                wst, wb, raw, o, ob = wst_[f % 2], wb_[f % 2], raw_[f % 2], o_[f % 2], ob_[f % 2]
                self.G(lambda e, wb=wb, wst=wst: e.tensor_copy(out=wb[:], in_=wst[:]), [wst], [wb])
                for si, (a, b) in enumerate(segs):
                    p = pp[si % 2]
                    for k in range(8):
                        self.P(lambda e, k=k, a=a, b=b, p=p, wb=wb: e.matmul(p[:, 0:b - a], lhsT=wb[:, k, :], rhs=uT[:, k, a:b],
                                                                       start=(k == 0), stop=(k == 7)), [wb, uT], [p])
                    self.A(lambda e, a=a, b=b, p=p, raw=raw: e.copy(out=raw[:, a:b], in_=p[:, 0:b - a]), [p], [raw])

            def stepB1(f):
                wst, wb, raw, o, ob = wst_[f % 2], wb_[f % 2], raw_[f % 2], o_[f % 2], ob_[f % 2]
                self.A(lambda e, f=f, o=o, raw=raw: e.activation(out=o[:], in_=raw[:], func=AF.Identity,
                                                   bias=cb[:, f:f + 1], scale=cw[:, f, 2:3]), [raw, cw, cb], [o])

            def stepB(f):
                wst, wb, raw, o, ob = wst_[f % 2], wb_[f % 2], raw_[f % 2], o_[f % 2], ob_[f % 2]
                for (a, b) in seqs:
                    for kk, off in ((0, -2), (1, -1), (3, 1), (4, 2)):
                        if off < 0:
                            osl = (a - off, b)
                            isl = (a, b + off)
                        else:
                            osl = (a, b - off)
                            isl = (a + off, b)
                        self.V(lambda e, f=f, kk=kk, osl=osl, isl=isl, o=o, raw=raw: e.scalar_tensor_tensor(
                            out=o[:, osl[0]:osl[1]], in0=raw[:, isl[0]:isl[1]], scalar=cw[:, f, kk:kk + 1],
                            in1=o[:, osl[0]:osl[1]], op0=ALU.mult, op1=ALU.add), [raw, cw, o], [o])
                self.A(lambda e, o=o, ob=ob: e.activation(out=ob[:], in_=o[:], func=AF.Silu), [o], [ob])
                if f >= 16:
                    g = (f - 16) % 4
                    dst = self.KT if f < 20 else self.QT
                    self.st(dst[g, 0], ob, ob[:])
                if f < 20:
                    dstm = self.Vtm if f < 16 else self.Ktm
                    fc = f if f < 16 else f - 16
                    dv = dstm.rearrange("(c p) f -> p c f", p=128)
                    for c0 in range(0, NCH, 8):
                        trs = trs_[(c0 // 8) % 2]
                        n = min(8, NCH - c0)
                        for cc in range(n):
                            self.P(lambda e, cc=cc, c0=c0, ob=ob: e.transpose(ptr[:, cc, :], ob[:, (c0 + cc) * 128:(c0 + cc + 1) * 128],
                                                                       identb[:]), [ob, identb], [ptr])
                        self.V(lambda e, n=n, trs=trs: e.tensor_copy(out=trs[:, 0:n, :], in_=ptr[:, 0:n, :]), [ptr], [trs])
                        self.st(dv[:, c0:c0 + n, fc * 128:(fc + 1) * 128], trs, trs[:, 0:n, :])

            loadW(0)
            loadW(1)
            stepA(0)
            for f in range(24):
                stepB1(f)
                if f + 1 < 24:
                    stepA(f + 1)
                if f + 2 < 24:
                    loadW(f + 2)
                stepB(f)

            wst2 = fw.sb("wst2", [128, 8, 512], F32)
            wb2 = fw.sb("wb2", [128, 8, 512], BF16)
            zt = fw.sb("zt", [128, 512], F32)
            for n in range(4):
                self.ld(wst2, wst2[:], wv[:, :, n * 512:(n + 1) * 512])
                self.G(lambda e: e.tensor_copy(out=wb2[:], in_=wst2[:]), [wst2], [wb2])
                for c in range(NCH):
                    p = pp[c % 2]
                    for k in range(8):
                        self.P(lambda e, k=k, c=c, p=p: e.matmul(p[:], lhsT=uT[:, k, c * 128:(c + 1) * 128], rhs=wb2[:, k, :],
                                                                 start=(k == 0), stop=(k == 7)), [uT, wb2], [p])
                    self.A(lambda e, p=p: e.copy(out=zt[:], in_=p[:]), [p], [zt])
                    self.st(self.gate[c * 128:(c + 1) * 128, n * 512:(n + 1) * 512], zt, zt[:])
            dtb = fw.sb("dtb", [128, 64], F32)
            nega = fw.sb("nega", [128, 64], F32)
            self.ld_row(dtb, dtb[:], self.ssd_dt_bias[j:j + 1, :])
            self.ld_row(nega, nega[:], self.ssd_a_log[j:j + 1, :])
            self.A(lambda e: e.activation(out=nega[:], in_=nega[:], func=AF.Exp), [nega], [nega])
            self.V(lambda e: e.tensor_scalar_mul(out=nega[:], in0=nega[:], scalar1=-1.0), [nega], [nega])
            self.ld(wst2, wst2[:, :, 0:64], wv[:, :, 5120:5184])
            self.G(lambda e: e.tensor_copy(out=wb2[:, :, 0:64], in_=wst2[:, :, 0:64]), [wst2], [wb2])
            d0 = fw.sb("d0", [128, 64], F32)
            d1 = fw.sb("d1", [128, 64], F32)
            d2 = fw.sb("d2", [128, 64], F32)
            for c in range(NCH):
                p = pp[c % 2]
                for k in range(8):
                    self.P(lambda e, k=k, c=c, p=p: e.matmul(p[:, 0:64], lhsT=uT[:, k, c * 128:(c + 1) * 128], rhs=wb2[:, k, 0:64],
                                                             start=(k == 0), stop=(k == 7)), [uT, wb2], [p])
                self.V(lambda e, p=p: e.tensor_tensor(out=d0[:], in0=p[:, 0:64], in1=dtb[:], op=ALU.add), [p, dtb], [d0])
                self.V(lambda e: e.tensor_scalar_mul(out=d1[:], in0=d0[:], scalar1=-1.0), [d0], [d1])
                self.V(lambda e: e.tensor_tensor(out=d1[:], in0=d1[:], in1=d0[:], op=ALU.max), [d0, d1], [d1])
                self.A(lambda e: e.activation(out=d1[:], in_=d1[:], func=AF.Exp, scale=-1.0), [d1], [d1])
                self.A(lambda e: e.activation(out=d1[:], in_=d1[:], func=AF.Ln, bias=1.0), [d1], [d1])
                self.V(lambda e: e.scalar_tensor_tensor(out=d2[:], in0=d0[:], scalar=0.0, in1=d1[:], op0=ALU.max, op1=ALU.add),
                       [d0, d1], [d2])
                self.st(self.dttm[c * 128:(c + 1) * 128, :], d2, d2[:])
                self.V(lambda e: e.tensor_tensor(out=d0[:], in0=d2[:], in1=nega[:], op=ALU.mult), [d2, nega], [d0])
                self.st(self.latm[c * 128:(c + 1) * 128, :], d0, d0[:])

    def phase_in_ret(self, li, j, xcur):
        fw = self.fw
        with fw.scope():
            identb = fw.sb("identb", [128, 128], BF16)
            self.ld(identb, identb[:], self.c_identb)
            rows = self.load_mod_rows(0, [("sh1", 0), ("sc1", 1)])
            sh1, sc1 = rows["sh1"], rows["sc1"]
            for v in range(2):
                self.V(lambda e, v=v: e.tensor_scalar_add(out=sc1[v][:], in0=sc1[v][:], scalar1=1.0), [sc1[v]], [sc1[v]])
            W = fw.sb("Wret", [128, 8, 6144], BF16)
            wst = fw.sb("wstR", [128, 8, 512], F32)
            wv = self.ret_in_w[j].rearrange("(k p) n -> p k n", p=128)
            for n in range(12):
                self.ld(wst, wst[:], wv[:, :, n * 512:(n + 1) * 512])
                eng = self.G if n % 2 == 0 else self.A
                if n % 2 == 0:
                    self.G(lambda e, n=n: e.tensor_copy(out=W[:, :, n * 512:(n + 1) * 512], in_=wst[:]), [wst], [W])
                else:
                    self.A(lambda e, n=n: e.copy(out=W[:, :, n * 512:(n + 1) * 512], in_=wst[:]), [wst], [W])
            uT = fw.sb("uTs", [128, 8, 512], BF16)
            xt = fw.sb("xt", [128, D], F32)
            u32 = fw.sb("u32", [128, D], F32)
            ub = fw.sb("ub", [128, D], BF16)
            ptr = fw.ps("ptr", [128, 8, 128], BF16)
            pp = [fw.ps("pp%d" % i, [128, 512], F32) for i in range(4)]
            cs = fw.sb("cs", [128, 512], F32)
            sn = fw.sb("sn", [128, 512], F32)
            r1_ = [fw.sb("r1%d" % i, [128, 512], F32) for i in range(2)]
            r2_ = [fw.sb("r2%d" % i, [128, 512], F32) for i in range(2)]
            ta_ = [fw.sb("ta%d" % i, [128, 512], F32) for i in range(2)]
            tb_ = [fw.sb("tb%d" % i, [128, 512], F32) for i in range(2)]
            o1_ = [fw.sb("o1%d" % i, [128, 512], BF16) for i in range(2)]
            o2_ = [fw.sb("o2%d" % i, [128, 512], BF16) for i in range(2)]
            trs_ = [fw.sb("trs%d" % i, [128, 8, 128], BF16) for i in range(2)]
            zt = fw.sb("zt", [128, 512], F32)
            vb = fw.sb("vb", [128, 512], BF16)
            segs = [(0, 256)] + [(256 + i * 512, 256 + (i + 1) * 512) for i in range(8)]
            ktv = self.Ktm.rearrange("(c p) f -> p c f", p=128)
            for (a, b) in segs:
                n = b - a
                nt = n // 128
                c0 = a // 128
                for ci in range(nt):
                    self.make_uT(xcur, uT, identb, ptr, sc1, sh1, xt, u32, ub, c0 + ci)
                    self.A(lambda e, ci=ci: e.copy(out=uT[:, :, ci * 128:(ci + 1) * 128], in_=ptr[:]), [ptr], [uT])
                self.ld(cs, cs[:, 0:n], self.c_rope[0, :, a:b])
                self.ld(sn, sn[:, 0:n], self.c_rope[1, :, a:b])
                def qkA(which, h):
                    ii = (which * 4 + h) % 2
                    r1, r2, ta, tb, o1, o2, trs = r1_[ii], r2_[ii], ta_[ii], tb_[ii], o1_[ii], o2_[ii], trs_[ii]
                    base = which * 1024 + h * 256
                    for half, (pt, rr) in enumerate(((pp[ii * 2], r1), (pp[ii * 2 + 1], r2))):
                        cb0 = base + half * 128
                        for k in range(8):
                            self.P(lambda e, k=k, cb0=cb0, pt=pt: e.matmul(pt[:, 0:n], lhsT=W[:, k, cb0:cb0 + 128], rhs=uT[:, k, 0:n],
                                                                           start=(k == 0), stop=(k == 7)), [W, uT], [pt])
                        sc = 1.0 if which == 0 else 0.0625
                        self.A(lambda e, pt=pt, rr=rr, sc=sc: e.activation(out=rr[:, 0:n], in_=pt[:, 0:n], func=AF.Copy, scale=sc),
                               [pt], [rr])

                def qkB(which, h):
                    ii = (which * 4 + h) % 2
                    r1, r2, ta, tb, o1, o2, trs = r1_[ii], r2_[ii], ta_[ii], tb_[ii], o1_[ii], o2_[ii], trs_[ii]
                    base = which * 1024 + h * 256
                    self.V(lambda e, ta=ta, r1=r1: e.tensor_tensor(out=ta[:, 0:n], in0=r1[:, 0:n], in1=cs[:, 0:n], op=ALU.mult), [r1, cs], [ta])
                    self.G(lambda e, tb=tb, r2=r2: e.tensor_tensor(out=tb[:, 0:n], in0=r2[:, 0:n], in1=sn[:, 0:n], op=ALU.mult), [r2, sn], [tb])
                    self.V(lambda e, ta=ta, tb=tb, o1=o1: e.tensor_tensor(out=o1[:, 0:n], in0=ta[:, 0:n], in1=tb[:, 0:n], op=ALU.subtract), [ta, tb], [o1])
                    self.V(lambda e, ta=ta, r1=r1: e.tensor_tensor(out=ta[:, 0:n], in0=r1[:, 0:n], in1=sn[:, 0:n], op=ALU.mult), [r1, sn], [ta])
                    self.G(lambda e, tb=tb, r2=r2: e.tensor_tensor(out=tb[:, 0:n], in0=r2[:, 0:n], in1=cs[:, 0:n], op=ALU.mult), [r2, cs], [tb])
                    self.V(lambda e, ta=ta, tb=tb, o2=o2: e.tensor_tensor(out=o2[:, 0:n], in0=ta[:, 0:n], in1=tb[:, 0:n], op=ALU.add), [ta, tb], [o2])
                    dst = self.QT if which == 0 else self.KT
                    self.st(dst[h, 0, :, a:b], o1, o1[:, 0:n])
                    self.st(dst[h, 1, :, a:b], o2, o2[:, 0:n])
                    if which == 1:
                        for half, oo in enumerate((o1, o2)):
                            for ci in range(nt):
                                self.P(lambda e, ci=ci, oo=oo, half=half: e.transpose(ptr[:, half * 4 + ci, :], oo[:, ci * 128:(ci + 1) * 128],
                                                                                      identb[:]), [oo, identb], [ptr])
                        self.V(lambda e, trs=trs: e.tensor_copy(out=trs[:], in_=ptr[:]), [ptr], [trs])
                        for half in range(2):
                            col = h * 256 + half * 128
                            self.st(ktv[:, c0:c0 + nt, col:col + 128], trs, trs[:, half * 4:half * 4 + nt, :])

                wh = [(w_, h_) for w_ in range(2) for h_ in range(4)]
                qkA(*wh[0])
                for t_ in range(8):
                    if t_ + 1 < 8:
                        qkA(*wh[t_ + 1])
                    qkB(*wh[t_])
                for ci in range(nt):
                    c = c0 + ci
                    for nn in range(8):
                        p = pp[2 + nn % 2]
                        colw = 2048 + nn * 512
                        for k in range(8):
                            self.P(lambda e, k=k, ci=ci, p=p, colw=colw: e.matmul(p[:], lhsT=uT[:, k, ci * 128:(ci + 1) * 128],
                                                                                  rhs=W[:, k, colw:colw + 512], start=(k == 0), stop=(k == 7)),
                                   [uT, W], [p])
                        if nn < 4:
                            self.A(lambda e, p=p: e.copy(out=vb[:], in_=p[:]), [p], [vb])
                            self.st(self.Vtm[c * 128:(c + 1) * 128, nn * 512:(nn + 1) * 512], vb, vb[:])
                        else:
                            self.V(lambda e, p=p: e.tensor_copy(out=zt[:], in_=p[:]), [p], [zt])
                            self.st(self.gate[c * 128:(c + 1) * 128, (nn - 4) * 512:(nn - 3) * 512], zt, zt[:])

    def phase_scan(self, li, j, ssd, xcur, xnext):
        fw = self.fw
        G = 4
        Hg = 8 if ssd else 1
        KC = 1 if ssd else 2
        H = G * Hg
        dv = 2048 // H
        dk = KC * 128
        with fw.scope():
            identb = fw.sb("identb", [128, 128], BF16)
            self.ld(identb, identb[:], self.c_identb)
            ones = fw.sb("ones", [128, 128], F32)
            self.ld(ones, ones[:], self.c_ones)
            tri = fw.sb("tri", [128, 128], F32)
            Wo = fw.sb("Wo", [128, 16, D], BF16)
            wst = fw.sb("wstO", [128, 4, D], F32)
            owv = (self.ssd_out_w if ssd else self.ret_out_w)[j].rearrange("(k p) n -> p k n", p=128)
            for q in range(4):
                self.ld(wst, wst[:], owv[:, q * 4:(q + 1) * 4, :])
                self.G(lambda e, q=q: e.tensor_copy(out=Wo[:, q * 4:(q + 1) * 4, :], in_=wst[:]), [wst], [Wo])
            rows = self.load_mod_rows(0, [("g1", 2), ("sh2", 3), ("sc2", 4)])
            g1, sh2, sc2 = rows["g1"], rows["sh2"], rows["sc2"]
            for v in range(2):
                self.V(lambda e, v=v: e.tensor_scalar_add(out=sc2[v][:], in0=sc2[v][:], scalar1=1.0), [sc2[v]], [sc2[v]])
            lng = fw.sb("lng", [128, D], F32)
            lnb = fw.sb("lnb", [128, D], F32)
            self.ld_row(lng, lng[:], self.ln_mix_g[li:li + 1, :])
            self.ld_row(lnb, lnb[:], self.ln_mix_b[li:li + 1, :])
            nw = fw.sb("nw", [128, 2048], F32)
            self.ld_row(nw, nw[:], (self.ssd_norm_w if ssd else self.ret_gn_w)[j:j + 1, :])
            if ssd:
                dsk = fw.sb("dsk", [128, 32], F32)
                self.ld_row(dsk, dsk[:], self.ssd_d_skip[j:j + 1, :])
            else:
                nb_ = fw.sb("nb", [128, 2048], F32)
                self.ld_row(nb_, nb_[:], self.ret_gn_b[j:j + 1, :])
            S = fw.sb("S", [128, KC, 2048], F32)
            Sb = fw.sb("Sb", [128, KC, 2048], BF16)
            qt = fw.sb("qt", [128, G * KC, 128], BF16)
            kt = fw.sb("kt", [128, G * KC, 128], BF16)
            ktm = fw.sb("ktm", [128, G * dk], BF16)
            vt = fw.sb("vt", [128, 2048], BF16)
            la = fw.sb("la", [128, 64], F32)
            dt = fw.sb("dt", [128, 64], F32)
            acs = fw.sb("acs", [128, 2 * H], F32)
            nacs = fw.sb("nacs", [128, H], F32)
            ea = fw.sb("ea", [128, H], F32)
            wend = fw.sb("wend", [128, H], F32)
            cd = fw.sb("cd", [128, H], F32)
            E = fw.sb("E", [128, H, 128], F32)
            lt = fw.sb("lt", [128, Hg, 128], F32)
            M = fw.sb("M", [128, Hg, 128], BF16)
            cbm = fw.sb("cbm", [128, G, 128], F32)
            xdt = fw.sb("xdt", [128, 2048], BF16) if ssd else vt
            xe = fw.sb("xe", [128, 2048], BF16)
            yc = fw.sb("yc", [128, 2048], F32)
            tmp = fw.sb("tmp", [128, 512], F32)
            yfl = fw.sb("yfl", [128, 2048], F32)
            gt = fw.sb("gt", [128, 2048], F32)
            yb = fw.sb("yb", [128, 2048], BF16)
            yT = fw.sb("yT", [128, 16, 128], BF16)
            st6 = fw.sb("st6", [128, 4, 6], F32)
            mv = fw.sb("mv", [128, 4, 2], F32)
            ss = fw.sb("ss", [128, 4], F32)
            rstd = fw.sb("rstd", [128, 4], F32)
            xt = fw.sb("xt", [128, D], F32)
            t1 = fw.sb("t1", [128, D], F32)
            t2 = fw.sb("t2", [128, D], F32)
            st2 = fw.sb("st2", [128, 2, 6], F32)
            mv2 = fw.sb("mv2", [128, 2], F32)
            rs2 = fw.sb("rs2", [128, 1], F32)
            psm = fw.ps("psm", [128, 512], F32)
            pcb = fw.ps("pcb", [128, 512], F32)
            pbc = [fw.ps("pbc%d" % i, [128, 512], F32) for i in range(2)]
            pA = fw.ps("pA", [128, 1024], F32)
            pst = fw.ps("pst", [128, 512], F32)
            ptr = fw.ps("ptr", [128, 8, 128], BF16)

            qtv = self.QT.rearrange("g k p t -> p g k t")
            ktv = self.KT.rearrange("g k p t -> p g k t")

            def decay_prep(dirn):
                lad = la[:, dirn * 32:dirn * 32 + H] if ssd else la[:, 0:H]
                self.P(lambda e: e.matmul(psm[:, 0:H], lhsT=tri[:], rhs=lad, start=True, stop=True), [tri, la], [psm])
                self.P(lambda e: e.matmul(psm[:, H:2 * H], lhsT=ones[:], rhs=lad, start=True, stop=True), [ones, la], [psm])
                self.V(lambda e: e.tensor_copy(out=acs[:], in_=psm[:, 0:2 * H]), [psm], [acs])
                self.V(lambda e: e.tensor_scalar_mul(out=nacs[:], in0=acs[:, 0:H], scalar1=-1.0), [acs], [nacs])
                self.A(lambda e: e.activation(out=ea[:], in_=acs[:, 0:H], func=AF.Exp), [acs], [ea])
                self.V(lambda e: e.tensor_tensor(out=wend[:], in0=acs[:, H:2 * H], in1=acs[:, 0:H], op=ALU.subtract), [acs], [wend])
                self.A(lambda e: e.activation(out=wend[:], in_=wend[:], func=AF.Exp), [wend], [wend])
                self.A(lambda e: e.activation(out=cd[:], in_=acs[:, H:2 * H], func=AF.Exp), [acs], [cd])
                for g in range(G):
                    hs = slice(g * Hg, (g + 1) * Hg)
                    ladg = la[:, dirn * 32 + g * Hg:dirn * 32 + (g + 1) * Hg] if ssd else la[:, g:g + 1]
                    self.G(lambda e, ladg=ladg: e.tensor_tensor(out=lt[:], in0=bcast(tri[:].unsqueeze(1), [128, Hg, 128]),
                                                                in1=bcast(ladg.unsqueeze(2), [128, Hg, 128]), op=ALU.mult),
                           [tri, la], [lt])
                    for q in range((Hg + 3) // 4):
                        nh = min(4, Hg - q * 4)
                        pb = pbc[q % 2]
                        self.P(lambda e, q=q, nh=nh, pb=pb: e.matmul(pb[:, 0:nh * 128], lhsT=ones[:], rhs=lt[:, q * 4:q * 4 + nh, :],
                                                                     start=True, stop=True), [ones, lt], [pb])
                        for hh in range(nh):
                            h = g * Hg + q * 4 + hh
                            self.A(lambda e, h=h, hh=hh, pb=pb: e.activation(out=E[:, h, :], in_=pb[:, hh * 128:(hh + 1) * 128], func=AF.Exp,
                                                                            bias=nacs[:, h:h + 1], scale=1.0), [pb, nacs], [E])

            for dirn in range(2):
                order = list(range(NCH)) if dirn == 0 else [1, 0] + list(range(NCH - 1, 1, -1))
                self.ld(tri, tri[:], self.c_trif[dirn])
                self.V(lambda e: e.memset(S[:], 0.0), [], [S])
                self.G(lambda e: e.memset(Sb[:], 0.0), [], [Sb])
                if not ssd:
                    self.ld_row(la, la[:, 0:4], self.ret_decay[j, dirn:dirn + 1, :])
                    self.A(lambda e: e.activation(out=la[:, 0:4], in_=la[:, 0:4], func=AF.Exp, scale=-1.0), [la], [la])
                    self.A(lambda e: e.activation(out=la[:, 0:4], in_=la[:, 0:4], func=AF.Ln, bias=1.0), [la], [la])
                    self.V(lambda e: e.tensor_scalar_mul(out=la[:, 0:4], in0=la[:, 0:4], scalar1=-1.0), [la], [la])
                    decay_prep(dirn)
                for c in order:
                    v = 1 if c < NCTX else 0
                    cs_ = slice(c * 128, (c + 1) * 128)
                    self.ld(qt, qt[:].rearrange("p (g k) t -> p g k t", g=G), qtv[:, :, 0:KC, cs_])
                    self.ld(kt, kt[:].rearrange("p (g k) t -> p g k t", g=G), ktv[:, :, 0:KC, cs_])
                    self.ld(ktm, ktm[:], self.Ktm[cs_, 0:G * dk])
                    self.ld(vt, vt[:], self.Vtm[cs_, :])
                    if ssd:
                        self.ld(la, la[:], self.latm[cs_, :])
                        self.ld(dt, dt[:], self.dttm[cs_, :])
                        decay_prep(dirn)
                        self.V(lambda e: e.tensor_tensor(out=xdt[:].rearrange("p (h d) -> p h d", h=H), in0=vt[:].rearrange("p (h d) -> p h d", h=H),
                                                         in1=bcast(dt[:, dirn * 32:dirn * 32 + 32].unsqueeze(2), [128, H, dv]), op=ALU.mult),
                               [vt, dt], [xdt])
                    self.G(lambda e: e.tensor_tensor(out=xe[:].rearrange("p (h d) -> p h d", h=H), in0=xdt[:].rearrange("p (h d) -> p h d", h=H),
                                                     in1=bcast(wend[:].unsqueeze(2), [128, H, dv]), op=ALU.mult), [xdt, wend], [xe])
                    for g in range(G):
                        for kc in range(KC):
                            self.P(lambda e, g=g, kc=kc: e.matmul(pcb[:, g * 128:(g + 1) * 128], lhsT=kt[:, g * KC + kc, :], rhs=qt[:, g * KC + kc, :],
                                                                  start=(kc == 0), stop=(kc == KC - 1)), [kt, qt], [pcb])
                    self.V(lambda e: e.tensor_tensor(out=cbm[:], in0=pcb[:].rearrange("p (g l) -> p g l", g=G),
                                                     in1=bcast(tri[:].unsqueeze(1), [128, G, 128]), op=ALU.mult), [pcb, tri], [cbm])
                    for g in range(G):
                        gs = slice(g * 512, (g + 1) * 512)
                        self.V(lambda e, g=g: e.scalar_tensor_tensor(out=M[:], in0=E[:, g * Hg:(g + 1) * Hg, :], scalar=1.0,
                                                                     in1=bcast(cbm[:, g:g + 1, :], [128, Hg, 128]), op0=ALU.min, op1=ALU.mult),
                               [E, cbm], [M])
                        for hh in range(Hg):
                            h = g * Hg + hh
                            self.P(lambda e, h=h, hh=hh: e.matmul(pA[:, h * dv:(h + 1) * dv] if False else pA[:, (h * dv) % 512:(h * dv) % 512 + dv],
                                                                  lhsT=M[:, hh, :], rhs=xdt[:, h * dv:(h + 1) * dv], start=True, stop=True),
                                   [M, xdt], [pA])
                        for kc in range(KC):
                            self.P(lambda e, g=g, kc=kc, gs=gs: e.matmul(pA[:, 512:1024], lhsT=qt[:, g * KC + kc, :], rhs=Sb[:, kc, gs],
                                                                         start=(kc == 0), stop=(kc == KC - 1)), [qt, Sb], [pA])
                        self.V(lambda e, g=g: e.tensor_tensor(out=tmp[:].rearrange("p (h d) -> p h d", h=Hg),
                                                              in0=pA[:, 512:1024].rearrange("p (h d) -> p h d", h=Hg),
                                                              in1=bcast(ea[:, g * Hg:(g + 1) * Hg].unsqueeze(2), [128, Hg, dv]), op=ALU.mult),
                               [pA, ea], [tmp])
                        self.V(lambda e, gs=gs: e.tensor_tensor(out=yc[:, gs], in0=tmp[:], in1=pA[:, 0:512], op=ALU.add), [tmp, pA], [yc])
                        for kc in range(KC):
                            self.P(lambda e, g=g, kc=kc, gs=gs: e.matmul(pst[:], lhsT=ktm[:, g * dk + kc * 128:g * dk + (kc + 1) * 128], rhs=xe[:, gs],
                                                                         start=True, stop=True), [ktm, xe], [pst])
                            self.V(lambda e, g=g, kc=kc, gs=gs: e.tensor_tensor(out=S[:, kc, gs].rearrange("p (h d) -> p h d", h=Hg),
                                                                                in0=S[:, kc, gs].rearrange("p (h d) -> p h d", h=Hg),
                                                                                in1=bcast(cd[:, g * Hg:(g + 1) * Hg].unsqueeze(2), [128, Hg, dv]),
                                                                                op=ALU.mult), [S, cd], [S])
                            self.V(lambda e, kc=kc, gs=gs: e.tensor_tensor(out=S[:, kc, gs], in0=S[:, kc, gs], in1=pst[:], op=ALU.add), [S, pst], [S])
                            self.A(lambda e, kc=kc, gs=gs: e.copy(out=Sb[:, kc, gs], in_=S[:, kc, gs]), [S], [Sb])
                    if dirn == 0:
                        self.st(self.yf[cs_, :], yc, yc[:])
                        continue
                    self.ld(yfl, yfl[:], self.yf[cs_, :])
                    self.ld(gt, gt[:], self.gate[cs_, :])
                    self.V(lambda e: e.tensor_tensor(out=yc[:], in0=yc[:], in1=yfl[:], op=ALU.add), [yc, yfl], [yc])
                    if ssd:
                        self.G(lambda e: e.tensor_tensor(out=yfl[:].rearrange("p (h d) -> p h d", h=H), in0=vt[:].rearrange("p (h d) -> p h d", h=H),
                                                         in1=bcast(dsk[:].unsqueeze(2), [128, H, dv]), op=ALU.mult), [vt, dsk], [yfl])
                        self.V(lambda e: e.tensor_tensor(out=yc[:], in0=yc[:], in1=yfl[:], op=ALU.add), [yc, yfl], [yc])
                        self.A(lambda e: e.activation(out=gt[:], in_=gt[:], func=AF.Silu), [gt], [gt])
                        self.V(lambda e: e.tensor_tensor(out=yc[:], in0=yc[:], in1=gt[:], op=ALU.mult), [yc, gt], [yc])
                        for g in range(4):
                            self.V(lambda e, g=g: e.bn_stats(out=st6[:, g, :], in_=yc[:, g * 512:(g + 1) * 512]), [yc], [st6])
                            self.V(lambda e, g=g: e.bn_aggr(out=mv[:, g, :], in_=st6[:, g, :]), [st6], [mv])
                        self.V(lambda e: e.tensor_tensor(out=ss[:], in0=mv[:, :, 0], in1=mv[:, :, 0], op=ALU.mult), [mv], [ss])
                        self.V(lambda e: e.tensor_tensor(out=ss[:], in0=ss[:], in1=mv[:, :, 1], op=ALU.add), [ss, mv], [ss])
                        self.A(lambda e: e.activation(out=ss[:], in_=ss[:], func=AF.Sqrt, bias=EPS), [ss], [ss])
                        self.V(lambda e: e.reciprocal(out=rstd[:], in_=ss[:]), [ss], [rstd])
                        for g in range(4):
                            gs = slice(g * 512, (g + 1) * 512)
                            self.V(lambda e, g=g, gs=gs: e.scalar_tensor_tensor(out=yb[:, gs], in0=yc[:, gs], scalar=rstd[:, g:g + 1], in1=nw[:, gs],
                                                                                op0=ALU.mult, op1=ALU.mult), [yc, rstd, nw], [yb])
                    else:
                        for g in range(4):
                            self.V(lambda e, g=g: e.bn_stats(out=st6[:, g, :], in_=yc[:, g * 512:(g + 1) * 512]), [yc], [st6])
                            self.V(lambda e, g=g: e.bn_aggr(out=mv[:, g, :], in_=st6[:, g, :]), [st6], [mv])
                        self.A(lambda e: e.activation(out=ss[:], in_=mv[:, :, 1], func=AF.Sqrt, bias=EPS), [mv], [ss])
                        self.V(lambda e: e.reciprocal(out=rstd[:], in_=ss[:]), [ss], [rstd])
                        self.A(lambda e: e.activation(out=gt[:], in_=gt[:], func=AF.Silu), [gt], [gt])
                        for g in range(4):
                            gs = slice(g * 512, (g + 1) * 512)
                            self.V(lambda e, g=g, gs=gs: e.tensor_scalar(out=yc[:, gs], in0=yc[:, gs], scalar1=mv[:, g, 0:1], scalar2=rstd[:, g:g + 1],
                                                                         op0=ALU.subtract, op1=ALU.mult), [yc, mv, rstd], [yc])
                        self.G(lambda e: e.tensor_tensor(out=yc[:], in0=yc[:], in1=nw[:], op=ALU.mult), [yc, nw], [yc])
                        self.V(lambda e: e.tensor_tensor(out=yc[:], in0=yc[:], in1=nb_[:], op=ALU.add), [yc, nb_], [yc])
                        self.V(lambda e: e.tensor_tensor(out=yb[:], in0=yc[:], in1=gt[:], op=ALU.mult), [yc, gt], [yb])
                    for half in range(2):
                        for kk in range(8):
                            k = half * 8 + kk
                            self.P(lambda e, k=k, kk=kk: e.transpose(ptr[:, kk, :], yb[:, k * 128:(k + 1) * 128], identb[:]), [yb, identb], [ptr])
                        self.A(lambda e, half=half: e.copy(out=yT[:, half * 8:(half + 1) * 8, :], in_=ptr[:]), [ptr], [yT])
                    for nn in range(2):
                        for k in range(16):
                            self.P(lambda e, k=k, nn=nn: e.matmul(pA[:, nn * 512:(nn + 1) * 512], lhsT=yT[:, k, :], rhs=Wo[:, k, nn * 512:(nn + 1) * 512],
                                                                  start=(k == 0), stop=(k == 15)), [yT, Wo], [pA])
                    self.ld(xt, xt[:], xcur[cs_, :])
                    self.V(lambda e: e.tensor_tensor(out=t1[:], in0=pA[:], in1=g1[v][:], op=ALU.mult), [pA, g1[v]], [t1])
                    self.V(lambda e: e.scalar_tensor_tensor(out=t2[:], in0=xt[:], scalar=ALPHA, in1=t1[:], op0=ALU.mult, op1=ALU.add), [xt, t1], [t2])
                    self.layer_norm(t2, t1, st2, mv2, rs2, lng, lnb)
                    self.st(xnext[cs_, :], t1, t1[:])
                    self.G(lambda e: e.tensor_tensor(out=t2[:], in0=t1[:], in1=sc2[v][:], op=ALU.mult), [t1, sc2[v]], [t2])
                    self.V(lambda e: e.tensor_tensor(out=t2[:], in0=t2[:], in1=sh2[v][:], op=ALU.add), [t2, sh2[v]], [t2])
                    self.st(self.tok[cs_, :], t2, t2[:])
                if dirn == 0:
                    fw.barrier()

    def layer_norm(self, xin_t, out_t, st2, mv2, rs2, lng, lnb, pool=True):
        for hf in range(2):
            self.V(lambda e, hf=hf: e.bn_stats(out=st2[:, hf, :], in_=xin_t[:, hf * 512:(hf + 1) * 512]), [xin_t], [st2])
        self.V(lambda e: e.bn_aggr(out=mv2[:], in_=st2[:].rearrange("p a b -> p (a b)")), [st2], [mv2])
        self.A(lambda e: e.activation(out=rs2[:], in_=mv2[:, 1:2], func=AF.Sqrt, bias=EPS), [mv2], [rs2])
        self.V(lambda e: e.reciprocal(out=rs2[:], in_=rs2[:]), [rs2], [rs2])
        self.V(lambda e: e.tensor_scalar(out=out_t[:], in0=xin_t[:], scalar1=mv2[:, 0:1], scalar2=rs2[:, 0:1],
                                         op0=ALU.subtract, op1=ALU.mult), [xin_t, mv2, rs2], [out_t])
        (self.G if pool else self.V)(lambda e: e.tensor_tensor(out=out_t[:], in0=out_t[:], in1=lng[:], op=ALU.mult), [out_t, lng], [out_t])
        self.V(lambda e: e.tensor_tensor(out=out_t[:], in0=out_t[:], in1=lnb[:], op=ALU.add), [out_t, lnb], [out_t])


def host_consts():
    bf = ml_dtypes.bfloat16
    c = {}
    c["c_identb"] = np.eye(128, dtype=np.float32).astype(bf)
    c["c_identf"] = np.eye(128, dtype=np.float32)
    s = np.arange(128)
    c["c_trif"] = np.stack([(s[:, None] <= s[None, :]), (s[:, None] >= s[None, :])]).astype(np.float32)
    c["c_ones"] = np.ones((128, 128), np.float32)
    c["c_slow"] = (s[:, None] < s[None, :]).astype(np.float32).astype(bf)
    n_freq = 64
    inv_freq = (10000.0 ** (-np.arange(n_freq, dtype=np.float32) / np.float32(n_freq))).astype(np.float32)
    pos = np.arange(4096)
    rows = (pos // 64).astype(np.float32)
    cols = (pos % 64).astype(np.float32)
    ang = np.concatenate([rows[:, None] * inv_freq[None, :], cols[:, None] * inv_freq[None, :]], -1).astype(np.float32)
    cos = np.ones((T, 128), np.float32)
    sin = np.zeros((T, 128), np.float32)
    cos[256:] = np.cos(ang)
    sin[256:] = np.sin(ang)
    c["c_rope"] = np.ascontiguousarray(np.stack([cos.T, sin.T])).astype(np.float32)
    c["c_iota"] = np.broadcast_to(np.arange(512, dtype=np.float32)[None, :], (128, 512)).copy()
    c["c_pidx"] = (np.arange(12)[None, :] * 128 + np.arange(128)[:, None]).astype(np.float32)
    return c


def host_inputs(inp, b):
    f = np.float32
    m = {}
    m["xin"] = np.ascontiguousarray(np.concatenate([inp["ctx"][b], inp["x"][b]], 0)).astype(f)
    cv = np.stack([inp["c"][b].reshape(8, 128).T, inp["c_ctx"].reshape(8, 128).T], -1)
    m["cvec"] = np.ascontiguousarray(cv).astype(f)
    m["mod_w"] = inp["mod_w"]
    m["mod_b"] = inp["mod_b"]
    m["ssd_in_w"] = inp["ssd_in_w"]
    cw = inp["ssd_conv_w"]
    m["convw"] = np.ascontiguousarray(cw.reshape(2, 5, 24, 128).transpose(0, 3, 2, 1)).astype(f)
    m["convb"] = np.ascontiguousarray(inp["ssd_conv_b"].reshape(2, 24, 128).transpose(0, 2, 1)).astype(f)
    m["ssd_dt_bias"] = np.ascontiguousarray(inp["ssd_dt_bias"].reshape(2, 64))
    m["ssd_a_log"] = np.ascontiguousarray(inp["ssd_a_log"].reshape(2, 64))
    m["ssd_d_skip"] = inp["ssd_d_skip"]
    m["ssd_norm_w"] = inp["ssd_norm_w"]
    m["ssd_out_w"] = inp["ssd_out_w"]
    m["ret_in_w"] = inp["ret_in_w"]
    m["ret_decay_logit"] = inp["ret_decay_logit"]
    m["ret_gn_w"] = inp["ret_gn_w"]
    m["ret_gn_b"] = inp["ret_gn_b"]
    m["ret_out_w"] = inp["ret_out_w"]
    for k in ("ln_mix_g", "ln_mix_b", "ln_ffn_g", "ln_ffn_b"):
        m[k] = inp[k]
    m["moe_rw"] = np.ascontiguousarray(np.concatenate([inp["moe_group_w"], inp["moe_expert_w"]], -1)).astype(f)
    m["moe_rb"] = np.ascontiguousarray(np.concatenate([inp["moe_group_b"], inp["moe_expert_b"]], -1)).astype(f)
    m["moe_w_gate_up"] = inp["moe_w_gate_up"].reshape(DEPTH * 32 * 128, 8 * D)
    m["moe_w_down"] = inp["moe_w_down"].reshape(DEPTH * 32 * 128, 4 * D)
    return m


def _phase_moe(self, li, xcur, xnext, final):
    fw = self.fw
    NT = NCH
    with fw.scope():
        DI = fw.sb("DI", [128, 2 * NT], I32)
        Wt = fw.sb("Wt", [128, 2 * NT], F32)
        IGU = fw.sb("IGU", [128, NB], I32)
        with fw.scope():
            identf = fw.sb("identf", [128, 128], F32)
            self.ld(identf, identf[:], self.c_identf)
            slow = fw.sb("slow", [128, 128], BF16)
            self.ld(slow, slow[:], self.c_slow)
            onesf = fw.sb("onesf", [128, 128], F32)
            self.ld(onesf, onesf[:], self.c_ones)
            onesb = fw.sb("onesb", [128, 128], BF16)
            self.V(lambda e: e.tensor_copy(out=onesb[:], in_=onesf[:]), [onesf], [onesb])
            iota = fw.sb("iota", [128, 512], F32)
            self.ld(iota, iota[:], self.c_iota)
            pidx = fw.sb("pidx", [128, 12], F32)
            self.ld(pidx, pidx[:], self.c_pidx)
            rw = fw.sb("rw", [128, 8, 36], F32)
            self.ld(rw, rw[:], self.moe_rw[li].rearrange("(k p) e -> p k e", p=128))
            rb = fw.sb("rb", [128, 36], F32)
            self.ld_row(rb, rb[:], self.moe_rb[li:li + 1, :])
            OH = fw.sb("OH", [128, 2 * NT, 32], F32)
            SL = fw.sb("SL", [128, 2 * NT], F32)
            Acum = fw.sb("Acum", [128, 32], F32)
            Acb = fw.sb("Acb", [128, 32], BF16)
            self.V(lambda e: e.memset(Acum[:], 0.0), [], [Acum])
            self.V(lambda e: e.memset(Acb[:], 0.0), [], [Acb])
            tk = fw.sb("tk", [128, D], F32)
            tkT = fw.sb("tkT", [128, 8, 128], F32)
            L = fw.sb("L", [128, 36], F32)
            gmax = fw.sb("gmax", [128, 1], F32)
            ngmax = fw.sb("ngmax", [128, 1], F32)
            goh = fw.sb("goh", [128, 4], F32)
            pen = fw.sb("pen", [128, 4], F32)
            ex = fw.sb("ex", [128, 4], F32)
            gs = fw.sb("gs", [128, 1], F32)
            em = fw.sb("em", [128, 32], F32)
            em2 = fw.sb("em2", [128, 32], F32)
            m1 = fw.sb("m1", [128, 1], F32)
            m2 = fw.sb("m2", [128, 1], F32)
            dd = fw.sb("dd", [128, 1], F32)
            den = fw.sb("den", [128, 1], F32)
            A_ = fw.sb("A_", [128, 32], F32)
            Ab = fw.sb("Ab", [128, 32], BF16)
            Pp = fw.sb("Pp", [128, 32], F32)
            junk = fw.sb("junk", [128, 32], F32)
            pT = fw.ps("pT", [128, 8, 128], F32)
            plog = fw.ps("plog", [128, 512], F32)
            pP = fw.ps("pP", [128, 512], F32)
            for c in range(NT):
                cs_ = slice(c * 128, (c + 1) * 128)
                self.ld(tk, tk[:], self.tok[cs_, :])
                for k in range(8):
                    self.P(lambda e, k=k: e.transpose(pT[:, k, :], tk[:, k * 128:(k + 1) * 128], identf[:]), [tk, identf], [pT])
                self.A(lambda e: e.copy(out=tkT[:], in_=pT[:]), [pT], [tkT])
                for k in range(8):
                    self.P(lambda e, k=k: e.matmul(plog[:, 0:36], lhsT=tkT[:, k, :], rhs=rw[:, k, :], start=(k == 0), stop=(k == 7)),
                           [tkT, rw], [plog])
                self.V(lambda e: e.tensor_tensor(out=L[:], in0=plog[:, 0:36], in1=rb[:], op=ALU.add), [plog, rb], [L])
                self.V(lambda e: e.reduce_max(out=gmax[:], in_=L[:, 0:4], axis=AX.X), [L], [gmax])
                self.V(lambda e: e.tensor_scalar(out=goh[:], in0=L[:, 0:4], scalar1=gmax[:, 0:1], scalar2=None, op0=ALU.is_equal), [L, gmax], [goh])
                self.V(lambda e: e.tensor_scalar_mul(out=ngmax[:], in0=gmax[:], scalar1=-1.0), [gmax], [ngmax])
                self.A(lambda e: e.activation(out=ex[:], in_=L[:, 0:4], func=AF.Exp, bias=ngmax[:, 0:1], scale=1.0), [L, ngmax], [ex])
                self.V(lambda e: e.reduce_sum(out=gs[:], in_=ex[:], axis=AX.X), [ex], [gs])
                self.V(lambda e: e.reciprocal(out=gs[:], in_=gs[:]), [gs], [gs])
                self.V(lambda e: e.tensor_scalar(out=pen[:], in0=goh[:], scalar1=1e30, scalar2=-1e30, op0=ALU.mult, op1=ALU.add), [goh], [pen])
                self.V(lambda e: e.tensor_tensor(out=em[:].rearrange("p (g j) -> p g j", g=4), in0=L[:, 4:36].rearrange("p (g j) -> p g j", g=4),
                                                 in1=bcast(pen[:].unsqueeze(2), [128, 4, 8]), op=ALU.add), [L, pen], [em])
                self.V(lambda e: e.reduce_max(out=m1[:], in_=em[:], axis=AX.X), [em], [m1])
                o1 = OH[:, 2 * c, :]
                o2 = OH[:, 2 * c + 1, :]
                self.V(lambda e, o1=o1: e.tensor_scalar(out=o1, in0=em[:], scalar1=m1[:, 0:1], scalar2=None, op0=ALU.is_equal), [em, m1], [OH])
                self.V(lambda e, o1=o1: e.scalar_tensor_tensor(out=em2[:], in0=o1, scalar=-1e30, in1=em[:], op0=ALU.mult, op1=ALU.add), [OH, em], [em2])
                self.V(lambda e: e.reduce_max(out=m2[:], in_=em2[:], axis=AX.X), [em2], [m2])
                self.V(lambda e, o2=o2: e.tensor_scalar(out=o2, in0=em2[:], scalar1=m2[:, 0:1], scalar2=None, op0=ALU.is_equal), [em2, m2], [OH])
                self.V(lambda e: e.tensor_tensor(out=dd[:], in0=m2[:], in1=m1[:], op=ALU.subtract), [m1, m2], [dd])
                self.A(lambda e: e.activation(out=dd[:], in_=dd[:], func=AF.Exp), [dd], [dd])
                self.V(lambda e: e.tensor_scalar_add(out=den[:], in0=dd[:], scalar1=1.0), [dd], [den])
                self.V(lambda e: e.reciprocal(out=den[:], in_=den[:]), [den], [den])
                self.V(lambda e, c=c: e.tensor_tensor(out=Wt[:, 2 * c:2 * c + 1], in0=den[:], in1=gs[:], op=ALU.mult), [den, gs], [Wt])
                self.V(lambda e, c=c: e.tensor_tensor(out=Wt[:, 2 * c + 1:2 * c + 2], in0=Wt[:, 2 * c:2 * c + 1], in1=dd[:], op=ALU.mult), [Wt, dd], [Wt])
                self.V(lambda e, o1=o1, o2=o2: e.tensor_tensor(out=A_[:], in0=o1, in1=o2, op=ALU.add), [OH], [A_])
                self.V(lambda e: e.tensor_copy(out=Ab[:], in_=A_[:]), [A_], [Ab])
                self.P(lambda e: e.matmul(pP[:, 0:32], lhsT=slow[:], rhs=Ab[:], start=True, stop=False), [slow, Ab], [pP])
                self.P(lambda e: e.matmul(pP[:, 0:32], lhsT=onesb[:], rhs=Acb[:], start=False, stop=True), [onesb, Acb], [pP])
                self.V(lambda e: e.tensor_copy(out=Pp[:], in_=pP[:, 0:32]), [pP], [Pp])
                for k, ok in enumerate((o1, o2)):
                    self.V(lambda e, ok=ok: e.tensor_tensor(out=junk[:], in0=ok, in1=Pp[:], op=ALU.mult), [OH, Pp], [junk])
                    self.V(lambda e, c=c, k=k: e.reduce_sum(out=SL[:, 2 * c + k:2 * c + k + 1], in_=junk[:], axis=AX.X), [junk], [SL])
                self.V(lambda e: e.tensor_tensor(out=Acum[:], in0=Acum[:], in1=A_[:], op=ALU.add), [Acum, A_], [Acum])
                self.V(lambda e: e.tensor_copy(out=Acb[:], in_=Acum[:]), [Acum], [Acb])
            cnt = fw.sb("cnt", [128, 32], F32)
            self.P(lambda e: e.matmul(pP[:, 0:32], lhsT=onesb[:], rhs=Acb[:], start=True, stop=True), [onesb, Acb], [pP])
            self.V(lambda e: e.tensor_copy(out=cnt[:], in_=pP[:, 0:32]), [pP], [cnt])
            thr = fw.sb("thr", [128, 34], F32)
            self.V(lambda e: e.tensor_scalar_mul(out=thr[:], in0=iota[:, 0:34], scalar1=128.0), [iota], [thr])
            cmp = fw.sb("cmp", [128, 32, 34], F32)
            self.V(lambda e: e.tensor_tensor(out=cmp[:], in0=bcast(cnt[:].unsqueeze(2), [128, 32, 34]), in1=bcast(thr[:].unsqueeze(1), [128, 32, 34]),
                                             op=ALU.is_gt), [cnt, thr], [cmp])
            nblk = fw.sb("nblk", [128, 32], F32)
            self.V(lambda e: e.reduce_sum(out=nblk[:], in_=cmp[:], axis=AX.X), [cmp], [nblk])
            pa = fw.sb("pa", [128, 32], F32)
            pb_ = fw.sb("pb", [128, 32], F32)
            self.V(lambda e: e.tensor_copy(out=pa[:], in_=nblk[:]), [nblk], [pa])
            cur, oth = pa, pb_
            for sft in (1, 2, 4, 8, 16):
                self.V(lambda e, cur=cur, oth=oth: e.tensor_copy(out=oth[:], in_=cur[:]), [cur], [oth])
                self.V(lambda e, cur=cur, oth=oth, sft=sft: e.tensor_tensor(out=oth[:, sft:32], in0=cur[:, sft:32], in1=cur[:, 0:32 - sft], op=ALU.add),
                       [cur], [oth])
                cur, oth = oth, cur
            pend = cur
            pstart = fw.sb("pstart", [128, 32], F32)
            self.V(lambda e: e.tensor_tensor(out=pstart[:], in0=pend[:], in1=nblk[:], op=ALU.subtract), [pend, nblk], [pstart])
            self.V(lambda e: e.tensor_scalar_mul(out=pstart[:], in0=pstart[:], scalar1=128.0), [pstart], [pstart])
            big = fw.sb("big", [128, 2 * NT, 32], F32)
            self.V(lambda e: e.tensor_tensor(out=big[:], in0=OH[:], in1=bcast(pstart[:].unsqueeze(1), [128, 2 * NT, 32]), op=ALU.mult), [OH, pstart], [big])
            dst = fw.sb("dstf", [128, 2 * NT], F32)
            self.V(lambda e: e.reduce_sum(out=dst[:], in_=big[:], axis=AX.X), [big], [dst])
            self.V(lambda e: e.tensor_tensor(out=dst[:], in0=dst[:], in1=SL[:], op=ALU.add), [dst, SL], [dst])
            self.V(lambda e: e.tensor_copy(out=DI[:], in_=dst[:]), [dst], [DI])
            cmp2 = fw.sb("cmp2", [128, NB, 32], F32)
            self.V(lambda e: e.tensor_tensor(out=cmp2[:], in0=bcast(pend[:].unsqueeze(1), [128, NB, 32]), in1=bcast(iota[:, 0:NB].unsqueeze(2), [128, NB, 32]),
                                             op=ALU.is_le), [pend, iota], [cmp2])
            be = fw.sb("be", [128, NB], F32)
            self.V(lambda e: e.reduce_sum(out=be[:], in_=cmp2[:], axis=AX.X), [cmp2], [be])
            self.V(lambda e: e.tensor_scalar_min(out=be[:], in0=be[:], scalar1=31.0), [be], [be])
            same = fw.sb("same", [128, NB], F32)
            self.V(lambda e: e.memset(same[:], 0.0), [], [same])
            self.V(lambda e: e.tensor_tensor(out=same[:, 2:NB], in0=be[:, 2:NB], in1=be[:, 0:NB - 2], op=ALU.is_equal), [be], [same])
            self.V(lambda e: e.tensor_scalar(out=be[:], in0=be[:], scalar1=128.0, scalar2=float(li * 32 * 128), op0=ALU.mult, op1=ALU.add), [be], [be])
            self.V(lambda e: e.scalar_tensor_tensor(out=be[:], in0=same[:], scalar=1.0e6, in1=be[:], op0=ALU.mult, op1=ALU.add), [same, be], [be])
            self.V(lambda e: e.tensor_tensor(out=be[:], in0=be[:], in1=bcast(pidx[:, 0:1], [128, NB]), op=ALU.add), [be, pidx], [be])
            self.V(lambda e: e.tensor_copy(out=IGU[:], in_=be[:]), [be], [IGU])
        with fw.scope():
            tk = fw.sb("tk", [128, D], F32)
            tkb = fw.sb("tkb", [128, D], BF16)
            for c in range(NT):
                self.ld(tk, tk[:], self.tok[c * 128:(c + 1) * 128, :])
                self.V(lambda e: e.tensor_copy(out=tkb[:], in_=tk[:]), [tk], [tkb])
                for k in range(2):
                    col = 2 * c + k
                    self.fw.dma("gpsimd", lambda e, col=col: e.indirect_dma_start(
                        out=self.xbuf, out_offset=bass.IndirectOffsetOnAxis(ap=DI[:, col:col + 1], axis=0), in_=tkb[:, :], in_offset=None),
                        reads=[tkb, DI])
        with fw.scope():
            identb = fw.sb("identb", [128, 128], BF16)
            self.ld(identb, identb[:], self.c_identb)
            xb = [fw.sb("xb%d" % i, [128, D], BF16) for i in range(2)]
            xT = [fw.sb("xT%d" % i, [128, 8, 128], BF16) for i in range(2)]
            g32 = [fw.sb("g32_%d" % i, [128, 8, D], F32) for i in range(2)]
            d32 = [fw.sb("d32_%d" % i, [128, 4, D], F32) for i in range(2)]
            gbf = [fw.sb("gbf_%d" % i, [128, 8, D], BF16) for i in range(2)]
            dbf = [fw.sb("dbf_%d" % i, [128, 4, D], BF16) for i in range(2)]
            sg = fw.sb("sg", [128, 4, 128], F32)
            hT = fw.sb("hT", [128, 4, 128], BF16)
            ob = fw.sb("ob", [128, D], F32)
            ptr = fw.ps("ptr", [128, 8, 128], BF16)
            pH = [fw.ps("pH%d" % i, [128, 8, 128], F32) for i in range(2)]
            pO = fw.ps("pO", [128, D], F32)
            if getattr(self, "_bcreg", None) is None:
                self._bcreg = self.nc.gpsimd.alloc_register("bcreg")
                self.nc.gpsimd.reg_mov(self._bcreg, DEPTH * 32 * 128 - 1)
            bcreg = self._bcreg
            for b in range(NB):
                i = b % 2
                self.ld(xb[i], xb[i][:], self.xbuf[b * 128:(b + 1) * 128, :])
                idx = IGU[:, b:b + 1]
                self.fw.dma("gpsimd", lambda e, i=i, idx=idx: e.indirect_dma_start(
                    out=g32[i][:].rearrange("p k n -> p (k n)"), out_offset=None, in_=self.moe_wgu,
                    in_offset=bass.IndirectOffsetOnAxis(ap=idx, axis=0), bounds_check=bcreg, oob_is_err=False),
                    reads=[IGU], writes=[g32[i]])
                self.fw.dma("gpsimd", lambda e, i=i, idx=idx: e.indirect_dma_start(
                    out=d32[i][:].rearrange("p k n -> p (k n)"), out_offset=None, in_=self.moe_wd,
                    in_offset=bass.IndirectOffsetOnAxis(ap=idx, axis=0), bounds_check=bcreg, oob_is_err=False),
                    reads=[IGU], writes=[d32[i]])
                xbv = xb[i][:].rearrange("s (p k) -> s k p", k=8)
                for k in range(8):
                    self.P(lambda e, k=k, xbv=xbv: e.transpose(ptr[:, k, :], xbv[:, k, :], identb[:]), [xb[i], identb], [ptr])
                self.V(lambda e, i=i: e.tensor_copy(out=xT[i][:], in_=ptr[:]), [ptr], [xT[i]])
                for q in range(4):
                    sl = slice(q * 2, q * 2 + 2)
                    if q % 2 == 0:
                        self.A(lambda e, i=i, sl=sl: e.copy(out=gbf[i][:, sl, :], in_=g32[i][:, sl, :]), [g32[i]], [gbf[i]])
                    else:
                        self.V(lambda e, i=i, sl=sl: e.tensor_copy(out=gbf[i][:, sl, :], in_=g32[i][:, sl, :]), [g32[i]], [gbf[i]])
                self.A(lambda e, i=i: e.copy(out=dbf[i][:, 0:2, :], in_=d32[i][:, 0:2, :]), [d32[i]], [dbf[i]])
                self.V(lambda e, i=i: e.tensor_copy(out=dbf[i][:, 2:4, :], in_=d32[i][:, 2:4, :]), [d32[i]], [dbf[i]])
                ph = pH[i]
                for m in range(8):
                    tt, kh = m // 4, m % 4
                    for k in range(8):
                        lw = gbf[i][:, k, :].rearrange("d (two p k) -> d two k p", two=2, k=4)[:, tt, kh, :]
                        self.P(lambda e, i=i, m=m, k=k, ph=ph, lw=lw: e.matmul(ph[:, m, :], lhsT=lw, rhs=xT[i][:, k, :],
                                                                               start=(k == 0), stop=(k == 7)), [gbf[i], xT[i]], [ph])
                self.A(lambda e, ph=ph: e.activation(out=sg[:], in_=ph[:, 0:4, :], func=AF.Silu), [ph], [sg])
                self.V(lambda e, ph=ph: e.tensor_tensor(out=hT[:], in0=sg[:], in1=ph[:, 4:8, :], op=ALU.mult), [sg, ph], [hT])
                for nn in range(2):
                    for k in range(4):
                        self.P(lambda e, i=i, nn=nn, k=k: e.matmul(pO[:, nn * 512:(nn + 1) * 512], lhsT=hT[:, k, :], rhs=dbf[i][:, k, nn * 512:(nn + 1) * 512],
                                                                   start=(k == 0), stop=(k == 3)), [hT, dbf[i]], [pO])
                self.A(lambda e: e.copy(out=ob[:], in_=pO[:]), [pO], [ob])
                self.st(self.ybuf[b * 128:(b + 1) * 128, :], ob, ob[:])
        with fw.scope():
            g2 = []
            for v in range(2):
                t = fw.sb("g2_%d" % v, [128, D], F32)
                self.ld_row(t, t[:], self.modrow[v:v + 1, 5 * D:6 * D])
                g2.append(t)
            lng = fw.sb("lng", [128, D], F32)
            lnb = fw.sb("lnb", [128, D], F32)
            self.ld_row(lng, lng[:], self.ln_ffn_g[li:li + 1, :])
            self.ld_row(lnb, lnb[:], self.ln_ffn_b[li:li + 1, :])
            o1_ = [fw.sb("o1%d" % i, [128, D], F32) for i in range(2)]
            o2_ = [fw.sb("o2%d" % i, [128, D], F32) for i in range(2)]
            xt_ = [fw.sb("xt%d" % i, [128, D], F32) for i in range(2)]
            t1_ = [fw.sb("t1%d" % i, [128, D], F32) for i in range(2)]
            st2 = fw.sb("st2", [128, 2, 6], F32)
            mv2 = fw.sb("mv2", [128, 2], F32)
            rs2 = fw.sb("rs2", [128, 1], F32)
            clist = [c for c in range(NT) if not (final and c < NCTX)]

            def fetch(c):
                o1, o2, xt = o1_[c % 2], o2_[c % 2], xt_[c % 2]
                for k, ot in enumerate((o1, o2)):
                    col = 2 * c + k
                    self.fw.dma("gpsimd", lambda e, col=col, ot=ot: e.indirect_dma_start(
                        out=ot[:, :], out_offset=None, in_=self.ybuf, in_offset=bass.IndirectOffsetOnAxis(ap=DI[:, col:col + 1], axis=0)),
                        reads=[DI], writes=[ot])
                self.ld(xt, xt[:], xcur[c * 128:(c + 1) * 128, :])

            fetch(clist[0])
            for ci, c in enumerate(clist):
                if ci + 1 < len(clist):
                    fetch(clist[ci + 1])
                v = 1 if c < NCTX else 0
                o1, o2, xt, t1 = o1_[c % 2], o2_[c % 2], xt_[c % 2], t1_[c % 2]
                cs_ = slice(c * 128, (c + 1) * 128)
                self.V(lambda e: e.tensor_scalar(out=o1[:], in0=o1[:], scalar1=Wt[:, 2 * c:2 * c + 1], scalar2=None, op0=ALU.mult), [o1, Wt], [o1])
                self.V(lambda e: e.scalar_tensor_tensor(out=o1[:], in0=o2[:], scalar=Wt[:, 2 * c + 1:2 * c + 2], in1=o1[:], op0=ALU.mult, op1=ALU.add),
                       [o2, Wt, o1], [o1])
                self.V(lambda e: e.tensor_tensor(out=o1[:], in0=o1[:], in1=g2[v][:], op=ALU.mult), [o1, g2[v]], [o1])
                self.V(lambda e: e.scalar_tensor_tensor(out=o2[:], in0=xt[:], scalar=ALPHA, in1=o1[:], op0=ALU.mult, op1=ALU.add), [xt, o1], [o2])
                self.layer_norm(o2, t1, st2, mv2, rs2, lng, lnb, pool=False)
                if final:
                    self.st(self.yout[(c - NCTX) * 128:(c - NCTX + 1) * 128, :], t1, t1[:])
                else:
                    self.st(xnext[cs_, :], t1, t1[:])


Builder.phase_moe = _phase_moe


def build_program(nlayers=DEPTH):
    nc = bass.Bass("TRN2", target_bir_lowering=False)
    b = Builder(nc)
    b.declare()
    xcur = b.xin
    for li in range(nlayers):
        j = li // 2
        b.phase_mod(li)
        if li % 2 == 0:
            b.phase_in_ssd(li, j, xcur)
            b.phase_scan(li, j, True, xcur, b.xA)
        else:
            b.phase_in_ret(li, j, xcur)
            b.phase_scan(li, j, False, xcur, b.xA)
        b.phase_moe(li, b.xA, b.xB, final=(li == nlayers - 1))
        xcur = b.xB
    b.fw.barrier()
    b.fw.root.close()
    return nc


def kernel(**inputs):
    inp = {k: np.asarray(v) for k, v in inputs.items()}
    nc = build_program()
    consts = host_consts()
    maps = []
    for c in range(8):
        m = host_inputs(inp, c)
        m.update(consts)
        maps.append(m)
    res = run_bass_kernel_spmd(nc, maps, core_ids=list(range(8)))
    out = np.stack([np.asarray(res.results[c]["yout"]) for c in range(8)], 0)
    return out.astype(np.float32)


def _phase_scan2(self, li, j, ssd, xcur, xnext):
    fw = self.fw
    G = 4
    Hg = 8 if ssd else 1
    KC = 1 if ssd else 2
    H = G * Hg
    dv = 2048 // H
    dk = KC * 128
    with fw.scope():
        identb = fw.sb("identb", [128, 128], BF16)
        self.ld(identb, identb[:], self.c_identb)
        ones = fw.sb("ones", [128, 128], F32)
        self.ld(ones, ones[:], self.c_ones)
        tri = fw.sb("tri", [128, 128], F32)
        Wo = fw.sb("Wo", [128, 16, D], BF16)
        owv = (self.ssd_out_w if ssd else self.ret_out_w)[j].rearrange("(k p) n -> p k n", p=128)
        with fw.scope():
            wst = fw.sb("wstO", [128, 4, D], F32)
            for q in range(4):
                self.ld(wst, wst[:], owv[:, q * 4:(q + 1) * 4, :])
                self.G(lambda e, q=q: e.tensor_copy(out=Wo[:, q * 4:(q + 1) * 4, :], in_=wst[:]), [wst], [Wo])
        g1 = fw.sb("g1", [128, D], F32)
        sh2 = fw.sb("sh2", [128, D], F32)
        sc2 = fw.sb("sc2", [128, D], F32)

        def load_rows(v):
            self.ld_row(g1, g1[:], self.modrow[v:v + 1, 2 * D:3 * D])
            self.ld_row(sh2, sh2[:], self.modrow[v:v + 1, 3 * D:4 * D])
            self.ld_row(sc2, sc2[:], self.modrow[v:v + 1, 4 * D:5 * D])
            self.V(lambda e: e.tensor_scalar_add(out=sc2[:], in0=sc2[:], scalar1=1.0), [sc2], [sc2])

        lng = fw.sb("lng", [128, D], F32)
        lnb = fw.sb("lnb", [128, D], F32)
        self.ld_row(lng, lng[:], self.ln_mix_g[li:li + 1, :])
        self.ld_row(lnb, lnb[:], self.ln_mix_b[li:li + 1, :])
        nw = fw.sb("nw", [128, 2048], F32)
        self.ld_row(nw, nw[:], (self.ssd_norm_w if ssd else self.ret_gn_w)[j:j + 1, :])
        if ssd:
            dsk = fw.sb("dsk", [128, 32], F32)
            self.ld_row(dsk, dsk[:], self.ssd_d_skip[j:j + 1, :])
        else:
            nb_ = fw.sb("nb", [128, 2048], F32)
            self.ld_row(nb_, nb_[:], self.ret_gn_b[j:j + 1, :])
        S = fw.sb("S", [128, KC, 2048], F32)
        Sb = fw.sb("Sb", [128, KC, 2048], BF16)

        def dbl(name, shape, dt):
            return [fw.sb(name + "0", shape, dt), fw.sb(name + "1", shape, dt)]

        def tpl(name, shape, dt):
            return [fw.sb(name + str(i_), shape, dt) for i_ in range(3)]

        qt3 = tpl("qt", [128, G * KC, 128], BF16)
        kt2 = dbl("kt", [128, G * KC, 128], BF16)
        ktm3 = tpl("ktm", [128, G * dk], BF16)
        vt3 = tpl("vt", [128, 2048], BF16)
        xdt2 = dbl("xdt", [128, 2048], BF16) if ssd else None
        xe = dbl("xe", [128, 2048], BF16)
        cbm = dbl("cbm", [128, G, 128], F32)
        la2 = dbl("la", [128, 64], F32)
        dt2 = dbl("dt", [128, 64], F32)
        acs = fw.sb("acs", [128, 2 * H], F32)
        wend = fw.sb("wend", [128, H], F32)
        if ssd:
            ea = dbl("ea", [128, H], F32)
            cd = dbl("cd", [128, H], F32)
            E = [[fw.sb("E%d_%d" % (p_, g), [128, Hg, 128], BF16) for g in range(G)] for p_ in range(2)]
        else:
            ea0 = fw.sb("ea", [128, H], F32)
            cd0 = fw.sb("cd", [128, H], F32)
            ea = [ea0, ea0]
            cd = [cd0, cd0]
            E0 = [fw.sb("E_%d" % g, [128, Hg, 128], F32) for g in range(G)]
            E = [E0, E0]
        lt = dbl("lt", [128, Hg, 128], F32)
        nlb = dbl("nlb", [128, Hg, 128], F32)
        nones = fw.sb("nones", [128, 128], F32)
        self.V(lambda e: e.memset(nones[:], -1.0), [], [nones])
        M = dbl("M", [128, Hg, 128], BF16)
        ycs = dbl("yc", [128, 2048], F32)
        tmp0 = fw.sb("tmp", [128, 512], F32)
        tmp = [tmp0, tmp0]
        yfl = fw.sb("yfl", [128, 2048], F32)
        gt = fw.sb("gt", [128, 2048], F32)
        yb = fw.sb("yb", [128, 2048], BF16)
        yT = fw.sb("yT", [128, 16, 128], BF16)
        st6 = fw.sb("st6", [128, 4, 6], F32)
        mv = fw.sb("mv", [128, 4, 2], F32)
        ss = fw.sb("ss", [128, 4], F32)
        rstd = fw.sb("rstd", [128, 4], F32)
        xt = fw.sb("xt", [128, D], F32)
        t1 = fw.sb("t1", [128, D], F32)
        t2 = fw.sb("t2", [128, D], F32)
        st2 = fw.sb("st2", [128, 2, 6], F32)
        mv2 = fw.sb("mv2", [128, 2], F32)
        rs2 = fw.sb("rs2", [128, 1], F32)
        psm = fw.ps("psm", [128, 512], F32)
        pcb = fw.ps("pcb", [128, 512], F32)
        pbc = [fw.ps("pbc%d" % i, [128, 512], F32) for i in range(2)]
        pyd = fw.ps("pyd", [128, 512], F32)
        pyo = fw.ps("pyo", [128, 512], F32)
        pst = fw.ps("pst", [128, 512], F32)
        ptr = fw.ps("ptr", [128, 8, 128], BF16)

        qtv = self.QT.rearrange("g k p t -> p g k t")
        ktv = self.KT.rearrange("g k p t -> p g k t")
        r3 = lambda ap, h: ap.rearrange("p (h d) -> p h d", h=h)

        def decay_pre(dirn, i):
            par = i % 2
            la = la2[par]
            lad = la[:, dirn * 32:dirn * 32 + H] if ssd else la[:, 0:H]
            self.P(lambda e: e.matmul(psm[:, 0:H], lhsT=tri[:], rhs=lad, start=True, stop=True), [tri, la], [psm])
            self.P(lambda e: e.matmul(psm[:, H:2 * H], lhsT=ones[:], rhs=lad, start=True, stop=True), [ones, la], [psm])
            self.V(lambda e: e.tensor_copy(out=acs[:], in_=psm[:, 0:2 * H]), [psm], [acs])
            self.A(lambda e: e.activation(out=ea[par][:], in_=acs[:, 0:H], func=AF.Exp), [acs], [ea[par]])
            self.V(lambda e: e.tensor_tensor(out=wend[:], in0=acs[:, H:2 * H], in1=acs[:, 0:H], op=ALU.subtract), [acs], [wend])
            self.A(lambda e: e.activation(out=wend[:], in_=wend[:], func=AF.Exp), [wend], [wend])
            self.A(lambda e: e.activation(out=cd[par][:], in_=acs[:, H:2 * H], func=AF.Exp), [acs], [cd[par]])

        def decay_g(dirn, i, g):
            par = i % 2
            la = la2[par]
            ltg = lt[g % 2]
            nlg = nlb[g % 2]
            ladg = la[:, dirn * 32 + g * Hg:dirn * 32 + (g + 1) * Hg] if ssd else la[:, g:g + 1]
            self.G(lambda e: e.tensor_tensor(out=ltg[:], in0=bcast(tri[:].unsqueeze(1), [128, Hg, 128]),
                                             in1=bcast(ladg.unsqueeze(2), [128, Hg, 128]), op=ALU.mult), [tri, la], [ltg])
            self.G(lambda e: e.tensor_tensor(out=nlg[:], in0=bcast(nones[:].unsqueeze(1), [128, Hg, 128]),
                                             in1=bcast(ladg.unsqueeze(2), [128, Hg, 128]), op=ALU.mult), [nones, la], [nlg])
            Eg = E[par][g]
            for q in range((Hg + 3) // 4):
                nh = min(4, Hg - q * 4)
                pb = pbc[q % 2]
                self.P(lambda e: e.matmul(pb[:, 0:nh * 128], lhsT=ones[:], rhs=ltg[:, q * 4:q * 4 + nh, :], start=True, stop=False), [ones, ltg], [pb])
                self.P(lambda e: e.matmul(pb[:, 0:nh * 128], lhsT=tri[:], rhs=nlg[:, q * 4:q * 4 + nh, :], start=False, stop=True), [tri, nlg], [pb])
                self.A(lambda e: e.activation(out=Eg[:, q * 4:q * 4 + nh, :].rearrange("p h l -> p (h l)"), in_=pb[:, 0:nh * 128], func=AF.Exp), [pb], [Eg])

        def loads(c, i):
            cs_ = slice(c * 128, (c + 1) * 128)
            t3, p2 = i % 3, i % 2
            self.ld(qt3[t3], qt3[t3][:].rearrange("p (g k) t -> p g k t", g=G), qtv[:, :, 0:KC, cs_])
            self.ld(kt2[p2], kt2[p2][:].rearrange("p (g k) t -> p g k t", g=G), ktv[:, :, 0:KC, cs_])
            self.ld(ktm3[t3], ktm3[t3][:], self.Ktm[cs_, 0:G * dk])
            self.ld(vt3[t3], vt3[t3][:], self.Vtm[cs_, :])
            if ssd:
                self.ld(la2[p2], la2[p2][:], self.latm[cs_, :])
                self.ld(dt2[p2], dt2[p2][:], self.dttm[cs_, :])

        def XDT(i):
            return xdt2[i % 2] if ssd else vt3[i % 3]

        def stage1_pre(dirn, i):
            par = i % 2
            qt, kt, vt, dt = qt3[i % 3], kt2[par], vt3[i % 3], dt2[par]
            xdt = XDT(i)
            if ssd:
                decay_pre(dirn, i)
                self.V(lambda e: e.tensor_tensor(out=r3(xdt[:], H), in0=r3(vt[:], H),
                                                 in1=bcast(dt[:, dirn * 32:dirn * 32 + 32].unsqueeze(2), [128, H, dv]), op=ALU.mult),
                       [vt, dt], [xdt])
            self.G(lambda e: e.tensor_tensor(out=r3(xe[par][:], H), in0=r3(xdt[:], H),
                                             in1=bcast(wend[:].unsqueeze(2), [128, H, dv]), op=ALU.mult), [xdt, wend], [xe[par]])
            for g in range(G):
                for kc in range(KC):
                    self.P(lambda e, g=g, kc=kc: e.matmul(pcb[:, g * 128:(g + 1) * 128], lhsT=kt[:, g * KC + kc, :], rhs=qt[:, g * KC + kc, :],
                                                          start=(kc == 0), stop=(kc == KC - 1)), [kt, qt], [pcb])
            self.V(lambda e: e.tensor_tensor(out=cbm[par][:], in0=pcb[:].rearrange("p (g l) -> p g l", g=G),
                                             in1=bcast(tri[:].unsqueeze(1), [128, G, 128]), op=ALU.mult), [pcb, tri], [cbm[par]])

        def emitM(g, par):
            Mg = M[g % 2]
            self.V(lambda e: e.scalar_tensor_tensor(out=Mg[:], in0=E[par][g][:], scalar=1.0,
                                                    in1=bcast(cbm[par][:, g:g + 1, :], [128, Hg, 128]), op0=ALU.min, op1=ALU.mult),
                   [E[par][g], cbm[par]], [Mg])

        def s2g(i, g):
            par = i % 2
            qt, ktm = qt3[i % 3], ktm3[i % 3]
            xdt = XDT(i)
            gs = slice(g * 512, (g + 1) * 512)
            yc = ycs[par]
            if g + 1 < G:
                emitM(g + 1, par)
            Mg = M[g % 2]
            tg = tmp[g % 2]
            for hh in range(Hg):
                h = g * Hg + hh
                self.P(lambda e, h=h, hh=hh: e.matmul(pyd[:, hh * dv:(hh + 1) * dv], lhsT=Mg[:, hh, :], rhs=xdt[:, h * dv:(h + 1) * dv],
                                                      start=True, stop=True), [Mg, xdt], [pyd])
            for kc in range(KC):
                self.P(lambda e, kc=kc: e.matmul(pyo[:], lhsT=qt[:, g * KC + kc, :], rhs=Sb[:, kc, gs],
                                                 start=(kc == 0), stop=(kc == KC - 1)), [qt, Sb], [pyo])
            self.V(lambda e: e.tensor_tensor(out=r3(tg[:], Hg), in0=r3(pyo[:], Hg),
                                             in1=bcast(ea[par][:, g * Hg:(g + 1) * Hg].unsqueeze(2), [128, Hg, dv]), op=ALU.mult),
                   [pyo, ea[par]], [tg])
            self.V(lambda e: e.tensor_tensor(out=yc[:, gs], in0=tg[:], in1=pyd[:], op=ALU.add), [tg, pyd], [yc])
            for kc in range(KC):
                self.P(lambda e, kc=kc: e.matmul(pst[:], lhsT=ktm[:, g * dk + kc * 128:g * dk + (kc + 1) * 128], rhs=xe[par][:, gs],
                                                 start=True, stop=True), [ktm, xe[par]], [pst])
                self.G(lambda e, kc=kc: e.tensor_tensor(out=r3(S[:, kc, gs], Hg), in0=r3(S[:, kc, gs], Hg),
                                                        in1=bcast(cd[par][:, g * Hg:(g + 1) * Hg].unsqueeze(2), [128, Hg, dv]),
                                                        op=ALU.mult), [S, cd[par]], [S])
                self.V(lambda e, kc=kc: e.tensor_tensor(out=S[:, kc, gs], in0=S[:, kc, gs], in1=pst[:], op=ALU.add), [S, pst], [S])
                self.A(lambda e, kc=kc: e.copy(out=Sb[:, kc, gs], in_=S[:, kc, gs]), [S], [Sb])

        def stage2_post(c, dirn, i):
            par = i % 2
            yc = ycs[par]
            cs_ = slice(c * 128, (c + 1) * 128)
            if dirn == 0:
                deferred.append(lambda: self.st(self.yf[cs_, :], yc, yc[:]))
                return
            self.ld(yfl, yfl[:], self.yf[cs_, :])
            self.ld(gt, gt[:], self.gate[cs_, :])
            self.V(lambda e: e.tensor_tensor(out=yc[:], in0=yc[:], in1=yfl[:], op=ALU.add), [yc, yfl], [yc])
            self.A(lambda e: e.activation(out=gt[:], in_=gt[:], func=AF.Silu), [gt], [gt])
            yield
            if ssd:
                self.V(lambda e: e.tensor_tensor(out=yc[:], in0=yc[:], in1=gt[:], op=ALU.mult), [yc, gt], [yc])
            for g in range(4):
                self.V(lambda e, g=g: e.bn_stats(out=st6[:, g, :], in_=yc[:, g * 512:(g + 1) * 512]), [yc], [st6])
                self.V(lambda e, g=g: e.bn_aggr(out=mv[:, g, :], in_=st6[:, g, :]), [st6], [mv])
            yield
            if ssd:
                self.V(lambda e: e.tensor_tensor(out=ss[:], in0=mv[:, :, 0], in1=mv[:, :, 0], op=ALU.mult), [mv], [ss])
                self.V(lambda e: e.tensor_tensor(out=ss[:], in0=ss[:], in1=mv[:, :, 1], op=ALU.add), [ss, mv], [ss])
                self.A(lambda e: e.activation(out=ss[:], in_=ss[:], func=AF.Sqrt, bias=EPS), [ss], [ss])
                self.V(lambda e: e.reciprocal(out=rstd[:], in_=ss[:]), [ss], [rstd])
                yield
                for g in range(4):
                    gs = slice(g * 512, (g + 1) * 512)
                    self.V(lambda e, g=g, gs=gs: e.scalar_tensor_tensor(out=yb[:, gs], in0=yc[:, gs], scalar=rstd[:, g:g + 1], in1=nw[:, gs],
                                                                        op0=ALU.mult, op1=ALU.mult), [yc, rstd, nw], [yb])
            else:
                self.A(lambda e: e.activation(out=ss[:], in_=mv[:, :, 1], func=AF.Sqrt, bias=EPS), [mv], [ss])
                self.V(lambda e: e.reciprocal(out=rstd[:], in_=ss[:]), [ss], [rstd])
                yield
                for g in range(4):
                    gs = slice(g * 512, (g + 1) * 512)
                    self.V(lambda e, g=g, gs=gs: e.tensor_scalar(out=yc[:, gs], in0=yc[:, gs], scalar1=mv[:, g, 0:1], scalar2=rstd[:, g:g + 1],
                                                                 op0=ALU.subtract, op1=ALU.mult), [yc, mv, rstd], [yc])
                self.G(lambda e: e.tensor_tensor(out=yc[:], in0=yc[:], in1=nw[:], op=ALU.mult), [yc, nw], [yc])
                self.V(lambda e: e.tensor_tensor(out=yc[:], in0=yc[:], in1=nb_[:], op=ALU.add), [yc, nb_], [yc])
                self.V(lambda e: e.tensor_tensor(out=yb[:], in0=yc[:], in1=gt[:], op=ALU.mult), [yc, gt], [yb])
            yield
            for half in range(2):
                for kk in range(8):
                    k = half * 8 + kk
                    self.P(lambda e, k=k, kk=kk: e.transpose(ptr[:, kk, :], yb[:, k * 128:(k + 1) * 128], identb[:]), [yb, identb], [ptr])
                self.A(lambda e, half=half: e.copy(out=yT[:, half * 8:(half + 1) * 8, :], in_=ptr[:]), [ptr], [yT])
                yield
            pos = (pyd, pyo)
            self.ld(xt, xt[:], xcur[cs_, :])
            for nn in range(2):
                for k in range(16):
                    self.P(lambda e, k=k, nn=nn: e.matmul(pos[nn][:], lhsT=yT[:, k, :], rhs=Wo[:, k, nn * 512:(nn + 1) * 512],
                                                          start=(k == 0), stop=(k == 15)), [yT, Wo], [pos[nn]])
                ns = slice(nn * 512, (nn + 1) * 512)
                self.V(lambda e, nn=nn, ns=ns: e.tensor_tensor(out=t1[:, ns], in0=pos[nn][:], in1=g1[:, ns], op=ALU.mult), [pos[nn], g1], [t1])
            yield
            self.V(lambda e: e.scalar_tensor_tensor(out=t2[:], in0=xt[:], scalar=ALPHA, in1=t1[:], op0=ALU.mult, op1=ALU.add), [xt, t1], [t2])
            self.layer_norm(t2, t1, st2, mv2, rs2, lng, lnb)
            yield
            self.G(lambda e: e.tensor_tensor(out=t2[:], in0=t1[:], in1=sc2[:], op=ALU.mult), [t1, sc2], [t2])
            self.V(lambda e: e.tensor_tensor(out=t2[:], in0=t2[:], in1=sh2[:], op=ALU.add), [t2, sh2], [t2])
            deferred.append(lambda: self.st(xnext[cs_, :], t1, t1[:]))
            deferred.append(lambda: self.st(self.tok[cs_, :], t2, t2[:]))

        deferred = []

        def flush():
            for f in deferred:
                f()
            del deferred[:]

        for dirn in range(2):
            order = list(range(NCH)) if dirn == 0 else [1, 0] + list(range(NCH - 1, 1, -1))
            self.ld(tri, tri[:], self.c_trif[dirn])
            self.V(lambda e: e.memset(S[:], 0.0), [], [S])
            self.G(lambda e: e.memset(Sb[:], 0.0), [], [Sb])
            if dirn == 1:
                load_rows(1)
            n_ = len(order)
            if not ssd:
                for la in la2:
                    self.ld_row(la, la[:, 0:4], self.ret_decay[j, dirn:dirn + 1, :])
                    self.A(lambda e: e.activation(out=la[:, 0:4], in_=la[:, 0:4], func=AF.Exp, scale=-1.0), [la], [la])
                    self.A(lambda e: e.activation(out=la[:, 0:4], in_=la[:, 0:4], func=AF.Ln, bias=1.0), [la], [la])
                    self.V(lambda e: e.tensor_scalar_mul(out=la[:, 0:4], in0=la[:, 0:4], scalar1=-1.0), [la], [la])
                decay_pre(dirn, 0)
                for g in range(G):
                    decay_g(dirn, 0, g)
            loads(order[0], 0)
            loads(order[1], 1)
            stage1_pre(dirn, 0)
            if ssd:
                for g in range(G):
                    decay_g(dirn, 0, g)
            pend = None

            def step_post():
                nonlocal pend
                if pend is not None:
                    try:
                        next(pend)
                    except StopIteration:
                        pend = None

            for i, c in enumerate(order):
                if i + 2 < n_:
                    loads(order[i + 2], i + 2)
                flush()
                if dirn == 1 and i == NCTX + 1:
                    load_rows(0)
                if i + 1 < n_:
                    stage1_pre(dirn, i + 1)
                step_post()
                emitM(0, i % 2)
                for g in range(G):
                    if ssd and i + 1 < n_:
                        decay_g(dirn, i + 1, g)
                    step_post()
                    s2g(i, g)
                    step_post()
                while pend is not None:
                    step_post()
                if dirn == 1 and ssd:
                    par = i % 2
                    self.G(lambda e: e.tensor_tensor(out=r3(xe[par][:], H), in0=r3(vt3[i % 3][:], H),
                                                     in1=bcast(dsk[:].unsqueeze(2), [128, H, dv]), op=ALU.mult), [vt3[i % 3], dsk], [xe[par]])
                    self.V(lambda e: e.tensor_tensor(out=ycs[par][:], in0=ycs[par][:], in1=xe[par][:], op=ALU.add), [ycs[par], xe[par]], [ycs[par]])
                pend = stage2_post(c, dirn, i)
                if dirn == 0:
                    for _ in pend:
                        pass
                    pend = None
            flush()
            while pend is not None:
                step_post()
            flush()
            if dirn == 0:
                fw.barrier()


Builder.phase_scan = _phase_scan2
```

```python
import numpy as np
import ml_dtypes
from contextlib import ExitStack, contextmanager
import concourse.bass as bass
import concourse.mybir as mybir
from concourse.bass_utils import run_bass_kernel_spmd

F32 = mybir.dt.float32
BF16 = mybir.dt.bfloat16
I32 = mybir.dt.int32
ALU = mybir.AluOpType
AF = mybir.ActivationFunctionType
AX = mybir.AxisListType

ENGS = ["tensor", "vector", "scalar", "gpsimd", "sync"]
EPOCH = 20000
NDMA_SEM = 8

D = 1024
T = 4352
NCH = 34
NCTX = 2
NB = 100
DEPTH = 4
ALPHA = (2.0 * DEPTH) ** 0.25
EPS = 1e-5


class Res:
    __slots__ = ("w", "r")

    def __init__(self):
        self.w = None
        self.r = {}


class Tl:
    def __init__(self, t):
        self.t = t
        self.r = Res()

    def __getitem__(self, k):
        return self.t[k]


class FW:
    def __init__(self, nc):
        self.nc = nc
        self.root = ExitStack()
        self.es = self.root
        self.cnt = {e: 0 for e in ENGS}
        self.sems = {}
        self.waited = {e: {} for e in ENGS}
        self.dma_i = {e: 0 for e in ENGS}
        self.dma_last = {}
        self.latest = {}
        self.uid = 0

    def sem(self, key):
        if key not in self.sems:
            self.sems[key] = self.root.enter_context(self.nc.semaphore("s_%s_%s" % key))
        return self.sems[key]

    def sb(self, name, shape, dt):
        self.uid += 1
        return Tl(self.es.enter_context(self.nc.sbuf_tensor("%s_%d" % (name, self.uid), list(shape), dt)))

    def ps(self, name, shape, dt):
        self.uid += 1
        return Tl(self.es.enter_context(self.nc.psum_tensor("%s_%d" % (name, self.uid), list(shape), dt)))

    @contextmanager
    def scope(self):
        old = self.es
        self.es = ExitStack()
        try:
            yield
        finally:
            self.barrier()
            self.es.close()
            self.es = old

    def barrier(self):
        for eng in ENGS:
            for key, val in list(self.latest.items()):
                self._wait(eng, (key, val))

    def _wait(self, eng, ev):
        if ev is None:
            return
        key, val = ev
        if self.waited[eng].get(key, 0) >= val:
            return
        self.waited[eng][key] = val
        getattr(self.nc, eng).wait_ge(self.sem(key), val)

    def _deps(self, eng, reads, writes):
        evs = []
        for r in reads:
            if r.w is not None:
                evs.append(r.w)
        for w in writes:
            if w.w is not None:
                evs.append(w.w)
            evs.extend(w.r.items())
        for ev in evs:
            if ev[0][0] == "tensor" and eng == "tensor":
                continue
            self._wait(eng, ev)

    def _record(self, ev, reads, writes):
        self.latest[ev[0]] = ev[1]
        for r in reads:
            if r.r.get(ev[0], 0) < ev[1]:
                r.r[ev[0]] = ev[1]
        for w in writes:
            w.w = ev
            w.r = {}

    def op(self, eng, fn, reads=(), writes=()):
        reads = [t.r for t in reads]
        writes = [t.r for t in writes]
        self._deps(eng, reads, writes)
        c = self.cnt[eng]
        key = (eng, c // EPOCH)
        val = c % EPOCH + 1
        self.cnt[eng] = c + 1
        fn(getattr(self.nc, eng)).then_inc(self.sem(key), 1)
        self._record((key, val), reads, writes)

    def dma(self, eng, fn, reads=(), writes=()):
        reads = [t.r for t in reads]
        writes = [t.r for t in writes]
        self._deps(eng, reads, writes)
        i = self.dma_i[eng]
        self.dma_i[eng] = i + 1
        key = ("d" + eng, i % NDMA_SEM)
        prev = self.dma_last.get(key, 0)
        if prev:
            self._wait(eng, (key, prev))
        val = prev + 16
        self.dma_last[key] = val
        fn(getattr(self.nc, eng)).then_inc(self.sem(key), 16)
        self._record((key, val), reads, writes)


def bcast(ap, shape):
    return ap.to_broadcast(list(shape))


class Builder:
    def __init__(self, nc, nlayers=DEPTH, stop=None):
        self.nc = nc
        self.fw = FW(nc)
        self.nlayers = nlayers
        self.stop = stop
        self.dram = {}
        self.debug = set()
        self.only = None

    def din(self, name, shape, dt):
        if self.only is not None and name not in self.only:
            return None
        a = self.nc.dram_tensor(name, list(shape), dt, kind="ExternalInput").ap()
        self.dram[name] = a
        return a

    def dscr(self, name, shape, dt):
        kind = "ExternalOutput" if name in self.debug else "Internal"
        a = self.nc.dram_tensor(name, list(shape), dt, kind=kind).ap()
        self.dram[name] = a
        return a

    def ld(self, tile, dst, src, eng="sync"):
        self.fw.dma(eng, lambda e: e.dma_start(out=dst, in_=src), writes=[tile])

    def st(self, dst, tile, src, eng="sync"):
        self.fw.dma(eng, lambda e: e.dma_start(out=dst, in_=src), reads=[tile])

    def V(self, fn, rd, wr):
        self.fw.op("vector", fn, rd, wr)

    def A(self, fn, rd, wr):
        self.fw.op("scalar", fn, rd, wr)

    def G(self, fn, rd, wr):
        self.fw.op("gpsimd", fn, rd, wr)

    def P(self, fn, rd, wr):
        self.fw.op("tensor", fn, rd, wr)

    def ld_row(self, tile, dst, src_row, n=128):
        self.ld(tile, dst, src_row.partition_broadcast(n))

    def declare(self):
        d = self.din
        self.xin = d("xin", [T, D], F32)
        self.cvec = d("cvec", [128, 8, 2], F32)
        self.mod_w = d("mod_w", [DEPTH, D, 6 * D], F32)
        self.mod_b = d("mod_b", [DEPTH, 6 * D], F32)
        self.ssd_in_w = d("ssd_in_w", [2, D, 5184], F32)
        self.convw = d("convw", [2, 128, 24, 5], F32)
        self.convb = d("convb", [2, 128, 24], F32)
        self.ssd_dt_bias = d("ssd_dt_bias", [2, 64], F32)
        self.ssd_a_log = d("ssd_a_log", [2, 64], F32)
        self.ssd_d_skip = d("ssd_d_skip", [2, 32], F32)
        self.ssd_norm_w = d("ssd_norm_w", [2, 2048], F32)
        self.ssd_out_w = d("ssd_out_w", [2, 2048, D], F32)
        self.ret_in_w = d("ret_in_w", [2, D, 6144], F32)
        self.ret_decay = d("ret_decay_logit", [2, 2, 4], F32)
        self.ret_gn_w = d("ret_gn_w", [2, 2048], F32)
        self.ret_gn_b = d("ret_gn_b", [2, 2048], F32)
        self.ret_out_w = d("ret_out_w", [2, 2048, D], F32)
        self.ln_mix_g = d("ln_mix_g", [DEPTH, D], F32)
        self.ln_mix_b = d("ln_mix_b", [DEPTH, D], F32)
        self.ln_ffn_g = d("ln_ffn_g", [DEPTH, D], F32)
        self.ln_ffn_b = d("ln_ffn_b", [DEPTH, D], F32)
        self.moe_rw = d("moe_rw", [DEPTH, D, 36], F32)
        self.moe_rb = d("moe_rb", [DEPTH, 36], F32)
        self.moe_wgu = d("moe_w_gate_up", [DEPTH * 32 * 128, 8 * D], F32)
        self.moe_wd = d("moe_w_down", [DEPTH * 32 * 128, 4 * D], F32)
        self.c_identb = d("c_identb", [128, 128], BF16)
        self.c_identf = d("c_identf", [128, 128], F32)
        self.c_trif = d("c_trif", [2, 128, 128], F32)
        self.c_ones = d("c_ones", [128, 128], F32)
        self.c_slow = d("c_slow", [128, 128], BF16)
        self.c_rope = d("c_rope", [2, 128, T], F32)
        self.c_iota = d("c_iota", [128, 512], F32)
        self.c_pidx = d("c_pidx", [128, 12], F32)
        self.yout = self.nc.dram_tensor("yout", [4096, D], F32, kind="ExternalOutput").ap()
        s = self.dscr
        self.xA = s("xA", [T, D], F32)
        self.xB = s("xB", [T, D], F32)
        self.modrow = s("modrow", [2, 6 * D], F32)
        self.QT = s("QT", [4, 2, 128, T], BF16)
        self.KT = s("KT", [4, 2, 128, T], BF16)
        self.Ktm = s("Ktm", [T, 1024], BF16)
        self.Vtm = s("Vtm", [T, 2048], BF16)
        self.gate = s("gate", [T, 2048], F32)
        self.latm = s("latm", [T, 64], F32)
        self.dttm = s("dttm", [T, 64], F32)
        self.yf = s("yf", [T, 2048], F32)
        self.tok = s("tok", [T, D], F32)
        self.xbuf = s("xbuf", [NB * 128, D], BF16)
        self.ybuf = s("ybuf", [NB * 128, D], F32)
        self.dbg = {}

    def phase_mod(self, li):
        fw = self.fw
        with fw.scope():
            cv = fw.sb("cv", [128, 8, 2], F32)
            sv = fw.sb("sv", [128, 8, 2], F32)
            mb = fw.sb("mb", [2, 6 * D], F32)
            mr = fw.sb("mr", [2, 6 * D], F32)
            wst = fw.sb("wst", [128, 8, 512], F32)
            pm = fw.ps("pm", [128, 512], F32)
            self.ld(cv, cv[:], self.cvec)
            self.A(lambda e: e.activation(out=sv[:], in_=cv[:], func=AF.Silu), [cv], [sv])
            self.ld(mb, mb[0:1, :], self.mod_b[li:li + 1, :])
            self.ld(mb, mb[1:2, :], self.mod_b[li:li + 1, :])
            wv = self.mod_w[li].rearrange("(k p) n -> p k n", p=128)
            for n in range(12):
                self.ld(wst, wst[:], wv[:, :, n * 512:(n + 1) * 512])
                for k in range(8):
                    self.P(lambda e, k=k: e.matmul(pm[0:2, :], lhsT=sv[:, k, :], rhs=wst[:, k, :],
                                                   start=(k == 0), stop=(k == 7)), [sv, wst], [pm])
                self.V(lambda e, n=n: e.tensor_tensor(out=mr[0:2, n * 512:(n + 1) * 512], in0=pm[0:2, :],
                                                      in1=mb[0:2, n * 512:(n + 1) * 512], op=ALU.add),
                       [pm, mb], [mr])
            self.st(self.modrow, mr, mr[0:2, :])

    def make_uT(self, xcur, uT, identb, ptr, sc1, sh1, xt, u32, ub, c):
        v = 1 if c < NCTX else 0
        self.ld(xt, xt[:], xcur[c * 128:(c + 1) * 128, :])
        self.V(lambda e: e.tensor_tensor(out=u32[:], in0=xt[:], in1=sc1[v][:], op=ALU.mult), [xt, sc1[v]], [u32])
        self.G(lambda e: e.tensor_tensor(out=ub[:], in0=u32[:], in1=sh1[v][:], op=ALU.add), [u32, sh1[v]], [ub])
        for k in range(8):
            self.P(lambda e, k=k: e.transpose(ptr[:, k, :], ub[:, k * 128:(k + 1) * 128], identb[:]),
                   [ub, identb], [ptr])

    def load_mod_rows(self, lo, names):
        out = {}
        for nm, idx in names:
            tl = []
            for v in range(2):
                t = self.fw.sb("row_%s%d" % (nm, v), [128, D], F32)
                self.ld_row(t, t[:], self.modrow[v:v + 1, idx * D:(idx + 1) * D])
                tl.append(t)
            out[nm] = tl
        return out

    def phase_in_ssd(self, li, j, xcur):
        fw = self.fw
        with fw.scope():
            identb = fw.sb("identb", [128, 128], BF16)
            self.ld(identb, identb[:], self.c_identb)
            rows = self.load_mod_rows(0, [("sh1", 0), ("sc1", 1)])
            sh1, sc1 = rows["sh1"], rows["sc1"]
            for v in range(2):
                self.V(lambda e, v=v: e.tensor_scalar_add(out=sc1[v][:], in0=sc1[v][:], scalar1=1.0), [sc1[v]], [sc1[v]])
            uT = fw.sb("uT", [128, 8, T], BF16)
            xt = fw.sb("xt", [128, D], F32)
            u32 = fw.sb("u32", [128, D], F32)
            ub = fw.sb("ub", [128, D], BF16)
            ptr = fw.ps("ptr", [128, 8, 128], BF16)
            for c in range(NCH):
                self.make_uT(xcur, uT, identb, ptr, sc1, sh1, xt, u32, ub, c)
                self.A(lambda e, c=c: e.copy(out=uT[:, :, c * 128:(c + 1) * 128], in_=ptr[:]), [ptr], [uT])
            cw = fw.sb("cw", [128, 24, 5], F32)
            cb = fw.sb("cb", [128, 24], F32)
            self.ld(cw, cw[:], self.convw[j])
            self.ld(cb, cb[:], self.convb[j])
            wst_ = [fw.sb("wstA%d" % i, [128, 8, 128], F32) for i in range(2)]
            wb_ = [fw.sb("wbA%d" % i, [128, 8, 128], BF16) for i in range(2)]
            raw_ = [fw.sb("raw%d" % i, [128, T], F32) for i in range(2)]
            o_single = fw.sb("o", [128, T], F32)
            o_ = [o_single, o_single]
            ob_ = [fw.sb("ob%d" % i, [128, T], BF16) for i in range(2)]
            pp = [fw.ps("pp%d" % i, [128, 512], F32) for i in range(2)]
            trs_ = [fw.sb("trs%d" % i, [128, 8, 128], BF16) for i in range(2)]
            wv = self.ssd_in_w[j].rearrange("(k p) n -> p k n", p=128)
            segs = [(0, 256)] + [(256 + i * 512, 256 + (i + 1) * 512) for i in range(8)]
            seqs = [(0, 256), (256, T)]
            def loadW(f):
                wst = wst_[f % 2]
                col0 = 2048 + f * 128
                self.ld(wst, wst[:], wv[:, :, col0:col0 + 128])

            def stepA(f):
                wst, wb, raw, o, ob = wst_[f % 2], wb_[f % 2], raw_[f % 2], o_[f % 2], ob_[f % 2]
                self.G(lambda e, wb=wb, wst=wst: e.tensor_copy(out=wb[:], in_=wst[:]), [wst], [wb])
                for si, (a, b) in enumerate(segs):
                    p = pp[si % 2]
                    for k in range(8):
                        self.P(lambda e, k=k, a=a, b=b, p=p, wb=wb: e.matmul(p[:, 0:b - a], lhsT=wb[:, k, :], rhs=uT[:, k, a:b],
                                                                       start=(k == 0), stop=(k == 7)), [wb, uT], [p])
                    self.A(lambda e, a=a, b=b, p=p, raw=raw: e.copy(out=raw[:, a:b], in_=p[:, 0:b - a]), [p], [raw])

            def stepB1(f):
                wst, wb, raw, o, ob = wst_[f % 2], wb_[f % 2], raw_[f % 2], o_[f % 2], ob_[f % 2]
                self.A(lambda e, f=f, o=o, raw=raw: e.activation(out=o[:], in_=raw[:], func=AF.Identity,
                                                   bias=cb[:, f:f + 1], scale=cw[:, f, 2:3]), [raw, cw, cb], [o])

            def stepB(f):
                wst, wb, raw, o, ob = wst_[f % 2], wb_[f % 2], raw_[f % 2], o_[f % 2], ob_[f % 2]
                for (a, b) in seqs:
                    for kk, off in ((0, -2), (1, -1), (3, 1), (4, 2)):
                        if off < 0:
                            osl = (a - off, b)
                            isl = (a, b + off)
                        else:
                            osl = (a, b - off)
                            isl = (a + off, b)
                        self.V(lambda e, f=f, kk=kk, osl=osl, isl=isl, o=o, raw=raw: e.scalar_tensor_tensor(
                            out=o[:, osl[0]:osl[1]], in0=raw[:, isl[0]:isl[1]], scalar=cw[:, f, kk:kk + 1],
                            in1=o[:, osl[0]:osl[1]], op0=ALU.mult, op1=ALU.add), [raw, cw, o], [o])
                self.A(lambda e, o=o, ob=ob: e.activation(out=ob[:], in_=o[:], func=AF.Silu), [o], [ob])
                if f >= 16:
                    g = (f - 16) % 4
                    dst = self.KT if f < 20 else self.QT
                    self.st(dst[g, 0], ob, ob[:])
                if f < 20:
                    dstm = self.Vtm if f < 16 else self.Ktm
                    fc = f if f < 16 else f - 16
                    dv = dstm.rearrange("(c p) f -> p c f", p=128)
                    for c0 in range(0, NCH, 8):
                        trs = trs_[(c0 // 8) % 2]
                        n = min(8, NCH - c0)
                        for cc in range(n):
                            self.P(lambda e, cc=cc, c0=c0, ob=ob: e.transpose(ptr[:, cc, :], ob[:, (c0 + cc) * 128:(c0 + cc + 1) * 128],
                                                                       identb[:]), [ob, identb], [ptr])
                        self.V(lambda e, n=n, trs=trs: e.tensor_copy(out=trs[:, 0:n, :], in_=ptr[:, 0:n, :]), [ptr], [trs])
                        self.st(dv[:, c0:c0 + n, fc * 128:(fc + 1) * 128], trs, trs[:, 0:n, :])

            loadW(0)
            loadW(1)
            stepA(0)
            for f in range(24):
                stepB1(f)
                if f + 1 < 24:
                    stepA(f + 1)
                if f + 2 < 24:
                    loadW(f + 2)
                stepB(f)

            wst2 = fw.sb("wst2", [128, 8, 512], F32)
            wb2 = fw.sb("wb2", [128, 8, 512], BF16)
            zt = fw.sb("zt", [128, 512], F32)
            for n in range(4):
                self.ld(wst2, wst2[:], wv[:, :, n * 512:(n + 1) * 512])
                self.G(lambda e: e.tensor_copy(out=wb2[:], in_=wst2[:]), [wst2], [wb2])
                for c in range(NCH):
                    p = pp[c % 2]
                    for k in range(8):
                        self.P(lambda e, k=k, c=c, p=p: e.matmul(p[:], lhsT=uT[:, k, c * 128:(c + 1) * 128], rhs=wb2[:, k, :],
                                                                 start=(k == 0), stop=(k == 7)), [uT, wb2], [p])
                    self.A(lambda e, p=p: e.copy(out=zt[:], in_=p[:]), [p], [zt])
                    self.st(self.gate[c * 128:(c + 1) * 128, n * 512:(n + 1) * 512], zt, zt[:])
            dtb = fw.sb("dtb", [128, 64], F32)
            nega = fw.sb("nega", [128, 64], F32)
            self.ld_row(dtb, dtb[:], self.ssd_dt_bias[j:j + 1, :])
            self.ld_row(nega, nega[:], self.ssd_a_log[j:j + 1, :])
            self.A(lambda e: e.activation(out=nega[:], in_=nega[:], func=AF.Exp), [nega], [nega])
            self.V(lambda e: e.tensor_scalar_mul(out=nega[:], in0=nega[:], scalar1=-1.0), [nega], [nega])
            self.ld(wst2, wst2[:, :, 0:64], wv[:, :, 5120:5184])
            self.G(lambda e: e.tensor_copy(out=wb2[:, :, 0:64], in_=wst2[:, :, 0:64]), [wst2], [wb2])
            d0 = fw.sb("d0", [128, 64], F32)
            d1 = fw.sb("d1", [128, 64], F32)
            d2 = fw.sb("d2", [128, 64], F32)
            for c in range(NCH):
                p = pp[c % 2]
                for k in range(8):
                    self.P(lambda e, k=k, c=c, p=p: e.matmul(p[:, 0:64], lhsT=uT[:, k, c * 128:(c + 1) * 128], rhs=wb2[:, k, 0:64],
                                                             start=(k == 0), stop=(k == 7)), [uT, wb2], [p])
                self.V(lambda e, p=p: e.tensor_tensor(out=d0[:], in0=p[:, 0:64], in1=dtb[:], op=ALU.add), [p, dtb], [d0])
                self.V(lambda e: e.tensor_scalar_mul(out=d1[:], in0=d0[:], scalar1=-1.0), [d0], [d1])
                self.V(lambda e: e.tensor_tensor(out=d1[:], in0=d1[:], in1=d0[:], op=ALU.max), [d0, d1], [d1])
                self.A(lambda e: e.activation(out=d1[:], in_=d1[:], func=AF.Exp, scale=-1.0), [d1], [d1])
                self.A(lambda e: e.activation(out=d1[:], in_=d1[:], func=AF.Ln, bias=1.0), [d1], [d1])
                self.V(lambda e: e.scalar_tensor_tensor(out=d2[:], in0=d0[:], scalar=0.0, in1=d1[:], op0=ALU.max, op1=ALU.add),
                       [d0, d1], [d2])
                self.st(self.dttm[c * 128:(c + 1) * 128, :], d2, d2[:])
                self.V(lambda e: e.tensor_tensor(out=d0[:], in0=d2[:], in1=nega[:], op=ALU.mult), [d2, nega], [d0])
                self.st(self.latm[c * 128:(c + 1) * 128, :], d0, d0[:])

    def phase_in_ret(self, li, j, xcur):
        fw = self.fw
        with fw.scope():
            identb = fw.sb("identb", [128, 128], BF16)
            self.ld(identb, identb[:], self.c_identb)
            rows = self.load_mod_rows(0, [("sh1", 0), ("sc1", 1)])
            sh1, sc1 = rows["sh1"], rows["sc1"]
            for v in range(2):
                self.V(lambda e, v=v: e.tensor_scalar_add(out=sc1[v][:], in0=sc1[v][:], scalar1=1.0), [sc1[v]], [sc1[v]])
            W = fw.sb("Wret", [128, 8, 6144], BF16)
            wst = fw.sb("wstR", [128, 8, 512], F32)
            wv = self.ret_in_w[j].rearrange("(k p) n -> p k n", p=128)
            for n in range(12):
                self.ld(wst, wst[:], wv[:, :, n * 512:(n + 1) * 512])
                eng = self.G if n % 2 == 0 else self.A
                if n % 2 == 0:
                    self.G(lambda e, n=n: e.tensor_copy(out=W[:, :, n * 512:(n + 1) * 512], in_=wst[:]), [wst], [W])
                else:
                    self.A(lambda e, n=n: e.copy(out=W[:, :, n * 512:(n + 1) * 512], in_=wst[:]), [wst], [W])
            uT = fw.sb("uTs", [128, 8, 512], BF16)
            xt = fw.sb("xt", [128, D], F32)
            u32 = fw.sb("u32", [128, D], F32)
            ub = fw.sb("ub", [128, D], BF16)
            ptr = fw.ps("ptr", [128, 8, 128], BF16)
            pp = [fw.ps("pp%d" % i, [128, 512], F32) for i in range(4)]
            cs = fw.sb("cs", [128, 512], F32)
            sn = fw.sb("sn", [128, 512], F32)
            r1_ = [fw.sb("r1%d" % i, [128, 512], F32) for i in range(2)]
            r2_ = [fw.sb("r2%d" % i, [128, 512], F32) for i in range(2)]
            ta_ = [fw.sb("ta%d" % i, [128, 512], F32) for i in range(2)]
            tb_ = [fw.sb("tb%d" % i, [128, 512], F32) for i in range(2)]
            o1_ = [fw.sb("o1%d" % i, [128, 512], BF16) for i in range(2)]
            o2_ = [fw.sb("o2%d" % i, [128, 512], BF16) for i in range(2)]
            trs_ = [fw.sb("trs%d" % i, [128, 8, 128], BF16) for i in range(2)]
            zt = fw.sb("zt", [128, 512], F32)
            vb = fw.sb("vb", [128, 512], BF16)
            segs = [(0, 256)] + [(256 + i * 512, 256 + (i + 1) * 512) for i in range(8)]
            ktv = self.Ktm.rearrange("(c p) f -> p c f", p=128)
            for (a, b) in segs:
                n = b - a
                nt = n // 128
                c0 = a // 128
                for ci in range(nt):
                    self.make_uT(xcur, uT, identb, ptr, sc1, sh1, xt, u32, ub, c0 + ci)
                    self.A(lambda e, ci=ci: e.copy(out=uT[:, :, ci * 128:(ci + 1) * 128], in_=ptr[:]), [ptr], [uT])
                self.ld(cs, cs[:, 0:n], self.c_rope[0, :, a:b])
                self.ld(sn, sn[:, 0:n], self.c_rope[1, :, a:b])
                def qkA(which, h):
                    ii = (which * 4 + h) % 2
                    r1, r2, ta, tb, o1, o2, trs = r1_[ii], r2_[ii], ta_[ii], tb_[ii], o1_[ii], o2_[ii], trs_[ii]
                    base = which * 1024 + h * 256
                    for half, (pt, rr) in enumerate(((pp[ii * 2], r1), (pp[ii * 2 + 1], r2))):
                        cb0 = base + half * 128
                        for k in range(8):
                            self.P(lambda e, k=k, cb0=cb0, pt=pt: e.matmul(pt[:, 0:n], lhsT=W[:, k, cb0:cb0 + 128], rhs=uT[:, k, 0:n],
                                                                           start=(k == 0), stop=(k == 7)), [W, uT], [pt])
                        sc = 1.0 if which == 0 else 0.0625
                        self.A(lambda e, pt=pt, rr=rr, sc=sc: e.activation(out=rr[:, 0:n], in_=pt[:, 0:n], func=AF.Copy, scale=sc),
                               [pt], [rr])

                def qkB(which, h):
                    ii = (which * 4 + h) % 2
                    r1, r2, ta, tb, o1, o2, trs = r1_[ii], r2_[ii], ta_[ii], tb_[ii], o1_[ii], o2_[ii], trs_[ii]
                    base = which * 1024 + h * 256
                    self.V(lambda e, ta=ta, r1=r1: e.tensor_tensor(out=ta[:, 0:n], in0=r1[:, 0:n], in1=cs[:, 0:n], op=ALU.mult), [r1, cs], [ta])
                    self.G(lambda e, tb=tb, r2=r2: e.tensor_tensor(out=tb[:, 0:n], in0=r2[:, 0:n], in1=sn[:, 0:n], op=ALU.mult), [r2, sn], [tb])
                    self.V(lambda e, ta=ta, tb=tb, o1=o1: e.tensor_tensor(out=o1[:, 0:n], in0=ta[:, 0:n], in1=tb[:, 0:n], op=ALU.subtract), [ta, tb], [o1])
                    self.V(lambda e, ta=ta, r1=r1: e.tensor_tensor(out=ta[:, 0:n], in0=r1[:, 0:n], in1=sn[:, 0:n], op=ALU.mult), [r1, sn], [ta])
                    self.G(lambda e, tb=tb, r2=r2: e.tensor_tensor(out=tb[:, 0:n], in0=r2[:, 0:n], in1=cs[:, 0:n], op=ALU.mult), [r2, cs], [tb])
                    self.V(lambda e, ta=ta, tb=tb, o2=o2: e.tensor_tensor(out=o2[:, 0:n], in0=ta[:, 0:n], in1=tb[:, 0:n], op=ALU.add), [ta, tb], [o2])
                    dst = self.QT if which == 0 else self.KT
                    self.st(dst[h, 0, :, a:b], o1, o1[:, 0:n])
                    self.st(dst[h, 1, :, a:b], o2, o2[:, 0:n])
                    if which == 1:
                        for half, oo in enumerate((o1, o2)):
                            for ci in range(nt):
                                self.P(lambda e, ci=ci, oo=oo, half=half: e.transpose(ptr[:, half * 4 + ci, :], oo[:, ci * 128:(ci + 1) * 128],
                                                                                      identb[:]), [oo, identb], [ptr])
                        self.V(lambda e, trs=trs: e.tensor_copy(out=trs[:], in_=ptr[:]), [ptr], [trs])
                        for half in range(2):
                            col = h * 256 + half * 128
                            self.st(ktv[:, c0:c0 + nt, col:col + 128], trs, trs[:, half * 4:half * 4 + nt, :])

                wh = [(w_, h_) for w_ in range(2) for h_ in range(4)]
                qkA(*wh[0])
                for t_ in range(8):
                    if t_ + 1 < 8:
                        qkA(*wh[t_ + 1])
                    qkB(*wh[t_])
                for ci in range(nt):
                    c = c0 + ci
                    for nn in range(8):
                        p = pp[2 + nn % 2]
                        colw = 2048 + nn * 512
                        for k in range(8):
                            self.P(lambda e, k=k, ci=ci, p=p, colw=colw: e.matmul(p[:], lhsT=uT[:, k, ci * 128:(ci + 1) * 128],
                                                                                  rhs=W[:, k, colw:colw + 512], start=(k == 0), stop=(k == 7)),
                                   [uT, W], [p])
                        if nn < 4:
                            self.A(lambda e, p=p: e.copy(out=vb[:], in_=p[:]), [p], [vb])
                            self.st(self.Vtm[c * 128:(c + 1) * 128, nn * 512:(nn + 1) * 512], vb, vb[:])
                        else:
                            self.V(lambda e, p=p: e.tensor_copy(out=zt[:], in_=p[:]), [p], [zt])
                            self.st(self.gate[c * 128:(c + 1) * 128, (nn - 4) * 512:(nn - 3) * 512], zt, zt[:])

    def phase_scan(self, li, j, ssd, xcur, xnext):
        fw = self.fw
        G = 4
        Hg = 8 if ssd else 1
        KC = 1 if ssd else 2
        H = G * Hg
        dv = 2048 // H
        dk = KC * 128
        with fw.scope():
            identb = fw.sb("identb", [128, 128], BF16)
            self.ld(identb, identb[:], self.c_identb)
            ones = fw.sb("ones", [128, 128], F32)
            self.ld(ones, ones[:], self.c_ones)
            tri = fw.sb("tri", [128, 128], F32)
            Wo = fw.sb("Wo", [128, 16, D], BF16)
            wst = fw.sb("wstO", [128, 4, D], F32)
            owv = (self.ssd_out_w if ssd else self.ret_out_w)[j].rearrange("(k p) n -> p k n", p=128)
            for q in range(4):
                self.ld(wst, wst[:], owv[:, q * 4:(q + 1) * 4, :])
                self.G(lambda e, q=q: e.tensor_copy(out=Wo[:, q * 4:(q + 1) * 4, :], in_=wst[:]), [wst], [Wo])
            rows = self.load_mod_rows(0, [("g1", 2), ("sh2", 3), ("sc2", 4)])
            g1, sh2, sc2 = rows["g1"], rows["sh2"], rows["sc2"]
            for v in range(2):
                self.V(lambda e, v=v: e.tensor_scalar_add(out=sc2[v][:], in0=sc2[v][:], scalar1=1.0), [sc2[v]], [sc2[v]])
            lng = fw.sb("lng", [128, D], F32)
            lnb = fw.sb("lnb", [128, D], F32)
            self.ld_row(lng, lng[:], self.ln_mix_g[li:li + 1, :])
            self.ld_row(lnb, lnb[:], self.ln_mix_b[li:li + 1, :])
            nw = fw.sb("nw", [128, 2048], F32)
            self.ld_row(nw, nw[:], (self.ssd_norm_w if ssd else self.ret_gn_w)[j:j + 1, :])
            if ssd:
                dsk = fw.sb("dsk", [128, 32], F32)
                self.ld_row(dsk, dsk[:], self.ssd_d_skip[j:j + 1, :])
            else:
                nb_ = fw.sb("nb", [128, 2048], F32)
                self.ld_row(nb_, nb_[:], self.ret_gn_b[j:j + 1, :])
            S = fw.sb("S", [128, KC, 2048], F32)
            Sb = fw.sb("Sb", [128, KC, 2048], BF16)
            qt = fw.sb("qt", [128, G * KC, 128], BF16)
            kt = fw.sb("kt", [128, G * KC, 128], BF16)
            ktm = fw.sb("ktm", [128, G * dk], BF16)
            vt = fw.sb("vt", [128, 2048], BF16)
            la = fw.sb("la", [128, 64], F32)
            dt = fw.sb("dt", [128, 64], F32)
            acs = fw.sb("acs", [128, 2 * H], F32)
            nacs = fw.sb("nacs", [128, H], F32)
            ea = fw.sb("ea", [128, H], F32)
            wend = fw.sb("wend", [128, H], F32)
            cd = fw.sb("cd", [128, H], F32)
            E = fw.sb("E", [128, H, 128], F32)
            lt = fw.sb("lt", [128, Hg, 128], F32)
            M = fw.sb("M", [128, Hg, 128], BF16)
            cbm = fw.sb("cbm", [128, G, 128], F32)
            xdt = fw.sb("xdt", [128, 2048], BF16) if ssd else vt
            xe = fw.sb("xe", [128, 2048], BF16)
            yc = fw.sb("yc", [128, 2048], F32)
            tmp = fw.sb("tmp", [128, 512], F32)
            yfl = fw.sb("yfl", [128, 2048], F32)
            gt = fw.sb("gt", [128, 2048], F32)
            yb = fw.sb("yb", [128, 2048], BF16)
            yT = fw.sb("yT", [128, 16, 128], BF16)
            st6 = fw.sb("st6", [128, 4, 6], F32)
            mv = fw.sb("mv", [128, 4, 2], F32)
            ss = fw.sb("ss", [128, 4], F32)
            rstd = fw.sb("rstd", [128, 4], F32)
            xt = fw.sb("xt", [128, D], F32)
            t1 = fw.sb("t1", [128, D], F32)
            t2 = fw.sb("t2", [128, D], F32)
            st2 = fw.sb("st2", [128, 2, 6], F32)
            mv2 = fw.sb("mv2", [128, 2], F32)
            rs2 = fw.sb("rs2", [128, 1], F32)
            psm = fw.ps("psm", [128, 512], F32)
            pcb = fw.ps("pcb", [128, 512], F32)
            pbc = [fw.ps("pbc%d" % i, [128, 512], F32) for i in range(2)]
            pA = fw.ps("pA", [128, 1024], F32)
            pst = fw.ps("pst", [128, 512], F32)
            ptr = fw.ps("ptr", [128, 8, 128], BF16)

            qtv = self.QT.rearrange("g k p t -> p g k t")
            ktv = self.KT.rearrange("g k p t -> p g k t")

            def decay_prep(dirn):
                lad = la[:, dirn * 32:dirn * 32 + H] if ssd else la[:, 0:H]
                self.P(lambda e: e.matmul(psm[:, 0:H], lhsT=tri[:], rhs=lad, start=True, stop=True), [tri, la], [psm])
                self.P(lambda e: e.matmul(psm[:, H:2 * H], lhsT=ones[:], rhs=lad, start=True, stop=True), [ones, la], [psm])
                self.V(lambda e: e.tensor_copy(out=acs[:], in_=psm[:, 0:2 * H]), [psm], [acs])
                self.V(lambda e: e.tensor_scalar_mul(out=nacs[:], in0=acs[:, 0:H], scalar1=-1.0), [acs], [nacs])
                self.A(lambda e: e.activation(out=ea[:], in_=acs[:, 0:H], func=AF.Exp), [acs], [ea])
                self.V(lambda e: e.tensor_tensor(out=wend[:], in0=acs[:, H:2 * H], in1=acs[:, 0:H], op=ALU.subtract), [acs], [wend])
                self.A(lambda e: e.activation(out=wend[:], in_=wend[:], func=AF.Exp), [wend], [wend])
                self.A(lambda e: e.activation(out=cd[:], in_=acs[:, H:2 * H], func=AF.Exp), [acs], [cd])
                for g in range(G):
                    hs = slice(g * Hg, (g + 1) * Hg)
                    ladg = la[:, dirn * 32 + g * Hg:dirn * 32 + (g + 1) * Hg] if ssd else la[:, g:g + 1]
                    self.G(lambda e, ladg=ladg: e.tensor_tensor(out=lt[:], in0=bcast(tri[:].unsqueeze(1), [128, Hg, 128]),
                                                                in1=bcast(ladg.unsqueeze(2), [128, Hg, 128]), op=ALU.mult),
                           [tri, la], [lt])
                    for q in range((Hg + 3) // 4):
                        nh = min(4, Hg - q * 4)
                        pb = pbc[q % 2]
                        self.P(lambda e, q=q, nh=nh, pb=pb: e.matmul(pb[:, 0:nh * 128], lhsT=ones[:], rhs=lt[:, q * 4:q * 4 + nh, :],
                                                                     start=True, stop=True), [ones, lt], [pb])
                        for hh in range(nh):
                            h = g * Hg + q * 4 + hh
                            self.A(lambda e, h=h, hh=hh, pb=pb: e.activation(out=E[:, h, :], in_=pb[:, hh * 128:(hh + 1) * 128], func=AF.Exp,
                                                                            bias=nacs[:, h:h + 1], scale=1.0), [pb, nacs], [E])

            for dirn in range(2):
                order = list(range(NCH)) if dirn == 0 else [1, 0] + list(range(NCH - 1, 1, -1))
                self.ld(tri, tri[:], self.c_trif[dirn])
                self.V(lambda e: e.memset(S[:], 0.0), [], [S])
                self.G(lambda e: e.memset(Sb[:], 0.0), [], [Sb])
                if not ssd:
                    self.ld_row(la, la[:, 0:4], self.ret_decay[j, dirn:dirn + 1, :])
                    self.A(lambda e: e.activation(out=la[:, 0:4], in_=la[:, 0:4], func=AF.Exp, scale=-1.0), [la], [la])
                    self.A(lambda e: e.activation(out=la[:, 0:4], in_=la[:, 0:4], func=AF.Ln, bias=1.0), [la], [la])
                    self.V(lambda e: e.tensor_scalar_mul(out=la[:, 0:4], in0=la[:, 0:4], scalar1=-1.0), [la], [la])
                    decay_prep(dirn)
                for c in order:
                    v = 1 if c < NCTX else 0
                    cs_ = slice(c * 128, (c + 1) * 128)
                    self.ld(qt, qt[:].rearrange("p (g k) t -> p g k t", g=G), qtv[:, :, 0:KC, cs_])
                    self.ld(kt, kt[:].rearrange("p (g k) t -> p g k t", g=G), ktv[:, :, 0:KC, cs_])
                    self.ld(ktm, ktm[:], self.Ktm[cs_, 0:G * dk])
                    self.ld(vt, vt[:], self.Vtm[cs_, :])
                    if ssd:
                        self.ld(la, la[:], self.latm[cs_, :])
                        self.ld(dt, dt[:], self.dttm[cs_, :])
                        decay_prep(dirn)
                        self.V(lambda e: e.tensor_tensor(out=xdt[:].rearrange("p (h d) -> p h d", h=H), in0=vt[:].rearrange("p (h d) -> p h d", h=H),
                                                         in1=bcast(dt[:, dirn * 32:dirn * 32 + 32].unsqueeze(2), [128, H, dv]), op=ALU.mult),
                               [vt, dt], [xdt])
                    self.G(lambda e: e.tensor_tensor(out=xe[:].rearrange("p (h d) -> p h d", h=H), in0=xdt[:].rearrange("p (h d) -> p h d", h=H),
                                                     in1=bcast(wend[:].unsqueeze(2), [128, H, dv]), op=ALU.mult), [xdt, wend], [xe])
                    for g in range(G):
                        for kc in range(KC):
                            self.P(lambda e, g=g, kc=kc: e.matmul(pcb[:, g * 128:(g + 1) * 128], lhsT=kt[:, g * KC + kc, :], rhs=qt[:, g * KC + kc, :],
                                                                  start=(kc == 0), stop=(kc == KC - 1)), [kt, qt], [pcb])
                    self.V(lambda e: e.tensor_tensor(out=cbm[:], in0=pcb[:].rearrange("p (g l) -> p g l", g=G),
                                                     in1=bcast(tri[:].unsqueeze(1), [128, G, 128]), op=ALU.mult), [pcb, tri], [cbm])
                    for g in range(G):
                        gs = slice(g * 512, (g + 1) * 512)
                        self.V(lambda e, g=g: e.scalar_tensor_tensor(out=M[:], in0=E[:, g * Hg:(g + 1) * Hg, :], scalar=1.0,
                                                                     in1=bcast(cbm[:, g:g + 1, :], [128, Hg, 128]), op0=ALU.min, op1=ALU.mult),
                               [E, cbm], [M])
                        for hh in range(Hg):
                            h = g * Hg + hh
                            self.P(lambda e, h=h, hh=hh: e.matmul(pA[:, h * dv:(h + 1) * dv] if False else pA[:, (h * dv) % 512:(h * dv) % 512 + dv],
                                                                  lhsT=M[:, hh, :], rhs=xdt[:, h * dv:(h + 1) * dv], start=True, stop=True),
                                   [M, xdt], [pA])
                        for kc in range(KC):
                            self.P(lambda e, g=g, kc=kc, gs=gs: e.matmul(pA[:, 512:1024], lhsT=qt[:, g * KC + kc, :], rhs=Sb[:, kc, gs],
                                                                         start=(kc == 0), stop=(kc == KC - 1)), [qt, Sb], [pA])
                        self.V(lambda e, g=g: e.tensor_tensor(out=tmp[:].rearrange("p (h d) -> p h d", h=Hg),
                                                              in0=pA[:, 512:1024].rearrange("p (h d) -> p h d", h=Hg),
                                                              in1=bcast(ea[:, g * Hg:(g + 1) * Hg].unsqueeze(2), [128, Hg, dv]), op=ALU.mult),
                               [pA, ea], [tmp])
                        self.V(lambda e, gs=gs: e.tensor_tensor(out=yc[:, gs], in0=tmp[:], in1=pA[:, 0:512], op=ALU.add), [tmp, pA], [yc])
                        for kc in range(KC):
                            self.P(lambda e, g=g, kc=kc, gs=gs: e.matmul(pst[:], lhsT=ktm[:, g * dk + kc * 128:g * dk + (kc + 1) * 128], rhs=xe[:, gs],
                                                                         start=True, stop=True), [ktm, xe], [pst])
                            self.V(lambda e, g=g, kc=kc, gs=gs: e.tensor_tensor(out=S[:, kc, gs].rearrange("p (h d) -> p h d", h=Hg),
                                                                                in0=S[:, kc, gs].rearrange("p (h d) -> p h d", h=Hg),
                                                                                in1=bcast(cd[:, g * Hg:(g + 1) * Hg].unsqueeze(2), [128, Hg, dv]),
                                                                                op=ALU.mult), [S, cd], [S])
                            self.V(lambda e, kc=kc, gs=gs: e.tensor_tensor(out=S[:, kc, gs], in0=S[:, kc, gs], in1=pst[:], op=ALU.add), [S, pst], [S])
                            self.A(lambda e, kc=kc, gs=gs: e.copy(out=Sb[:, kc, gs], in_=S[:, kc, gs]), [S], [Sb])
                    if dirn == 0:
                        self.st(self.yf[cs_, :], yc, yc[:])
                        continue
                    self.ld(yfl, yfl[:], self.yf[cs_, :])
                    self.ld(gt, gt[:], self.gate[cs_, :])
                    self.V(lambda e: e.tensor_tensor(out=yc[:], in0=yc[:], in1=yfl[:], op=ALU.add), [yc, yfl], [yc])
                    if ssd:
                        self.G(lambda e: e.tensor_tensor(out=yfl[:].rearrange("p (h d) -> p h d", h=H), in0=vt[:].rearrange("p (h d) -> p h d", h=H),
                                                         in1=bcast(dsk[:].unsqueeze(2), [128, H, dv]), op=ALU.mult), [vt, dsk], [yfl])
                        self.V(lambda e: e.tensor_tensor(out=yc[:], in0=yc[:], in1=yfl[:], op=ALU.add), [yc, yfl], [yc])
                        self.A(lambda e: e.activation(out=gt[:], in_=gt[:], func=AF.Silu), [gt], [gt])
                        self.V(lambda e: e.tensor_tensor(out=yc[:], in0=yc[:], in1=gt[:], op=ALU.mult), [yc, gt], [yc])
                        for g in range(4):
                            self.V(lambda e, g=g: e.bn_stats(out=st6[:, g, :], in_=yc[:, g * 512:(g + 1) * 512]), [yc], [st6])
                            self.V(lambda e, g=g: e.bn_aggr(out=mv[:, g, :], in_=st6[:, g, :]), [st6], [mv])
                        self.V(lambda e: e.tensor_tensor(out=ss[:], in0=mv[:, :, 0], in1=mv[:, :, 0], op=ALU.mult), [mv], [ss])
                        self.V(lambda e: e.tensor_tensor(out=ss[:], in0=ss[:], in1=mv[:, :, 1], op=ALU.add), [ss, mv], [ss])
                        self.A(lambda e: e.activation(out=ss[:], in_=ss[:], func=AF.Sqrt, bias=EPS), [ss], [ss])
                        self.V(lambda e: e.reciprocal(out=rstd[:], in_=ss[:]), [ss], [rstd])
                        for g in range(4):
                            gs = slice(g * 512, (g + 1) * 512)
                            self.V(lambda e, g=g, gs=gs: e.scalar_tensor_tensor(out=yb[:, gs], in0=yc[:, gs], scalar=rstd[:, g:g + 1], in1=nw[:, gs],
                                                                                op0=ALU.mult, op1=ALU.mult), [yc, rstd, nw], [yb])
                    else:
                        for g in range(4):
                            self.V(lambda e, g=g: e.bn_stats(out=st6[:, g, :], in_=yc[:, g * 512:(g + 1) * 512]), [yc], [st6])
                            self.V(lambda e, g=g: e.bn_aggr(out=mv[:, g, :], in_=st6[:, g, :]), [st6], [mv])
                        self.A(lambda e: e.activation(out=ss[:], in_=mv[:, :, 1], func=AF.Sqrt, bias=EPS), [mv], [ss])
                        self.V(lambda e: e.reciprocal(out=rstd[:], in_=ss[:]), [ss], [rstd])
                        self.A(lambda e: e.activation(out=gt[:], in_=gt[:], func=AF.Silu), [gt], [gt])
                        for g in range(4):
                            gs = slice(g * 512, (g + 1) * 512)
                            self.V(lambda e, g=g, gs=gs: e.tensor_scalar(out=yc[:, gs], in0=yc[:, gs], scalar1=mv[:, g, 0:1], scalar2=rstd[:, g:g + 1],
                                                                         op0=ALU.subtract, op1=ALU.mult), [yc, mv, rstd], [yc])
                        self.G(lambda e: e.tensor_tensor(out=yc[:], in0=yc[:], in1=nw[:], op=ALU.mult), [yc, nw], [yc])
                        self.V(lambda e: e.tensor_tensor(out=yc[:], in0=yc[:], in1=nb_[:], op=ALU.add), [yc, nb_], [yc])
                        self.V(lambda e: e.tensor_tensor(out=yb[:], in0=yc[:], in1=gt[:], op=ALU.mult), [yc, gt], [yb])
                    for half in range(2):
                        for kk in range(8):
                            k = half * 8 + kk
                            self.P(lambda e, k=k, kk=kk: e.transpose(ptr[:, kk, :], yb[:, k * 128:(k + 1) * 128], identb[:]), [yb, identb], [ptr])
                        self.A(lambda e, half=half: e.copy(out=yT[:, half * 8:(half + 1) * 8, :], in_=ptr[:]), [ptr], [yT])
                    for nn in range(2):
                        for k in range(16):
                            self.P(lambda e, k=k, nn=nn: e.matmul(pA[:, nn * 512:(nn + 1) * 512], lhsT=yT[:, k, :], rhs=Wo[:, k, nn * 512:(nn + 1) * 512],
                                                                  start=(k == 0), stop=(k == 15)), [yT, Wo], [pA])
                    self.ld(xt, xt[:], xcur[cs_, :])
                    self.V(lambda e: e.tensor_tensor(out=t1[:], in0=pA[:], in1=g1[v][:], op=ALU.mult), [pA, g1[v]], [t1])
                    self.V(lambda e: e.scalar_tensor_tensor(out=t2[:], in0=xt[:], scalar=ALPHA, in1=t1[:], op0=ALU.mult, op1=ALU.add), [xt, t1], [t2])
                    self.layer_norm(t2, t1, st2, mv2, rs2, lng, lnb)
                    self.st(xnext[cs_, :], t1, t1[:])
                    self.G(lambda e: e.tensor_tensor(out=t2[:], in0=t1[:], in1=sc2[v][:], op=ALU.mult), [t1, sc2[v]], [t2])
                    self.V(lambda e: e.tensor_tensor(out=t2[:], in0=t2[:], in1=sh2[v][:], op=ALU.add), [t2, sh2[v]], [t2])
                    self.st(self.tok[cs_, :], t2, t2[:])
                if dirn == 0:
                    fw.barrier()

    def layer_norm(self, xin_t, out_t, st2, mv2, rs2, lng, lnb, pool=True):
        for hf in range(2):
            self.V(lambda e, hf=hf: e.bn_stats(out=st2[:, hf, :], in_=xin_t[:, hf * 512:(hf + 1) * 512]), [xin_t], [st2])
        self.V(lambda e: e.bn_aggr(out=mv2[:], in_=st2[:].rearrange("p a b -> p (a b)")), [st2], [mv2])
        self.A(lambda e: e.activation(out=rs2[:], in_=mv2[:, 1:2], func=AF.Sqrt, bias=EPS), [mv2], [rs2])
        self.V(lambda e: e.reciprocal(out=rs2[:], in_=rs2[:]), [rs2], [rs2])
        self.V(lambda e: e.tensor_scalar(out=out_t[:], in0=xin_t[:], scalar1=mv2[:, 0:1], scalar2=rs2[:, 0:1],
                                         op0=ALU.subtract, op1=ALU.mult), [xin_t, mv2, rs2], [out_t])
        (self.G if pool else self.V)(lambda e: e.tensor_tensor(out=out_t[:], in0=out_t[:], in1=lng[:], op=ALU.mult), [out_t, lng], [out_t])
        self.V(lambda e: e.tensor_tensor(out=out_t[:], in0=out_t[:], in1=lnb[:], op=ALU.add), [out_t, lnb], [out_t])


def host_consts():
    bf = ml_dtypes.bfloat16
    c = {}
    c["c_identb"] = np.eye(128, dtype=np.float32).astype(bf)
    c["c_identf"] = np.eye(128, dtype=np.float32)
    s = np.arange(128)
    c["c_trif"] = np.stack([(s[:, None] <= s[None, :]), (s[:, None] >= s[None, :])]).astype(np.float32)
    c["c_ones"] = np.ones((128, 128), np.float32)
    c["c_slow"] = (s[:, None] < s[None, :]).astype(np.float32).astype(bf)
    n_freq = 64
    inv_freq = (10000.0 ** (-np.arange(n_freq, dtype=np.float32) / np.float32(n_freq))).astype(np.float32)
    pos = np.arange(4096)
    rows = (pos // 64).astype(np.float32)
    cols = (pos % 64).astype(np.float32)
    ang = np.concatenate([rows[:, None] * inv_freq[None, :], cols[:, None] * inv_freq[None, :]], -1).astype(np.float32)
    cos = np.ones((T, 128), np.float32)
    sin = np.zeros((T, 128), np.float32)
    cos[256:] = np.cos(ang)
    sin[256:] = np.sin(ang)
    c["c_rope"] = np.ascontiguousarray(np.stack([cos.T, sin.T])).astype(np.float32)
    c["c_iota"] = np.broadcast_to(np.arange(512, dtype=np.float32)[None, :], (128, 512)).copy()
    c["c_pidx"] = (np.arange(12)[None, :] * 128 + np.arange(128)[:, None]).astype(np.float32)
    return c


def host_inputs(inp, b):
    f = np.float32
    m = {}
    m["xin"] = np.ascontiguousarray(np.concatenate([inp["ctx"][b], inp["x"][b]], 0)).astype(f)
    cv = np.stack([inp["c"][b].reshape(8, 128).T, inp["c_ctx"].reshape(8, 128).T], -1)
    m["cvec"] = np.ascontiguousarray(cv).astype(f)
    m["mod_w"] = inp["mod_w"]
    m["mod_b"] = inp["mod_b"]
    m["ssd_in_w"] = inp["ssd_in_w"]
    cw = inp["ssd_conv_w"]
    m["convw"] = np.ascontiguousarray(cw.reshape(2, 5, 24, 128).transpose(0, 3, 2, 1)).astype(f)
    m["convb"] = np.ascontiguousarray(inp["ssd_conv_b"].reshape(2, 24, 128).transpose(0, 2, 1)).astype(f)
    m["ssd_dt_bias"] = np.ascontiguousarray(inp["ssd_dt_bias"].reshape(2, 64))
    m["ssd_a_log"] = np.ascontiguousarray(inp["ssd_a_log"].reshape(2, 64))
    m["ssd_d_skip"] = inp["ssd_d_skip"]
    m["ssd_norm_w"] = inp["ssd_norm_w"]
    m["ssd_out_w"] = inp["ssd_out_w"]
    m["ret_in_w"] = inp["ret_in_w"]
    m["ret_decay_logit"] = inp["ret_decay_logit"]
    m["ret_gn_w"] = inp["ret_gn_w"]
    m["ret_gn_b"] = inp["ret_gn_b"]
    m["ret_out_w"] = inp["ret_out_w"]
    for k in ("ln_mix_g", "ln_mix_b", "ln_ffn_g", "ln_ffn_b"):
        m[k] = inp[k]
    m["moe_rw"] = np.ascontiguousarray(np.concatenate([inp["moe_group_w"], inp["moe_expert_w"]], -1)).astype(f)
    m["moe_rb"] = np.ascontiguousarray(np.concatenate([inp["moe_group_b"], inp["moe_expert_b"]], -1)).astype(f)
    m["moe_w_gate_up"] = inp["moe_w_gate_up"].reshape(DEPTH * 32 * 128, 8 * D)
    m["moe_w_down"] = inp["moe_w_down"].reshape(DEPTH * 32 * 128, 4 * D)
    return m


def _phase_moe(self, li, xcur, xnext, final):
    fw = self.fw
    NT = NCH
    with fw.scope():
        DI = fw.sb("DI", [128, 2 * NT], I32)
        Wt = fw.sb("Wt", [128, 2 * NT], F32)
        IGU = fw.sb("IGU", [128, NB], I32)
        with fw.scope():
            identf = fw.sb("identf", [128, 128], F32)
            self.ld(identf, identf[:], self.c_identf)
            slow = fw.sb("slow", [128, 128], BF16)
            self.ld(slow, slow[:], self.c_slow)
            onesf = fw.sb("onesf", [128, 128], F32)
            self.ld(onesf, onesf[:], self.c_ones)
            onesb = fw.sb("onesb", [128, 128], BF16)
            self.V(lambda e: e.tensor_copy(out=onesb[:], in_=onesf[:]), [onesf], [onesb])
            iota = fw.sb("iota", [128, 512], F32)
            self.ld(iota, iota[:], self.c_iota)
            pidx = fw.sb("pidx", [128, 12], F32)
            self.ld(pidx, pidx[:], self.c_pidx)
            rw = fw.sb("rw", [128, 8, 36], F32)
            self.ld(rw, rw[:], self.moe_rw[li].rearrange("(k p) e -> p k e", p=128))
            rb = fw.sb("rb", [128, 36], F32)
            self.ld_row(rb, rb[:], self.moe_rb[li:li + 1, :])
            OH = fw.sb("OH", [128, 2 * NT, 32], F32)
            SL = fw.sb("SL", [128, 2 * NT], F32)
            Acum = fw.sb("Acum", [128, 32], F32)
            Acb = fw.sb("Acb", [128, 32], BF16)
            self.V(lambda e: e.memset(Acum[:], 0.0), [], [Acum])
            self.V(lambda e: e.memset(Acb[:], 0.0), [], [Acb])
            tk = fw.sb("tk", [128, D], F32)
            tkT = fw.sb("tkT", [128, 8, 128], F32)
            L = fw.sb("L", [128, 36], F32)
            gmax = fw.sb("gmax", [128, 1], F32)
            ngmax = fw.sb("ngmax", [128, 1], F32)
            goh = fw.sb("goh", [128, 4], F32)
            pen = fw.sb("pen", [128, 4], F32)
            ex = fw.sb("ex", [128, 4], F32)
            gs = fw.sb("gs", [128, 1], F32)
            em = fw.sb("em", [128, 32], F32)
            em2 = fw.sb("em2", [128, 32], F32)
            m1 = fw.sb("m1", [128, 1], F32)
            m2 = fw.sb("m2", [128, 1], F32)
            dd = fw.sb("dd", [128, 1], F32)
            den = fw.sb("den", [128, 1], F32)
            A_ = fw.sb("A_", [128, 32], F32)
            Ab = fw.sb("Ab", [128, 32], BF16)
            Pp = fw.sb("Pp", [128, 32], F32)
            junk = fw.sb("junk", [128, 32], F32)
            pT = fw.ps("pT", [128, 8, 128], F32)
            plog = fw.ps("plog", [128, 512], F32)
            pP = fw.ps("pP", [128, 512], F32)
            for c in range(NT):
                cs_ = slice(c * 128, (c + 1) * 128)
                self.ld(tk, tk[:], self.tok[cs_, :])
                for k in range(8):
                    self.P(lambda e, k=k: e.transpose(pT[:, k, :], tk[:, k * 128:(k + 1) * 128], identf[:]), [tk, identf], [pT])
                self.A(lambda e: e.copy(out=tkT[:], in_=pT[:]), [pT], [tkT])
                for k in range(8):
                    self.P(lambda e, k=k: e.matmul(plog[:, 0:36], lhsT=tkT[:, k, :], rhs=rw[:, k, :], start=(k == 0), stop=(k == 7)),
                           [tkT, rw], [plog])
                self.V(lambda e: e.tensor_tensor(out=L[:], in0=plog[:, 0:36], in1=rb[:], op=ALU.add), [plog, rb], [L])
                self.V(lambda e: e.reduce_max(out=gmax[:], in_=L[:, 0:4], axis=AX.X), [L], [gmax])
                self.V(lambda e: e.tensor_scalar(out=goh[:], in0=L[:, 0:4], scalar1=gmax[:, 0:1], scalar2=None, op0=ALU.is_equal), [L, gmax], [goh])
                self.V(lambda e: e.tensor_scalar_mul(out=ngmax[:], in0=gmax[:], scalar1=-1.0), [gmax], [ngmax])
                self.A(lambda e: e.activation(out=ex[:], in_=L[:, 0:4], func=AF.Exp, bias=ngmax[:, 0:1], scale=1.0), [L, ngmax], [ex])
                self.V(lambda e: e.reduce_sum(out=gs[:], in_=ex[:], axis=AX.X), [ex], [gs])
                self.V(lambda e: e.reciprocal(out=gs[:], in_=gs[:]), [gs], [gs])
                self.V(lambda e: e.tensor_scalar(out=pen[:], in0=goh[:], scalar1=1e30, scalar2=-1e30, op0=ALU.mult, op1=ALU.add), [goh], [pen])
                self.V(lambda e: e.tensor_tensor(out=em[:].rearrange("p (g j) -> p g j", g=4), in0=L[:, 4:36].rearrange("p (g j) -> p g j", g=4),
                                                 in1=bcast(pen[:].unsqueeze(2), [128, 4, 8]), op=ALU.add), [L, pen], [em])
                self.V(lambda e: e.reduce_max(out=m1[:], in_=em[:], axis=AX.X), [em], [m1])
                o1 = OH[:, 2 * c, :]
                o2 = OH[:, 2 * c + 1, :]
                self.V(lambda e, o1=o1: e.tensor_scalar(out=o1, in0=em[:], scalar1=m1[:, 0:1], scalar2=None, op0=ALU.is_equal), [em, m1], [OH])
                self.V(lambda e, o1=o1: e.scalar_tensor_tensor(out=em2[:], in0=o1, scalar=-1e30, in1=em[:], op0=ALU.mult, op1=ALU.add), [OH, em], [em2])
                self.V(lambda e: e.reduce_max(out=m2[:], in_=em2[:], axis=AX.X), [em2], [m2])
                self.V(lambda e, o2=o2: e.tensor_scalar(out=o2, in0=em2[:], scalar1=m2[:, 0:1], scalar2=None, op0=ALU.is_equal), [em2, m2], [OH])
                self.V(lambda e: e.tensor_tensor(out=dd[:], in0=m2[:], in1=m1[:], op=ALU.subtract), [m1, m2], [dd])
                self.A(lambda e: e.activation(out=dd[:], in_=dd[:], func=AF.Exp), [dd], [dd])
                self.V(lambda e: e.tensor_scalar_add(out=den[:], in0=dd[:], scalar1=1.0), [dd], [den])
                self.V(lambda e: e.reciprocal(out=den[:], in_=den[:]), [den], [den])
                self.V(lambda e, c=c: e.tensor_tensor(out=Wt[:, 2 * c:2 * c + 1], in0=den[:], in1=gs[:], op=ALU.mult), [den, gs], [Wt])
                self.V(lambda e, c=c: e.tensor_tensor(out=Wt[:, 2 * c + 1:2 * c + 2], in0=Wt[:, 2 * c:2 * c + 1], in1=dd[:], op=ALU.mult), [Wt, dd], [Wt])
                self.V(lambda e, o1=o1, o2=o2: e.tensor_tensor(out=A_[:], in0=o1, in1=o2, op=ALU.add), [OH], [A_])
                self.V(lambda e: e.tensor_copy(out=Ab[:], in_=A_[:]), [A_], [Ab])
                self.P(lambda e: e.matmul(pP[:, 0:32], lhsT=slow[:], rhs=Ab[:], start=True, stop=False), [slow, Ab], [pP])
                self.P(lambda e: e.matmul(pP[:, 0:32], lhsT=onesb[:], rhs=Acb[:], start=False, stop=True), [onesb, Acb], [pP])
                self.V(lambda e: e.tensor_copy(out=Pp[:], in_=pP[:, 0:32]), [pP], [Pp])
                for k, ok in enumerate((o1, o2)):
                    self.V(lambda e, ok=ok: e.tensor_tensor(out=junk[:], in0=ok, in1=Pp[:], op=ALU.mult), [OH, Pp], [junk])
                    self.V(lambda e, c=c, k=k: e.reduce_sum(out=SL[:, 2 * c + k:2 * c + k + 1], in_=junk[:], axis=AX.X), [junk], [SL])
                self.V(lambda e: e.tensor_tensor(out=Acum[:], in0=Acum[:], in1=A_[:], op=ALU.add), [Acum, A_], [Acum])
                self.V(lambda e: e.tensor_copy(out=Acb[:], in_=Acum[:]), [Acum], [Acb])
            cnt = fw.sb("cnt", [128, 32], F32)
            self.P(lambda e: e.matmul(pP[:, 0:32], lhsT=onesb[:], rhs=Acb[:], start=True, stop=True), [onesb, Acb], [pP])
            self.V(lambda e: e.tensor_copy(out=cnt[:], in_=pP[:, 0:32]), [pP], [cnt])
            thr = fw.sb("thr", [128, 34], F32)
            self.V(lambda e: e.tensor_scalar_mul(out=thr[:], in0=iota[:, 0:34], scalar1=128.0), [iota], [thr])
            cmp = fw.sb("cmp", [128, 32, 34], F32)
            self.V(lambda e: e.tensor_tensor(out=cmp[:], in0=bcast(cnt[:].unsqueeze(2), [128, 32, 34]), in1=bcast(thr[:].unsqueeze(1), [128, 32, 34]),
                                             op=ALU.is_gt), [cnt, thr], [cmp])
            nblk = fw.sb("nblk", [128, 32], F32)
            self.V(lambda e: e.reduce_sum(out=nblk[:], in_=cmp[:], axis=AX.X), [cmp], [nblk])
            pa = fw.sb("pa", [128, 32], F32)
            pb_ = fw.sb("pb", [128, 32], F32)
            self.V(lambda e: e.tensor_copy(out=pa[:], in_=nblk[:]), [nblk], [pa])
            cur, oth = pa, pb_
            for sft in (1, 2, 4, 8, 16):
                self.V(lambda e, cur=cur, oth=oth: e.tensor_copy(out=oth[:], in_=cur[:]), [cur], [oth])
                self.V(lambda e, cur=cur, oth=oth, sft=sft: e.tensor_tensor(out=oth[:, sft:32], in0=cur[:, sft:32], in1=cur[:, 0:32 - sft], op=ALU.add),
                       [cur], [oth])
                cur, oth = oth, cur
            pend = cur
            pstart = fw.sb("pstart", [128, 32], F32)
            self.V(lambda e: e.tensor_tensor(out=pstart[:], in0=pend[:], in1=nblk[:], op=ALU.subtract), [pend, nblk], [pstart])
            self.V(lambda e: e.tensor_scalar_mul(out=pstart[:], in0=pstart[:], scalar1=128.0), [pstart], [pstart])
            big = fw.sb("big", [128, 2 * NT, 32], F32)
            self.V(lambda e: e.tensor_tensor(out=big[:], in0=OH[:], in1=bcast(pstart[:].unsqueeze(1), [128, 2 * NT, 32]), op=ALU.mult), [OH, pstart], [big])
            dst = fw.sb("dstf", [128, 2 * NT], F32)
            self.V(lambda e: e.reduce_sum(out=dst[:], in_=big[:], axis=AX.X), [big], [dst])
            self.V(lambda e: e.tensor_tensor(out=dst[:], in0=dst[:], in1=SL[:], op=ALU.add), [dst, SL], [dst])
            self.V(lambda e: e.tensor_copy(out=DI[:], in_=dst[:]), [dst], [DI])
            cmp2 = fw.sb("cmp2", [128, NB, 32], F32)
            self.V(lambda e: e.tensor_tensor(out=cmp2[:], in0=bcast(pend[:].unsqueeze(1), [128, NB, 32]), in1=bcast(iota[:, 0:NB].unsqueeze(2), [128, NB, 32]),
                                             op=ALU.is_le), [pend, iota], [cmp2])
            be = fw.sb("be", [128, NB], F32)
            self.V(lambda e: e.reduce_sum(out=be[:], in_=cmp2[:], axis=AX.X), [cmp2], [be])
            self.V(lambda e: e.tensor_scalar_min(out=be[:], in0=be[:], scalar1=31.0), [be], [be])
            same = fw.sb("same", [128, NB], F32)
            self.V(lambda e: e.memset(same[:], 0.0), [], [same])
            self.V(lambda e: e.tensor_tensor(out=same[:, 2:NB], in0=be[:, 2:NB], in1=be[:, 0:NB - 2], op=ALU.is_equal), [be], [same])
            self.V(lambda e: e.tensor_scalar(out=be[:], in0=be[:], scalar1=128.0, scalar2=float(li * 32 * 128), op0=ALU.mult, op1=ALU.add), [be], [be])
            self.V(lambda e: e.scalar_tensor_tensor(out=be[:], in0=same[:], scalar=1.0e6, in1=be[:], op0=ALU.mult, op1=ALU.add), [same, be], [be])
            self.V(lambda e: e.tensor_tensor(out=be[:], in0=be[:], in1=bcast(pidx[:, 0:1], [128, NB]), op=ALU.add), [be, pidx], [be])
            self.V(lambda e: e.tensor_copy(out=IGU[:], in_=be[:]), [be], [IGU])
        with fw.scope():
            tk = fw.sb("tk", [128, D], F32)
            tkb = fw.sb("tkb", [128, D], BF16)
            for c in range(NT):
                self.ld(tk, tk[:], self.tok[c * 128:(c + 1) * 128, :])
                self.V(lambda e: e.tensor_copy(out=tkb[:], in_=tk[:]), [tk], [tkb])
                for k in range(2):
                    col = 2 * c + k
                    self.fw.dma("gpsimd", lambda e, col=col: e.indirect_dma_start(
                        out=self.xbuf, out_offset=bass.IndirectOffsetOnAxis(ap=DI[:, col:col + 1], axis=0), in_=tkb[:, :], in_offset=None),
                        reads=[tkb, DI])
        with fw.scope():
            identb = fw.sb("identb", [128, 128], BF16)
            self.ld(identb, identb[:], self.c_identb)
            xb = [fw.sb("xb%d" % i, [128, D], BF16) for i in range(2)]
            xT = [fw.sb("xT%d" % i, [128, 8, 128], BF16) for i in range(2)]
            g32 = [fw.sb("g32_%d" % i, [128, 8, D], F32) for i in range(2)]
            d32 = [fw.sb("d32_%d" % i, [128, 4, D], F32) for i in range(2)]
            gbf = [fw.sb("gbf_%d" % i, [128, 8, D], BF16) for i in range(2)]
            dbf = [fw.sb("dbf_%d" % i, [128, 4, D], BF16) for i in range(2)]
            sg = fw.sb("sg", [128, 4, 128], F32)
            hT = fw.sb("hT", [128, 4, 128], BF16)
            ob = fw.sb("ob", [128, D], F32)
            ptr = fw.ps("ptr", [128, 8, 128], BF16)
            pH = [fw.ps("pH%d" % i, [128, 8, 128], F32) for i in range(2)]
            pO = fw.ps("pO", [128, D], F32)
            if getattr(self, "_bcreg", None) is None:
                self._bcreg = self.nc.gpsimd.alloc_register("bcreg")
                self.nc.gpsimd.reg_mov(self._bcreg, DEPTH * 32 * 128 - 1)
            bcreg = self._bcreg
            self.ld(xb[0], xb[0][:], self.xbuf[0:128, :])
            for b in range(NB):
                i = b % 2
                if b + 1 < NB:
                    self.ld(xb[(b + 1) % 2], xb[(b + 1) % 2][:], self.xbuf[(b + 1) * 128:(b + 2) * 128, :])
                idx = IGU[:, b:b + 1]
                self.fw.dma("gpsimd", lambda e, i=i, idx=idx: e.indirect_dma_start(
                    out=g32[i][:].rearrange("p k n -> p (k n)"), out_offset=None, in_=self.moe_wgu,
                    in_offset=bass.IndirectOffsetOnAxis(ap=idx, axis=0), bounds_check=bcreg, oob_is_err=False),
                    reads=[IGU], writes=[g32[i]])
                self.fw.dma("gpsimd", lambda e, i=i, idx=idx: e.indirect_dma_start(
                    out=d32[i][:].rearrange("p k n -> p (k n)"), out_offset=None, in_=self.moe_wd,
                    in_offset=bass.IndirectOffsetOnAxis(ap=idx, axis=0), bounds_check=bcreg, oob_is_err=False),
                    reads=[IGU], writes=[d32[i]])
                xbv = xb[i][:].rearrange("s (p k) -> s k p", k=8)
                for k in range(8):
                    self.P(lambda e, k=k, xbv=xbv: e.transpose(ptr[:, k, :], xbv[:, k, :], identb[:]), [xb[i], identb], [ptr])
                self.V(lambda e, i=i: e.tensor_copy(out=xT[i][:], in_=ptr[:]), [ptr], [xT[i]])
                for q in range(4):
                    sl = slice(q * 2, q * 2 + 2)
                    if q % 2 == 0:
                        self.A(lambda e, i=i, sl=sl: e.copy(out=gbf[i][:, sl, :], in_=g32[i][:, sl, :]), [g32[i]], [gbf[i]])
                    else:
                        self.V(lambda e, i=i, sl=sl: e.tensor_copy(out=gbf[i][:, sl, :], in_=g32[i][:, sl, :]), [g32[i]], [gbf[i]])
                self.A(lambda e, i=i: e.copy(out=dbf[i][:, 0:2, :], in_=d32[i][:, 0:2, :]), [d32[i]], [dbf[i]])
                self.V(lambda e, i=i: e.tensor_copy(out=dbf[i][:, 2:4, :], in_=d32[i][:, 2:4, :]), [d32[i]], [dbf[i]])
                ph = pH[i]
                for m in range(8):
                    tt, kh = m // 4, m % 4
                    for k in range(8):
                        lw = gbf[i][:, k, :].rearrange("d (two p k) -> d two k p", two=2, k=4)[:, tt, kh, :]
                        self.P(lambda e, i=i, m=m, k=k, ph=ph, lw=lw: e.matmul(ph[:, m, :], lhsT=lw, rhs=xT[i][:, k, :],
                                                                               start=(k == 0), stop=(k == 7)), [gbf[i], xT[i]], [ph])
                self.A(lambda e, ph=ph: e.activation(out=sg[:], in_=ph[:, 0:4, :], func=AF.Silu), [ph], [sg])
                self.V(lambda e, ph=ph: e.tensor_tensor(out=hT[:], in0=sg[:], in1=ph[:, 4:8, :], op=ALU.mult), [sg, ph], [hT])
                for nn in range(2):
                    for k in range(4):
                        self.P(lambda e, i=i, nn=nn, k=k: e.matmul(pO[:, nn * 512:(nn + 1) * 512], lhsT=hT[:, k, :], rhs=dbf[i][:, k, nn * 512:(nn + 1) * 512],
                                                                   start=(k == 0), stop=(k == 3)), [hT, dbf[i]], [pO])
                self.A(lambda e: e.copy(out=ob[:], in_=pO[:]), [pO], [ob])
                self.st(self.ybuf[b * 128:(b + 1) * 128, :], ob, ob[:])
        with fw.scope():
            g2 = []
            for v in range(2):
                t = fw.sb("g2_%d" % v, [128, D], F32)
                self.ld_row(t, t[:], self.modrow[v:v + 1, 5 * D:6 * D])
                g2.append(t)
            lng = fw.sb("lng", [128, D], F32)
            lnb = fw.sb("lnb", [128, D], F32)
            self.ld_row(lng, lng[:], self.ln_ffn_g[li:li + 1, :])
            self.ld_row(lnb, lnb[:], self.ln_ffn_b[li:li + 1, :])
            o1_ = [fw.sb("o1%d" % i, [128, D], F32) for i in range(2)]
            o2_ = [fw.sb("o2%d" % i, [128, D], F32) for i in range(2)]
            xt_ = [fw.sb("xt%d" % i, [128, D], F32) for i in range(2)]
            t1_ = [fw.sb("t1%d" % i, [128, D], F32) for i in range(2)]
            st2 = fw.sb("st2", [128, 2, 6], F32)
            mv2 = fw.sb("mv2", [128, 2], F32)
            rs2 = fw.sb("rs2", [128, 1], F32)
            clist = [c for c in range(NT) if not (final and c < NCTX)]

            def fetch(c):
                o1, o2, xt = o1_[c % 2], o2_[c % 2], xt_[c % 2]
                for k, ot in enumerate((o1, o2)):
                    col = 2 * c + k
                    self.fw.dma("gpsimd", lambda e, col=col, ot=ot: e.indirect_dma_start(
                        out=ot[:, :], out_offset=None, in_=self.ybuf, in_offset=bass.IndirectOffsetOnAxis(ap=DI[:, col:col + 1], axis=0)),
                        reads=[DI], writes=[ot])
                self.ld(xt, xt[:], xcur[c * 128:(c + 1) * 128, :])

            fetch(clist[0])
            for ci, c in enumerate(clist):
                if ci + 1 < len(clist):
                    fetch(clist[ci + 1])
                v = 1 if c < NCTX else 0
                o1, o2, xt, t1 = o1_[c % 2], o2_[c % 2], xt_[c % 2], t1_[c % 2]
                cs_ = slice(c * 128, (c + 1) * 128)
                self.V(lambda e: e.tensor_scalar(out=o1[:], in0=o1[:], scalar1=Wt[:, 2 * c:2 * c + 1], scalar2=None, op0=ALU.mult), [o1, Wt], [o1])
                self.V(lambda e: e.scalar_tensor_tensor(out=o1[:], in0=o2[:], scalar=Wt[:, 2 * c + 1:2 * c + 2], in1=o1[:], op0=ALU.mult, op1=ALU.add),
                       [o2, Wt, o1], [o1])
                self.V(lambda e: e.tensor_tensor(out=o1[:], in0=o1[:], in1=g2[v][:], op=ALU.mult), [o1, g2[v]], [o1])
                self.V(lambda e: e.scalar_tensor_tensor(out=o2[:], in0=xt[:], scalar=ALPHA, in1=o1[:], op0=ALU.mult, op1=ALU.add), [xt, o1], [o2])
                self.layer_norm(o2, t1, st2, mv2, rs2, lng, lnb, pool=False)
                if final:
                    self.st(self.yout[(c - NCTX) * 128:(c - NCTX + 1) * 128, :], t1, t1[:])
                else:
                    self.st(xnext[cs_, :], t1, t1[:])


Builder.phase_moe = _phase_moe


def build_program(nlayers=DEPTH):
    nc = bass.Bass("TRN2", target_bir_lowering=False)
    b = Builder(nc)
    b.declare()
    xcur = b.xin
    for li in range(nlayers):
        j = li // 2
        b.phase_mod(li)
        if li % 2 == 0:
            b.phase_in_ssd(li, j, xcur)
            b.phase_scan(li, j, True, xcur, b.xA)
        else:
            b.phase_in_ret(li, j, xcur)
            b.phase_scan(li, j, False, xcur, b.xA)
        b.phase_moe(li, b.xA, b.xB, final=(li == nlayers - 1))
        xcur = b.xB
    b.fw.barrier()
    b.fw.root.close()
    return nc


def kernel(**inputs):
    inp = {k: np.asarray(v) for k, v in inputs.items()}
    nc = build_program()
    consts = host_consts()
    maps = []
    for c in range(8):
        m = host_inputs(inp, c)
        m.update(consts)
        maps.append(m)
    res = run_bass_kernel_spmd(nc, maps, core_ids=list(range(8)))
    out = np.stack([np.asarray(res.results[c]["yout"]) for c in range(8)], 0)
    return out.astype(np.float32)


def _phase_scan2(self, li, j, ssd, xcur, xnext):
    fw = self.fw
    G = 4
    Hg = 8 if ssd else 1
    KC = 1 if ssd else 2
    H = G * Hg
    dv = 2048 // H
    dk = KC * 128
    with fw.scope():
        identb = fw.sb("identb", [128, 128], BF16)
        self.ld(identb, identb[:], self.c_identb)
        ones = fw.sb("ones", [128, 128], F32)
        self.ld(ones, ones[:], self.c_ones)
        tri = fw.sb("tri", [128, 128], F32)
        Wo = fw.sb("Wo", [128, 16, D], BF16)
        owv = (self.ssd_out_w if ssd else self.ret_out_w)[j].rearrange("(k p) n -> p k n", p=128)
        with fw.scope():
            wst = fw.sb("wstO", [128, 4, D], F32)
            for q in range(4):
                self.ld(wst, wst[:], owv[:, q * 4:(q + 1) * 4, :])
                self.G(lambda e, q=q: e.tensor_copy(out=Wo[:, q * 4:(q + 1) * 4, :], in_=wst[:]), [wst], [Wo])
        g1 = fw.sb("g1", [128, D], F32)
        sh2 = fw.sb("sh2", [128, D], F32)
        sc2 = fw.sb("sc2", [128, D], F32)

        def load_rows(v):
            self.ld_row(g1, g1[:], self.modrow[v:v + 1, 2 * D:3 * D])
            self.ld_row(sh2, sh2[:], self.modrow[v:v + 1, 3 * D:4 * D])
            self.ld_row(sc2, sc2[:], self.modrow[v:v + 1, 4 * D:5 * D])
            self.V(lambda e: e.tensor_scalar_add(out=sc2[:], in0=sc2[:], scalar1=1.0), [sc2], [sc2])

        lng = fw.sb("lng", [128, D], F32)
        lnb = fw.sb("lnb", [128, D], F32)
        self.ld_row(lng, lng[:], self.ln_mix_g[li:li + 1, :])
        self.ld_row(lnb, lnb[:], self.ln_mix_b[li:li + 1, :])
        nw = fw.sb("nw", [128, 2048], F32)
        self.ld_row(nw, nw[:], (self.ssd_norm_w if ssd else self.ret_gn_w)[j:j + 1, :])
        if ssd:
            dsk = fw.sb("dsk", [128, 32], F32)
            self.ld_row(dsk, dsk[:], self.ssd_d_skip[j:j + 1, :])
        else:
            nb_ = fw.sb("nb", [128, 2048], F32)
            self.ld_row(nb_, nb_[:], self.ret_gn_b[j:j + 1, :])
        S = fw.sb("S", [128, KC, 2048], F32)
        Sb = fw.sb("Sb", [128, KC, 2048], BF16)

        def dbl(name, shape, dt):
            return [fw.sb(name + "0", shape, dt), fw.sb(name + "1", shape, dt)]

        def tpl(name, shape, dt):
            return [fw.sb(name + str(i_), shape, dt) for i_ in range(3)]

        qt3 = tpl("qt", [128, G * KC, 128], BF16)
        kt2 = dbl("kt", [128, G * KC, 128], BF16)
        ktm3 = tpl("ktm", [128, G * dk], BF16)
        vt3 = tpl("vt", [128, 2048], BF16)
        xdt2 = dbl("xdt", [128, 2048], BF16) if ssd else None
        xe = dbl("xe", [128, 2048], BF16)
        cbm = dbl("cbm", [128, G, 128], F32)
        la2 = dbl("la", [128, 64], F32)
        dt2 = dbl("dt", [128, 64], F32)
        acs = fw.sb("acs", [128, 2 * H], F32)
        wend = fw.sb("wend", [128, H], F32)
        if ssd:
            ea = dbl("ea", [128, H], F32)
            cd = dbl("cd", [128, H], F32)
            E = [[fw.sb("E%d_%d" % (p_, g), [128, Hg, 128], BF16) for g in range(G)] for p_ in range(2)]
        else:
            ea0 = fw.sb("ea", [128, H], F32)
            cd0 = fw.sb("cd", [128, H], F32)
            ea = [ea0, ea0]
            cd = [cd0, cd0]
            E0 = [fw.sb("E_%d" % g, [128, Hg, 128], F32) for g in range(G)]
            E = [E0, E0]
        lt = dbl("lt", [128, Hg, 128], F32)
        nlb = dbl("nlb", [128, Hg, 128], F32)
        nones = fw.sb("nones", [128, 128], F32)
        self.V(lambda e: e.memset(nones[:], -1.0), [], [nones])
        M = dbl("M", [128, Hg, 128], BF16)
        ycs = dbl("yc", [128, 2048], F32)
        tmp0 = fw.sb("tmp", [128, 512], F32)
        tmp = [tmp0, tmp0]
        yfl = fw.sb("yfl", [128, 2048], F32)
        gt = fw.sb("gt", [128, 2048], F32)
        yb = fw.sb("yb", [128, 2048], BF16)
        yT = fw.sb("yT", [128, 16, 128], BF16)
        st6 = fw.sb("st6", [128, 4, 6], F32)
        mv = fw.sb("mv", [128, 4, 2], F32)
        ss = fw.sb("ss", [128, 4], F32)
        rstd = fw.sb("rstd", [128, 4], F32)
        xt = fw.sb("xt", [128, D], F32)
        t1 = fw.sb("t1", [128, D], F32)
        t2 = fw.sb("t2", [128, D], F32)
        st2 = fw.sb("st2", [128, 2, 6], F32)
        mv2 = fw.sb("mv2", [128, 2], F32)
        rs2 = fw.sb("rs2", [128, 1], F32)
        psm = fw.ps("psm", [128, 512], F32)
        pcb = fw.ps("pcb", [128, 512], F32)
        pbc = [fw.ps("pbc%d" % i, [128, 512], F32) for i in range(2)]
        pyd = fw.ps("pyd", [128, 512], F32)
        pyo = fw.ps("pyo", [128, 512], F32)
        pst = fw.ps("pst", [128, 512], F32)
        ptr = fw.ps("ptr", [128, 8, 128], BF16)

        qtv = self.QT.rearrange("g k p t -> p g k t")
        ktv = self.KT.rearrange("g k p t -> p g k t")
        r3 = lambda ap, h: ap.rearrange("p (h d) -> p h d", h=h)

        def decay_pre(dirn, i):
            par = i % 2
            la = la2[par]
            lad = la[:, dirn * 32:dirn * 32 + H] if ssd else la[:, 0:H]
            self.P(lambda e: e.matmul(psm[:, 0:H], lhsT=tri[:], rhs=lad, start=True, stop=True), [tri, la], [psm])
            self.P(lambda e: e.matmul(psm[:, H:2 * H], lhsT=ones[:], rhs=lad, start=True, stop=True), [ones, la], [psm])
            self.V(lambda e: e.tensor_copy(out=acs[:], in_=psm[:, 0:2 * H]), [psm], [acs])
            self.A(lambda e: e.activation(out=ea[par][:], in_=acs[:, 0:H], func=AF.Exp), [acs], [ea[par]])
            self.V(lambda e: e.tensor_tensor(out=wend[:], in0=acs[:, H:2 * H], in1=acs[:, 0:H], op=ALU.subtract), [acs], [wend])
            self.A(lambda e: e.activation(out=wend[:], in_=wend[:], func=AF.Exp), [wend], [wend])
            self.A(lambda e: e.activation(out=cd[par][:], in_=acs[:, H:2 * H], func=AF.Exp), [acs], [cd[par]])

        def decay_g(dirn, i, g):
            par = i % 2
            la = la2[par]
            ltg = lt[g % 2]
            nlg = nlb[g % 2]
            ladg = la[:, dirn * 32 + g * Hg:dirn * 32 + (g + 1) * Hg] if ssd else la[:, g:g + 1]
            self.G(lambda e: e.tensor_tensor(out=ltg[:], in0=bcast(tri[:].unsqueeze(1), [128, Hg, 128]),
                                             in1=bcast(ladg.unsqueeze(2), [128, Hg, 128]), op=ALU.mult), [tri, la], [ltg])
            self.G(lambda e: e.tensor_tensor(out=nlg[:], in0=bcast(nones[:].unsqueeze(1), [128, Hg, 128]),
                                             in1=bcast(ladg.unsqueeze(2), [128, Hg, 128]), op=ALU.mult), [nones, la], [nlg])
            Eg = E[par][g]
            for q in range((Hg + 3) // 4):
                nh = min(4, Hg - q * 4)
                pb = pbc[q % 2]
                self.P(lambda e: e.matmul(pb[:, 0:nh * 128], lhsT=ones[:], rhs=ltg[:, q * 4:q * 4 + nh, :], start=True, stop=False), [ones, ltg], [pb])
                self.P(lambda e: e.matmul(pb[:, 0:nh * 128], lhsT=tri[:], rhs=nlg[:, q * 4:q * 4 + nh, :], start=False, stop=True), [tri, nlg], [pb])
                self.A(lambda e: e.activation(out=Eg[:, q * 4:q * 4 + nh, :].rearrange("p h l -> p (h l)"), in_=pb[:, 0:nh * 128], func=AF.Exp), [pb], [Eg])

        def loads(c, i):
            cs_ = slice(c * 128, (c + 1) * 128)
            t3, p2 = i % 3, i % 2
            self.ld(qt3[t3], qt3[t3][:].rearrange("p (g k) t -> p g k t", g=G), qtv[:, :, 0:KC, cs_])
            self.ld(kt2[p2], kt2[p2][:].rearrange("p (g k) t -> p g k t", g=G), ktv[:, :, 0:KC, cs_])
            self.ld(ktm3[t3], ktm3[t3][:], self.Ktm[cs_, 0:G * dk])
            self.ld(vt3[t3], vt3[t3][:], self.Vtm[cs_, :])
            if ssd:
                self.ld(la2[p2], la2[p2][:], self.latm[cs_, :])
                self.ld(dt2[p2], dt2[p2][:], self.dttm[cs_, :])

        def XDT(i):
            return xdt2[i % 2] if ssd else vt3[i % 3]

        def stage1_pre(dirn, i):
            par = i % 2
            qt, kt, vt, dt = qt3[i % 3], kt2[par], vt3[i % 3], dt2[par]
            xdt = XDT(i)
            if ssd:
                decay_pre(dirn, i)
                self.V(lambda e: e.tensor_tensor(out=r3(xdt[:], H), in0=r3(vt[:], H),
                                                 in1=bcast(dt[:, dirn * 32:dirn * 32 + 32].unsqueeze(2), [128, H, dv]), op=ALU.mult),
                       [vt, dt], [xdt])
            self.G(lambda e: e.tensor_tensor(out=r3(xe[par][:], H), in0=r3(xdt[:], H),
                                             in1=bcast(wend[:].unsqueeze(2), [128, H, dv]), op=ALU.mult), [xdt, wend], [xe[par]])
            for g in range(G):
                for kc in range(KC):
                    self.P(lambda e, g=g, kc=kc: e.matmul(pcb[:, g * 128:(g + 1) * 128], lhsT=kt[:, g * KC + kc, :], rhs=qt[:, g * KC + kc, :],
                                                          start=(kc == 0), stop=(kc == KC - 1)), [kt, qt], [pcb])
            self.V(lambda e: e.tensor_tensor(out=cbm[par][:], in0=pcb[:].rearrange("p (g l) -> p g l", g=G),
                                             in1=bcast(tri[:].unsqueeze(1), [128, G, 128]), op=ALU.mult), [pcb, tri], [cbm[par]])

        def emitM(g, par):
            Mg = M[g % 2]
            self.V(lambda e: e.scalar_tensor_tensor(out=Mg[:], in0=E[par][g][:], scalar=1.0,
                                                    in1=bcast(cbm[par][:, g:g + 1, :], [128, Hg, 128]), op0=ALU.min, op1=ALU.mult),
                   [E[par][g], cbm[par]], [Mg])

        def s2g(i, g):
            par = i % 2
            qt, ktm = qt3[i % 3], ktm3[i % 3]
            xdt = XDT(i)
            gs = slice(g * 512, (g + 1) * 512)
            yc = ycs[par]
            if g + 1 < G:
                emitM(g + 1, par)
            Mg = M[g % 2]
            tg = tmp[g % 2]
            for hh in range(Hg):
                h = g * Hg + hh
                self.P(lambda e, h=h, hh=hh: e.matmul(pyd[:, hh * dv:(hh + 1) * dv], lhsT=Mg[:, hh, :], rhs=xdt[:, h * dv:(h + 1) * dv],
                                                      start=True, stop=True), [Mg, xdt], [pyd])
            for kc in range(KC):
                self.P(lambda e, kc=kc: e.matmul(pyo[:], lhsT=qt[:, g * KC + kc, :], rhs=Sb[:, kc, gs],
                                                 start=(kc == 0), stop=(kc == KC - 1)), [qt, Sb], [pyo])
            self.V(lambda e: e.tensor_tensor(out=r3(tg[:], Hg), in0=r3(pyo[:], Hg),
                                             in1=bcast(ea[par][:, g * Hg:(g + 1) * Hg].unsqueeze(2), [128, Hg, dv]), op=ALU.mult),
                   [pyo, ea[par]], [tg])
            self.V(lambda e: e.tensor_tensor(out=yc[:, gs], in0=tg[:], in1=pyd[:], op=ALU.add), [tg, pyd], [yc])
            for kc in range(KC):
                self.P(lambda e, kc=kc: e.matmul(pst[:], lhsT=ktm[:, g * dk + kc * 128:g * dk + (kc + 1) * 128], rhs=xe[par][:, gs],
                                                 start=True, stop=True), [ktm, xe[par]], [pst])
                self.G(lambda e, kc=kc: e.tensor_tensor(out=r3(S[:, kc, gs], Hg), in0=r3(S[:, kc, gs], Hg),
                                                        in1=bcast(cd[par][:, g * Hg:(g + 1) * Hg].unsqueeze(2), [128, Hg, dv]),
                                                        op=ALU.mult), [S, cd[par]], [S])
                self.V(lambda e, kc=kc: e.tensor_tensor(out=S[:, kc, gs], in0=S[:, kc, gs], in1=pst[:], op=ALU.add), [S, pst], [S])
                self.A(lambda e, kc=kc: e.copy(out=Sb[:, kc, gs], in_=S[:, kc, gs]), [S], [Sb])

        def stage2_post(c, dirn, i):
            par = i % 2
            yc = ycs[par]
            cs_ = slice(c * 128, (c + 1) * 128)
            if dirn == 0:
                deferred.append(lambda: self.st(self.yf[cs_, :], yc, yc[:]))
                return
            self.ld(yfl, yfl[:], self.yf[cs_, :])
            self.ld(gt, gt[:], self.gate[cs_, :])
            self.V(lambda e: e.tensor_tensor(out=yc[:], in0=yc[:], in1=yfl[:], op=ALU.add), [yc, yfl], [yc])
            self.A(lambda e: e.activation(out=gt[:], in_=gt[:], func=AF.Silu), [gt], [gt])
            yield
            if ssd:
                self.V(lambda e: e.tensor_tensor(out=yc[:], in0=yc[:], in1=gt[:], op=ALU.mult), [yc, gt], [yc])
            for g in range(4):
                self.V(lambda e, g=g: e.bn_stats(out=st6[:, g, :], in_=yc[:, g * 512:(g + 1) * 512]), [yc], [st6])
                self.V(lambda e, g=g: e.bn_aggr(out=mv[:, g, :], in_=st6[:, g, :]), [st6], [mv])
            yield
            if ssd:
                self.V(lambda e: e.tensor_tensor(out=ss[:], in0=mv[:, :, 0], in1=mv[:, :, 0], op=ALU.mult), [mv], [ss])
                self.V(lambda e: e.tensor_tensor(out=ss[:], in0=ss[:], in1=mv[:, :, 1], op=ALU.add), [ss, mv], [ss])
                self.A(lambda e: e.activation(out=ss[:], in_=ss[:], func=AF.Sqrt, bias=EPS), [ss], [ss])
                self.V(lambda e: e.reciprocal(out=rstd[:], in_=ss[:]), [ss], [rstd])
                yield
                for g in range(4):
                    gs = slice(g * 512, (g + 1) * 512)
                    self.V(lambda e, g=g, gs=gs: e.scalar_tensor_tensor(out=yb[:, gs], in0=yc[:, gs], scalar=rstd[:, g:g + 1], in1=nw[:, gs],
                                                                        op0=ALU.mult, op1=ALU.mult), [yc, rstd, nw], [yb])
            else:
                self.A(lambda e: e.activation(out=ss[:], in_=mv[:, :, 1], func=AF.Sqrt, bias=EPS), [mv], [ss])
                self.V(lambda e: e.reciprocal(out=rstd[:], in_=ss[:]), [ss], [rstd])
                yield
                for g in range(4):
                    gs = slice(g * 512, (g + 1) * 512)
                    self.V(lambda e, g=g, gs=gs: e.tensor_scalar(out=yc[:, gs], in0=yc[:, gs], scalar1=mv[:, g, 0:1], scalar2=rstd[:, g:g + 1],
                                                                 op0=ALU.subtract, op1=ALU.mult), [yc, mv, rstd], [yc])
                self.G(lambda e: e.tensor_tensor(out=yc[:], in0=yc[:], in1=nw[:], op=ALU.mult), [yc, nw], [yc])
                self.V(lambda e: e.tensor_tensor(out=yc[:], in0=yc[:], in1=nb_[:], op=ALU.add), [yc, nb_], [yc])
                self.V(lambda e: e.tensor_tensor(out=yb[:], in0=yc[:], in1=gt[:], op=ALU.mult), [yc, gt], [yb])
            yield
            for half in range(2):
                for kk in range(8):
                    k = half * 8 + kk
                    self.P(lambda e, k=k, kk=kk: e.transpose(ptr[:, kk, :], yb[:, k * 128:(k + 1) * 128], identb[:]), [yb, identb], [ptr])
                self.A(lambda e, half=half: e.copy(out=yT[:, half * 8:(half + 1) * 8, :], in_=ptr[:]), [ptr], [yT])
                yield
            pos = (pyd, pyo)
            self.ld(xt, xt[:], xcur[cs_, :])
            for nn in range(2):
                for k in range(16):
                    self.P(lambda e, k=k, nn=nn: e.matmul(pos[nn][:], lhsT=yT[:, k, :], rhs=Wo[:, k, nn * 512:(nn + 1) * 512],
                                                          start=(k == 0), stop=(k == 15)), [yT, Wo], [pos[nn]])
                ns = slice(nn * 512, (nn + 1) * 512)
                self.V(lambda e, nn=nn, ns=ns: e.tensor_tensor(out=t1[:, ns], in0=pos[nn][:], in1=g1[:, ns], op=ALU.mult), [pos[nn], g1], [t1])
            yield
            self.V(lambda e: e.scalar_tensor_tensor(out=t2[:], in0=xt[:], scalar=ALPHA, in1=t1[:], op0=ALU.mult, op1=ALU.add), [xt, t1], [t2])
            self.layer_norm(t2, t1, st2, mv2, rs2, lng, lnb)
            yield
            self.G(lambda e: e.tensor_tensor(out=t2[:], in0=t1[:], in1=sc2[:], op=ALU.mult), [t1, sc2], [t2])
            self.V(lambda e: e.tensor_tensor(out=t2[:], in0=t2[:], in1=sh2[:], op=ALU.add), [t2, sh2], [t2])
            deferred.append(lambda: self.st(xnext[cs_, :], t1, t1[:]))
            deferred.append(lambda: self.st(self.tok[cs_, :], t2, t2[:]))

        deferred = []

        def flush():
            for f in deferred:
                f()
            del deferred[:]

        for dirn in range(2):
            order = list(range(NCH)) if dirn == 0 else [1, 0] + list(range(NCH - 1, 1, -1))
            self.ld(tri, tri[:], self.c_trif[dirn])
            self.V(lambda e: e.memset(S[:], 0.0), [], [S])
            self.G(lambda e: e.memset(Sb[:], 0.0), [], [Sb])
            if dirn == 1:
                load_rows(1)
            n_ = len(order)
            if not ssd:
                for la in la2:
                    self.ld_row(la, la[:, 0:4], self.ret_decay[j, dirn:dirn + 1, :])
                    self.A(lambda e: e.activation(out=la[:, 0:4], in_=la[:, 0:4], func=AF.Exp, scale=-1.0), [la], [la])
                    self.A(lambda e: e.activation(out=la[:, 0:4], in_=la[:, 0:4], func=AF.Ln, bias=1.0), [la], [la])
                    self.V(lambda e: e.tensor_scalar_mul(out=la[:, 0:4], in0=la[:, 0:4], scalar1=-1.0), [la], [la])
                decay_pre(dirn, 0)
                for g in range(G):
                    decay_g(dirn, 0, g)
            loads(order[0], 0)
            loads(order[1], 1)
            stage1_pre(dirn, 0)
            if ssd:
                for g in range(G):
                    decay_g(dirn, 0, g)
            pend = None

            def step_post():
                nonlocal pend
                if pend is not None:
                    try:
                        next(pend)
                    except StopIteration:
                        pend = None

            for i, c in enumerate(order):
                if i + 2 < n_:
                    loads(order[i + 2], i + 2)
                flush()
                if dirn == 1 and i == NCTX + 1:
                    load_rows(0)
                if i + 1 < n_:
                    stage1_pre(dirn, i + 1)
                step_post()
                emitM(0, i % 2)
                for g in range(G):
                    if ssd and i + 1 < n_:
                        decay_g(dirn, i + 1, g)
                    step_post()
                    s2g(i, g)
                    step_post()
                while pend is not None:
                    step_post()
                if dirn == 1 and ssd:
                    par = i % 2
                    self.G(lambda e: e.tensor_tensor(out=r3(xe[par][:], H), in0=r3(vt3[i % 3][:], H),
                                                     in1=bcast(dsk[:].unsqueeze(2), [128, H, dv]), op=ALU.mult), [vt3[i % 3], dsk], [xe[par]])
                    self.V(lambda e: e.tensor_tensor(out=ycs[par][:], in0=ycs[par][:], in1=xe[par][:], op=ALU.add), [ycs[par], xe[par]], [ycs[par]])
                pend = stage2_post(c, dirn, i)
                if dirn == 0:
                    for _ in pend:
                        pass
                    pend = None
            flush()
            while pend is not None:
                step_post()
            flush()
            if dirn == 0:
                fw.barrier()


Builder.phase_scan = _phase_scan2
```
